# Optimizing a Trainium2 kernel written in Bass

```python
import jax, jax.numpy as jnp
from jax import lax
import numpy as np

D_MODEL = 1024
BATCH = 8
SEQ = 8192
DEPTH = 1

ATTN_GROUPS = ((128, 1), (512, 4), (2048, 16))
N_ATTN_GROUPS = 3
HEADS_PER_GROUP = 8
HEAD_DIM_A = 64
ATTN_GROUP_WIDTH = HEADS_PER_GROUP * HEAD_DIM_A
ATTN_QKV_WIDTH = N_ATTN_GROUPS * ATTN_GROUP_WIDTH
DN_HEADS = 8
DN_HEAD_DIM = 128
DN_WIDTH = DN_HEADS * DN_HEAD_DIM
CONV_WIDTH = 4
CHUNK = 64
N_EXPERT_GROUPS = 4
EXPERTS_PER_GROUP = 4
N_EXPERTS = N_EXPERT_GROUPS * EXPERTS_PER_GROUP
TOP_K = 2
D_EXPERT = 256
PLE_DIM = 256
EPS = 1e-6
IN_WIDTHS = (3 * ATTN_QKV_WIDTH, 3 * DN_WIDTH, DN_WIDTH, DN_HEADS, DN_HEADS, D_MODEL, D_MODEL)
IN_TOTAL = 3 * ATTN_QKV_WIDTH + 4 * DN_WIDTH + 2 * DN_HEADS + 2 * D_MODEL

kernel_name = "hybrid_dilated_attn_gated_deltanet_hier_moe"


def rms_norm(x, gain):
    xf = x.astype(jnp.float32)
    y = xf * lax.rsqrt(jnp.mean(xf * xf, axis=-1, keepdims=True) + EPS)
    return y * gain.astype(jnp.float32)


def l2_norm(x):
    return x * lax.rsqrt(jnp.sum(x * x, axis=-1, keepdims=True) + EPS)


def split_columns(t, widths):
    out, start = [], 0
    for w in widths:
        out.append(t[..., start:start + w])
        start += w
    return out


def dilated_window_attention(q, k, v, window, dilation):
    b, s, h, dh = q.shape
    blk = window // dilation
    span = blk * dilation
    s_pad = -(-s // span) * span
    n_sub = s_pad // dilation
    nblk = n_sub // blk

    def to_blocks(t):
        t = jnp.pad(t, ((0, 0), (0, s_pad - s), (0, 0), (0, 0)))
        t = t.reshape(b, n_sub, dilation, h, dh).transpose(0, 2, 3, 1, 4)
        return t.reshape(b, dilation, h, nblk, blk, dh)

    def with_prev(t):
        prev = jnp.pad(t[:, :, :, :-1], ((0, 0), (0, 0), (0, 0), (1, 0), (0, 0), (0, 0)))
        return jnp.concatenate([prev, t], axis=4)

    qb = to_blocks(q)
    kb = with_prev(to_blocks(k))
    vb = with_prev(to_blocks(v))
    scores = jnp.einsum('brhnqd,brhnkd->brhnqk', qb, kb) * (dh ** -0.5)
    qi = jnp.arange(blk)[:, None]
    kj = jnp.arange(2 * blk)[None, :]
    dist = blk + qi - kj
    band = (dist >= 0) & (dist <= blk)
    has_prev = jnp.arange(nblk)[:, None, None] > 0
    valid = band[None] & (has_prev | (kj >= blk)[None])
    scores = jnp.where(valid, scores, -jnp.inf)
    m = jnp.max(scores, axis=-1, keepdims=True)
    e = jnp.exp(scores - m)
    denom = jnp.sum(e, axis=-1)
    o = jnp.einsum('brhnqk,brhnkd->brhnqd', e, vb) / denom[..., None]
    lse = m[..., 0] + jnp.log(denom)
    o = o.reshape(b, dilation, h, n_sub, dh).transpose(0, 3, 1, 2, 4).reshape(b, s_pad, h, dh)[:, :s]
    lse = lse.reshape(b, dilation, h, n_sub).transpose(0, 3, 1, 2).reshape(b, s_pad, h)[:, :s]
    return o, lse


def causal_short_conv(x, w):
    kw = w.shape[0]
    s = x.shape[1]
    xp = jnp.pad(x, ((0, 0), (kw - 1, 0), (0, 0)))
    y = xp[:, kw - 1:kw - 1 + s] * w[kw - 1]
    for j in range(kw - 1):
        y = y + xp[:, j:j + s] * w[j]
    return jax.nn.silu(y)


def gated_delta_rule(q, k, v, beta, g):
    b, s, h, dk = q.shape
    dv = v.shape[-1]
    n = s // CHUNK

    def chunkify(t):
        return t.reshape(b, n, CHUNK, h, -1).transpose(1, 0, 3, 2, 4)

    qc, kc, vc = chunkify(q), chunkify(k), chunkify(v)
    bc = beta.reshape(b, n, CHUNK, h).transpose(1, 0, 3, 2)
    gcum = jnp.cumsum(g.reshape(b, n, CHUNK, h).transpose(1, 0, 3, 2), axis=-1)
    tril = jnp.tril(jnp.ones((CHUNK, CHUNK), dtype=bool))
    tril_strict = jnp.tril(jnp.ones((CHUNK, CHUNK), dtype=bool), -1)
    decay_mat = jnp.exp(jnp.where(tril, gcum[..., :, None] - gcum[..., None, :], -jnp.inf))
    kbeta = kc * bc[..., None]
    a_mat = jnp.where(tril_strict, jnp.einsum('nbhid,nbhjd->nbhij', kbeta, kc) * decay_mat, 0.0)
    eye = jnp.eye(CHUNK, dtype=jnp.float32)
    t_mat = lax.linalg.triangular_solve(eye + a_mat, jnp.broadcast_to(eye, a_mat.shape),
                                        left_side=True, lower=True, unit_diagonal=True)
    u = jnp.einsum('nbhij,nbhjd->nbhid', t_mat, vc * bc[..., None])
    w = jnp.einsum('nbhij,nbhjd->nbhid', t_mat, kbeta * jnp.exp(gcum)[..., None])
    attn_intra = jnp.where(tril, jnp.einsum('nbhid,nbhjd->nbhij', qc, kc) * decay_mat, 0.0)
    g_last = gcum[..., -1]
    q_dec = qc * jnp.exp(gcum)[..., None]
    k_tail = kc * jnp.exp(g_last[..., None] - gcum)[..., None]

    def step(state, xs):
        qd, ui, wi, ai, gl, kt = xs
        v_new = ui - jnp.einsum('bhcd,bhde->bhce', wi, state)
        o = jnp.einsum('bhcd,bhde->bhce', qd, state) + jnp.einsum('bhij,bhje->bhie', ai, v_new)
        state = state * jnp.exp(gl)[..., None, None] + jnp.einsum('bhcd,bhce->bhde', kt, v_new)
        return state, o

    s0 = jnp.zeros((b, h, dk, dv), jnp.float32)
    _, o = lax.scan(step, s0, (q_dec, u, w, attn_intra, g_last, k_tail))
    return o.transpose(1, 0, 3, 2, 4).reshape(b, s, h, dv)


def hierarchical_moe(hn, w_rg, b_rg, w_re, b_re, w_gate, w_up, w_down):
    bsz, s, d = hn.shape
    t = hn.reshape(-1, d)
    p_group = jax.nn.softmax(t @ w_rg.astype(jnp.float32) + b_rg.astype(jnp.float32), axis=-1)
    g_idx = jnp.argmax(p_group, axis=-1)
    p_g = jnp.take_along_axis(p_group, g_idx[:, None], axis=-1)
    e_logits = (t @ w_re.astype(jnp.float32) + b_re.astype(jnp.float32)).reshape(-1, N_EXPERT_GROUPS, EXPERTS_PER_GROUP)
    sel_logits = jnp.take_along_axis(e_logits, g_idx[:, None, None], axis=1)[:, 0]
    p_exp = jax.nn.softmax(sel_logits, axis=-1)
    top_p, top_i = lax.top_k(p_exp, TOP_K)
    top_p = top_p / jnp.sum(top_p, axis=-1, keepdims=True)
    w_in_group = jnp.sum(jax.nn.one_hot(top_i, EXPERTS_PER_GROUP) * top_p[..., None], axis=1)
    combine = jax.nn.one_hot(g_idx, N_EXPERT_GROUPS)[:, :, None] * (p_g * w_in_group)[:, None, :]
    y = jnp.zeros_like(t)
    for gi in range(N_EXPERT_GROUPS):
        hg = jnp.einsum('td,edf->tef', t, w_gate[gi])
        hu = jnp.einsum('td,edf->tef', t, w_up[gi])
        act = jax.nn.silu(hg) * hu * combine[:, gi, :, None]
        y = y + jnp.einsum('tef,efd->td', act, w_down[gi])
    return y.reshape(bsz, s, d)


def setup_inputs(seed: int = 0) -> dict:
    key = jax.random.key(seed)
    ks = jax.random.split(key, 24)
    f32 = jnp.float32

    def nrm(k, shape, scale):
        return jax.random.normal(k, shape, f32) * scale

    def gain(k, n):
        return 1.0 + 0.05 * jax.random.normal(k, (DEPTH, n), f32)

    a_init = jax.random.uniform(ks[6], (DEPTH, DN_HEADS), f32, 1.0, 16.0)
    dt = jnp.exp(jax.random.uniform(ks[7], (DEPTH, DN_HEADS), f32, float(np.log(1e-3)), float(np.log(1e-1))))
    dt_bias = dt + jnp.log(-jnp.expm1(-dt))
    return {
        "x": nrm(ks[0], (BATCH, SEQ, D_MODEL), 1.0),
        "p": nrm(ks[1], (DEPTH, BATCH, SEQ, PLE_DIM), 1.0),
        "norm_mix": gain(ks[2], D_MODEL),
        "w_in": nrm(ks[3], (DEPTH, D_MODEL, IN_TOTAL), D_MODEL ** -0.5),
        "q_norm": gain(ks[4], HEAD_DIM_A),
        "k_norm": gain(ks[5], HEAD_DIM_A),
        "conv_w": nrm(ks[8], (DEPTH, CONV_WIDTH, 3 * DN_WIDTH), CONV_WIDTH ** -0.5),
        "a_log": jnp.log(a_init),
        "dt_bias": dt_bias,
        "dn_out_norm": gain(ks[9], DN_HEAD_DIM),
        "w_branch_a": nrm(ks[10], (DEPTH, ATTN_GROUP_WIDTH, D_MODEL), ATTN_GROUP_WIDTH ** -0.5),
        "w_branch_b": nrm(ks[11], (DEPTH, DN_WIDTH, D_MODEL), DN_WIDTH ** -0.5),
        "w_out": nrm(ks[12], (DEPTH, D_MODEL, D_MODEL), D_MODEL ** -0.5),
        "norm_ffn": gain(ks[13], D_MODEL),
        "w_router_group": nrm(ks[14], (DEPTH, D_MODEL, N_EXPERT_GROUPS), D_MODEL ** -0.5),
        "b_router_group": nrm(ks[15], (DEPTH, N_EXPERT_GROUPS), 0.01),
        "w_router_expert": nrm(ks[16], (DEPTH, D_MODEL, N_EXPERTS), D_MODEL ** -0.5),
        "b_router_expert": nrm(ks[17], (DEPTH, N_EXPERTS), 0.01),
        "w_expert_gate": nrm(ks[18], (DEPTH, N_EXPERT_GROUPS, EXPERTS_PER_GROUP, D_MODEL, D_EXPERT), D_MODEL ** -0.5),
        "w_expert_up": nrm(ks[19], (DEPTH, N_EXPERT_GROUPS, EXPERTS_PER_GROUP, D_MODEL, D_EXPERT), D_MODEL ** -0.5),
        "w_expert_down": nrm(ks[20], (DEPTH, N_EXPERT_GROUPS, EXPERTS_PER_GROUP, D_EXPERT, D_MODEL), D_EXPERT ** -0.5),
        "norm_ple": gain(ks[21], D_MODEL),
        "w_ple": nrm(ks[22], (DEPTH, PLE_DIM, D_MODEL), PLE_DIM ** -0.5),
        "w_ple_gate": nrm(ks[23], (DEPTH, D_MODEL, D_MODEL), D_MODEL ** -0.5),
    }


def reference(x, p, norm_mix, w_in, q_norm, k_norm, conv_w, a_log, dt_bias, dn_out_norm,
              w_branch_a, w_branch_b, w_out, norm_ffn, w_router_group, b_router_group,
              w_router_expert, b_router_expert, w_expert_gate, w_expert_up, w_expert_down,
              norm_ple, w_ple, w_ple_gate):
    b, s, d = x.shape
    x = x.astype(jnp.float32)
    for i in range(DEPTH):
        h = rms_norm(x, norm_mix[i])
        proj = h @ w_in[i].astype(jnp.float32)
        qkv_a, qkv_b, z_b, beta_b, alpha_b, gate_a, gate_b = split_columns(proj, IN_WIDTHS)

        qkv_a = qkv_a.reshape(b, s, 3, N_ATTN_GROUPS, HEADS_PER_GROUP, HEAD_DIM_A)
        qa = rms_norm(qkv_a[:, :, 0], q_norm[i])
        ka = rms_norm(qkv_a[:, :, 1], k_norm[i])
        va = qkv_a[:, :, 2]
        outs, lses = [], []
        for gi, (win, dil) in enumerate(ATTN_GROUPS):
            o_g, lse_g = dilated_window_attention(qa[:, :, gi], ka[:, :, gi], va[:, :, gi], win, dil)
            outs.append(o_g)
            lses.append(lse_g)
        mix_w = jax.nn.softmax(jnp.stack(lses, axis=0), axis=0)
        o_a = jnp.sum(mix_w[..., None] * jnp.stack(outs, axis=0), axis=0).reshape(b, s, ATTN_GROUP_WIDTH)

        qkv_b = causal_short_conv(qkv_b, conv_w[i].astype(jnp.float32))
        qb, kb, vb = split_columns(qkv_b, (DN_WIDTH, DN_WIDTH, DN_WIDTH))
        qb = l2_norm(qb.reshape(b, s, DN_HEADS, DN_HEAD_DIM)) * (DN_HEAD_DIM ** -0.5)
        kb = l2_norm(kb.reshape(b, s, DN_HEADS, DN_HEAD_DIM))
        vb = vb.reshape(b, s, DN_HEADS, DN_HEAD_DIM)
        beta = jax.nn.sigmoid(beta_b)
        g = -jnp.exp(a_log[i].astype(jnp.float32)) * jax.nn.softplus(alpha_b + dt_bias[i].astype(jnp.float32))
        o_b = gated_delta_rule(qb, kb, vb, beta, g)
        o_b = rms_norm(o_b, dn_out_norm[i]) * jax.nn.silu(z_b.reshape(b, s, DN_HEADS, DN_HEAD_DIM))
        o_b = o_b.reshape(b, s, DN_WIDTH)

        merged = (jax.nn.sigmoid(gate_a) * (o_a @ w_branch_a[i].astype(jnp.float32))
                  + jax.nn.sigmoid(gate_b) * (o_b @ w_branch_b[i].astype(jnp.float32)))
        x = x + merged @ w_out[i].astype(jnp.float32)

        h2 = rms_norm(x, norm_ffn[i])
        x = x + hierarchical_moe(h2, w_router_group[i], b_router_group[i], w_router_expert[i],
                                 b_router_expert[i], w_expert_gate[i].astype(jnp.float32),
                                 w_expert_up[i].astype(jnp.float32), w_expert_down[i].astype(jnp.float32))

        h3 = rms_norm(x, norm_ple[i])
        ple = p[i].astype(jnp.float32) @ w_ple[i].astype(jnp.float32)
        x = x + jax.nn.sigmoid(h3 @ w_ple_gate[i].astype(jnp.float32)) * ple
    return x
```

```python
import numpy as np
import concourse.bass as bass
import concourse.mybir as mybir
from concourse.bass_utils import run_bass_kernel_spmd

F32 = mybir.dt.float32
BF16 = mybir.dt.bfloat16
ALU = mybir.AluOpType
AF = mybir.ActivationFunctionType
AX = mybir.AxisListType

ENGS = ("pe", "act", "dve", "pool")
DMAQ = ("sp", "gq")
NDSEM = 24
EPS = 1e-6
SB_BASE = 16512
SB_TOP = 229344


class T:
    __slots__ = ("name", "w", "rs", "banks")

    def __init__(self, name="", banks=()):
        self.name = name
        self.w = None
        self.rs = []
        self.banks = banks


class Bank:
    __slots__ = ("last",)

    def __init__(self):
        self.last = None


class Op:
    __slots__ = ("eng", "fn", "idx", "deps", "sig", "signo", "dslot", "dval", "clock", "gorder")


class Prog:
    def __init__(self):
        self.ops = {e: [] for e in ENGS + DMAQ}
        self.clock = {e: {} for e in ENGS + DMAQ}
        self.ndma = {q: 0 for q in DMAQ}
        self.g = 0
        self.pending = {e: [] for e in ENGS + DMAQ}

    def barrier(self):
        lasts = []
        for e in ENGS:
            if self.ops[e]:
                lasts.append(self.ops[e][-1])
        for q in DMAQ:
            lasts.extend(self.ops[q][-NDSEM:])
        self.pending["sp"] = list(lasts)
        op = self.add("sp", self.bar_fn)
        for e in ENGS + ("gq",):
            self.pending[e] = [op]

    def add(self, eng, fn, reads=(), writes=()):
        op = Op()
        op.eng = eng
        op.fn = fn
        op.idx = len(self.ops[eng])
        op.sig = False
        op.signo = None
        op.gorder = self.g
        self.g += 1
        deps = []
        for t in reads:
            if t.w is not None:
                deps.append((t.w, True))
        for t in writes:
            if t.w is not None:
                deps.append((t.w, False))
            for r in t.rs:
                deps.append((r, False))
        bks = []
        for t in tuple(reads) + tuple(writes):
            for b in t.banks:
                if b not in bks:
                    bks.append(b)
        for b in bks:
            if b.last is not None and b.last.eng != eng:
                deps.append((b.last, True))
        if self.pending[eng]:
            for d in self.pending[eng]:
                if d.eng != eng or eng in DMAQ:
                    deps.append((d, True))
            self.pending[eng] = []
        clk = self.clock[eng]
        need = {}
        for d, raw in deps:
            if d.eng == eng and eng not in DMAQ:
                if eng == "pe":
                    continue
            if d.eng in DMAQ:
                key = (d.eng, d.idx)
                if clk.get(key, False):
                    continue
                need[key] = d
            else:
                if clk.get(d.eng, -1) >= d.idx:
                    continue
                cur = need.get(d.eng)
                if cur is None or cur.idx < d.idx:
                    need[d.eng] = d
        op.deps = list(need.values())
        for d in op.deps:
            d.sig = True
            for k, v in d.clock.items():
                if isinstance(k, tuple):
                    clk[k] = True
                elif clk.get(k, -1) < v:
                    clk[k] = v
            if d.eng in DMAQ:
                clk[(d.eng, d.idx)] = True
            elif clk.get(d.eng, -1) < d.idx:
                clk[d.eng] = d.idx
        if eng in DMAQ:
            op.dslot = self.ndma[eng] % NDSEM
            op.dval = 16 * (self.ndma[eng] // NDSEM + 1)
            self.ndma[eng] += 1
            prev_i = op.idx - NDSEM
            if prev_i >= 0:
                pd = self.ops[eng][prev_i]
                if not clk.get((eng, prev_i), False):
                    op.deps.append(pd)
                    clk[(eng, prev_i)] = True
            op.sig = True
        if len(clk) > 400:
            for k in [k for k in clk if isinstance(k, tuple) and k[1] < self.ndma[k[0]] - 4 * NDSEM]:
                del clk[k]
        op.clock = dict(clk)
        if eng not in DMAQ:
            op.clock[eng] = op.idx
        self.ops[eng].append(op)
        for b in bks:
            b.last = op
        for t in reads:
            t.rs.append(op)
        for t in writes:
            t.w = op
            t.rs = []
        return op

    def emit(self, nc, final_waits=()):
        from contextlib import ExitStack
        with ExitStack() as es:
            sems = {e: es.enter_context(nc.semaphore("s_" + e)) for e in ENGS}
            dsems = {q: [es.enter_context(nc.semaphore(f"d_{q}{i}")) for i in range(NDSEM)] for q in DMAQ}
            for d in final_waits:
                d.sig = True
            for e in ENGS:
                n = 0
                for op in self.ops[e]:
                    if op.sig:
                        n += 1
                        op.signo = n
            block = es.enter_context(nc.Block())

            def waits(engobj, op):
                for d in op.deps:
                    if d.eng in DMAQ:
                        engobj.wait_ge(dsems[d.eng][d.dslot], d.dval)
                    else:
                        engobj.wait_ge(sems[d.eng], d.signo)

            def run(oplist, engobj, extra=None):
                for op in oplist:
                    waits(engobj, op)
                    ins = op.fn(engobj)
                    if op.sig:
                        if op.eng in DMAQ:
                            ins.then_inc(dsems[op.eng][op.dslot], 16)
                        else:
                            ins.then_inc(sems[op.eng], 1)
                if extra:
                    extra(engobj)

            def fin(engobj):
                for d in final_waits:
                    if d.eng in DMAQ:
                        engobj.wait_ge(dsems[d.eng][d.dslot], d.dval)
                    else:
                        engobj.wait_ge(sems[d.eng], d.signo)

            @block.tensor
            def _(e):
                run(self.ops["pe"], e)

            @block.scalar
            def _(e):
                run(self.ops["act"], e)

            @block.vector
            def _(e):
                run(self.ops["dve"], e)

            @block.gpsimd
            def _(e):
                merged = sorted(self.ops["pool"] + self.ops["gq"], key=lambda o: o.gorder)
                run(merged, e)

            @block.sync
            def _(e):
                run(self.ops["sp"], e, fin)


class KB:
    def __init__(self, nc):
        self.nc = nc
        self.P = Prog()
        self.off = SB_BASE
        self.n = 0
        self.banks = [nc.alloc_psum_tensor(f"bank{i}", [128, 512], F32) for i in range(8)]
        self.bk = [Bank() for _ in range(8)]

    def pst(self, *bank_ids):
        return T("ps", banks=tuple(self.bk[b] for b in bank_ids))

    def sb(self, shape, dt, name="t"):
        sz = int(np.prod(shape[1:])) * (2 if dt == BF16 else 4)
        sz = (sz + 31) // 32 * 32
        assert self.off + sz <= SB_TOP, f"sbuf overflow {name} {self.off} {sz}"
        self.n += 1
        t = self.nc.alloc_sbuf_tensor_at(f"{name}_{self.n}", list(shape), dt, offset=self.off)
        self.off += sz
        return t

    def ring(self, n, shape, dt, name="r"):
        return Ring([(self.sb(shape, dt, name), T(name)) for _ in range(n)])

    def psring(self, banks, nf32, name="ps"):
        per = 512 // nf32
        slots = []
        for i in range(per):
            for b in banks:
                slots.append((self.banks[b][:, i * nf32:(i + 1) * nf32], self.pst(b)))
        return Ring(slots)

    def mm(self, out, lhsT, rhs, r, w, start=True, stop=True):
        return self.P.add("pe", lambda e: e.matmul(out, lhsT=lhsT, rhs=rhs, start=start, stop=stop), reads=r, writes=w)

    def tr(self, out, in_, ident, r, w):
        return self.P.add("pe", lambda e: e.transpose(out=out, in_=in_, identity=ident), reads=r, writes=w)

    def act(self, out, in_, func, r, w, bias=None, scale=None, accum_out=None, eng="act"):
        kw = {}
        if bias is not None:
            kw["bias"] = bias
        if scale is not None:
            kw["scale"] = scale
        if accum_out is not None:
            kw["accum_out"] = accum_out
        return self.P.add("act", lambda e: e.activation(out=out, in_=in_, func=func, **kw), reads=r, writes=w)

    def cp(self, eng, out, in_, r, w):
        if eng == "act":
            return self.P.add("act", lambda e: e.copy(out=out, in_=in_), reads=r, writes=w)
        return self.P.add(eng, lambda e: e.tensor_copy(out=out, in_=in_), reads=r, writes=w)

    def tt(self, eng, out, in0, in1, op, r, w):
        return self.P.add(eng, lambda e: e.tensor_tensor(out=out, in0=in0, in1=in1, op=op), reads=r, writes=w)

    def ts(self, eng, out, in0, s1, op0, r, w, s2=None, op1=None):
        if op1 is None:
            return self.P.add(eng, lambda e: e.tensor_scalar(out=out, in0=in0, scalar1=s1, scalar2=None, op0=op0), reads=r, writes=w)
        return self.P.add(eng, lambda e: e.tensor_scalar(out=out, in0=in0, scalar1=s1, scalar2=s2, op0=op0, op1=op1), reads=r, writes=w)

    def stt(self, eng, out, in0, scalar, in1, op0, op1, r, w):
        return self.P.add(eng, lambda e: e.scalar_tensor_tensor(out=out, in0=in0, scalar=scalar, in1=in1, op0=op0, op1=op1), reads=r, writes=w)

    def red(self, eng, out, in_, r, w, op=ALU.add):
        return self.P.add(eng, lambda e: e.tensor_reduce(out=out, in_=in_, axis=AX.X, op=op), reads=r, writes=w)

    def recip(self, out, in_, r, w):
        return self.P.add("dve", lambda e: e.reciprocal(out=out, in_=in_), reads=r, writes=w)

    def memset(self, eng, ap, val, w):
        return self.P.add(eng, lambda e: e.memset(ap, val), writes=w)

    def dma(self, q, out, in_, r, w):
        return self.P.add(q, lambda e: e.dma_start(out=out, in_=in_), reads=r, writes=w)


class Ring:
    def __init__(self, slots):
        self.slots = slots
        self.i = 0

    def next(self):
        s = self.slots[self.i % len(self.slots)]
        self.i += 1
        return s


def bfv(ap):
    return ap.bitcast(BF16)


def build(S, debug=False, stop=None, skip_pre=False):
    nc = bass.Bass("TRN2", target_bir_lowering=False)
    NT = S // 128
    NSP = S // 512
    kb = KB(nc)
    P = kb.P

    def din(name, shape, dt=F32):
        return nc.dram_tensor(name, list(shape), dt, kind="ExternalInput").ap()

    def dscr(name, shape, dt):
        return nc.dram_tensor(name, list(shape), dt, kind="ExternalOutput" if debug else "Internal").ap()

    x = din("x", [S, 1024])
    p_in = din("p", [S, 256])
    norm_mix = din("norm_mix", [1024])
    w_in = din("w_in", [1024, 10768])
    q_norm = din("q_norm", [64])
    k_norm = din("k_norm", [64])
    conv_w = din("conv_w", [128, 24, 4])
    a_log = din("a_log", [8])
    dt_bias = din("dt_bias", [8])
    dn_out_norm = din("dn_out_norm", [128])
    w_branch_a = din("w_branch_a", [512, 1024])
    w_branch_b = din("w_branch_b", [1024, 1024])
    w_out = din("w_out", [1024, 1024])
    norm_ffn = din("norm_ffn", [1024])
    w_rg = din("w_router_group", [1024, 4])
    b_rg = din("b_router_group", [4])
    w_re = din("w_router_expert", [1024, 16])
    b_re = din("b_router_expert", [16])
    w_eg = din("w_expert_gate", [16, 1024, 256])
    w_eu = din("w_expert_up", [16, 1024, 256])
    w_ed = din("w_expert_down", [16, 256, 1024])
    norm_ple = din("norm_ple", [1024])
    w_ple = din("w_ple", [256, 1024])
    w_pg = din("w_ple_gate", [1024, 1024])
    c_ident = din("c_ident", [128, 128])
    c_triu = din("c_triu", [128, 128])
    c_tril = din("c_tril", [128, 128])
    out = nc.dram_tensor("out", [S, 1024], F32, kind="ExternalOutput").ap()

    qkva_s = dscr("qkva_s", [9, S, 512], BF16)
    qkvb_s = dscr("qkvb_s", [24, 128, S], BF16)
    z_s = dscr("z_s", [S, 1024], BF16)
    bg_s = dscr("bg_s", [S, 16], F32)
    gates_s = dscr("gates_s", [S, 2048], BF16)
    ua_s = dscr("ua_s", [3, S, 520], F32)
    ob_s = dscr("ob_s", [S, 1024], BF16)
    wgu_s = nc.dram_tensor("wgu_s", [16, 1024, 512], BF16, kind="Internal").ap()
    wd_s = nc.dram_tensor("wd_s", [16, 256, 1024], BF16, kind="Internal").ap()

    ident = kb.sb([128, 128], F32, "ident"); t_ident = T()
    ident_bf = kb.sb([128, 128], BF16, "identbf"); t_identbf = T()
    triu = kb.sb([128, 128], F32, "triu"); t_triu = T()
    tril_bf = kb.sb([128, 128], BF16, "trilbf"); t_trilbf = T()
    triu_bf = kb.sb([128, 128], BF16, "triubf"); t_triubf = T()
    tril = kb.sb([128, 128], F32, "tril"); t_tril = T()
    mbm = kb.sb([128, 128], F32, "mbm"); t_mbm = T()
    strict = kb.sb([128, 128], F32, "strict"); t_strict = T()
    ones_bf = kb.sb([128, 128], BF16, "ones"); t_ones = T()
    epsc = kb.sb([128, 1], F32, "eps"); t_eps = T()
    eps128 = kb.sb([128, 1], F32, "eps128"); t_eps128 = T()
    kb.dma("sp", ident[:], c_ident[:, :], [], [t_ident])
    kb.dma("sp", triu[:], c_triu[:, :], [], [t_triu])
    kb.dma("sp", tril[:], c_tril[:, :], [], [t_tril])
    kb.cp("dve", ident_bf[:], ident[:], [t_ident], [t_identbf])
    kb.cp("dve", triu_bf[:], triu[:], [t_triu], [t_triubf])
    kb.cp("dve", tril_bf[:], tril[:], [t_tril], [t_trilbf])
    kb.ts("dve", mbm[:], triu[:], -1.0, ALU.add, [t_triu], [t_mbm], s2=1e9, op1=ALU.mult)
    kb.tt("dve", strict[:], triu[:], ident[:], ALU.subtract, [t_triu, t_ident], [t_strict])
    kb.memset("pool", ones_bf[:], 1.0, [t_ones])
    kb.memset("pool", epsc[:], EPS, [t_eps])
    kb.memset("pool", eps128[:], 128.0 * EPS, [t_eps128])

    t_wgu = [T() for _ in range(16)]
    t_wd = [T() for _ in range(16)]
    for e_ in range(0 if not skip_pre else 16, 16):
        kb.dma("gq", wgu_s[e_, :, 0:256], w_eg[e_], [], [t_wgu[e_]])
        kb.dma("gq", wgu_s[e_, :, 256:512], w_eu[e_], [], [t_wgu[e_]])
        kb.dma("gq", wd_s[e_], w_ed[e_], [], [t_wd[e_]])

    bar_a = kb.sb([128, 8], F32, "bar_a")
    bar_b = kb.sb([128, 8], F32, "bar_b")
    kb.memset("pool", bar_a[:], 0.0, [])
    P.bar_fn = lambda e: e.dma_start(out=bar_b[:], in_=bar_a[:])
    persist_off = kb.off

    def finish_early():
        fw = []
        for q in DMAQ:
            fw.extend(P.ops[q][-NDSEM:])
        for e in ENGS:
            if P.ops[e]:
                fw.append(P.ops[e][-1])
        P.emit(nc, final_waits=fw)
        return nc
    gain_mix = kb.sb([128, 1024], F32, "gmix"); t_gmix = T()
    kb.dma("sp", gain_mix[:], norm_mix.partition_broadcast(128), [], [t_gmix])
    hT = kb.sb([128, 8, S], BF16, "hT")
    t_hT = [T() for _ in range(NT)]
    a_off = kb.off
    xr = kb.ring(2, [128, 1024], F32, "xt")
    junk = kb.ring(2, [128, 1024], BF16, "junk")
    hb = kb.ring(2, [128, 1024], BF16, "hb")
    ssr = kb.ring(4, [128, 1], F32, "ss")
    psA0 = kb.psring([0, 1], 512, "psA0")

    def rms_rstd(ss_ap, t_ss, nfeat_scale_done=True):
        kb.act(ss_ap, ss_ap, AF.Sqrt, [t_ss, t_eps], [t_ss], bias=epsc[:, 0:1])
        kb.recip(ss_ap, ss_ap, [t_ss], [t_ss])

    for t in range(NT):
        xt, t_xt = xr.next()
        jk, t_jk = junk.next()
        h_, t_h = hb.next()
        ss, t_ss = ssr.next()
        kb.dma("sp", xt[:], x[t * 128:(t + 1) * 128, :], [], [t_xt])
        kb.memset("pool", ss[:], 0.0, [t_ss])
        kb.act(jk[:], xt[:], AF.Square, [t_xt, t_ss], [t_jk, t_ss], scale=1.0 / 32.0, accum_out=ss[:])
        rms_rstd(ss[:], t_ss)
        kb.stt("dve", h_[:], xt[:], ss[:, 0:1], gain_mix[:], ALU.mult, ALU.mult, [t_xt, t_ss, t_gmix], [t_h])
        ps, t_ps = psA0.next()
        psv = bfv(ps).rearrange("p (k c) -> p k c", k=8)
        for k in range(8):
            kb.tr(psv[:, k, :], h_[:, k * 128:(k + 1) * 128], ident_bf[:], [t_h, t_identbf], [t_ps])
        kb.cp("act" if t % 2 else "dve", hT[:, :, t * 128:(t + 1) * 128], psv, [t_ps], [t_hT[t]])

    if stop == "A0":
        return finish_early()
    P.barrier()
    kb.off = a_off
    qg = kb.sb([128, 64], F32, "qg"); t_qg = T()
    kg = kb.sb([128, 64], F32, "kg"); t_kg = T()
    kb.dma("sp", qg[:], q_norm.partition_broadcast(128), [], [t_qg])
    kb.dma("sp", kg[:], k_norm.partition_broadcast(128), [], [t_kg])
    kb.ts("dve", qg[:], qg[:], 0.125, ALU.mult, [t_qg], [t_qg])
    convw = kb.sb([128, 24, 4], F32, "convw"); t_convw = T()
    kb.dma("sp", convw[:], conv_w[:, :, :], [], [t_convw])
    dtb = kb.sb([128, 8], F32, "dtb"); t_dtb = T()
    negA = kb.sb([128, 8], F32, "negA"); t_negA = T()
    kb.dma("sp", dtb[:], dt_bias.partition_broadcast(128), [], [t_dtb])
    kb.dma("sp", negA[:], a_log.partition_broadcast(128), [], [t_negA])
    kb.act(negA[:], negA[:], AF.Exp, [t_negA], [t_negA])
    kb.ts("dve", negA[:], negA[:], -1.0, ALU.mult, [t_negA], [t_negA])

    wring = kb.ring(2, [128, 8, 512], BF16, "wg")
    wsm = kb.sb([128, 8, 16], BF16, "wsm"); t_wsm = T()
    psA = kb.psring([0, 1, 2, 3, 4, 5], 512, "psA")
    psS = kb.psring([6, 7], 512, "psS")
    f32r = kb.ring(3, [128, 512], F32, "f32r")
    f32r2 = kb.ring(3, [128, 512], F32, "f32r2")
    bfr = kb.ring(4, [128, 512], BF16, "bfr")
    s8r = kb.ring(4, [128, 8], F32, "s8")
    rawr = kb.ring(3, [128, 515], F32, "raw")
    carry = [(kb.sb([128, 3], F32, "carry"), T()) for _ in range(4)]
    yr = kb.ring(3, [128, 512], F32, "y")
    sqr = kb.ring(2, [128, 512], BF16, "sqb")
    bgr = kb.ring(3, [128, 16], F32, "bg")
    w_in_v = w_in.rearrange("(k p) c -> p k c", p=128)

    for cg in range(22):
        if cg < 17:
            c0 = cg * 512
        elif cg == 17:
            c0 = 8704
        else:
            c0 = 8720 + (cg - 18) * 512
        if cg == 17:
            W, t_W = wsm, t_wsm
            kb.dma("gq", wsm[:], w_in_v[:, :, c0:c0 + 16], [], [t_wsm])
        else:
            W, t_W = wring.next()
            kb.dma("gq", W[:, 0:4, :], w_in_v[:, 0:4, c0:c0 + 512], [], [t_W])
            kb.dma("gq", W[:, 4:8, :], w_in_v[:, 4:8, c0:c0 + 512], [], [t_W])
        if 9 <= cg <= 14:
            for s in range(NSP):
                for cb in range(4):
                    cbg = (cg - 9) * 4 + cb
                    which = cbg // 8
                    ps, t_ps = psA.next()
                    for k in range(8):
                        kb.mm(ps, W[:, k, cb * 128:(cb + 1) * 128], hT[:, k, s * 512:(s + 1) * 512],
                              [t_W] + t_hT[4 * s:4 * s + 4], [t_ps], start=(k == 0), stop=(k == 7))
                    raw, t_raw = rawr.next()
                    cy, t_cy = carry[cb]
                    if s == 0:
                        kb.memset("pool", raw[:, 0:3], 0.0, [t_raw])
                    else:
                        kb.cp("pool", raw[:, 0:3], cy[:], [t_cy], [t_raw])
                    kb.cp("act", raw[:, 3:515], ps, [t_ps], [t_raw])
                    kb.cp("pool", cy[:], raw[:, 512:515], [t_raw], [t_cy])
                    y, t_y = yr.next()
                    kb.ts("dve", y[:], raw[:, 3:515], convw[:, cbg, 3:4], ALU.mult, [t_raw, t_convw], [t_y])
                    for j in range(3):
                        kb.stt("dve", y[:], raw[:, j:j + 512], convw[:, cbg, j:j + 1], y[:], ALU.mult, ALU.add,
                               [t_raw, t_convw, t_y], [t_y])
                    ob, t_ob = bfr.next()
                    if which == 2:
                        kb.act(ob[:], y[:], AF.Silu, [t_y], [t_ob])
                    else:
                        kb.act(y[:], y[:], AF.Silu, [t_y], [t_y])
                        sq, t_sq = sqr.next()
                        kb.tt("pool", sq[:], y[:], y[:], ALU.mult, [t_y], [t_sq])
                        pss, t_pss = psS.next()
                        kb.mm(pss, ones_bf[:], sq[:], [t_ones, t_sq], [t_pss])
                        rn, t_rn = f32r.next()
                        if which == 0:
                            kb.act(rn[:], pss, AF.Sqrt, [t_pss, t_eps128], [t_rn], bias=eps128[:, 0:1], scale=128.0)
                        else:
                            kb.act(rn[:], pss, AF.Sqrt, [t_pss, t_eps], [t_rn], bias=epsc[:, 0:1])
                        kb.recip(rn[:], rn[:], [t_rn], [t_rn])
                        kb.tt("pool", ob[:], y[:], rn[:], ALU.mult, [t_y, t_rn], [t_ob])
                    kb.dma("sp", qkvb_s[cbg, :, s * 512:(s + 1) * 512], ob[:], [t_ob], [])
        else:
            for t in range(NT):
                rows = slice(t * 128, (t + 1) * 128)
                ps, t_ps = psA.next()
                ncol = 16 if cg == 17 else 512
                pso = ps[:, 0:ncol]
                for k in range(8):
                    kb.mm(pso, hT[:, k, t * 128:(t + 1) * 128], W[:, k, :], [t_W, t_hT[t]], [t_ps],
                          start=(k == 0), stop=(k == 7))
                if cg < 6:
                    gain, t_gain = (qg, t_qg) if cg < 3 else (kg, t_kg)
                    sqf, t_sqf = f32r.next()
                    kb.act(sqf[:], ps, AF.Square, [t_ps], [t_sqf], scale=0.125)
                    s8, t_s8 = s8r.next()
                    kb.red("dve", s8[:], sqf[:].rearrange("p (h d) -> p h d", d=64), [t_sqf], [t_s8])
                    rms_rstd(s8[:], t_s8)
                    tmp, t_tmp = f32r2.next()
                    kb.tt("dve", tmp[:].rearrange("p (h d) -> p h d", d=64), ps.rearrange("p (h d) -> p h d", d=64),
                          s8[:, :].unsqueeze(2).to_broadcast([128, 8, 64]), ALU.mult, [t_ps, t_s8], [t_tmp])
                    ob, t_ob = bfr.next()
                    kb.tt("pool", ob[:].rearrange("p (h d) -> p h d", d=64), tmp[:].rearrange("p (h d) -> p h d", d=64),
                          gain[:, :].unsqueeze(1).to_broadcast([128, 8, 64]), ALU.mult, [t_tmp, t_gain], [t_ob])
                    kb.dma("sp", qkva_s[cg, rows, :], ob[:], [t_ob], [])
                elif cg < 9:
                    ob, t_ob = bfr.next()
                    kb.cp("act" if t % 2 else "dve", ob[:], ps, [t_ps], [t_ob])
                    kb.dma("sp", qkva_s[cg, rows, :], ob[:], [t_ob], [])
                elif cg in (15, 16):
                    ob, t_ob = bfr.next()
                    kb.act(ob[:], ps, AF.Silu, [t_ps], [t_ob])
                    kb.dma("sp", z_s[rows, (cg - 15) * 512:(cg - 14) * 512], ob[:], [t_ob], [])
                elif cg == 17:
                    bg, t_bg = bgr.next()
                    kb.act(bg[:, 0:8], ps[:, 0:8], AF.Sigmoid, [t_ps], [t_bg])
                    kb.tt("dve", bg[:, 8:16], ps[:, 8:16], dtb[:], ALU.add, [t_ps, t_dtb], [t_bg])
                    kb.act(bg[:, 8:16], bg[:, 8:16], AF.Exp, [t_bg], [t_bg])
                    kb.act(bg[:, 8:16], bg[:, 8:16], AF.Ln, [t_bg], [t_bg], bias=1.0)
                    kb.tt("dve", bg[:, 8:16], bg[:, 8:16], negA[:], ALU.mult, [t_bg, t_negA], [t_bg])
                    kb.dma("sp", bg_s[rows, :], bg[:], [t_bg], [])
                else:
                    ob, t_ob = bfr.next()
                    kb.act(ob[:], ps, AF.Sigmoid, [t_ps], [t_ob])
                    kb.dma("sp", gates_s[rows, (cg - 18) * 512:(cg - 17) * 512], ob[:], [t_ob], [])

    print("sbuf end A", kb.off)
    if stop == "A":
        return finish_early()
    P.barrier()
    kb.off = persist_off
    qr_ = kb.ring(2, [128, 512], BF16, "qb")
    kr_ = kb.ring(2, [128, 512], BF16, "kb")
    vr_ = [(kb.sb([128, 8, 65], BF16, "v1"), T()) for _ in range(3)]
    for v1, t_v1 in vr_:
        kb.memset("pool", v1[:, :, 64:65], 1.0, [t_v1])
    vst_r = kb.ring(2, [128, 512], BF16, "vst")
    qTr = kb.ring(2, [128, 8, 128], BF16, "qT")
    for qz, t_qz in qTr.slots:
        kb.memset("pool", qz[:], 0.0, [t_qz])
    kTr = [(kb.sb([128, 4, 128], BF16, "kT"), T()) for _ in range(3)]
    er_ = kb.ring(3, [128, 4, 2, 128], BF16, "E")
    ur_ = kb.ring(2, [128, 8, 65], F32, "U")
    psSc = kb.psring([0, 1, 2, 3], 1024, "psSc") if False else None
    sc_slots = Ring([((kb.banks[0][:, :], kb.banks[1][:, :]), kb.pst(0, 1)), ((kb.banks[2][:, :], kb.banks[3][:, :]), kb.pst(2, 3))])
    pv_slots = Ring([(kb.banks[4][:, :], kb.pst(4)), (kb.banks[5][:, :], kb.pst(5))])
    psT = Ring([(kb.banks[6][:, :], kb.pst(6)), (kb.banks[7][:, :], kb.pst(7))])
    ATT = ((128, 1), (512, 4), (2048, 16))
    for g, (win, dil) in enumerate(ATT):
        nblk = S // dil // 128
        qs = qkva_s[g].rearrange("(u r) c -> r u c", r=dil)
        ks = qkva_s[3 + g].rearrange("(u r) c -> r u c", r=dil)
        vs = qkva_s[6 + g].rearrange("(u r) c -> r u c", r=dil)
        us = ua_s[g].rearrange("(u r) c -> r u c", r=dil)
        cnt = 0
        for r_ in range(dil):
            for nb in range(nblk):
                ur = slice(nb * 128, (nb + 1) * 128)
                qb_, t_qb = qr_.next()
                kb_, t_kb = kr_.next()
                v1, t_v1 = vr_[cnt % 3]
                kb.dma("sp", qb_[:], qs[r_, ur, :], [], [t_qb])
                kb.dma("sp", kb_[:], ks[r_, ur, :], [], [t_kb])
                vst, t_vst = vst_r.next()
                kb.dma("sp", vst[:], vs[r_, ur, :], [], [t_vst])
                kb.cp("dve", v1[:, :, 0:64], vst[:].rearrange("p (h d) -> p h d", d=64), [t_vst], [t_v1])
                pt, t_pt = psT.next()
                ptv = bfv(pt).rearrange("p (a h c) -> p a h c", a=2, h=4)
                for hp in range(4):
                    kb.tr(ptv[:, 0, hp, :], qb_[:, hp * 128:(hp + 1) * 128], ident_bf[:], [t_qb, t_identbf], [t_pt])
                    kb.tr(ptv[:, 1, hp, :], kb_[:, hp * 128:(hp + 1) * 128], ident_bf[:], [t_kb, t_identbf], [t_pt])
                qT, t_qT = qTr.next()
                kT, t_kT = kTr[cnt % 3]
                qTv = qT[:].rearrange("p (hp j) c -> p hp j c", j=2)
                kb.cp("dve", qTv[0:64, :, 0, :], ptv[0:64, 0], [t_pt], [t_qT])
                kb.cp("dve", qTv[64:128, :, 1, :], ptv[64:128, 0], [t_pt], [t_qT])
                kb.cp("act", kT[:], ptv[:, 1], [t_pt], [t_kT])
                have_prev = nb > 0
                if have_prev:
                    kTp, t_kTp = kTr[(cnt - 1) % 3]
                    v1p, t_v1p = vr_[(cnt - 1) % 3]
                U_, t_U = ur_.next()
                for half in range(2):
                    (b0, b1), t_sc = sc_slots.next()
                    E, t_E = er_.next()
                    for hh in range(4):
                        h = half * 4 + hh
                        hp, lo = h // 2, (h % 2) * 64
                        bank = b0 if hh < 2 else b1
                        base = (hh % 2) * 256
                        if have_prev:
                            kb.mm(bank[:, base:base + 128], kTp[:, hp, :], qT[:, h, :],
                                  [t_kTp, t_qT], [t_sc])
                        kb.mm(bank[:, base + 128:base + 256], kT[:, hp, :], qT[:, h, :],
                              [t_kT, t_qT], [t_sc])
                    for bi, bank in enumerate((b0, b1)):
                        ev = E[:, 2 * bi:2 * bi + 2, :, :]
                        bv = bank.rearrange("p (h a c) -> p h a c", h=2, a=2)
                        if have_prev:
                            kb.act(ev, bv, AF.Exp, [t_sc], [t_E])
                        else:
                            kb.act(ev[:, :, 1, :], bv[:, :, 1, :], AF.Exp, [t_sc], [t_E])
                    if have_prev:
                        kb.tt("pool", E[:, :, 0, :], E[:, :, 0, :], tril_bf[:, :].unsqueeze(1).to_broadcast([128, 4, 128]),
                              ALU.mult, [t_E, t_trilbf], [t_E])
                    kb.tt("dve", E[:, :, 1, :], E[:, :, 1, :], triu_bf[:, :].unsqueeze(1).to_broadcast([128, 4, 128]),
                          ALU.mult, [t_E, t_triubf], [t_E])
                    pv, t_pv = pv_slots.next()
                    pvv = pv[:, 0:260].rearrange("p (h c) -> p h c", h=4)
                    for hh in range(4):
                        h = half * 4 + hh
                        if have_prev:
                            kb.mm(pvv[:, hh, :], E[:, hh, 0, :], v1p[:, h, :], [t_E, t_v1p], [t_pv], start=True, stop=False)
                        kb.mm(pvv[:, hh, :], E[:, hh, 1, :], v1[:, h, :], [t_E, t_v1], [t_pv], start=not have_prev, stop=True)
                    kb.cp("act" if half else "dve", U_[:, half * 4:half * 4 + 4, :], pvv, [t_pv], [t_U])
                kb.dma("sp", us[r_, ur, :], U_[:].rearrange("p h c -> p (h c)"), [t_U], [])
                cnt += 1

    print("sbuf end B", kb.off)
    if stop == "B":
        return finish_early()
    P.barrier()
    kb.off = persist_off
    dng = kb.sb([128, 128], F32, "dng"); t_dng = T()
    kb.dma("sp", dng[:], dn_out_norm.partition_broadcast(128), [], [t_dng])
    S32 = [(kb.sb([128, 128], F32, "S32"), T()) for _ in range(8)]
    Sbf = [(kb.sb([128, 128], BF16, "Sbf"), T()) for _ in range(8)]
    for h in range(8):
        kb.memset("pool", S32[h][0][:], 0.0, [S32[h][1]])
        kb.memset("pool", Sbf[h][0][:], 0.0, [Sbf[h][1]])
    qkvr = kb.ring(2, [128, 24, 128], BF16, "qkvT")
    bgr2 = kb.ring(2, [128, 16], F32, "bgc")
    zr = kb.ring(2, [128, 1024], BF16, "z")
    gbc_r = kb.ring(2, [128, 8, 128], F32, "gbc")
    gcum_r = kb.ring(2, [128, 8], F32, "gcum")
    ngc_r = kb.ring(2, [128, 8], F32, "ngc")
    eg_r = kb.ring(2, [128, 8], F32, "eg")
    neg_r = kb.ring(2, [128, 8], F32, "neg")
    kts_r = kb.ring(2, [128, 8], F32, "kts")
    egl_r = kb.ring(2, [128, 8], F32, "egl")
    H = 8
    pre_r = kb.ring(4, [128, 128], F32, "pre")
    dtm_r = kb.ring(2 * H, [128, 128], F32, "dtm")
    dts_r = kb.ring(2 * H, [128, 128], F32, "dts")
    B_r = kb.ring(H + 2, [128, 128], F32, "B")
    BT_r = kb.ring(H + 2, [128, 128], F32, "BT")
    SA_r = kb.ring(2 * H + 2, [128, 128], F32, "SA")
    SB_r = kb.ring(2 * H + 2, [128, 128], F32, "SB")
    R_r = kb.ring(2 * H + 2, [128, 128], F32, "R")
    PT_r = kb.ring(2 * H, [128, 128], BF16, "PT")
    AIT_r = kb.ring(2 * H, [128, 128], BF16, "AIT")
    Ktl_r = kb.ring(2 * H, [128, 128], BF16, "Ktl")
    Vtm_r = kb.ring(2 * H, [128, 128], BF16, "Vtm")
    Y_r = kb.ring(4, [128, 128], BF16, "Y")
    vn_r = kb.ring(4, [128, 128], BF16, "vn")
    o1_r = kb.ring(4, [128, 128], F32, "o1")
    O_r = kb.ring(2, [128, 8, 128], F32, "O")
    osq_r = kb.ring(2, [128, 8, 128], F32, "osq")
    obf_r = kb.ring(2, [128, 1024], BF16, "obf")
    s8c_r = kb.ring(2, [128, 8], F32, "s8c")
    glast_r = kb.ring(2, [128, 8], F32, "glast")
    psN = kb.psring([0, 1, 2, 3], 128, "psN")
    psXO = kb.psring([4, 5], 128, "psXO")
    psG = kb.psring([6], 128, "psG")
    psX = Ring([(kb.banks[7][:, 0:128], kb.pst(7)), (kb.banks[7][:, 128:256], kb.pst(7))])
    psB = Ring([(kb.banks[7][:, 256 + 64 * i:256 + 64 * (i + 1)], kb.pst(7)) for i in range(4)])

    for b in range(NT):
        cols = slice(b * 128, (b + 1) * 128)
        qkvT, t_qkvT = qkvr.next()
        bgc, t_bgc = bgr2.next()
        zt, t_zt = zr.next()
        kb.dma("sp", qkvT[:], qkvb_s[:, :, cols].rearrange("c p t -> p c t"), [], [t_qkvT])
        kb.dma("sp", bgc[:], bg_s[cols, :], [], [t_bgc])
        kb.dma("sp", zt[:], z_s[cols, :], [], [t_zt])
        gcum, t_gcum = gcum_r.next()
        psg, t_psg = psG.next()
        kb.mm(psg[:, 0:8], triu[:], bgc[:, 8:16], [t_triu, t_bgc], [t_psg])
        kb.cp("dve", gcum[:], psg[:, 0:8], [t_psg], [t_gcum])
        gbc, t_gbc = gbc_r.next()
        kb.cp("pool", gbc[:], bgc[:, 8:16].unsqueeze(2).to_broadcast([128, 8, 128]), [t_bgc], [t_gbc])
        eg, t_eg = eg_r.next()
        neg, t_neg = neg_r.next()
        kb.act(eg[:], gcum[:], AF.Exp, [t_gcum], [t_eg])
        kb.ts("dve", neg[:], eg[:], -1.0, ALU.mult, [t_eg], [t_neg])
        kts, t_kts = kts_r.next()
        egl, t_egl = egl_r.next()
        O_, t_O = O_r.next()
        glast, t_glast = glast_r.next()
        st = []
        for h in range(H):
            psg, t_psg = psG.next()
            kb.mm(psg, gbc[:, h, :], triu[:], [t_gbc, t_triu], [t_psg])
            pre, t_pre = pre_r.next()
            kb.stt("dve", pre[:], psg, gcum[:, h:h + 1], mbm[:], ALU.subtract, ALU.add, [t_psg, t_gcum, t_mbm], [t_pre])
            dtm, t_dtm = dtm_r.next()
            kb.act(dtm[:], pre[:], AF.Exp, [t_pre], [t_dtm])
            dts, t_dts = dts_r.next()
            kb.tt("pool", dts[:], dtm[:], strict[:], ALU.mult, [t_dtm, t_strict], [t_dts])
            kb.cp("act", glast[:, h:h + 1], psg[:, 127:128], [t_psg], [t_glast])
            st.append(dict(dtm=dtm, t_dtm=t_dtm, dts=dts, t_dts=t_dts))
        kb.act(egl[:], glast[:], AF.Exp, [t_glast], [t_egl])
        kb.tt("dve", kts[:], glast[:], gcum[:], ALU.subtract, [t_glast, t_gcum], [t_kts])
        kb.act(kts[:], kts[:], AF.Exp, [t_kts], [t_kts])
        for h in range(H):
            d = st[h]
            dtm, t_dtm, dts, t_dts = d["dtm"], d["t_dtm"], d["dts"], d["t_dts"]
            KT = qkvT[:, 8 + h, :]
            QT = qkvT[:, h, :]
            VT = qkvT[:, 16 + h, :]
            pkk, t_pkk = psN.next()
            pkq, t_pkq = psN.next()
            kb.mm(pkk, KT, KT, [t_qkvT], [t_pkk])
            kb.mm(pkq, KT, QT, [t_qkvT], [t_pkq])
            B, t_B = B_r.next()
            kb.stt("dve", B[:], pkk, bgc[:, h:h + 1], dts[:], ALU.mult, ALU.mult, [t_pkk, t_bgc, t_dts], [t_B])
            AIT, t_AIT = AIT_r.next()
            kb.tt("dve", AIT[:], pkq, dtm[:], ALU.mult, [t_pkq, t_dtm], [t_AIT])
            pb, t_pb = psB.next()
            kb.tr(bfv(pb), KT, ident_bf[:], [t_qkvT, t_identbf], [t_pb])
            Ktl, t_Ktl = Ktl_r.next()
            kb.act(Ktl[:], bfv(pb), AF.Copy, [t_pb, t_kts], [t_Ktl], scale=kts[:, h:h + 1])
            pb2, t_pb2 = psB.next()
            kb.tr(bfv(pb2), VT, ident_bf[:], [t_qkvT, t_identbf], [t_pb2])
            Vtm, t_Vtm = Vtm_r.next()
            kb.cp("act", Vtm[:], bfv(pb2), [t_pb2], [t_Vtm])
            d.update(B=B, t_B=t_B, AIT=AIT, t_AIT=t_AIT, Ktl=Ktl, t_Ktl=t_Ktl, Vtm=Vtm, t_Vtm=t_Vtm, KT=KT, QT=QT)
        for h in range(H):
            d = st[h]
            pt_, t_pt_ = psN.next()
            kb.tr(pt_, d["B"][:], ident[:], [d["t_B"], t_ident], [t_pt_])
            BT, t_BT = BT_r.next()
            kb.cp("act", BT[:], pt_, [t_pt_], [t_BT])
            R, t_R = R_r.next()
            kb.tt("pool", R[:], ident[:], d["B"][:], ALU.subtract, [t_ident, d["t_B"]], [t_R])
            d.update(Sk=d["B"], t_Sk=d["t_B"], SkT=BT, t_SkT=t_BT, R=R, t_R=t_R)
        NL = 6
        for lvl in range(1, NL + 1):
            last = lvl == NL
            for h in range(H):
                d = st[h]
                pT, t_pT = psN.next()
                kb.mm(pT, d["Sk"][:], d["SkT"][:], [d["t_Sk"], d["t_SkT"]], [t_pT])
                if not last:
                    pS, t_pS = psN.next()
                    kb.mm(pS, d["SkT"][:], d["Sk"][:], [d["t_Sk"], d["t_SkT"]], [t_pS])
                nT, t_nT = SB_r.next()
                kb.cp("act", nT[:], pT, [t_pT], [t_nT])
                if not last:
                    nS, t_nS = SA_r.next()
                    kb.cp("dve", nS[:], pS, [t_pS], [t_nS])
                    d.update(Sk=nS, t_Sk=t_nS)
                d.update(SkT=nT, t_SkT=t_nT)
            for h in range(H):
                d = st[h]
                pR, t_pR = psN.next()
                kb.mm(pR, d["SkT"][:], d["R"][:], [d["t_SkT"], d["t_R"]], [t_pR])
                if last:
                    PT, t_PT = PT_r.next()
                    kb.tt("dve", PT[:], pR, d["R"][:], ALU.add, [t_pR, d["t_R"]], [t_PT])
                    d.update(PT=PT, t_PT=t_PT)
                else:
                    nR, t_nR = R_r.next()
                    kb.tt("dve", nR[:], pR, d["R"][:], ALU.add, [t_pR, d["t_R"]], [t_nR])
                    d.update(R=nR, t_R=t_nR)
        for hg in range(0, H, 4):
            for h in range(hg, hg + 4):
                d = st[h]
                sbf, t_sbf = Sbf[h]
                px, t_px = psXO.next()
                kb.mm(px, d["KT"], sbf[:], [t_qkvT, t_sbf], [t_px])
                po1, t_po1 = psXO.next()
                kb.mm(po1, d["QT"], sbf[:], [t_qkvT, t_sbf], [t_po1])
                d.update(px=px, t_px=t_px, po1=po1, t_po1=t_po1)
            for h in range(hg, hg + 4):
                d = st[h]
                Y, t_Y = Y_r.next()
                kb.stt("dve", Y[:], d["px"], neg[:, h:h + 1], d["Vtm"][:], ALU.mult, ALU.add,
                       [d["t_px"], t_neg, d["t_Vtm"]], [t_Y])
                o1, t_o1 = o1_r.next()
                kb.act(o1[:], d["po1"], AF.Copy, [d["t_po1"], t_eg], [t_o1], scale=eg[:, h:h + 1])
                ppy, t_ppy = psX.next()
                kb.mm(ppy, d["PT"][:], Y[:], [d["t_PT"], t_Y], [t_ppy])
                vn, t_vn = vn_r.next()
                kb.act(vn[:], ppy, AF.Copy, [t_ppy, t_bgc], [t_vn], scale=bgc[:, h:h + 1])
                d.update(vn=vn, t_vn=t_vn, o1=o1, t_o1=t_o1)
            for h in range(hg, hg + 4):
                d = st[h]
                vn, t_vn = d["vn"], d["t_vn"]
                s32, t_s32 = S32[h]
                sbf, t_sbf = Sbf[h]
                pst, t_pst = psN.next()
                kb.mm(pst, d["Ktl"][:], vn[:], [d["t_Ktl"], t_vn], [t_pst])
                po2, t_po2 = psN.next()
                kb.mm(po2, d["AIT"][:], vn[:], [d["t_AIT"], t_vn], [t_po2])
                kb.stt("dve", s32[:], s32[:], egl[:, h:h + 1], pst, ALU.mult, ALU.add, [t_s32, t_egl, t_pst], [t_s32])
                kb.cp("act", sbf[:], s32[:], [t_s32], [t_sbf])
                kb.tt("dve", O_[:, h, :], po2, d["o1"][:], ALU.add, [t_po2, d["t_o1"]], [t_O])
        osq, t_osq = osq_r.next()
        kb.act(osq[:], O_[:], AF.Square, [t_O], [t_osq], scale=float(128.0 ** -0.5))
        s8, t_s8 = s8c_r.next()
        kb.red("dve", s8[:], osq[:], [t_osq], [t_s8])
        kb.act(s8[:], s8[:], AF.Sqrt, [t_s8, t_eps], [t_s8], bias=epsc[:, 0:1])
        kb.recip(s8[:], s8[:], [t_s8], [t_s8])
        kb.tt("pool", osq[:], O_[:], s8[:, :].unsqueeze(2).to_broadcast([128, 8, 128]), ALU.mult, [t_O, t_s8], [t_osq])
        kb.tt("pool", osq[:], osq[:], dng[:, :].unsqueeze(1).to_broadcast([128, 8, 128]), ALU.mult, [t_osq, t_dng], [t_osq])
        obf, t_obf = obf_r.next()
        kb.tt("dve", obf[:].rearrange("p (h d) -> p h d", d=128), osq[:], zt[:].rearrange("p (h d) -> p h d", d=128),
              ALU.mult, [t_osq, t_zt], [t_obf])
        kb.dma("sp", ob_s[cols, :], obf[:], [t_obf], [])

    print("sbuf end C", kb.off)
    if stop == "C":
        return finish_early()
    P.barrier()
    kb.off = persist_off
    TC = 8 if NT >= 8 else NT
    wa = kb.sb([128, 4, 1024], BF16, "wa"); t_wa = T()
    wb = kb.sb([128, 8, 1024], BF16, "wb"); t_wb = T()
    wo = kb.sb([128, 8, 1024], BF16, "wo"); t_wo = T()
    wpg = kb.sb([128, 8, 1024], BF16, "wpg"); t_wpg = T()
    wpl = kb.sb([128, 2, 1024], BF16, "wpl"); t_wpl = T()
    kb.dma("gq", wa[:], w_branch_a.rearrange("(k p) c -> p k c", p=128), [], [t_wa])
    kb.dma("gq", wb[:], w_branch_b.rearrange("(k p) c -> p k c", p=128), [], [t_wb])
    kb.dma("gq", wo[:], w_out.rearrange("(k p) c -> p k c", p=128), [], [t_wo])
    kb.dma("gq", wpg[:], w_pg.rearrange("(k p) c -> p k c", p=128), [], [t_wpg])
    kb.dma("gq", wpl[:], w_ple.rearrange("(k p) c -> p k c", p=128), [], [t_wpl])
    wr = kb.sb([128, 8, 20], F32, "wr"); t_wr = T()
    kb.dma("sp", wr[:, :, 0:4], w_rg.rearrange("(k p) c -> p k c", p=128), [], [t_wr])
    kb.dma("sp", wr[:, :, 4:20], w_re.rearrange("(k p) c -> p k c", p=128), [], [t_wr])
    br = kb.sb([128, 20], F32, "br"); t_br = T()
    kb.dma("sp", br[:, 0:4], b_rg.partition_broadcast(128), [], [t_br])
    kb.dma("sp", br[:, 4:20], b_re.partition_broadcast(128), [], [t_br])
    gffn = kb.sb([128, 1024], F32, "gffn"); t_gffn = T()
    gple = kb.sb([128, 1024], F32, "gple"); t_gple = T()
    kb.dma("sp", gffn[:], norm_ffn.partition_broadcast(128), [], [t_gffn])
    kb.dma("sp", gple[:], norm_ple.partition_broadcast(128), [], [t_gple])
    yacc = [(kb.sb([128, 1024], F32, "yacc"), T()) for _ in range(TC)]
    h2T = [(kb.sb([128, 8, 128], BF16, "h2T"), T()) for _ in range(TC)]
    comb = [(kb.sb([128, 16], F32, "comb"), T()) for _ in range(TC)]
    wgu_r = kb.ring(2, [128, 8, 512], BF16, "wgu")
    wd_r = kb.ring(2, [128, 2, 1024], BF16, "wdn")
    u_r = kb.ring(3, [128, 520], F32, "ua")
    un_r = kb.ring(2, [128, 8, 65], F32, "un")
    ga_r = kb.ring(1, [128, 2048], BF16, "gates")
    obr = kb.ring(2, [128, 1024], BF16, "ob")
    oab_r = kb.ring(2, [128, 512], BF16, "oab")
    rden_r = kb.ring(2, [128, 8], F32, "rden")
    tT_r = kb.ring(2, [128, 8, 128], BF16, "tT")
    mg_r = kb.ring(1, [128, 1024], F32, "mg")
    mgb_r = kb.ring(2, [128, 1024], BF16, "mgb")
    hf_r = kb.ring(1, [128, 1024], F32, "hf")
    hfT_r = kb.ring(1, [128, 8, 128], F32, "hfT")
    hbf_r = kb.ring(2, [128, 1024], BF16, "hbf")
    jk_r = kb.ring(1, [128, 1024], BF16, "jk2")
    ss_r = kb.ring(4, [128, 1], F32, "ss2")
    lg_r = kb.ring(2, [128, 20], F32, "lg")
    sm_r = kb.ring(24, [128, 16], F32, "sm")
    sil_r = kb.ring(3, [128, 256], F32, "sil")
    act_r = kb.ring(3, [128, 256], BF16, "actb")
    actT_r = kb.ring(3, [128, 2, 128], BF16, "actT")
    pin_r = kb.ring(2, [128, 256], F32, "pin")
    pbf_r = kb.ring(2, [128, 256], BF16, "pbf")
    sg_r = kb.ring(1, [128, 1024], F32, "sg")
    psM = kb.psring([0, 1, 2, 3], 512, "psM")
    psTr = kb.psring([4, 5], 512, "psTr")
    psD = kb.psring([6, 7], 512, "psD")
    final_ops = []

    def transpose_bf(src, t_src, nk):
        ps, t_ps = psTr.next()
        psv = bfv(ps).rearrange("p (k c) -> p k c", k=8)
        for k in range(nk):
            kb.tr(psv[:, k, :], src[:, k * 128:(k + 1) * 128], ident_bf[:], [t_src, t_identbf], [t_ps])
        tT, t_tT = tT_r.next()
        kb.cp("act", tT[:, 0:nk, :], psv[:, 0:nk, :], [t_ps], [t_tT])
        return tT, t_tT

    def proj(tT, t_tT, W, t_W, nk):
        res = []
        for c in range(2):
            ps, t_ps = psM.next()
            for k in range(nk):
                kb.mm(ps, tT[:, k, :], W[:, k, c * 512:(c + 1) * 512], [t_tT, t_W], [t_ps], start=(k == 0), stop=(k == nk - 1))
            res.append((ps, t_ps))
        return res

    def rmsnorm_to(xsrc, t_x, gain, t_gain, out_ap, t_out):
        jk, t_jk = jk_r.next()
        ss, t_ss = ss_r.next()
        kb.memset("pool", ss[:], 0.0, [t_ss])
        kb.act(jk[:], xsrc, AF.Square, [t_x, t_ss], [t_jk, t_ss], scale=1.0 / 32.0, accum_out=ss[:])
        rms_rstd(ss[:], t_ss)
        kb.stt("dve", out_ap, xsrc, ss[:, 0:1], gain[:], ALU.mult, ALU.mult, [t_x, t_ss, t_gain], [t_out])

    for c0 in range(0, NT, TC):
        for ti in range(TC):
            t = c0 + ti
            rows = slice(t * 128, (t + 1) * 128)
            ya, t_ya = yacc[ti]
            kb.dma("sp", ya[:], x[rows, :], [], [t_ya])
            us_ = []
            for g in range(3):
                u, t_u = u_r.next()
                kb.dma("sp", u[:], ua_s[g, rows, :], [], [t_u])
                us_.append((u, t_u))
            ga, t_ga = ga_r.next()
            kb.dma("sp", ga[:], gates_s[rows, :], [], [t_ga])
            ob, t_ob = obr.next()
            kb.dma("sp", ob[:], ob_s[rows, :], [], [t_ob])
            un, t_un = un_r.next()
            unf = un[:].rearrange("p h c -> p (h c)")
            kb.tt("pool", unf, us_[0][0][:], us_[1][0][:], ALU.add, [us_[0][1], us_[1][1]], [t_un])
            kb.tt("pool", unf, unf, us_[2][0][:], ALU.add, [t_un, us_[2][1]], [t_un])
            rden, t_rden = rden_r.next()
            kb.recip(rden[:], un[:, :, 64], [t_un], [t_rden])
            oab, t_oab = oab_r.next()
            kb.tt("dve", oab[:].rearrange("p (h d) -> p h d", d=64), un[:, :, 0:64],
                  rden[:, :].unsqueeze(2).to_broadcast([128, 8, 64]), ALU.mult, [t_un, t_rden], [t_oab])
            aT, t_aT = transpose_bf(oab, t_oab, 4)
            pa = proj(aT, t_aT, wa, t_wa, 4)
            bT, t_bT = transpose_bf(ob, t_ob, 8)
            pb_ = proj(bT, t_bT, wb, t_wb, 8)
            mg, t_mg = mg_r.next()
            mgb, t_mgb = mgb_r.next()
            for c in range(2):
                cs = slice(c * 512, (c + 1) * 512)
                kb.tt("dve", mg[:, cs], pa[c][0], ga[:, c * 512:(c + 1) * 512], ALU.mult, [pa[c][1], t_ga], [t_mg])
                kb.tt("dve", mgb[:, cs], pb_[c][0], ga[:, 1024 + c * 512:1024 + (c + 1) * 512], ALU.mult, [pb_[c][1], t_ga], [t_mgb])
            kb.tt("pool", mgb[:], mg[:], mgb[:], ALU.add, [t_mg, t_mgb], [t_mgb])
            mT, t_mT = transpose_bf(mgb, t_mgb, 8)
            po = proj(mT, t_mT, wo, t_wo, 8)
            for c in range(2):
                cs = slice(c * 512, (c + 1) * 512)
                kb.tt("dve", ya[:, cs], po[c][0], ya[:, cs], ALU.add, [po[c][1], t_ya], [t_ya])
            hf, t_hf = hf_r.next()
            rmsnorm_to(ya[:], t_ya, gffn, t_gffn, hf[:], t_hf)
            hbf, t_hbf = hbf_r.next()
            kb.cp("pool", hbf[:], hf[:], [t_hf], [t_hbf])
            hT2, t_hT2 = h2T[ti]
            ps, t_ps = psTr.next()
            psv = bfv(ps).rearrange("p (k c) -> p k c", k=8)
            for k in range(8):
                kb.tr(psv[:, k, :], hbf[:, k * 128:(k + 1) * 128], ident_bf[:], [t_hbf, t_identbf], [t_ps])
            kb.cp("act", hT2[:], psv, [t_ps], [t_hT2])
            hfT, t_hfT = hfT_r.next()
            for hv in range(2):
                ps, t_ps = psTr.next()
                psv4 = ps.rearrange("p (k c) -> p k c", k=4)
                for k in range(4):
                    kk = hv * 4 + k
                    kb.tr(psv4[:, k, :], hf[:, kk * 128:(kk + 1) * 128], ident[:], [t_hf, t_ident], [t_ps])
                kb.cp("act" if hv else "dve", hfT[:, hv * 4:hv * 4 + 4, :], psv4, [t_ps], [t_hfT])
            ps, t_ps = psD.next()
            for k in range(8):
                kb.mm(ps[:, 0:20], hfT[:, k, :], wr[:, k, :], [t_hfT, t_wr], [t_ps], start=(k == 0), stop=(k == 7))
            lg, t_lg = lg_r.next()
            kb.tt("dve", lg[:], ps[:, 0:20], br[:], ALU.add, [t_ps, t_br], [t_lg])
            cm, t_cm = comb[ti]

            def sm(n=16):
                a, t_a = sm_r.next()
                return a[:, 0:n], t_a
            gmax, t_gmax = sm(1)
            kb.red("dve", gmax, lg[:, 0:4], [t_lg], [t_gmax], op=ALU.max)
            ngmax, t_ngmax = sm(1)
            kb.ts("dve", ngmax, gmax, -1.0, ALU.mult, [t_gmax], [t_ngmax])
            gex, t_gex = sm(4)
            gsum, t_gsum = sm(1)
            kb.memset("pool", gsum, 0.0, [t_gsum])
            kb.act(gex, lg[:, 0:4], AF.Exp, [t_lg, t_ngmax, t_gsum], [t_gex, t_gsum], bias=ngmax, accum_out=gsum)
            pg, t_pg = sm(1)
            kb.recip(pg, gsum, [t_gsum], [t_pg])
            goh, t_goh = sm(4)
            kb.ts("dve", goh, lg[:, 0:4], gmax, ALU.is_ge, [t_lg, t_gmax], [t_goh])
            el, t_el = sm(16)
            kb.tt("dve", el.rearrange("p (g e) -> p g e", e=4), lg[:, 4:20].rearrange("p (g e) -> p g e", e=4),
                  goh.unsqueeze(2).to_broadcast([128, 4, 4]), ALU.mult, [t_lg, t_goh], [t_el])
            sel, t_sel = sm(4)
            kb.red("dve", sel, el.rearrange("p (g e) -> p e g", e=4), [t_el], [t_sel])
            m1, t_m1 = sm(1)
            kb.red("dve", m1, sel, [t_sel], [t_m1], op=ALU.max)
            oh1, t_oh1 = sm(4)
            kb.ts("dve", oh1, sel, m1, ALU.is_ge, [t_sel, t_m1], [t_oh1])
            sel2, t_sel2 = sm(4)
            kb.stt("dve", sel2, oh1, -1e30, sel, ALU.mult, ALU.add, [t_oh1, t_sel], [t_sel2])
            m2, t_m2 = sm(1)
            kb.red("dve", m2, sel2, [t_sel2], [t_m2], op=ALU.max)
            oh2, t_oh2 = sm(4)
            kb.ts("dve", oh2, sel2, m2, ALU.is_ge, [t_sel2, t_m2], [t_oh2])
            dd, t_dd = sm(1)
            kb.tt("dve", dd, m2, m1, ALU.subtract, [t_m2, t_m1], [t_dd])
            kb.act(dd, dd, AF.Exp, [t_dd], [t_dd])
            kb.ts("dve", dd, dd, 1.0, ALU.add, [t_dd], [t_dd])
            w1, t_w1 = sm(1)
            kb.recip(w1, dd, [t_dd], [t_w1])
            w2, t_w2 = sm(1)
            kb.ts("dve", w2, w1, -1.0, ALU.mult, [t_w1], [t_w2], s2=1.0, op1=ALU.add)
            kb.tt("dve", w1, w1, pg, ALU.mult, [t_w1, t_pg], [t_w1])
            kb.tt("dve", w2, w2, pg, ALU.mult, [t_w2, t_pg], [t_w2])
            wig, t_wig = sm(4)
            kb.ts("dve", wig, oh1, w1, ALU.mult, [t_oh1, t_w1], [t_wig])
            kb.stt("dve", wig, oh2, w2, wig, ALU.mult, ALU.add, [t_oh2, t_w2, t_wig], [t_wig])
            for g in range(4):
                kb.ts("dve", cm[:, g * 4:(g + 1) * 4], wig, goh[:, g:g + 1], ALU.mult, [t_wig, t_goh], [t_cm])
        for e_ in range(16):
            wgu, t_wgu_sb = wgu_r.next()
            wdn, t_wdn = wd_r.next()
            kb.dma("sp", wgu[:], wgu_s[e_].rearrange("(k p) c -> p k c", p=128), [t_wgu[e_]], [t_wgu_sb])
            kb.dma("sp", wdn[:], wd_s[e_].rearrange("(k p) c -> p k c", p=128), [t_wd[e_]], [t_wdn])
            for ti in range(TC):
                ya, t_ya = yacc[ti]
                hT2, t_hT2 = h2T[ti]
                cm, t_cm = comb[ti]
                ps, t_ps = psM.next()
                for k in range(8):
                    kb.mm(ps, hT2[:, k, :], wgu[:, k, :], [t_hT2, t_wgu_sb], [t_ps], start=(k == 0), stop=(k == 7))
                sil, t_sil = sil_r.next()
                kb.act(sil[:], ps[:, 0:256], AF.Silu, [t_ps], [t_sil])
                ab, t_ab = act_r.next()
                kb.stt("dve", ab[:], ps[:, 256:512], cm[:, e_:e_ + 1], sil[:], ALU.mult, ALU.mult, [t_ps, t_cm, t_sil], [t_ab])
                pst_, t_pst_ = psTr.next()
                pstv = bfv(pst_).rearrange("p (k c) -> p k c", k=8)
                for k in range(2):
                    kb.tr(pstv[:, k, :], ab[:, k * 128:(k + 1) * 128], ident_bf[:], [t_ab, t_identbf], [t_pst_])
                aT, t_aT = actT_r.next()
                kb.cp("act", aT[:], pstv[:, 0:2, :], [t_pst_], [t_aT])
                for c in range(2):
                    pd, t_pd = psD.next()
                    for k in range(2):
                        kb.mm(pd, aT[:, k, :], wdn[:, k, c * 512:(c + 1) * 512], [t_aT, t_wdn], [t_pd], start=(k == 0), stop=(k == 1))
                    cs = slice(c * 512, (c + 1) * 512)
                    kb.tt("dve", ya[:, cs], ya[:, cs], pd, ALU.add, [t_ya, t_pd], [t_ya])
        for ti in range(TC):
            t = c0 + ti
            rows = slice(t * 128, (t + 1) * 128)
            ya, t_ya = yacc[ti]
            hbf, t_hbf = hbf_r.next()
            rmsnorm_to(ya[:], t_ya, gple, t_gple, hbf[:], t_hbf)
            h3T, t_h3T = transpose_bf(hbf, t_hbf, 8)
            pgate = proj(h3T, t_h3T, wpg, t_wpg, 8)
            pin, t_pin = pin_r.next()
            kb.dma("sp", pin[:], p_in[rows, :], [], [t_pin])
            pbf, t_pbf = pbf_r.next()
            kb.cp("pool", pbf[:], pin[:], [t_pin], [t_pbf])
            pT, t_pT = transpose_bf(pbf, t_pbf, 2)
            sg, t_sg = sg_r.next()
            for c in range(2):
                cs = slice(c * 512, (c + 1) * 512)
                kb.act(sg[:, cs], pgate[c][0], AF.Sigmoid, [pgate[c][1]], [t_sg])
            pple = proj(pT, t_pT, wpl, t_wpl, 2)
            for c in range(2):
                cs = slice(c * 512, (c + 1) * 512)
                kb.tt("dve", sg[:, cs], sg[:, cs], pple[c][0], ALU.mult, [t_sg, pple[c][1]], [t_sg])
            kb.tt("pool", ya[:], sg[:], ya[:], ALU.add, [t_sg, t_ya], [t_ya])
            final_ops.append(kb.dma("sp", out[rows, :], ya[:], [t_ya], []))

    print("sbuf end D", kb.off, "ops", {k: len(v) for k, v in P.ops.items()})
    P.emit(nc, final_waits=final_ops)
    return nc


_CONSTS = None


def _consts():
    global _CONSTS
    if _CONSTS is None:
        _CONSTS = {
            "c_ident": np.eye(128, dtype=np.float32),
            "c_triu": np.triu(np.ones((128, 128), dtype=np.float32)),
            "c_tril": np.tril(np.ones((128, 128), dtype=np.float32)),
        }
    return _CONSTS


def make_in_map(inputs, b, S):
    f = lambda a: np.ascontiguousarray(np.asarray(a, dtype=np.float32))
    m = {
        "x": f(inputs["x"][b, :S]),
        "p": f(inputs["p"][0, b, :S]),
        "norm_mix": f(inputs["norm_mix"][0]),
        "w_in": f(inputs["w_in"][0]),
        "q_norm": f(inputs["q_norm"][0]),
        "k_norm": f(inputs["k_norm"][0]),
        "conv_w": f(np.asarray(inputs["conv_w"][0]).reshape(4, 24, 128).transpose(2, 1, 0)),
        "a_log": f(inputs["a_log"][0]),
        "dt_bias": f(inputs["dt_bias"][0]),
        "dn_out_norm": f(inputs["dn_out_norm"][0]),
        "w_branch_a": f(inputs["w_branch_a"][0]),
        "w_branch_b": f(inputs["w_branch_b"][0]),
        "w_out": f(inputs["w_out"][0]),
        "norm_ffn": f(inputs["norm_ffn"][0]),
        "w_router_group": f(inputs["w_router_group"][0]),
        "b_router_group": f(inputs["b_router_group"][0]),
        "w_router_expert": f(inputs["w_router_expert"][0]),
        "b_router_expert": f(inputs["b_router_expert"][0]),
        "w_expert_gate": f(np.asarray(inputs["w_expert_gate"][0]).reshape(16, 1024, 256)),
        "w_expert_up": f(np.asarray(inputs["w_expert_up"][0]).reshape(16, 1024, 256)),
        "w_expert_down": f(np.asarray(inputs["w_expert_down"][0]).reshape(16, 256, 1024)),
        "norm_ple": f(inputs["norm_ple"][0]),
        "w_ple": f(inputs["w_ple"][0]),
        "w_ple_gate": f(inputs["w_ple_gate"][0]),
    }
    m.update(_consts())
    return m


def kernel(**inputs):
    S = 8192
    nc = build(S)
    in_maps = [make_in_map(inputs, b, S) for b in range(8)]
    res = run_bass_kernel_spmd(nc, in_maps, core_ids=list(range(8)))
    return np.stack([np.asarray(r["out"], dtype=np.float32) for r in res.results], axis=0)
```

```python
import numpy as np
import concourse.bass as bass
import concourse.mybir as mybir
from concourse.bass_utils import run_bass_kernel_spmd

F32 = mybir.dt.float32
BF16 = mybir.dt.bfloat16
ALU = mybir.AluOpType
AF = mybir.ActivationFunctionType
AX = mybir.AxisListType

ENGS = ("pe", "act", "dve", "pool")
DMAQ = ("sp", "gq")
NDSEM = 24
EPS = 1e-6
SB_BASE = 16512
SB_TOP = 229344


class T:
    __slots__ = ("name", "w", "rs", "banks")

    def __init__(self, name="", banks=()):
        self.name = name
        self.w = None
        self.rs = []
        self.banks = banks


class Bank:
    __slots__ = ("last",)

    def __init__(self):
        self.last = None


class Op:
    __slots__ = ("eng", "fn", "idx", "deps", "sig", "signo", "dslot", "dval", "clock", "gorder")


class Prog:
    def __init__(self):
        self.ops = {e: [] for e in ENGS + DMAQ}
        self.clock = {e: {} for e in ENGS + DMAQ}
        self.ndma = {q: 0 for q in DMAQ}
        self.g = 0
        self.pending = {e: [] for e in ENGS + DMAQ}

    def barrier(self):
        lasts = []
        for e in ENGS:
            if self.ops[e]:
                lasts.append(self.ops[e][-1])
        for q in DMAQ:
            lasts.extend(self.ops[q][-NDSEM:])
        self.pending["sp"] = list(lasts)
        op = self.add("sp", self.bar_fn)
        for e in ENGS + ("gq",):
            self.pending[e] = [op]

    def add(self, eng, fn, reads=(), writes=()):
        op = Op()
        op.eng = eng
        op.fn = fn
        op.idx = len(self.ops[eng])
        op.sig = False
        op.signo = None
        op.gorder = self.g
        self.g += 1
        deps = []
        for t in reads:
            if t.w is not None:
                deps.append((t.w, True))
        for t in writes:
            if t.w is not None:
                deps.append((t.w, False))
            for r in t.rs:
                deps.append((r, False))
        bks = []
        for t in tuple(reads) + tuple(writes):
            for b in t.banks:
                if b not in bks:
                    bks.append(b)
        for b in bks:
            if b.last is not None and b.last.eng != eng:
                deps.append((b.last, True))
        if self.pending[eng]:
            for d in self.pending[eng]:
                if d.eng != eng or eng in DMAQ:
                    deps.append((d, True))
            self.pending[eng] = []
        clk = self.clock[eng]
        need = {}
        for d, raw in deps:
            if d.eng == eng and eng not in DMAQ:
                if eng == "pe":
                    continue
            if d.eng in DMAQ:
                key = (d.eng, d.idx)
                if clk.get(key, False):
                    continue
                need[key] = d
            else:
                if clk.get(d.eng, -1) >= d.idx:
                    continue
                cur = need.get(d.eng)
                if cur is None or cur.idx < d.idx:
                    need[d.eng] = d
        op.deps = list(need.values())
        for d in op.deps:
            d.sig = True
            for k, v in d.clock.items():
                if isinstance(k, tuple):
                    clk[k] = True
                elif clk.get(k, -1) < v:
                    clk[k] = v
            if d.eng in DMAQ:
                clk[(d.eng, d.idx)] = True
            elif clk.get(d.eng, -1) < d.idx:
                clk[d.eng] = d.idx
        if eng in DMAQ:
            op.dslot = self.ndma[eng] % NDSEM
            op.dval = 16 * (self.ndma[eng] // NDSEM + 1)
            self.ndma[eng] += 1
            prev_i = op.idx - NDSEM
            if prev_i >= 0:
                pd = self.ops[eng][prev_i]
                if not clk.get((eng, prev_i), False):
                    op.deps.append(pd)
                    clk[(eng, prev_i)] = True
            op.sig = True
        if len(clk) > 400:
            for k in [k for k in clk if isinstance(k, tuple) and k[1] < self.ndma[k[0]] - 4 * NDSEM]:
                del clk[k]
        op.clock = dict(clk)
        if eng not in DMAQ:
            op.clock[eng] = op.idx
        self.ops[eng].append(op)
        for b in bks:
            b.last = op
        for t in reads:
            t.rs.append(op)
        for t in writes:
            t.w = op
            t.rs = []
        return op

    def emit(self, nc, final_waits=()):
        from contextlib import ExitStack
        with ExitStack() as es:
            sems = {e: es.enter_context(nc.semaphore("s_" + e)) for e in ENGS}
            dsems = {q: [es.enter_context(nc.semaphore(f"d_{q}{i}")) for i in range(NDSEM)] for q in DMAQ}
            for d in final_waits:
                d.sig = True
            for e in ENGS:
                n = 0
                for op in self.ops[e]:
                    if op.sig:
                        n += 1
                        op.signo = n
            block = es.enter_context(nc.Block())

            def waits(engobj, op):
                for d in op.deps:
                    if d.eng in DMAQ:
                        engobj.wait_ge(dsems[d.eng][d.dslot], d.dval)
                    else:
                        engobj.wait_ge(sems[d.eng], d.signo)

            def run(oplist, engobj, extra=None):
                for op in oplist:
                    waits(engobj, op)
                    ins = op.fn(engobj)
                    if op.sig:
                        if op.eng in DMAQ:
                            ins.then_inc(dsems[op.eng][op.dslot], 16)
                        else:
                            ins.then_inc(sems[op.eng], 1)
                if extra:
                    extra(engobj)

            def fin(engobj):
                for d in final_waits:
                    if d.eng in DMAQ:
                        engobj.wait_ge(dsems[d.eng][d.dslot], d.dval)
                    else:
                        engobj.wait_ge(sems[d.eng], d.signo)

            @block.tensor
            def _(e):
                run(self.ops["pe"], e)

            @block.scalar
            def _(e):
                run(self.ops["act"], e)

            @block.vector
            def _(e):
                run(self.ops["dve"], e)

            @block.gpsimd
            def _(e):
                merged = sorted(self.ops["pool"] + self.ops["gq"], key=lambda o: o.gorder)
                run(merged, e)

            @block.sync
            def _(e):
                run(self.ops["sp"], e, fin)


class KB:
    def __init__(self, nc):
        self.nc = nc
        self.P = Prog()
        self.off = SB_BASE
        self.n = 0
        self.banks = [nc.alloc_psum_tensor(f"bank{i}", [128, 512], F32) for i in range(8)]
        self.bk = [Bank() for _ in range(8)]

    def pst(self, *bank_ids):
        return T("ps", banks=tuple(self.bk[b] for b in bank_ids))

    def sb(self, shape, dt, name="t"):
        sz = int(np.prod(shape[1:])) * (2 if dt == BF16 else 4)
        sz = (sz + 31) // 32 * 32
        assert self.off + sz <= SB_TOP, f"sbuf overflow {name} {self.off} {sz}"
        self.n += 1
        t = self.nc.alloc_sbuf_tensor_at(f"{name}_{self.n}", list(shape), dt, offset=self.off)
        self.off += sz
        return t

    def ring(self, n, shape, dt, name="r"):
        return Ring([(self.sb(shape, dt, name), T(name)) for _ in range(n)])

    def psring(self, banks, nf32, name="ps"):
        per = 512 // nf32
        slots = []
        for i in range(per):
            for b in banks:
                slots.append((self.banks[b][:, i * nf32:(i + 1) * nf32], self.pst(b)))
        return Ring(slots)

    def mm(self, out, lhsT, rhs, r, w, start=True, stop=True):
        return self.P.add("pe", lambda e: e.matmul(out, lhsT=lhsT, rhs=rhs, start=start, stop=stop), reads=r, writes=w)

    def tr(self, out, in_, ident, r, w):
        return self.P.add("pe", lambda e: e.transpose(out=out, in_=in_, identity=ident), reads=r, writes=w)

    def act(self, out, in_, func, r, w, bias=None, scale=None, accum_out=None, eng="act"):
        kw = {}
        if bias is not None:
            kw["bias"] = bias
        if scale is not None:
            kw["scale"] = scale
        if accum_out is not None:
            kw["accum_out"] = accum_out
        return self.P.add("act", lambda e: e.activation(out=out, in_=in_, func=func, **kw), reads=r, writes=w)

    def cp(self, eng, out, in_, r, w):
        if eng == "act":
            return self.P.add("act", lambda e: e.copy(out=out, in_=in_), reads=r, writes=w)
        return self.P.add(eng, lambda e: e.tensor_copy(out=out, in_=in_), reads=r, writes=w)

    def tt(self, eng, out, in0, in1, op, r, w):
        return self.P.add(eng, lambda e: e.tensor_tensor(out=out, in0=in0, in1=in1, op=op), reads=r, writes=w)

    def ts(self, eng, out, in0, s1, op0, r, w, s2=None, op1=None):
        if op1 is None:
            return self.P.add(eng, lambda e: e.tensor_scalar(out=out, in0=in0, scalar1=s1, scalar2=None, op0=op0), reads=r, writes=w)
        return self.P.add(eng, lambda e: e.tensor_scalar(out=out, in0=in0, scalar1=s1, scalar2=s2, op0=op0, op1=op1), reads=r, writes=w)

    def stt(self, eng, out, in0, scalar, in1, op0, op1, r, w):
        return self.P.add(eng, lambda e: e.scalar_tensor_tensor(out=out, in0=in0, scalar=scalar, in1=in1, op0=op0, op1=op1), reads=r, writes=w)

    def red(self, eng, out, in_, r, w, op=ALU.add):
        return self.P.add(eng, lambda e: e.tensor_reduce(out=out, in_=in_, axis=AX.X, op=op), reads=r, writes=w)

    def recip(self, out, in_, r, w):
        return self.P.add("dve", lambda e: e.reciprocal(out=out, in_=in_), reads=r, writes=w)

    def memset(self, eng, ap, val, w):
        return self.P.add(eng, lambda e: e.memset(ap, val), writes=w)

    def dma(self, q, out, in_, r, w):
        return self.P.add(q, lambda e: e.dma_start(out=out, in_=in_), reads=r, writes=w)


class Ring:
    def __init__(self, slots):
        self.slots = slots
        self.i = 0

    def next(self):
        s = self.slots[self.i % len(self.slots)]
        self.i += 1
        return s


def pipeline(units):
    n = len(units)
    nst = max(len(u) for u in units)
    for i in range(n + nst - 1):
        for st in range(nst - 1, -1, -1):
            u = i - st
            if 0 <= u < n and st < len(units[u]) and units[u][st] is not None:
                units[u][st]()


def bfv(ap):
    return ap.bitcast(BF16)


def build(S, debug=False, stop=None, skip_pre=False):
    nc = bass.Bass("TRN2", target_bir_lowering=False)
    NT = S // 128
    NSP = S // 512
    kb = KB(nc)
    P = kb.P

    def din(name, shape, dt=F32):
        return nc.dram_tensor(name, list(shape), dt, kind="ExternalInput").ap()

    def dscr(name, shape, dt):
        return nc.dram_tensor(name, list(shape), dt, kind="ExternalOutput" if debug else "Internal").ap()

    x = din("x", [S, 1024])
    p_in = din("p", [S, 256])
    norm_mix = din("norm_mix", [1024])
    w_in = din("w_in", [1024, 10768])
    q_norm = din("q_norm", [64])
    k_norm = din("k_norm", [64])
    conv_w = din("conv_w", [128, 24, 4])
    a_log = din("a_log", [8])
    dt_bias = din("dt_bias", [8])
    dn_out_norm = din("dn_out_norm", [128])
    w_branch_a = din("w_branch_a", [512, 1024])
    w_branch_b = din("w_branch_b", [1024, 1024])
    w_out = din("w_out", [1024, 1024])
    norm_ffn = din("norm_ffn", [1024])
    w_rg = din("w_router_group", [1024, 4])
    b_rg = din("b_router_group", [4])
    w_re = din("w_router_expert", [1024, 16])
    b_re = din("b_router_expert", [16])
    w_eg = din("w_expert_gate", [16, 1024, 256])
    w_eu = din("w_expert_up", [16, 1024, 256])
    w_ed = din("w_expert_down", [16, 256, 1024])
    norm_ple = din("norm_ple", [1024])
    w_ple = din("w_ple", [256, 1024])
    w_pg = din("w_ple_gate", [1024, 1024])
    c_ident = din("c_ident", [128, 128])
    c_triu = din("c_triu", [128, 128])
    c_tril = din("c_tril", [128, 128])
    out = nc.dram_tensor("out", [S, 1024], F32, kind="ExternalOutput").ap()

    qkva_s = dscr("qkva_s", [9, S, 512], BF16)
    qkvb_s = dscr("qkvb_s", [24, 128, S], BF16)
    z_s = dscr("z_s", [S, 1024], BF16)
    bg_s = dscr("bg_s", [S, 16], F32)
    gates_s = dscr("gates_s", [S, 2048], BF16)
    ua_s = dscr("ua_s", [3, S, 520], F32)
    ob_s = dscr("ob_s", [S, 1024], BF16)
    wgu_s = nc.dram_tensor("wgu_s", [16, 1024, 512], BF16, kind="Internal").ap()
    wd_s = nc.dram_tensor("wd_s", [16, 256, 1024], BF16, kind="Internal").ap()

    ident = kb.sb([128, 128], F32, "ident"); t_ident = T()
    ident_bf = kb.sb([128, 128], BF16, "identbf"); t_identbf = T()
    triu = kb.sb([128, 128], F32, "triu"); t_triu = T()
    tril_bf = kb.sb([128, 128], BF16, "trilbf"); t_trilbf = T()
    triu_bf = kb.sb([128, 128], BF16, "triubf"); t_triubf = T()
    tril = kb.sb([128, 128], F32, "tril"); t_tril = T()
    mbm = kb.sb([128, 128], F32, "mbm"); t_mbm = T()
    strict = kb.sb([128, 128], F32, "strict"); t_strict = T()
    ones_bf = kb.sb([128, 128], BF16, "ones"); t_ones = T()
    epsc = kb.sb([128, 1], F32, "eps"); t_eps = T()
    eps128 = kb.sb([128, 1], F32, "eps128"); t_eps128 = T()
    kb.dma("sp", ident[:], c_ident[:, :], [], [t_ident])
    kb.dma("sp", triu[:], c_triu[:, :], [], [t_triu])
    kb.dma("sp", tril[:], c_tril[:, :], [], [t_tril])
    kb.cp("dve", ident_bf[:], ident[:], [t_ident], [t_identbf])
    kb.cp("dve", triu_bf[:], triu[:], [t_triu], [t_triubf])
    kb.cp("dve", tril_bf[:], tril[:], [t_tril], [t_trilbf])
    kb.ts("dve", mbm[:], triu[:], -1.0, ALU.add, [t_triu], [t_mbm], s2=1e9, op1=ALU.mult)
    kb.tt("dve", strict[:], triu[:], ident[:], ALU.subtract, [t_triu, t_ident], [t_strict])
    kb.memset("pool", ones_bf[:], 1.0, [t_ones])
    kb.memset("pool", epsc[:], EPS, [t_eps])
    kb.memset("pool", eps128[:], 128.0 * EPS, [t_eps128])

    t_wgu = [T() for _ in range(16)]
    t_wd = [T() for _ in range(16)]
    for e_ in range(0 if not skip_pre else 16, 16):
        kb.dma("gq", wgu_s[e_, :, 0:256], w_eg[e_], [], [t_wgu[e_]])
        kb.dma("gq", wgu_s[e_, :, 256:512], w_eu[e_], [], [t_wgu[e_]])
        kb.dma("gq", wd_s[e_], w_ed[e_], [], [t_wd[e_]])

    bar_a = kb.sb([128, 8], F32, "bar_a")
    bar_b = kb.sb([128, 8], F32, "bar_b")
    kb.memset("pool", bar_a[:], 0.0, [])
    P.bar_fn = lambda e: e.dma_start(out=bar_b[:], in_=bar_a[:])
    persist_off = kb.off

    def finish_early():
        fw = []
        for q in DMAQ:
            fw.extend(P.ops[q][-NDSEM:])
        for e in ENGS:
            if P.ops[e]:
                fw.append(P.ops[e][-1])
        P.emit(nc, final_waits=fw)
        return nc
    gain_mix = kb.sb([128, 1024], F32, "gmix"); t_gmix = T()
    kb.dma("sp", gain_mix[:], norm_mix.partition_broadcast(128), [], [t_gmix])
    hT = kb.sb([128, 8, S], BF16, "hT")
    t_hT = [T() for _ in range(NT)]
    a_off = kb.off
    xr = kb.ring(2, [128, 1024], F32, "xt")
    junk = kb.ring(2, [128, 1024], BF16, "junk")
    hb = kb.ring(2, [128, 1024], BF16, "hb")
    ssr = kb.ring(4, [128, 1], F32, "ss")
    psA0 = kb.psring([0, 1], 512, "psA0")

    def rms_rstd(ss_ap, t_ss, nfeat_scale_done=True):
        kb.act(ss_ap, ss_ap, AF.Sqrt, [t_ss, t_eps], [t_ss], bias=epsc[:, 0:1])
        kb.recip(ss_ap, ss_ap, [t_ss], [t_ss])

    for t in range(NT):
        xt, t_xt = xr.next()
        jk, t_jk = junk.next()
        h_, t_h = hb.next()
        ss, t_ss = ssr.next()
        kb.dma("sp", xt[:], x[t * 128:(t + 1) * 128, :], [], [t_xt])
        kb.memset("pool", ss[:], 0.0, [t_ss])
        kb.act(jk[:], xt[:], AF.Square, [t_xt, t_ss], [t_jk, t_ss], scale=1.0 / 32.0, accum_out=ss[:])
        rms_rstd(ss[:], t_ss)
        kb.stt("dve", h_[:], xt[:], ss[:, 0:1], gain_mix[:], ALU.mult, ALU.mult, [t_xt, t_ss, t_gmix], [t_h])
        ps, t_ps = psA0.next()
        psv = bfv(ps).rearrange("p (k c) -> p k c", k=8)
        for k in range(8):
            kb.tr(psv[:, k, :], h_[:, k * 128:(k + 1) * 128], ident_bf[:], [t_h, t_identbf], [t_ps])
        kb.cp("act" if t % 2 else "dve", hT[:, :, t * 128:(t + 1) * 128], psv, [t_ps], [t_hT[t]])

    if stop == "A0":
        return finish_early()
    P.barrier()
    kb.off = a_off
    qg = kb.sb([128, 64], F32, "qg"); t_qg = T()
    kg = kb.sb([128, 64], F32, "kg"); t_kg = T()
    kb.dma("sp", qg[:], q_norm.partition_broadcast(128), [], [t_qg])
    kb.dma("sp", kg[:], k_norm.partition_broadcast(128), [], [t_kg])
    kb.ts("dve", qg[:], qg[:], 0.125, ALU.mult, [t_qg], [t_qg])
    convw = kb.sb([128, 24, 4], F32, "convw"); t_convw = T()
    kb.dma("sp", convw[:], conv_w[:, :, :], [], [t_convw])
    dtb = kb.sb([128, 8], F32, "dtb"); t_dtb = T()
    negA = kb.sb([128, 8], F32, "negA"); t_negA = T()
    kb.dma("sp", dtb[:], dt_bias.partition_broadcast(128), [], [t_dtb])
    kb.dma("sp", negA[:], a_log.partition_broadcast(128), [], [t_negA])
    kb.act(negA[:], negA[:], AF.Exp, [t_negA], [t_negA])
    kb.ts("dve", negA[:], negA[:], -1.0, ALU.mult, [t_negA], [t_negA])

    wring = kb.ring(2, [128, 8, 512], BF16, "wg")
    wsm = kb.sb([128, 8, 16], BF16, "wsm"); t_wsm = T()
    psA = kb.psring([0, 1, 2, 3, 4, 5], 512, "psA")
    psS = kb.psring([6, 7], 512, "psS")
    f32r = kb.ring(3, [128, 512], F32, "f32r")
    f32r2 = kb.ring(3, [128, 512], F32, "f32r2")
    bfr = kb.ring(4, [128, 512], BF16, "bfr")
    s8r = kb.ring(4, [128, 8], F32, "s8")
    rawr = kb.ring(3, [128, 515], F32, "raw")
    carry = [(kb.sb([128, 3], F32, "carry"), T()) for _ in range(4)]
    yr = kb.ring(5, [128, 512], F32, "y")
    sqr = kb.ring(3, [128, 512], BF16, "sqb")
    rnr = kb.ring(3, [128, 512], F32, "rn")
    bgr = kb.ring(3, [128, 16], F32, "bg")
    w_in_v = w_in.rearrange("(k p) c -> p k c", p=128)

    def col0(cg):
        if cg < 17:
            return cg * 512
        if cg == 17:
            return 8704
        return 8720 + (cg - 18) * 512

    def load_W(cg):
        c0 = col0(cg)
        if cg == 17:
            kb.dma("gq", wsm[:], w_in_v[:, :, c0:c0 + 16], [], [t_wsm])
            return wsm, t_wsm
        W, t_W = wring.next()
        kb.dma("gq", W[:, 0:4, :], w_in_v[:, 0:4, c0:c0 + 512], [], [t_W])
        kb.dma("gq", W[:, 4:8, :], w_in_v[:, 4:8, c0:c0 + 512], [], [t_W])
        return W, t_W

    def conv_unit(cg, s, cb, W, t_W):
        cbg = (cg - 9) * 4 + cb
        which = cbg // 8
        d = {}

        def sa():
            ps, t_ps = psA.next()
            for k in range(8):
                kb.mm(ps, W[:, k, cb * 128:(cb + 1) * 128], hT[:, k, s * 512:(s + 1) * 512],
                      [t_W] + t_hT[4 * s:4 * s + 4], [t_ps], start=(k == 0), stop=(k == 7))
            raw, t_raw = rawr.next()
            cy, t_cy = carry[cb]
            if s == 0:
                kb.memset("pool", raw[:, 0:3], 0.0, [t_raw])
            else:
                kb.cp("pool", raw[:, 0:3], cy[:], [t_cy], [t_raw])
            kb.cp("act", raw[:, 3:515], ps, [t_ps], [t_raw])
            kb.cp("pool", cy[:], raw[:, 512:515], [t_raw], [t_cy])
            d.update(raw=raw, t_raw=t_raw)

        def sb_():
            raw, t_raw = d["raw"], d["t_raw"]
            y, t_y = yr.next()
            kb.ts("dve", y[:], raw[:, 3:515], convw[:, cbg, 3:4], ALU.mult, [t_raw, t_convw], [t_y])
            for j in range(3):
                kb.stt("dve", y[:], raw[:, j:j + 512], convw[:, cbg, j:j + 1], y[:], ALU.mult, ALU.add,
                       [t_raw, t_convw, t_y], [t_y])
            d.update(y=y, t_y=t_y)

        def sc():
            y, t_y = d["y"], d["t_y"]
            if which == 2:
                ob, t_ob = bfr.next()
                kb.act(ob[:], y[:], AF.Silu, [t_y], [t_ob])
                kb.dma("sp", qkvb_s[cbg, :, s * 512:(s + 1) * 512], ob[:], [t_ob], [])
            else:
                kb.act(y[:], y[:], AF.Silu, [t_y], [t_y])
                sq, t_sq = sqr.next()
                kb.tt("pool", sq[:], y[:], y[:], ALU.mult, [t_y], [t_sq])
                d.update(sq=sq, t_sq=t_sq)

        def sd():
            if which == 2:
                return
            pss, t_pss = psS.next()
            kb.mm(pss, ones_bf[:], d["sq"][:], [t_ones, d["t_sq"]], [t_pss])
            rn, t_rn = rnr.next()
            if which == 0:
                kb.act(rn[:], pss, AF.Sqrt, [t_pss, t_eps128], [t_rn], bias=eps128[:, 0:1], scale=128.0)
            else:
                kb.act(rn[:], pss, AF.Sqrt, [t_pss, t_eps], [t_rn], bias=epsc[:, 0:1])
            d.update(rn=rn, t_rn=t_rn)

        def se():
            if which == 2:
                return
            rn, t_rn, y, t_y = d["rn"], d["t_rn"], d["y"], d["t_y"]
            kb.recip(rn[:], rn[:], [t_rn], [t_rn])
            ob, t_ob = bfr.next()
            kb.tt("pool", ob[:], y[:], rn[:], ALU.mult, [t_y, t_rn], [t_ob])
            kb.dma("sp", qkvb_s[cbg, :, s * 512:(s + 1) * 512], ob[:], [t_ob], [])

        return [sa, sb_, sc, sd, se]

    Wnext = load_W(0)
    for cg in range(22):
        c0 = col0(cg)
        W, t_W = Wnext
        if cg + 1 < 22:
            Wnext = load_W(cg + 1)
        if 9 <= cg <= 14:
            units = []
            for s in range(NSP):
                for cb in range(4):
                    units.append(conv_unit(cg, s, cb, W, t_W))
            pipeline(units)
        else:
            for t in range(NT):
                rows = slice(t * 128, (t + 1) * 128)
                ps, t_ps = psA.next()
                ncol = 16 if cg == 17 else 512
                pso = ps[:, 0:ncol]
                for k in range(8):
                    kb.mm(pso, hT[:, k, t * 128:(t + 1) * 128], W[:, k, :], [t_W, t_hT[t]], [t_ps],
                          start=(k == 0), stop=(k == 7))
                if cg < 6:
                    gain, t_gain = (qg, t_qg) if cg < 3 else (kg, t_kg)
                    sqf, t_sqf = f32r.next()
                    kb.act(sqf[:], ps, AF.Square, [t_ps], [t_sqf], scale=0.125)
                    s8, t_s8 = s8r.next()
                    kb.red("dve", s8[:], sqf[:].rearrange("p (h d) -> p h d", d=64), [t_sqf], [t_s8])
                    rms_rstd(s8[:], t_s8)
                    tmp, t_tmp = f32r2.next()
                    kb.tt("dve", tmp[:].rearrange("p (h d) -> p h d", d=64), ps.rearrange("p (h d) -> p h d", d=64),
                          s8[:, :].unsqueeze(2).to_broadcast([128, 8, 64]), ALU.mult, [t_ps, t_s8], [t_tmp])
                    ob, t_ob = bfr.next()
                    kb.tt("pool", ob[:].rearrange("p (h d) -> p h d", d=64), tmp[:].rearrange("p (h d) -> p h d", d=64),
                          gain[:, :].unsqueeze(1).to_broadcast([128, 8, 64]), ALU.mult, [t_tmp, t_gain], [t_ob])
                    kb.dma("sp", qkva_s[cg, rows, :], ob[:], [t_ob], [])
                elif cg < 9:
                    ob, t_ob = bfr.next()
                    kb.cp("act" if t % 2 else "dve", ob[:], ps, [t_ps], [t_ob])
                    kb.dma("sp", qkva_s[cg, rows, :], ob[:], [t_ob], [])
                elif cg in (15, 16):
                    ob, t_ob = bfr.next()
                    kb.act(ob[:], ps, AF.Silu, [t_ps], [t_ob])
                    kb.dma("sp", z_s[rows, (cg - 15) * 512:(cg - 14) * 512], ob[:], [t_ob], [])
                elif cg == 17:
                    bg, t_bg = bgr.next()
                    kb.act(bg[:, 0:8], ps[:, 0:8], AF.Sigmoid, [t_ps], [t_bg])
                    kb.tt("dve", bg[:, 8:16], ps[:, 8:16], dtb[:], ALU.add, [t_ps, t_dtb], [t_bg])
                    kb.act(bg[:, 8:16], bg[:, 8:16], AF.Exp, [t_bg], [t_bg])
                    kb.act(bg[:, 8:16], bg[:, 8:16], AF.Ln, [t_bg], [t_bg], bias=1.0)
                    kb.tt("dve", bg[:, 8:16], bg[:, 8:16], negA[:], ALU.mult, [t_bg, t_negA], [t_bg])
                    kb.dma("sp", bg_s[rows, :], bg[:], [t_bg], [])
                else:
                    ob, t_ob = bfr.next()
                    kb.act(ob[:], ps, AF.Sigmoid, [t_ps], [t_ob])
                    kb.dma("sp", gates_s[rows, (cg - 18) * 512:(cg - 17) * 512], ob[:], [t_ob], [])

    print("sbuf end A", kb.off)
    if stop == "A":
        return finish_early()
    P.barrier()
    kb.off = persist_off
    qr_ = kb.ring(3, [128, 512], BF16, "qb")
    kr_ = kb.ring(3, [128, 512], BF16, "kb")
    vst_r = kb.ring(3, [128, 512], BF16, "vst")
    vr_ = kb.ring(6, [128, 8, 65], BF16, "v1")
    for v1, t_v1 in vr_.slots:
        kb.memset("pool", v1[:, :, 64:65], 1.0, [t_v1])
    qTr = kb.ring(3, [128, 8, 128], BF16, "qT")
    for qz, t_qz in qTr.slots:
        kb.memset("pool", qz[:], 0.0, [t_qz])
    kTr = kb.ring(4, [128, 4, 128], BF16, "kT")
    er_ = kb.ring(4, [128, 4, 2, 128], BF16, "E")
    ur_ = kb.ring(3, [128, 8, 65], F32, "U")
    sc_slots = Ring([((kb.banks[0][:, :], kb.banks[1][:, :]), kb.pst(0, 1)), ((kb.banks[2][:, :], kb.banks[3][:, :]), kb.pst(2, 3))])
    pv_slots = Ring([(kb.banks[4][:, :], kb.pst(4)), (kb.banks[5][:, :], kb.pst(5))])
    psT = Ring([(kb.banks[6][:, :], kb.pst(6)), (kb.banks[7][:, :], kb.pst(7))])
    ATT = ((128, 1), (512, 4), (2048, 16))

    def attn_unit(g, dil, r_, nb, prev):
        qs = qkva_s[g].rearrange("(u r) c -> r u c", r=dil)
        ks = qkva_s[3 + g].rearrange("(u r) c -> r u c", r=dil)
        vs = qkva_s[6 + g].rearrange("(u r) c -> r u c", r=dil)
        us = ua_s[g].rearrange("(u r) c -> r u c", r=dil)
        ur = slice(nb * 128, (nb + 1) * 128)
        have_prev = nb > 0
        d = {}

        def s0():
            qb_, t_qb = qr_.next()
            kb_, t_kb = kr_.next()
            vst, t_vst = vst_r.next()
            kb.dma("sp", qb_[:], qs[r_, ur, :], [], [t_qb])
            kb.dma("sp", kb_[:], ks[r_, ur, :], [], [t_kb])
            kb.dma("sp", vst[:], vs[r_, ur, :], [], [t_vst])
            d.update(qb=qb_, t_qb=t_qb, kb=kb_, t_kb=t_kb, vst=vst, t_vst=t_vst)

        def s1():
            v1, t_v1 = vr_.next()
            kb.cp("dve", v1[:, :, 0:64], d["vst"][:].rearrange("p (h d) -> p h d", d=64), [d["t_vst"]], [t_v1])
            pt, t_pt = psT.next()
            ptv = bfv(pt).rearrange("p (a h c) -> p a h c", a=2, h=4)
            for hp in range(4):
                kb.tr(ptv[:, 0, hp, :], d["qb"][:, hp * 128:(hp + 1) * 128], ident_bf[:], [d["t_qb"], t_identbf], [t_pt])
                kb.tr(ptv[:, 1, hp, :], d["kb"][:, hp * 128:(hp + 1) * 128], ident_bf[:], [d["t_kb"], t_identbf], [t_pt])
            d.update(v1=v1, t_v1=t_v1, ptv=ptv, t_pt=t_pt)

        def s2():
            qT, t_qT = qTr.next()
            kT, t_kT = kTr.next()
            ptv, t_pt = d["ptv"], d["t_pt"]
            qTv = qT[:].rearrange("p (hp j) c -> p hp j c", j=2)
            kb.cp("dve", qTv[0:64, :, 0, :], ptv[0:64, 0], [t_pt], [t_qT])
            kb.cp("dve", qTv[64:128, :, 1, :], ptv[64:128, 0], [t_pt], [t_qT])
            kb.cp("act", kT[:], ptv[:, 1], [t_pt], [t_kT])
            d.update(qT=qT, t_qT=t_qT, kT=kT, t_kT=t_kT)

        def mk_half(half):
            hd = {}

            def h3():
                (b0, b1), t_sc = sc_slots.next()
                qT, t_qT, kT, t_kT = d["qT"], d["t_qT"], d["kT"], d["t_kT"]
                for hh in range(4):
                    h = half * 4 + hh
                    hp = h // 2
                    bank = b0 if hh < 2 else b1
                    base = (hh % 2) * 256
                    if have_prev:
                        kb.mm(bank[:, base:base + 128], prev["kT"][:, hp, :], qT[:, h, :],
                              [prev["t_kT"], t_qT], [t_sc])
                    kb.mm(bank[:, base + 128:base + 256], kT[:, hp, :], qT[:, h, :],
                          [t_kT, t_qT], [t_sc])
                hd.update(b0=b0, b1=b1, t_sc=t_sc)

            def h4():
                E, t_E = er_.next()
                t_sc = hd["t_sc"]
                for bi, bank in enumerate((hd["b0"], hd["b1"])):
                    ev = E[:, 2 * bi:2 * bi + 2, :, :]
                    bv = bank.rearrange("p (h a c) -> p h a c", h=2, a=2)
                    if have_prev:
                        kb.act(ev, bv, AF.Exp, [t_sc], [t_E])
                    else:
                        kb.act(ev[:, :, 1, :], bv[:, :, 1, :], AF.Exp, [t_sc], [t_E])
                hd.update(E=E, t_E=t_E)

            def h5():
                E, t_E = hd["E"], hd["t_E"]
                if have_prev:
                    kb.tt("pool", E[:, :, 0, :], E[:, :, 0, :], tril_bf[:, :].unsqueeze(1).to_broadcast([128, 4, 128]),
                          ALU.mult, [t_E, t_trilbf], [t_E])
                kb.tt("dve", E[:, :, 1, :], E[:, :, 1, :], triu_bf[:, :].unsqueeze(1).to_broadcast([128, 4, 128]),
                      ALU.mult, [t_E, t_triubf], [t_E])

            def h6():
                E, t_E = hd["E"], hd["t_E"]
                pv, t_pv = pv_slots.next()
                pvv = pv[:, 0:260].rearrange("p (h c) -> p h c", h=4)
                for hh in range(4):
                    h = half * 4 + hh
                    if have_prev:
                        kb.mm(pvv[:, hh, :], E[:, hh, 0, :], prev["v1"][:, h, :], [t_E, prev["t_v1"]], [t_pv], start=True, stop=False)
                    kb.mm(pvv[:, hh, :], E[:, hh, 1, :], d["v1"][:, h, :], [t_E, d["t_v1"]], [t_pv], start=not have_prev, stop=True)
                hd.update(pvv=pvv, t_pv=t_pv)

            def h7():
                if half == 0:
                    U_, t_U = ur_.next()
                    d.update(U=U_, t_U=t_U)
                U_, t_U = d["U"], d["t_U"]
                kb.cp("act" if half else "dve", U_[:, half * 4:half * 4 + 4, :], hd["pvv"], [hd["t_pv"]], [t_U])
                if half == 1:
                    kb.dma("sp", us[r_, ur, :], U_[:].rearrange("p h c -> p (h c)"), [t_U], [])

            return [h3, h4, h5, h6, h7]

        return d, [[s0, s1, s2] + mk_half(0), [None, None, None] + mk_half(1)]

    items = []
    for g, (win, dil) in enumerate(ATT):
        nblk = S // dil // 128
        for r_ in range(dil):
            prev = None
            for nb in range(nblk):
                prev, its = attn_unit(g, dil, r_, nb, prev)
                items.extend(its)
    pipeline(items)

    print("sbuf end B", kb.off)
    if stop == "B":
        return finish_early()
    P.barrier()
    kb.off = persist_off
    dng = kb.sb([128, 128], F32, "dng"); t_dng = T()
    kb.dma("sp", dng[:], dn_out_norm.partition_broadcast(128), [], [t_dng])
    S32 = [(kb.sb([128, 128], F32, "S32"), T()) for _ in range(8)]
    Sbf = [(kb.sb([128, 128], BF16, "Sbf"), T()) for _ in range(8)]
    for h in range(8):
        kb.memset("pool", S32[h][0][:], 0.0, [S32[h][1]])
        kb.memset("pool", Sbf[h][0][:], 0.0, [Sbf[h][1]])
    qkvr = kb.ring(2, [128, 24, 128], BF16, "qkvT")
    bgr2 = kb.ring(2, [128, 16], F32, "bgc")
    zr = kb.ring(2, [128, 1024], BF16, "z")
    gbc_r = kb.ring(2, [128, 8, 128], F32, "gbc")
    gcum_r = kb.ring(2, [128, 8], F32, "gcum")
    ngc_r = kb.ring(2, [128, 8], F32, "ngc")
    eg_r = kb.ring(2, [128, 8], F32, "eg")
    neg_r = kb.ring(2, [128, 8], F32, "neg")
    kts_r = kb.ring(2, [128, 8], F32, "kts")
    egl_r = kb.ring(2, [128, 8], F32, "egl")
    H = 8
    pre_r = kb.ring(4, [128, 128], F32, "pre")
    dtm_r = kb.ring(2 * H, [128, 128], F32, "dtm")
    dts_r = kb.ring(2 * H, [128, 128], F32, "dts")
    B_r = kb.ring(H + 2, [128, 128], F32, "B")
    BT_r = kb.ring(H + 2, [128, 128], F32, "BT")
    SA_r = kb.ring(2 * H + 2, [128, 128], F32, "SA")
    SB_r = kb.ring(2 * H + 2, [128, 128], F32, "SB")
    R_r = kb.ring(2 * H + 2, [128, 128], F32, "R")
    PT_r = kb.ring(2 * H, [128, 128], BF16, "PT")
    AIT_r = kb.ring(2 * H, [128, 128], BF16, "AIT")
    Ktl_r = kb.ring(2 * H, [128, 128], BF16, "Ktl")
    Vtm_r = kb.ring(2 * H, [128, 128], BF16, "Vtm")
    Y_r = kb.ring(4, [128, 128], BF16, "Y")
    vn_r = kb.ring(4, [128, 128], BF16, "vn")
    o1_r = kb.ring(4, [128, 128], F32, "o1")
    O_r = kb.ring(2, [128, 8, 128], F32, "O")
    osq_r = kb.ring(2, [128, 8, 128], F32, "osq")
    obf_r = kb.ring(2, [128, 1024], BF16, "obf")
    s8c_r = kb.ring(2, [128, 8], F32, "s8c")
    glast_r = kb.ring(2, [128, 8], F32, "glast")
    psN = kb.psring([0, 1, 2, 3], 128, "psN")
    psXO = kb.psring([4, 5], 128, "psXO")
    psG = kb.psring([6], 128, "psG")
    psX = Ring([(kb.banks[7][:, 0:128], kb.pst(7)), (kb.banks[7][:, 128:256], kb.pst(7))])
    psB = Ring([(kb.banks[7][:, 256 + 64 * i:256 + 64 * (i + 1)], kb.pst(7)) for i in range(4)])

    for b in range(NT):
        cols = slice(b * 128, (b + 1) * 128)
        qkvT, t_qkvT = qkvr.next()
        bgc, t_bgc = bgr2.next()
        zt, t_zt = zr.next()
        kb.dma("sp", qkvT[:], qkvb_s[:, :, cols].rearrange("c p t -> p c t"), [], [t_qkvT])
        kb.dma("sp", bgc[:], bg_s[cols, :], [], [t_bgc])
        kb.dma("sp", zt[:], z_s[cols, :], [], [t_zt])
        gcum, t_gcum = gcum_r.next()
        psg, t_psg = psG.next()
        kb.mm(psg[:, 0:8], triu[:], bgc[:, 8:16], [t_triu, t_bgc], [t_psg])
        kb.cp("dve", gcum[:], psg[:, 0:8], [t_psg], [t_gcum])
        gbc, t_gbc = gbc_r.next()
        kb.cp("pool", gbc[:], bgc[:, 8:16].unsqueeze(2).to_broadcast([128, 8, 128]), [t_bgc], [t_gbc])
        eg, t_eg = eg_r.next()
        neg, t_neg = neg_r.next()
        kb.act(eg[:], gcum[:], AF.Exp, [t_gcum], [t_eg])
        kb.ts("dve", neg[:], eg[:], -1.0, ALU.mult, [t_eg], [t_neg])
        kts, t_kts = kts_r.next()
        egl, t_egl = egl_r.next()
        O_, t_O = O_r.next()
        glast, t_glast = glast_r.next()
        st = []
        for h in range(H):
            psg, t_psg = psG.next()
            kb.mm(psg, gbc[:, h, :], triu[:], [t_gbc, t_triu], [t_psg])
            pre, t_pre = pre_r.next()
            kb.stt("dve", pre[:], psg, gcum[:, h:h + 1], mbm[:], ALU.subtract, ALU.add, [t_psg, t_gcum, t_mbm], [t_pre])
            dtm, t_dtm = dtm_r.next()
            kb.act(dtm[:], pre[:], AF.Exp, [t_pre], [t_dtm])
            dts, t_dts = dts_r.next()
            kb.tt("pool", dts[:], dtm[:], strict[:], ALU.mult, [t_dtm, t_strict], [t_dts])
            kb.cp("act", glast[:, h:h + 1], psg[:, 127:128], [t_psg], [t_glast])
            st.append(dict(dtm=dtm, t_dtm=t_dtm, dts=dts, t_dts=t_dts))
        kb.act(egl[:], glast[:], AF.Exp, [t_glast], [t_egl])
        kb.tt("dve", kts[:], glast[:], gcum[:], ALU.subtract, [t_glast, t_gcum], [t_kts])
        kb.act(kts[:], kts[:], AF.Exp, [t_kts], [t_kts])
        for h in range(H):
            d = st[h]
            dtm, t_dtm, dts, t_dts = d["dtm"], d["t_dtm"], d["dts"], d["t_dts"]
            KT = qkvT[:, 8 + h, :]
            QT = qkvT[:, h, :]
            VT = qkvT[:, 16 + h, :]
            pkk, t_pkk = psN.next()
            pkq, t_pkq = psN.next()
            kb.mm(pkk, KT, KT, [t_qkvT], [t_pkk])
            kb.mm(pkq, KT, QT, [t_qkvT], [t_pkq])
            B, t_B = B_r.next()
            kb.stt("dve", B[:], pkk, bgc[:, h:h + 1], dts[:], ALU.mult, ALU.mult, [t_pkk, t_bgc, t_dts], [t_B])
            AIT, t_AIT = AIT_r.next()
            kb.tt("dve", AIT[:], pkq, dtm[:], ALU.mult, [t_pkq, t_dtm], [t_AIT])
            pb, t_pb = psB.next()
            kb.tr(bfv(pb), KT, ident_bf[:], [t_qkvT, t_identbf], [t_pb])
            Ktl, t_Ktl = Ktl_r.next()
            kb.act(Ktl[:], bfv(pb), AF.Copy, [t_pb, t_kts], [t_Ktl], scale=kts[:, h:h + 1])
            pb2, t_pb2 = psB.next()
            kb.tr(bfv(pb2), VT, ident_bf[:], [t_qkvT, t_identbf], [t_pb2])
            Vtm, t_Vtm = Vtm_r.next()
            kb.cp("act", Vtm[:], bfv(pb2), [t_pb2], [t_Vtm])
            d.update(B=B, t_B=t_B, AIT=AIT, t_AIT=t_AIT, Ktl=Ktl, t_Ktl=t_Ktl, Vtm=Vtm, t_Vtm=t_Vtm, KT=KT, QT=QT)
        for h in range(H):
            d = st[h]
            pt_, t_pt_ = psN.next()
            kb.tr(pt_, d["B"][:], ident[:], [d["t_B"], t_ident], [t_pt_])
            BT, t_BT = BT_r.next()
            kb.cp("act", BT[:], pt_, [t_pt_], [t_BT])
            R, t_R = R_r.next()
            kb.tt("pool", R[:], ident[:], d["B"][:], ALU.subtract, [t_ident, d["t_B"]], [t_R])
            d.update(Sk=d["B"], t_Sk=d["t_B"], SkT=BT, t_SkT=t_BT, R=R, t_R=t_R)
        NL = 6
        for lvl in range(1, NL + 1):
            last = lvl == NL
            for h in range(H):
                d = st[h]
                pT, t_pT = psN.next()
                kb.mm(pT, d["Sk"][:], d["SkT"][:], [d["t_Sk"], d["t_SkT"]], [t_pT])
                if not last:
                    pS, t_pS = psN.next()
                    kb.mm(pS, d["SkT"][:], d["Sk"][:], [d["t_Sk"], d["t_SkT"]], [t_pS])
                nT, t_nT = SB_r.next()
                kb.cp("act", nT[:], pT, [t_pT], [t_nT])
                if not last:
                    nS, t_nS = SA_r.next()
                    kb.cp("dve", nS[:], pS, [t_pS], [t_nS])
                    d.update(Sk=nS, t_Sk=t_nS)
                d.update(SkT=nT, t_SkT=t_nT)
            for h in range(H):
                d = st[h]
                pR, t_pR = psN.next()
                kb.mm(pR, d["SkT"][:], d["R"][:], [d["t_SkT"], d["t_R"]], [t_pR])
                if last:
                    PT, t_PT = PT_r.next()
                    kb.tt("dve", PT[:], pR, d["R"][:], ALU.add, [t_pR, d["t_R"]], [t_PT])
                    d.update(PT=PT, t_PT=t_PT)
                else:
                    nR, t_nR = R_r.next()
                    kb.tt("dve", nR[:], pR, d["R"][:], ALU.add, [t_pR, d["t_R"]], [t_nR])
                    d.update(R=nR, t_R=t_nR)
        for hg in range(0, H, 4):
            for h in range(hg, hg + 4):
                d = st[h]
                sbf, t_sbf = Sbf[h]
                px, t_px = psXO.next()
                kb.mm(px, d["KT"], sbf[:], [t_qkvT, t_sbf], [t_px])
                po1, t_po1 = psXO.next()
                kb.mm(po1, d["QT"], sbf[:], [t_qkvT, t_sbf], [t_po1])
                d.update(px=px, t_px=t_px, po1=po1, t_po1=t_po1)
            for h in range(hg, hg + 4):
                d = st[h]
                Y, t_Y = Y_r.next()
                kb.stt("dve", Y[:], d["px"], neg[:, h:h + 1], d["Vtm"][:], ALU.mult, ALU.add,
                       [d["t_px"], t_neg, d["t_Vtm"]], [t_Y])
                o1, t_o1 = o1_r.next()
                kb.act(o1[:], d["po1"], AF.Copy, [d["t_po1"], t_eg], [t_o1], scale=eg[:, h:h + 1])
                ppy, t_ppy = psX.next()
                kb.mm(ppy, d["PT"][:], Y[:], [d["t_PT"], t_Y], [t_ppy])
                vn, t_vn = vn_r.next()
                kb.act(vn[:], ppy, AF.Copy, [t_ppy, t_bgc], [t_vn], scale=bgc[:, h:h + 1])
                d.update(vn=vn, t_vn=t_vn, o1=o1, t_o1=t_o1)
            for h in range(hg, hg + 4):
                d = st[h]
                vn, t_vn = d["vn"], d["t_vn"]
                s32, t_s32 = S32[h]
                sbf, t_sbf = Sbf[h]
                pst, t_pst = psN.next()
                kb.mm(pst, d["Ktl"][:], vn[:], [d["t_Ktl"], t_vn], [t_pst])
                po2, t_po2 = psN.next()
                kb.mm(po2, d["AIT"][:], vn[:], [d["t_AIT"], t_vn], [t_po2])
                kb.stt("dve", s32[:], s32[:], egl[:, h:h + 1], pst, ALU.mult, ALU.add, [t_s32, t_egl, t_pst], [t_s32])
                kb.cp("act", sbf[:], s32[:], [t_s32], [t_sbf])
                kb.tt("dve", O_[:, h, :], po2, d["o1"][:], ALU.add, [t_po2, d["t_o1"]], [t_O])
        osq, t_osq = osq_r.next()
        kb.act(osq[:], O_[:], AF.Square, [t_O], [t_osq], scale=float(128.0 ** -0.5))
        s8, t_s8 = s8c_r.next()
        kb.red("dve", s8[:], osq[:], [t_osq], [t_s8])
        kb.act(s8[:], s8[:], AF.Sqrt, [t_s8, t_eps], [t_s8], bias=epsc[:, 0:1])
        kb.recip(s8[:], s8[:], [t_s8], [t_s8])
        kb.tt("pool", osq[:], O_[:], s8[:, :].unsqueeze(2).to_broadcast([128, 8, 128]), ALU.mult, [t_O, t_s8], [t_osq])
        kb.tt("pool", osq[:], osq[:], dng[:, :].unsqueeze(1).to_broadcast([128, 8, 128]), ALU.mult, [t_osq, t_dng], [t_osq])
        obf, t_obf = obf_r.next()
        kb.tt("dve", obf[:].rearrange("p (h d) -> p h d", d=128), osq[:], zt[:].rearrange("p (h d) -> p h d", d=128),
              ALU.mult, [t_osq, t_zt], [t_obf])
        kb.dma("sp", ob_s[cols, :], obf[:], [t_obf], [])

    print("sbuf end C", kb.off)
    if stop == "C":
        return finish_early()
    P.barrier()
    kb.off = persist_off
    TC = 8 if NT >= 8 else NT
    wa = kb.sb([128, 4, 1024], BF16, "wa"); t_wa = T()
    wb = kb.sb([128, 8, 1024], BF16, "wb"); t_wb = T()
    wo = kb.sb([128, 8, 1024], BF16, "wo"); t_wo = T()
    wpg = kb.sb([128, 8, 1024], BF16, "wpg"); t_wpg = T()
    wpl = kb.sb([128, 2, 1024], BF16, "wpl"); t_wpl = T()
    kb.dma("gq", wa[:], w_branch_a.rearrange("(k p) c -> p k c", p=128), [], [t_wa])
    kb.dma("gq", wb[:], w_branch_b.rearrange("(k p) c -> p k c", p=128), [], [t_wb])
    kb.dma("gq", wo[:], w_out.rearrange("(k p) c -> p k c", p=128), [], [t_wo])
    kb.dma("gq", wpg[:], w_pg.rearrange("(k p) c -> p k c", p=128), [], [t_wpg])
    kb.dma("gq", wpl[:], w_ple.rearrange("(k p) c -> p k c", p=128), [], [t_wpl])
    wr = kb.sb([128, 8, 20], F32, "wr"); t_wr = T()
    kb.dma("sp", wr[:, :, 0:4], w_rg.rearrange("(k p) c -> p k c", p=128), [], [t_wr])
    kb.dma("sp", wr[:, :, 4:20], w_re.rearrange("(k p) c -> p k c", p=128), [], [t_wr])
    br = kb.sb([128, 20], F32, "br"); t_br = T()
    kb.dma("sp", br[:, 0:4], b_rg.partition_broadcast(128), [], [t_br])
    kb.dma("sp", br[:, 4:20], b_re.partition_broadcast(128), [], [t_br])
    gffn = kb.sb([128, 1024], F32, "gffn"); t_gffn = T()
    gple = kb.sb([128, 1024], F32, "gple"); t_gple = T()
    kb.dma("sp", gffn[:], norm_ffn.partition_broadcast(128), [], [t_gffn])
    kb.dma("sp", gple[:], norm_ple.partition_broadcast(128), [], [t_gple])
    yacc = [(kb.sb([128, 1024], F32, "yacc"), T()) for _ in range(TC)]
    h2T = [(kb.sb([128, 8, 128], BF16, "h2T"), T()) for _ in range(TC)]
    comb = [(kb.sb([128, 16], F32, "comb"), T()) for _ in range(TC)]
    wgu_r = kb.ring(2, [128, 8, 512], BF16, "wgu")
    wd_r = kb.ring(2, [128, 2, 1024], BF16, "wdn")
    u_r = kb.ring(3, [128, 520], F32, "ua")
    un_r = kb.ring(2, [128, 8, 65], F32, "un")
    ga_r = kb.ring(1, [128, 2048], BF16, "gates")
    obr = kb.ring(2, [128, 1024], BF16, "ob")
    oab_r = kb.ring(2, [128, 512], BF16, "oab")
    rden_r = kb.ring(2, [128, 8], F32, "rden")
    tT_r = kb.ring(2, [128, 8, 128], BF16, "tT")
    mg_r = kb.ring(1, [128, 1024], F32, "mg")
    mgb_r = kb.ring(2, [128, 1024], BF16, "mgb")
    hf_r = kb.ring(1, [128, 1024], F32, "hf")
    hfT_r = kb.ring(1, [128, 8, 128], F32, "hfT")
    hbf_r = kb.ring(2, [128, 1024], BF16, "hbf")
    jk_r = kb.ring(1, [128, 1024], BF16, "jk2")
    ss_r = kb.ring(4, [128, 1], F32, "ss2")
    lg_r = kb.ring(2, [128, 20], F32, "lg")
    sm_r = kb.ring(24, [128, 16], F32, "sm")
    sil_r = kb.ring(3, [128, 256], F32, "sil")
    act_r = kb.ring(4, [128, 256], BF16, "actb")
    actT_r = kb.ring(4, [128, 2, 128], BF16, "actT")
    pin_r = kb.ring(2, [128, 256], F32, "pin")
    pbf_r = kb.ring(2, [128, 256], BF16, "pbf")
    sg_r = kb.ring(1, [128, 1024], F32, "sg")
    psM = kb.psring([0, 1, 2, 3], 512, "psM")
    psTr = kb.psring([4, 5], 512, "psTr")
    psD = kb.psring([6, 7], 512, "psD")
    final_ops = []

    def transpose_bf(src, t_src, nk):
        ps, t_ps = psTr.next()
        psv = bfv(ps).rearrange("p (k c) -> p k c", k=8)
        for k in range(nk):
            kb.tr(psv[:, k, :], src[:, k * 128:(k + 1) * 128], ident_bf[:], [t_src, t_identbf], [t_ps])
        tT, t_tT = tT_r.next()
        kb.cp("act", tT[:, 0:nk, :], psv[:, 0:nk, :], [t_ps], [t_tT])
        return tT, t_tT

    def proj(tT, t_tT, W, t_W, nk):
        res = []
        for c in range(2):
            ps, t_ps = psM.next()
            for k in range(nk):
                kb.mm(ps, tT[:, k, :], W[:, k, c * 512:(c + 1) * 512], [t_tT, t_W], [t_ps], start=(k == 0), stop=(k == nk - 1))
            res.append((ps, t_ps))
        return res

    def rmsnorm_to(xsrc, t_x, gain, t_gain, out_ap, t_out):
        jk, t_jk = jk_r.next()
        ss, t_ss = ss_r.next()
        kb.memset("pool", ss[:], 0.0, [t_ss])
        kb.act(jk[:], xsrc, AF.Square, [t_x, t_ss], [t_jk, t_ss], scale=1.0 / 32.0, accum_out=ss[:])
        rms_rstd(ss[:], t_ss)
        kb.stt("dve", out_ap, xsrc, ss[:, 0:1], gain[:], ALU.mult, ALU.mult, [t_x, t_ss, t_gain], [t_out])

    for c0 in range(0, NT, TC):
        for ti in range(TC):
            t = c0 + ti
            rows = slice(t * 128, (t + 1) * 128)
            ya, t_ya = yacc[ti]
            kb.dma("sp", ya[:], x[rows, :], [], [t_ya])
            us_ = []
            for g in range(3):
                u, t_u = u_r.next()
                kb.dma("sp", u[:], ua_s[g, rows, :], [], [t_u])
                us_.append((u, t_u))
            ga, t_ga = ga_r.next()
            kb.dma("sp", ga[:], gates_s[rows, :], [], [t_ga])
            ob, t_ob = obr.next()
            kb.dma("sp", ob[:], ob_s[rows, :], [], [t_ob])
            un, t_un = un_r.next()
            unf = un[:].rearrange("p h c -> p (h c)")
            kb.tt("pool", unf, us_[0][0][:], us_[1][0][:], ALU.add, [us_[0][1], us_[1][1]], [t_un])
            kb.tt("pool", unf, unf, us_[2][0][:], ALU.add, [t_un, us_[2][1]], [t_un])
            rden, t_rden = rden_r.next()
            kb.recip(rden[:], un[:, :, 64], [t_un], [t_rden])
            oab, t_oab = oab_r.next()
            kb.tt("dve", oab[:].rearrange("p (h d) -> p h d", d=64), un[:, :, 0:64],
                  rden[:, :].unsqueeze(2).to_broadcast([128, 8, 64]), ALU.mult, [t_un, t_rden], [t_oab])
            aT, t_aT = transpose_bf(oab, t_oab, 4)
            pa = proj(aT, t_aT, wa, t_wa, 4)
            bT, t_bT = transpose_bf(ob, t_ob, 8)
            pb_ = proj(bT, t_bT, wb, t_wb, 8)
            mg, t_mg = mg_r.next()
            mgb, t_mgb = mgb_r.next()
            for c in range(2):
                cs = slice(c * 512, (c + 1) * 512)
                kb.tt("dve", mg[:, cs], pa[c][0], ga[:, c * 512:(c + 1) * 512], ALU.mult, [pa[c][1], t_ga], [t_mg])
                kb.tt("dve", mgb[:, cs], pb_[c][0], ga[:, 1024 + c * 512:1024 + (c + 1) * 512], ALU.mult, [pb_[c][1], t_ga], [t_mgb])
            kb.tt("pool", mgb[:], mg[:], mgb[:], ALU.add, [t_mg, t_mgb], [t_mgb])
            mT, t_mT = transpose_bf(mgb, t_mgb, 8)
            po = proj(mT, t_mT, wo, t_wo, 8)
            for c in range(2):
                cs = slice(c * 512, (c + 1) * 512)
                kb.tt("dve", ya[:, cs], po[c][0], ya[:, cs], ALU.add, [po[c][1], t_ya], [t_ya])
            hf, t_hf = hf_r.next()
            rmsnorm_to(ya[:], t_ya, gffn, t_gffn, hf[:], t_hf)
            hbf, t_hbf = hbf_r.next()
            kb.cp("pool", hbf[:], hf[:], [t_hf], [t_hbf])
            hT2, t_hT2 = h2T[ti]
            ps, t_ps = psTr.next()
            psv = bfv(ps).rearrange("p (k c) -> p k c", k=8)
            for k in range(8):
                kb.tr(psv[:, k, :], hbf[:, k * 128:(k + 1) * 128], ident_bf[:], [t_hbf, t_identbf], [t_ps])
            kb.cp("act", hT2[:], psv, [t_ps], [t_hT2])
            hfT, t_hfT = hfT_r.next()
            for hv in range(2):
                ps, t_ps = psTr.next()
                psv4 = ps.rearrange("p (k c) -> p k c", k=4)
                for k in range(4):
                    kk = hv * 4 + k
                    kb.tr(psv4[:, k, :], hf[:, kk * 128:(kk + 1) * 128], ident[:], [t_hf, t_ident], [t_ps])
                kb.cp("act" if hv else "dve", hfT[:, hv * 4:hv * 4 + 4, :], psv4, [t_ps], [t_hfT])
            ps, t_ps = psD.next()
            for k in range(8):
                kb.mm(ps[:, 0:20], hfT[:, k, :], wr[:, k, :], [t_hfT, t_wr], [t_ps], start=(k == 0), stop=(k == 7))
            lg, t_lg = lg_r.next()
            kb.tt("dve", lg[:], ps[:, 0:20], br[:], ALU.add, [t_ps, t_br], [t_lg])
            cm, t_cm = comb[ti]

            def sm(n=16):
                a, t_a = sm_r.next()
                return a[:, 0:n], t_a
            gmax, t_gmax = sm(1)
            kb.red("dve", gmax, lg[:, 0:4], [t_lg], [t_gmax], op=ALU.max)
            ngmax, t_ngmax = sm(1)
            kb.ts("dve", ngmax, gmax, -1.0, ALU.mult, [t_gmax], [t_ngmax])
            gex, t_gex = sm(4)
            gsum, t_gsum = sm(1)
            kb.memset("pool", gsum, 0.0, [t_gsum])
            kb.act(gex, lg[:, 0:4], AF.Exp, [t_lg, t_ngmax, t_gsum], [t_gex, t_gsum], bias=ngmax, accum_out=gsum)
            pg, t_pg = sm(1)
            kb.recip(pg, gsum, [t_gsum], [t_pg])
            goh, t_goh = sm(4)
            kb.ts("dve", goh, lg[:, 0:4], gmax, ALU.is_ge, [t_lg, t_gmax], [t_goh])
            el, t_el = sm(16)
            kb.tt("dve", el.rearrange("p (g e) -> p g e", e=4), lg[:, 4:20].rearrange("p (g e) -> p g e", e=4),
                  goh.unsqueeze(2).to_broadcast([128, 4, 4]), ALU.mult, [t_lg, t_goh], [t_el])
            sel, t_sel = sm(4)
            kb.red("dve", sel, el.rearrange("p (g e) -> p e g", e=4), [t_el], [t_sel])
            m1, t_m1 = sm(1)
            kb.red("dve", m1, sel, [t_sel], [t_m1], op=ALU.max)
            oh1, t_oh1 = sm(4)
            kb.ts("dve", oh1, sel, m1, ALU.is_ge, [t_sel, t_m1], [t_oh1])
            sel2, t_sel2 = sm(4)
            kb.stt("dve", sel2, oh1, -1e30, sel, ALU.mult, ALU.add, [t_oh1, t_sel], [t_sel2])
            m2, t_m2 = sm(1)
            kb.red("dve", m2, sel2, [t_sel2], [t_m2], op=ALU.max)
            oh2, t_oh2 = sm(4)
            kb.ts("dve", oh2, sel2, m2, ALU.is_ge, [t_sel2, t_m2], [t_oh2])
            dd, t_dd = sm(1)
            kb.tt("dve", dd, m2, m1, ALU.subtract, [t_m2, t_m1], [t_dd])
            kb.act(dd, dd, AF.Exp, [t_dd], [t_dd])
            kb.ts("dve", dd, dd, 1.0, ALU.add, [t_dd], [t_dd])
            w1, t_w1 = sm(1)
            kb.recip(w1, dd, [t_dd], [t_w1])
            w2, t_w2 = sm(1)
            kb.ts("dve", w2, w1, -1.0, ALU.mult, [t_w1], [t_w2], s2=1.0, op1=ALU.add)
            kb.tt("dve", w1, w1, pg, ALU.mult, [t_w1, t_pg], [t_w1])
            kb.tt("dve", w2, w2, pg, ALU.mult, [t_w2, t_pg], [t_w2])
            wig, t_wig = sm(4)
            kb.ts("dve", wig, oh1, w1, ALU.mult, [t_oh1, t_w1], [t_wig])
            kb.stt("dve", wig, oh2, w2, wig, ALU.mult, ALU.add, [t_oh2, t_w2, t_wig], [t_wig])
            for g in range(4):
                kb.ts("dve", cm[:, g * 4:(g + 1) * 4], wig, goh[:, g:g + 1], ALU.mult, [t_wig, t_goh], [t_cm])
        def load_gu(e_):
            wgu, t_wgu_sb = wgu_r.next()
            kb.dma("sp", wgu[:, 0:4, :], wgu_s[e_].rearrange("(k p) c -> p k c", p=128)[:, 0:4, :], [t_wgu[e_]], [t_wgu_sb])
            kb.dma("gq", wgu[:, 4:8, :], wgu_s[e_].rearrange("(k p) c -> p k c", p=128)[:, 4:8, :], [t_wgu[e_]], [t_wgu_sb])
            return wgu, t_wgu_sb

        def load_dn(e_):
            wdn, t_wdn = wd_r.next()
            kb.dma("sp", wdn[:], wd_s[e_].rearrange("(k p) c -> p k c", p=128), [t_wd[e_]], [t_wdn])
            return wdn, t_wdn

        Wgu = {0: load_gu(0)}
        Wdn = {0: load_dn(0)}

        def exp_item(e_, ti):
            ya, t_ya = yacc[ti]
            hT2, t_hT2 = h2T[ti]
            cm, t_cm = comb[ti]
            d = {}

            def e0():
                if ti == 0 and e_ + 1 < 16:
                    Wgu[e_ + 1] = load_gu(e_ + 1)
                wgu, t_wgu_sb = Wgu[e_]
                ps, t_ps = psM.next()
                for k in range(8):
                    kb.mm(ps, hT2[:, k, :], wgu[:, k, :], [t_hT2, t_wgu_sb], [t_ps], start=(k == 0), stop=(k == 7))
                d.update(ps=ps, t_ps=t_ps)

            def e1():
                ps, t_ps = d["ps"], d["t_ps"]
                sil, t_sil = sil_r.next()
                kb.act(sil[:], ps[:, 0:256], AF.Silu, [t_ps], [t_sil])
                ab, t_ab = act_r.next()
                kb.stt("dve", ab[:], ps[:, 256:512], cm[:, e_:e_ + 1], sil[:], ALU.mult, ALU.mult, [t_ps, t_cm, t_sil], [t_ab])
                d.update(ab=ab, t_ab=t_ab)

            def e2():
                pst_, t_pst_ = psTr.next()
                pstv = bfv(pst_).rearrange("p (k c) -> p k c", k=8)
                for k in range(2):
                    kb.tr(pstv[:, k, :], d["ab"][:, k * 128:(k + 1) * 128], ident_bf[:], [d["t_ab"], t_identbf], [t_pst_])
                d.update(pstv=pstv, t_pst=t_pst_)

            def e3():
                aT, t_aT = actT_r.next()
                kb.cp("act", aT[:], d["pstv"][:, 0:2, :], [d["t_pst"]], [t_aT])
                d.update(aT=aT, t_aT=t_aT)

            def e4():
                if ti == 0 and e_ + 1 < 16:
                    Wdn[e_ + 1] = load_dn(e_ + 1)
                wdn, t_wdn = Wdn[e_]
                pds = []
                for c in range(2):
                    pd, t_pd = psD.next()
                    for k in range(2):
                        kb.mm(pd, d["aT"][:, k, :], wdn[:, k, c * 512:(c + 1) * 512], [d["t_aT"], t_wdn], [t_pd], start=(k == 0), stop=(k == 1))
                    pds.append((pd, t_pd))
                d.update(pds=pds)

            def e5():
                for c in range(2):
                    pd, t_pd = d["pds"][c]
                    cs = slice(c * 512, (c + 1) * 512)
                    kb.tt("dve", ya[:, cs], ya[:, cs], pd, ALU.add, [t_ya, t_pd], [t_ya])

            return [e0, e1, e2, e3, e4, e5]

        pipeline([exp_item(e_, ti) for e_ in range(16) for ti in range(TC)])
        for ti in range(TC):
            t = c0 + ti
            rows = slice(t * 128, (t + 1) * 128)
            ya, t_ya = yacc[ti]
            hbf, t_hbf = hbf_r.next()
            rmsnorm_to(ya[:], t_ya, gple, t_gple, hbf[:], t_hbf)
            h3T, t_h3T = transpose_bf(hbf, t_hbf, 8)
            pgate = proj(h3T, t_h3T, wpg, t_wpg, 8)
            pin, t_pin = pin_r.next()
            kb.dma("sp", pin[:], p_in[rows, :], [], [t_pin])
            pbf, t_pbf = pbf_r.next()
            kb.cp("pool", pbf[:], pin[:], [t_pin], [t_pbf])
            pT, t_pT = transpose_bf(pbf, t_pbf, 2)
            sg, t_sg = sg_r.next()
            for c in range(2):
                cs = slice(c * 512, (c + 1) * 512)
                kb.act(sg[:, cs], pgate[c][0], AF.Sigmoid, [pgate[c][1]], [t_sg])
            pple = proj(pT, t_pT, wpl, t_wpl, 2)
            for c in range(2):
                cs = slice(c * 512, (c + 1) * 512)
                kb.tt("dve", sg[:, cs], sg[:, cs], pple[c][0], ALU.mult, [t_sg, pple[c][1]], [t_sg])
            kb.tt("pool", ya[:], sg[:], ya[:], ALU.add, [t_sg, t_ya], [t_ya])
            final_ops.append(kb.dma("sp", out[rows, :], ya[:], [t_ya], []))

    print("sbuf end D", kb.off, "ops", {k: len(v) for k, v in P.ops.items()})
    P.emit(nc, final_waits=final_ops)
    return nc


_CONSTS = None


def _consts():
    global _CONSTS
    if _CONSTS is None:
        _CONSTS = {
            "c_ident": np.eye(128, dtype=np.float32),
            "c_triu": np.triu(np.ones((128, 128), dtype=np.float32)),
            "c_tril": np.tril(np.ones((128, 128), dtype=np.float32)),
        }
    return _CONSTS


def make_in_map(inputs, b, S):
    f = lambda a: np.ascontiguousarray(np.asarray(a, dtype=np.float32))
    m = {
        "x": f(inputs["x"][b, :S]),
        "p": f(inputs["p"][0, b, :S]),
        "norm_mix": f(inputs["norm_mix"][0]),
        "w_in": f(inputs["w_in"][0]),
        "q_norm": f(inputs["q_norm"][0]),
        "k_norm": f(inputs["k_norm"][0]),
        "conv_w": f(np.asarray(inputs["conv_w"][0]).reshape(4, 24, 128).transpose(2, 1, 0)),
        "a_log": f(inputs["a_log"][0]),
        "dt_bias": f(inputs["dt_bias"][0]),
        "dn_out_norm": f(inputs["dn_out_norm"][0]),
        "w_branch_a": f(inputs["w_branch_a"][0]),
        "w_branch_b": f(inputs["w_branch_b"][0]),
        "w_out": f(inputs["w_out"][0]),
        "norm_ffn": f(inputs["norm_ffn"][0]),
        "w_router_group": f(inputs["w_router_group"][0]),
        "b_router_group": f(inputs["b_router_group"][0]),
        "w_router_expert": f(inputs["w_router_expert"][0]),
        "b_router_expert": f(inputs["b_router_expert"][0]),
        "w_expert_gate": f(np.asarray(inputs["w_expert_gate"][0]).reshape(16, 1024, 256)),
        "w_expert_up": f(np.asarray(inputs["w_expert_up"][0]).reshape(16, 1024, 256)),
        "w_expert_down": f(np.asarray(inputs["w_expert_down"][0]).reshape(16, 256, 1024)),
        "norm_ple": f(inputs["norm_ple"][0]),
        "w_ple": f(inputs["w_ple"][0]),
        "w_ple_gate": f(inputs["w_ple_gate"][0]),
    }
    m.update(_consts())
    return m


def kernel(**inputs):
    S = 8192
    nc = build(S)
    in_maps = [make_in_map(inputs, b, S) for b in range(8)]
    res = run_bass_kernel_spmd(nc, in_maps, core_ids=list(range(8)))
    return np.stack([np.asarray(r["out"], dtype=np.float32) for r in res.results], axis=0)
```

```python
import numpy as np
import concourse.bass as bass
import concourse.mybir as mybir
from concourse.bass_utils import run_bass_kernel_spmd

F32 = mybir.dt.float32
BF16 = mybir.dt.bfloat16
ALU = mybir.AluOpType
AF = mybir.ActivationFunctionType
AX = mybir.AxisListType

ENGS = ("pe", "act", "dve", "pool")
DMAQ = ("sp", "gq")
NDSEM = 24
EPS = 1e-6
SB_BASE = 16512
SB_TOP = 229344


class T:
    __slots__ = ("name", "w", "rs", "banks")

    def __init__(self, name="", banks=()):
        self.name = name
        self.w = None
        self.rs = []
        self.banks = banks


class Bank:
    __slots__ = ("last",)

    def __init__(self):
        self.last = None


class Op:
    __slots__ = ("eng", "fn", "idx", "deps", "sig", "signo", "dslot", "dval", "clock", "gorder")


class Prog:
    def __init__(self):
        self.ops = {e: [] for e in ENGS + DMAQ}
        self.clock = {e: {} for e in ENGS + DMAQ}
        self.ndma = {q: 0 for q in DMAQ}
        self.g = 0
        self.pending = {e: [] for e in ENGS + DMAQ}

    def barrier(self):
        lasts = []
        for e in ENGS:
            if self.ops[e]:
                lasts.append(self.ops[e][-1])
        for q in DMAQ:
            lasts.extend(self.ops[q][-NDSEM:])
        self.pending["sp"] = list(lasts)
        op = self.add("sp", self.bar_fn)
        for e in ENGS + ("gq",):
            self.pending[e] = [op]

    def add(self, eng, fn, reads=(), writes=()):
        op = Op()
        op.eng = eng
        op.fn = fn
        op.idx = len(self.ops[eng])
        op.sig = False
        op.signo = None
        op.gorder = self.g
        self.g += 1
        deps = []
        for t in reads:
            if t.w is not None:
                deps.append((t.w, True))
        for t in writes:
            if t.w is not None:
                deps.append((t.w, False))
            for r in t.rs:
                deps.append((r, False))
        bks = []
        for t in tuple(reads) + tuple(writes):
            for b in t.banks:
                if b not in bks:
                    bks.append(b)
        for b in bks:
            if b.last is not None and b.last.eng != eng:
                deps.append((b.last, True))
        if self.pending[eng]:
            for d in self.pending[eng]:
                if d.eng != eng or eng in DMAQ:
                    deps.append((d, True))
            self.pending[eng] = []
        clk = self.clock[eng]
        need = {}
        for d, raw in deps:
            if d.eng == eng and eng not in DMAQ:
                if eng == "pe":
                    continue
            if d.eng in DMAQ:
                key = (d.eng, d.idx)
                if clk.get(key, False):
                    continue
                need[key] = d
            else:
                if clk.get(d.eng, -1) >= d.idx:
                    continue
                cur = need.get(d.eng)
                if cur is None or cur.idx < d.idx:
                    need[d.eng] = d
        op.deps = list(need.values())
        for d in op.deps:
            d.sig = True
            for k, v in d.clock.items():
                if isinstance(k, tuple):
                    clk[k] = True
                elif clk.get(k, -1) < v:
                    clk[k] = v
            if d.eng in DMAQ:
                clk[(d.eng, d.idx)] = True
            elif clk.get(d.eng, -1) < d.idx:
                clk[d.eng] = d.idx
        if eng in DMAQ:
            op.dslot = self.ndma[eng] % NDSEM
            op.dval = 16 * (self.ndma[eng] // NDSEM + 1)
            self.ndma[eng] += 1
            prev_i = op.idx - NDSEM
            if prev_i >= 0:
                pd = self.ops[eng][prev_i]
                if not clk.get((eng, prev_i), False):
                    op.deps.append(pd)
                    clk[(eng, prev_i)] = True
            op.sig = True
        if len(clk) > 400:
            for k in [k for k in clk if isinstance(k, tuple) and k[1] < self.ndma[k[0]] - 4 * NDSEM]:
                del clk[k]
        op.clock = dict(clk)
        if eng not in DMAQ:
            op.clock[eng] = op.idx
        self.ops[eng].append(op)
        for b in bks:
            b.last = op
        for t in reads:
            t.rs.append(op)
        for t in writes:
            t.w = op
            t.rs = []
        return op

    def emit(self, nc, final_waits=()):
        from contextlib import ExitStack
        with ExitStack() as es:
            sems = {e: es.enter_context(nc.semaphore("s_" + e)) for e in ENGS}
            dsems = {q: [es.enter_context(nc.semaphore(f"d_{q}{i}")) for i in range(NDSEM)] for q in DMAQ}
            for d in final_waits:
                d.sig = True
            for e in ENGS:
                n = 0
                for op in self.ops[e]:
                    if op.sig:
                        n += 1
                        op.signo = n
            block = es.enter_context(nc.Block())

            def waits(engobj, op):
                for d in op.deps:
                    if d.eng in DMAQ:
                        engobj.wait_ge(dsems[d.eng][d.dslot], d.dval)
                    else:
                        engobj.wait_ge(sems[d.eng], d.signo)

            def run(oplist, engobj, extra=None):
                for op in oplist:
                    waits(engobj, op)
                    ins = op.fn(engobj)
                    if op.sig:
                        if op.eng in DMAQ:
                            ins.then_inc(dsems[op.eng][op.dslot], 16)
                        else:
                            ins.then_inc(sems[op.eng], 1)
                if extra:
                    extra(engobj)

            def fin(engobj):
                for d in final_waits:
                    if d.eng in DMAQ:
                        engobj.wait_ge(dsems[d.eng][d.dslot], d.dval)
                    else:
                        engobj.wait_ge(sems[d.eng], d.signo)

            @block.tensor
            def _(e):
                run(self.ops["pe"], e)

            @block.scalar
            def _(e):
                run(self.ops["act"], e)

            @block.vector
            def _(e):
                run(self.ops["dve"], e)

            @block.gpsimd
            def _(e):
                merged = sorted(self.ops["pool"] + self.ops["gq"], key=lambda o: o.gorder)
                run(merged, e)

            @block.sync
            def _(e):
                run(self.ops["sp"], e, fin)


class KB:
    def __init__(self, nc):
        self.nc = nc
        self.P = Prog()
        self.off = SB_BASE
        self.n = 0
        self.banks = [nc.alloc_psum_tensor(f"bank{i}", [128, 512], F32) for i in range(8)]
        self.bk = [Bank() for _ in range(8)]

    def pst(self, *bank_ids):
        return T("ps", banks=tuple(self.bk[b] for b in bank_ids))

    def sb(self, shape, dt, name="t"):
        sz = int(np.prod(shape[1:])) * (2 if dt == BF16 else 4)
        sz = (sz + 31) // 32 * 32
        assert self.off + sz <= SB_TOP, f"sbuf overflow {name} {self.off} {sz}"
        self.n += 1
        t = self.nc.alloc_sbuf_tensor_at(f"{name}_{self.n}", list(shape), dt, offset=self.off)
        self.off += sz
        return t

    def ring(self, n, shape, dt, name="r"):
        return Ring([(self.sb(shape, dt, name), T(name)) for _ in range(n)])

    def psring(self, banks, nf32, name="ps"):
        per = 512 // nf32
        slots = []
        for i in range(per):
            for b in banks:
                slots.append((self.banks[b][:, i * nf32:(i + 1) * nf32], self.pst(b)))
        return Ring(slots)

    def mm(self, out, lhsT, rhs, r, w, start=True, stop=True):
        return self.P.add("pe", lambda e: e.matmul(out, lhsT=lhsT, rhs=rhs, start=start, stop=stop), reads=r, writes=w)

    def tr(self, out, in_, ident, r, w):
        return self.P.add("pe", lambda e: e.transpose(out=out, in_=in_, identity=ident), reads=r, writes=w)

    def act(self, out, in_, func, r, w, bias=None, scale=None, accum_out=None, eng="act"):
        kw = {}
        if bias is not None:
            kw["bias"] = bias
        if scale is not None:
            kw["scale"] = scale
        if accum_out is not None:
            kw["accum_out"] = accum_out
        return self.P.add("act", lambda e: e.activation(out=out, in_=in_, func=func, **kw), reads=r, writes=w)

    def cp(self, eng, out, in_, r, w):
        if eng == "act":
            return self.P.add("act", lambda e: e.copy(out=out, in_=in_), reads=r, writes=w)
        return self.P.add(eng, lambda e: e.tensor_copy(out=out, in_=in_), reads=r, writes=w)

    def tt(self, eng, out, in0, in1, op, r, w):
        return self.P.add(eng, lambda e: e.tensor_tensor(out=out, in0=in0, in1=in1, op=op), reads=r, writes=w)

    def ts(self, eng, out, in0, s1, op0, r, w, s2=None, op1=None):
        if op1 is None:
            return self.P.add(eng, lambda e: e.tensor_scalar(out=out, in0=in0, scalar1=s1, scalar2=None, op0=op0), reads=r, writes=w)
        return self.P.add(eng, lambda e: e.tensor_scalar(out=out, in0=in0, scalar1=s1, scalar2=s2, op0=op0, op1=op1), reads=r, writes=w)

    def stt(self, eng, out, in0, scalar, in1, op0, op1, r, w):
        return self.P.add(eng, lambda e: e.scalar_tensor_tensor(out=out, in0=in0, scalar=scalar, in1=in1, op0=op0, op1=op1), reads=r, writes=w)

    def red(self, eng, out, in_, r, w, op=ALU.add):
        return self.P.add(eng, lambda e: e.tensor_reduce(out=out, in_=in_, axis=AX.X, op=op), reads=r, writes=w)

    def recip(self, out, in_, r, w):
        return self.P.add("dve", lambda e: e.reciprocal(out=out, in_=in_), reads=r, writes=w)

    def memset(self, eng, ap, val, w):
        return self.P.add(eng, lambda e: e.memset(ap, val), writes=w)

    def dma(self, q, out, in_, r, w):
        return self.P.add(q, lambda e: e.dma_start(out=out, in_=in_), reads=r, writes=w)


class Ring:
    def __init__(self, slots):
        self.slots = slots
        self.i = 0

    def next(self):
        s = self.slots[self.i % len(self.slots)]
        self.i += 1
        return s


def pipeline(units):
    n = len(units)
    nst = max(len(u) for u in units)
    for i in range(n + nst - 1):
        for st in range(nst - 1, -1, -1):
            u = i - st
            if 0 <= u < n and st < len(units[u]) and units[u][st] is not None:
                units[u][st]()


def bfv(ap):
    return ap.bitcast(BF16)


def build(S, debug=False, stop=None, skip_pre=False):
    nc = bass.Bass("TRN2", target_bir_lowering=False)
    NT = S // 128
    NSP = S // 512
    kb = KB(nc)
    P = kb.P

    def din(name, shape, dt=F32):
        return nc.dram_tensor(name, list(shape), dt, kind="ExternalInput").ap()

    def dscr(name, shape, dt):
        return nc.dram_tensor(name, list(shape), dt, kind="ExternalOutput" if debug else "Internal").ap()

    x = din("x", [S, 1024])
    p_in = din("p", [S, 256])
    norm_mix = din("norm_mix", [1024])
    w_in = din("w_in", [1024, 10768])
    q_norm = din("q_norm", [64])
    k_norm = din("k_norm", [64])
    conv_w = din("conv_w", [128, 24, 4])
    a_log = din("a_log", [8])
    dt_bias = din("dt_bias", [8])
    dn_out_norm = din("dn_out_norm", [128])
    w_branch_a = din("w_branch_a", [512, 1024])
    w_branch_b = din("w_branch_b", [1024, 1024])
    w_out = din("w_out", [1024, 1024])
    norm_ffn = din("norm_ffn", [1024])
    w_rg = din("w_router_group", [1024, 4])
    b_rg = din("b_router_group", [4])
    w_re = din("w_router_expert", [1024, 16])
    b_re = din("b_router_expert", [16])
    w_eg = din("w_expert_gate", [16, 1024, 256])
    w_eu = din("w_expert_up", [16, 1024, 256])
    w_ed = din("w_expert_down", [16, 256, 1024])
    norm_ple = din("norm_ple", [1024])
    w_ple = din("w_ple", [256, 1024])
    w_pg = din("w_ple_gate", [1024, 1024])
    c_ident = din("c_ident", [128, 128])
    c_triu = din("c_triu", [128, 128])
    c_tril = din("c_tril", [128, 128])
    out = nc.dram_tensor("out", [S, 1024], F32, kind="ExternalOutput").ap()

    qkva_s = dscr("qkva_s", [9, S, 512], BF16)
    qkvb_s = dscr("qkvb_s", [24, 128, S], BF16)
    z_s = dscr("z_s", [S, 1024], BF16)
    bg_s = dscr("bg_s", [S, 16], F32)
    gates_s = dscr("gates_s", [S, 2048], BF16)
    ua_s = dscr("ua_s", [3, S, 520], F32)
    ob_s = dscr("ob_s", [S, 1024], BF16)
    wgu_s = nc.dram_tensor("wgu_s", [16, 1024, 512], BF16, kind="Internal").ap()
    wd_s = nc.dram_tensor("wd_s", [16, 256, 1024], BF16, kind="Internal").ap()

    ident = kb.sb([128, 128], F32, "ident"); t_ident = T()
    ident_bf = kb.sb([128, 128], BF16, "identbf"); t_identbf = T()
    triu = kb.sb([128, 128], F32, "triu"); t_triu = T()
    tril_bf = kb.sb([128, 128], BF16, "trilbf"); t_trilbf = T()
    triu_bf = kb.sb([128, 128], BF16, "triubf"); t_triubf = T()
    tril = kb.sb([128, 128], F32, "tril"); t_tril = T()
    mbm = kb.sb([128, 128], F32, "mbm"); t_mbm = T()
    strict = kb.sb([128, 128], F32, "strict"); t_strict = T()
    ones_bf = kb.sb([128, 128], BF16, "ones"); t_ones = T()
    epsc = kb.sb([128, 1], F32, "eps"); t_eps = T()
    eps128 = kb.sb([128, 1], F32, "eps128"); t_eps128 = T()
    kb.dma("sp", ident[:], c_ident[:, :], [], [t_ident])
    kb.dma("sp", triu[:], c_triu[:, :], [], [t_triu])
    kb.dma("sp", tril[:], c_tril[:, :], [], [t_tril])
    kb.cp("dve", ident_bf[:], ident[:], [t_ident], [t_identbf])
    kb.cp("dve", triu_bf[:], triu[:], [t_triu], [t_triubf])
    kb.cp("dve", tril_bf[:], tril[:], [t_tril], [t_trilbf])
    kb.ts("dve", mbm[:], triu[:], -1.0, ALU.add, [t_triu], [t_mbm], s2=1e9, op1=ALU.mult)
    kb.tt("dve", strict[:], triu[:], ident[:], ALU.subtract, [t_triu, t_ident], [t_strict])
    kb.memset("pool", ones_bf[:], 1.0, [t_ones])
    kb.memset("pool", epsc[:], EPS, [t_eps])
    kb.memset("pool", eps128[:], 128.0 * EPS, [t_eps128])

    t_wgu = [T() for _ in range(16)]
    t_wd = [T() for _ in range(16)]
    for e_ in range(0 if not skip_pre else 16, 16):
        kb.dma("gq", wgu_s[e_, :, 0:256], w_eg[e_], [], [t_wgu[e_]])
        kb.dma("gq", wgu_s[e_, :, 256:512], w_eu[e_], [], [t_wgu[e_]])
        kb.dma("gq", wd_s[e_], w_ed[e_], [], [t_wd[e_]])

    bar_a = kb.sb([128, 8], F32, "bar_a")
    bar_b = kb.sb([128, 8], F32, "bar_b")
    kb.memset("pool", bar_a[:], 0.0, [])
    P.bar_fn = lambda e: e.dma_start(out=bar_b[:], in_=bar_a[:])
    persist_off = kb.off

    def finish_early():
        fw = []
        for q in DMAQ:
            fw.extend(P.ops[q][-NDSEM:])
        for e in ENGS:
            if P.ops[e]:
                fw.append(P.ops[e][-1])
        P.emit(nc, final_waits=fw)
        return nc
    gain_mix = kb.sb([128, 1024], F32, "gmix"); t_gmix = T()
    kb.dma("sp", gain_mix[:], norm_mix.partition_broadcast(128), [], [t_gmix])
    hT = kb.sb([128, 8, S], BF16, "hT")
    t_hT = [T() for _ in range(NT)]
    a_off = kb.off
    xr = kb.ring(4, [128, 1024], F32, "xt")
    junk = kb.ring(2, [128, 1024], BF16, "junk")
    hb = kb.ring(3, [128, 1024], BF16, "hb")
    ssr = kb.ring(6, [128, 1], F32, "ss")
    psA0 = kb.psring([0, 1], 512, "psA0")

    def rms_rstd(ss_ap, t_ss, nfeat_scale_done=True):
        kb.act(ss_ap, ss_ap, AF.Sqrt, [t_ss, t_eps], [t_ss], bias=epsc[:, 0:1])
        kb.recip(ss_ap, ss_ap, [t_ss], [t_ss])

    def a0_unit(t):
        d = {}

        def s0():
            xt, t_xt = xr.next()
            kb.dma("sp", xt[:], x[t * 128:(t + 1) * 128, :], [], [t_xt])
            d.update(xt=xt, t_xt=t_xt)

        def s1():
            xt, t_xt = d["xt"], d["t_xt"]
            jk, t_jk = junk.next()
            ss, t_ss = ssr.next()
            kb.memset("pool", ss[:], 0.0, [t_ss])
            kb.act(jk[:], xt[:], AF.Square, [t_xt, t_ss], [t_jk, t_ss], scale=1.0 / 32.0, accum_out=ss[:])
            kb.act(ss[:], ss[:], AF.Sqrt, [t_ss, t_eps], [t_ss], bias=epsc[:, 0:1])
            d.update(ss=ss, t_ss=t_ss)

        def s2():
            xt, t_xt, ss, t_ss = d["xt"], d["t_xt"], d["ss"], d["t_ss"]
            kb.recip(ss[:], ss[:], [t_ss], [t_ss])
            h_, t_h = hb.next()
            kb.stt("dve", h_[:], xt[:], ss[:, 0:1], gain_mix[:], ALU.mult, ALU.mult, [t_xt, t_ss, t_gmix], [t_h])
            d.update(h=h_, t_h=t_h)

        def s3():
            ps, t_ps = psA0.next()
            psv = bfv(ps).rearrange("p (k c) -> p k c", k=8)
            for k in range(8):
                kb.tr(psv[:, k, :], d["h"][:, k * 128:(k + 1) * 128], ident_bf[:], [d["t_h"], t_identbf], [t_ps])
            d.update(psv=psv, t_ps=t_ps)

        def s4():
            kb.cp("act" if t % 2 else "dve", hT[:, :, t * 128:(t + 1) * 128], d["psv"], [d["t_ps"]], [t_hT[t]])

        return [s0, s1, s2, s3, s4]

    pipeline([a0_unit(t) for t in range(NT)])

    if stop == "A0":
        return finish_early()
    P.barrier()
    kb.off = a_off
    qg = kb.sb([128, 64], F32, "qg"); t_qg = T()
    kg = kb.sb([128, 64], F32, "kg"); t_kg = T()
    kb.dma("sp", qg[:], q_norm.partition_broadcast(128), [], [t_qg])
    kb.dma("sp", kg[:], k_norm.partition_broadcast(128), [], [t_kg])
    kb.ts("dve", qg[:], qg[:], 0.125, ALU.mult, [t_qg], [t_qg])
    convw = kb.sb([128, 24, 4], F32, "convw"); t_convw = T()
    kb.dma("sp", convw[:], conv_w[:, :, :], [], [t_convw])
    dtb = kb.sb([128, 8], F32, "dtb"); t_dtb = T()
    negA = kb.sb([128, 8], F32, "negA"); t_negA = T()
    kb.dma("sp", dtb[:], dt_bias.partition_broadcast(128), [], [t_dtb])
    kb.dma("sp", negA[:], a_log.partition_broadcast(128), [], [t_negA])
    kb.act(negA[:], negA[:], AF.Exp, [t_negA], [t_negA])
    kb.ts("dve", negA[:], negA[:], -1.0, ALU.mult, [t_negA], [t_negA])

    wring = kb.ring(2, [128, 8, 512], BF16, "wg")
    wsm = kb.sb([128, 8, 16], BF16, "wsm"); t_wsm = T()
    psA = kb.psring([0, 1, 2, 3, 4, 5], 512, "psA")
    psS = kb.psring([6, 7], 512, "psS")
    f32r = kb.ring(3, [128, 512], F32, "f32r")
    f32r2 = kb.ring(3, [128, 512], F32, "f32r2")
    bfr = kb.ring(4, [128, 512], BF16, "bfr")
    s8r = kb.ring(4, [128, 8], F32, "s8")
    rawr = kb.ring(3, [128, 515], F32, "raw")
    carry = [(kb.sb([128, 3], F32, "carry"), T()) for _ in range(4)]
    yr = kb.ring(5, [128, 512], F32, "y")
    sqr = kb.ring(3, [128, 512], BF16, "sqb")
    rnr = kb.ring(3, [128, 512], F32, "rn")
    bgr = kb.ring(3, [128, 16], F32, "bg")
    w_in_v = w_in.rearrange("(k p) c -> p k c", p=128)

    def col0(cg):
        if cg < 17:
            return cg * 512
        if cg == 17:
            return 8704
        return 8720 + (cg - 18) * 512

    def load_W(cg):
        c0 = col0(cg)
        if cg == 17:
            kb.dma("gq", wsm[:], w_in_v[:, :, c0:c0 + 16], [], [t_wsm])
            return wsm, t_wsm
        W, t_W = wring.next()
        kb.dma("gq", W[:, 0:4, :], w_in_v[:, 0:4, c0:c0 + 512], [], [t_W])
        kb.dma("gq", W[:, 4:8, :], w_in_v[:, 4:8, c0:c0 + 512], [], [t_W])
        return W, t_W

    def conv_unit(cg, s, cb, W, t_W):
        cbg = (cg - 9) * 4 + cb
        which = cbg // 8
        d = {}

        def sa():
            ps, t_ps = psA.next()
            for k in range(8):
                kb.mm(ps, W[:, k, cb * 128:(cb + 1) * 128], hT[:, k, s * 512:(s + 1) * 512],
                      [t_W] + t_hT[4 * s:4 * s + 4], [t_ps], start=(k == 0), stop=(k == 7))
            raw, t_raw = rawr.next()
            cy, t_cy = carry[cb]
            if s == 0:
                kb.memset("pool", raw[:, 0:3], 0.0, [t_raw])
            else:
                kb.cp("pool", raw[:, 0:3], cy[:], [t_cy], [t_raw])
            kb.cp("act", raw[:, 3:515], ps, [t_ps], [t_raw])
            kb.cp("pool", cy[:], raw[:, 512:515], [t_raw], [t_cy])
            d.update(raw=raw, t_raw=t_raw)

        def sb_():
            raw, t_raw = d["raw"], d["t_raw"]
            y, t_y = yr.next()
            kb.ts("dve", y[:], raw[:, 3:515], convw[:, cbg, 3:4], ALU.mult, [t_raw, t_convw], [t_y])
            for j in range(3):
                kb.stt("dve", y[:], raw[:, j:j + 512], convw[:, cbg, j:j + 1], y[:], ALU.mult, ALU.add,
                       [t_raw, t_convw, t_y], [t_y])
            d.update(y=y, t_y=t_y)

        def sc():
            y, t_y = d["y"], d["t_y"]
            if which == 2:
                ob, t_ob = bfr.next()
                kb.act(ob[:], y[:], AF.Silu, [t_y], [t_ob])
                kb.dma("sp", qkvb_s[cbg, :, s * 512:(s + 1) * 512], ob[:], [t_ob], [])
            else:
                kb.act(y[:], y[:], AF.Silu, [t_y], [t_y])
                sq, t_sq = sqr.next()
                kb.tt("pool", sq[:], y[:], y[:], ALU.mult, [t_y], [t_sq])
                d.update(sq=sq, t_sq=t_sq)

        def sd():
            if which == 2:
                return
            pss, t_pss = psS.next()
            kb.mm(pss, ones_bf[:], d["sq"][:], [t_ones, d["t_sq"]], [t_pss])
            rn, t_rn = rnr.next()
            if which == 0:
                kb.act(rn[:], pss, AF.Sqrt, [t_pss, t_eps128], [t_rn], bias=eps128[:, 0:1], scale=128.0)
            else:
                kb.act(rn[:], pss, AF.Sqrt, [t_pss, t_eps], [t_rn], bias=epsc[:, 0:1])
            d.update(rn=rn, t_rn=t_rn)

        def se():
            if which == 2:
                return
            rn, t_rn, y, t_y = d["rn"], d["t_rn"], d["y"], d["t_y"]
            kb.recip(rn[:], rn[:], [t_rn], [t_rn])
            ob, t_ob = bfr.next()
            kb.tt("pool", ob[:], y[:], rn[:], ALU.mult, [t_y, t_rn], [t_ob])
            kb.dma("sp", qkvb_s[cbg, :, s * 512:(s + 1) * 512], ob[:], [t_ob], [])

        return [sa, sb_, sc, sd, se]

    Wnext = load_W(0)
    for cg in range(22):
        c0 = col0(cg)
        W, t_W = Wnext
        if cg + 1 < 22:
            Wnext = load_W(cg + 1)
        if 9 <= cg <= 14:
            units = []
            for s in range(NSP):
                for cb in range(4):
                    units.append(conv_unit(cg, s, cb, W, t_W))
            pipeline(units)
        else:
            for t in range(NT):
                rows = slice(t * 128, (t + 1) * 128)
                ps, t_ps = psA.next()
                ncol = 16 if cg == 17 else 512
                pso = ps[:, 0:ncol]
                for k in range(8):
                    kb.mm(pso, hT[:, k, t * 128:(t + 1) * 128], W[:, k, :], [t_W, t_hT[t]], [t_ps],
                          start=(k == 0), stop=(k == 7))
                if cg < 6:
                    gain, t_gain = (qg, t_qg) if cg < 3 else (kg, t_kg)
                    sqf, t_sqf = f32r.next()
                    kb.act(sqf[:], ps, AF.Square, [t_ps], [t_sqf], scale=0.125)
                    s8, t_s8 = s8r.next()
                    kb.red("dve", s8[:], sqf[:].rearrange("p (h d) -> p h d", d=64), [t_sqf], [t_s8])
                    rms_rstd(s8[:], t_s8)
                    tmp, t_tmp = f32r2.next()
                    kb.tt("dve", tmp[:].rearrange("p (h d) -> p h d", d=64), ps.rearrange("p (h d) -> p h d", d=64),
                          s8[:, :].unsqueeze(2).to_broadcast([128, 8, 64]), ALU.mult, [t_ps, t_s8], [t_tmp])
                    ob, t_ob = bfr.next()
                    kb.tt("pool", ob[:].rearrange("p (h d) -> p h d", d=64), tmp[:].rearrange("p (h d) -> p h d", d=64),
                          gain[:, :].unsqueeze(1).to_broadcast([128, 8, 64]), ALU.mult, [t_tmp, t_gain], [t_ob])
                    kb.dma("sp", qkva_s[cg, rows, :], ob[:], [t_ob], [])
                elif cg < 9:
                    ob, t_ob = bfr.next()
                    kb.cp("act" if t % 2 else "dve", ob[:], ps, [t_ps], [t_ob])
                    kb.dma("sp", qkva_s[cg, rows, :], ob[:], [t_ob], [])
                elif cg in (15, 16):
                    ob, t_ob = bfr.next()
                    kb.act(ob[:], ps, AF.Silu, [t_ps], [t_ob])
                    kb.dma("sp", z_s[rows, (cg - 15) * 512:(cg - 14) * 512], ob[:], [t_ob], [])
                elif cg == 17:
                    bg, t_bg = bgr.next()
                    kb.act(bg[:, 0:8], ps[:, 0:8], AF.Sigmoid, [t_ps], [t_bg])
                    kb.tt("dve", bg[:, 8:16], ps[:, 8:16], dtb[:], ALU.add, [t_ps, t_dtb], [t_bg])
                    kb.act(bg[:, 8:16], bg[:, 8:16], AF.Exp, [t_bg], [t_bg])
                    kb.act(bg[:, 8:16], bg[:, 8:16], AF.Ln, [t_bg], [t_bg], bias=1.0)
                    kb.tt("dve", bg[:, 8:16], bg[:, 8:16], negA[:], ALU.mult, [t_bg, t_negA], [t_bg])
                    kb.dma("sp", bg_s[rows, :], bg[:], [t_bg], [])
                else:
                    ob, t_ob = bfr.next()
                    kb.act(ob[:], ps, AF.Sigmoid, [t_ps], [t_ob])
                    kb.dma("sp", gates_s[rows, (cg - 18) * 512:(cg - 17) * 512], ob[:], [t_ob], [])

    print("sbuf end A", kb.off)
    if stop == "A":
        return finish_early()
    P.barrier()
    kb.off = persist_off
    qr_ = kb.ring(3, [128, 512], BF16, "qb")
    kr_ = kb.ring(3, [128, 512], BF16, "kb")
    vst_r = kb.ring(3, [128, 512], BF16, "vst")
    vr_ = kb.ring(6, [128, 8, 65], BF16, "v1")
    for v1, t_v1 in vr_.slots:
        kb.memset("pool", v1[:, :, 64:65], 1.0, [t_v1])
    qTr = kb.ring(3, [128, 8, 128], BF16, "qT")
    for qz, t_qz in qTr.slots:
        kb.memset("pool", qz[:], 0.0, [t_qz])
    kTr = kb.ring(4, [128, 4, 128], BF16, "kT")
    er_ = kb.ring(4, [128, 4, 2, 128], BF16, "E")
    ur_ = kb.ring(3, [128, 8, 65], F32, "U")
    sc_slots = Ring([((kb.banks[0][:, :], kb.banks[1][:, :]), kb.pst(0, 1)), ((kb.banks[2][:, :], kb.banks[3][:, :]), kb.pst(2, 3))])
    pv_slots = Ring([(kb.banks[4][:, :], kb.pst(4)), (kb.banks[5][:, :], kb.pst(5))])
    psT = Ring([(kb.banks[6][:, :], kb.pst(6)), (kb.banks[7][:, :], kb.pst(7))])
    ATT = ((128, 1), (512, 4), (2048, 16))

    def attn_unit(g, dil, r_, nb, prev):
        qs = qkva_s[g].rearrange("(u r) c -> r u c", r=dil)
        ks = qkva_s[3 + g].rearrange("(u r) c -> r u c", r=dil)
        vs = qkva_s[6 + g].rearrange("(u r) c -> r u c", r=dil)
        us = ua_s[g].rearrange("(u r) c -> r u c", r=dil)
        ur = slice(nb * 128, (nb + 1) * 128)
        have_prev = nb > 0
        d = {}

        def s0():
            qb_, t_qb = qr_.next()
            kb_, t_kb = kr_.next()
            vst, t_vst = vst_r.next()
            kb.dma("sp", qb_[:], qs[r_, ur, :], [], [t_qb])
            kb.dma("sp", kb_[:], ks[r_, ur, :], [], [t_kb])
            kb.dma("sp", vst[:], vs[r_, ur, :], [], [t_vst])
            d.update(qb=qb_, t_qb=t_qb, kb=kb_, t_kb=t_kb, vst=vst, t_vst=t_vst)

        def s1():
            v1, t_v1 = vr_.next()
            kb.cp("dve", v1[:, :, 0:64], d["vst"][:].rearrange("p (h d) -> p h d", d=64), [d["t_vst"]], [t_v1])
            pt, t_pt = psT.next()
            ptv = bfv(pt).rearrange("p (a h c) -> p a h c", a=2, h=4)
            for hp in range(4):
                kb.tr(ptv[:, 0, hp, :], d["qb"][:, hp * 128:(hp + 1) * 128], ident_bf[:], [d["t_qb"], t_identbf], [t_pt])
                kb.tr(ptv[:, 1, hp, :], d["kb"][:, hp * 128:(hp + 1) * 128], ident_bf[:], [d["t_kb"], t_identbf], [t_pt])
            d.update(v1=v1, t_v1=t_v1, ptv=ptv, t_pt=t_pt)

        def s2():
            qT, t_qT = qTr.next()
            kT, t_kT = kTr.next()
            ptv, t_pt = d["ptv"], d["t_pt"]
            qTv = qT[:].rearrange("p (hp j) c -> p hp j c", j=2)
            kb.cp("dve", qTv[0:64, :, 0, :], ptv[0:64, 0], [t_pt], [t_qT])
            kb.cp("dve", qTv[64:128, :, 1, :], ptv[64:128, 0], [t_pt], [t_qT])
            kb.cp("act", kT[:], ptv[:, 1], [t_pt], [t_kT])
            d.update(qT=qT, t_qT=t_qT, kT=kT, t_kT=t_kT)

        def mk_half(half):
            hd = {}

            def h3():
                (b0, b1), t_sc = sc_slots.next()
                qT, t_qT, kT, t_kT = d["qT"], d["t_qT"], d["kT"], d["t_kT"]
                for hh in range(4):
                    h = half * 4 + hh
                    hp = h // 2
                    bank = b0 if hh < 2 else b1
                    base = (hh % 2) * 256
                    if have_prev:
                        kb.mm(bank[:, base:base + 128], prev["kT"][:, hp, :], qT[:, h, :],
                              [prev["t_kT"], t_qT], [t_sc])
                    kb.mm(bank[:, base + 128:base + 256], kT[:, hp, :], qT[:, h, :],
                          [t_kT, t_qT], [t_sc])
                hd.update(b0=b0, b1=b1, t_sc=t_sc)

            def h4():
                E, t_E = er_.next()
                t_sc = hd["t_sc"]
                for bi, bank in enumerate((hd["b0"], hd["b1"])):
                    ev = E[:, 2 * bi:2 * bi + 2, :, :]
                    bv = bank.rearrange("p (h a c) -> p h a c", h=2, a=2)
                    if have_prev:
                        kb.act(ev, bv, AF.Exp, [t_sc], [t_E])
                    else:
                        kb.act(ev[:, :, 1, :], bv[:, :, 1, :], AF.Exp, [t_sc], [t_E])
                hd.update(E=E, t_E=t_E)

            def h5():
                E, t_E = hd["E"], hd["t_E"]
                if have_prev:
                    kb.tt("pool", E[:, :, 0, :], E[:, :, 0, :], tril_bf[:, :].unsqueeze(1).to_broadcast([128, 4, 128]),
                          ALU.mult, [t_E, t_trilbf], [t_E])
                kb.tt("dve", E[:, :, 1, :], E[:, :, 1, :], triu_bf[:, :].unsqueeze(1).to_broadcast([128, 4, 128]),
                      ALU.mult, [t_E, t_triubf], [t_E])

            def h6():
                E, t_E = hd["E"], hd["t_E"]
                pv, t_pv = pv_slots.next()
                pvv = pv[:, 0:260].rearrange("p (h c) -> p h c", h=4)
                for hh in range(4):
                    h = half * 4 + hh
                    if have_prev:
                        kb.mm(pvv[:, hh, :], E[:, hh, 0, :], prev["v1"][:, h, :], [t_E, prev["t_v1"]], [t_pv], start=True, stop=False)
                    kb.mm(pvv[:, hh, :], E[:, hh, 1, :], d["v1"][:, h, :], [t_E, d["t_v1"]], [t_pv], start=not have_prev, stop=True)
                hd.update(pvv=pvv, t_pv=t_pv)

            def h7():
                if half == 0:
                    U_, t_U = ur_.next()
                    d.update(U=U_, t_U=t_U)
                U_, t_U = d["U"], d["t_U"]
                kb.cp("act" if half else "dve", U_[:, half * 4:half * 4 + 4, :], hd["pvv"], [hd["t_pv"]], [t_U])
                if half == 1:
                    kb.dma("sp", us[r_, ur, :], U_[:].rearrange("p h c -> p (h c)"), [t_U], [])

            return [h3, h4, h5, h6, h7]

        return d, [[s0, s1, s2] + mk_half(0), [None, None, None] + mk_half(1)]

    items = []
    for g, (win, dil) in enumerate(ATT):
        nblk = S // dil // 128
        for r_ in range(dil):
            prev = None
            for nb in range(nblk):
                prev, its = attn_unit(g, dil, r_, nb, prev)
                items.extend(its)
    pipeline(items)

    print("sbuf end B", kb.off)
    if stop == "B":
        return finish_early()
    P.barrier()
    kb.off = persist_off
    dng = kb.sb([128, 128], F32, "dng"); t_dng = T()
    kb.dma("sp", dng[:], dn_out_norm.partition_broadcast(128), [], [t_dng])
    S32 = [(kb.sb([128, 128], F32, "S32"), T()) for _ in range(8)]
    Sbf = [(kb.sb([128, 128], BF16, "Sbf"), T()) for _ in range(8)]
    for h in range(8):
        kb.memset("pool", S32[h][0][:], 0.0, [S32[h][1]])
        kb.memset("pool", Sbf[h][0][:], 0.0, [Sbf[h][1]])
    qkvr = kb.ring(2, [128, 24, 128], BF16, "qkvT")
    bgr2 = kb.ring(2, [128, 16], F32, "bgc")
    zr = kb.ring(2, [128, 1024], BF16, "z")
    gbc_r = kb.ring(2, [128, 8, 128], F32, "gbc")
    gcum_r = kb.ring(2, [128, 8], F32, "gcum")
    ngc_r = kb.ring(2, [128, 8], F32, "ngc")
    eg_r = kb.ring(2, [128, 8], F32, "eg")
    neg_r = kb.ring(2, [128, 8], F32, "neg")
    kts_r = kb.ring(2, [128, 8], F32, "kts")
    egl_r = kb.ring(2, [128, 8], F32, "egl")
    H = 8
    pre_r = kb.ring(4, [128, 128], F32, "pre")
    dtm_r = kb.ring(2 * H, [128, 128], F32, "dtm")
    dts_r = kb.ring(2 * H, [128, 128], F32, "dts")
    B_r = kb.ring(H + 2, [128, 128], F32, "B")
    BT_r = kb.ring(H + 2, [128, 128], F32, "BT")
    SA_r = kb.ring(2 * H + 2, [128, 128], F32, "SA")
    SB_r = kb.ring(2 * H + 2, [128, 128], F32, "SB")
    R_r = kb.ring(2 * H + 2, [128, 128], F32, "R")
    PT_r = kb.ring(2 * H, [128, 128], BF16, "PT")
    AIT_r = kb.ring(2 * H, [128, 128], BF16, "AIT")
    Ktl_r = kb.ring(2 * H, [128, 128], BF16, "Ktl")
    Vtm_r = kb.ring(2 * H, [128, 128], BF16, "Vtm")
    Y_r = kb.ring(4, [128, 128], BF16, "Y")
    vn_r = kb.ring(4, [128, 128], BF16, "vn")
    o1_r = kb.ring(4, [128, 128], F32, "o1")
    O_r = kb.ring(2, [128, 8, 128], F32, "O")
    osq_r = kb.ring(2, [128, 8, 128], F32, "osq")
    obf_r = kb.ring(2, [128, 1024], BF16, "obf")
    s8c_r = kb.ring(2, [128, 8], F32, "s8c")
    glast_r = kb.ring(2, [128, 8], F32, "glast")
    psN = kb.psring([0, 1, 2, 3], 128, "psN")
    psXO = kb.psring([4, 5], 128, "psXO")
    psG = kb.psring([6], 128, "psG")
    psX = Ring([(kb.banks[7][:, 0:128], kb.pst(7)), (kb.banks[7][:, 128:256], kb.pst(7))])
    psB = Ring([(kb.banks[7][:, 256 + 64 * i:256 + 64 * (i + 1)], kb.pst(7)) for i in range(4)])

    def chunk_gen(b):
        cols = slice(b * 128, (b + 1) * 128)
        qkvT, t_qkvT = qkvr.next()
        bgc, t_bgc = bgr2.next()
        zt, t_zt = zr.next()
        kb.dma("sp", qkvT[:], qkvb_s[:, :, cols].rearrange("c p t -> p c t"), [], [t_qkvT])
        kb.dma("sp", bgc[:], bg_s[cols, :], [], [t_bgc])
        kb.dma("sp", zt[:], z_s[cols, :], [], [t_zt])
        gcum, t_gcum = gcum_r.next()
        psg, t_psg = psG.next()
        kb.mm(psg[:, 0:8], triu[:], bgc[:, 8:16], [t_triu, t_bgc], [t_psg])
        kb.cp("dve", gcum[:], psg[:, 0:8], [t_psg], [t_gcum])
        gbc, t_gbc = gbc_r.next()
        kb.cp("pool", gbc[:], bgc[:, 8:16].unsqueeze(2).to_broadcast([128, 8, 128]), [t_bgc], [t_gbc])
        eg, t_eg = eg_r.next()
        neg, t_neg = neg_r.next()
        kb.act(eg[:], gcum[:], AF.Exp, [t_gcum], [t_eg])
        kb.ts("dve", neg[:], eg[:], -1.0, ALU.mult, [t_eg], [t_neg])
        kts, t_kts = kts_r.next()
        egl, t_egl = egl_r.next()
        O_, t_O = O_r.next()
        glast, t_glast = glast_r.next()
        st = []
        for h in range(H):
            psg, t_psg = psG.next()
            kb.mm(psg, gbc[:, h, :], triu[:], [t_gbc, t_triu], [t_psg])
            pre, t_pre = pre_r.next()
            kb.stt("dve", pre[:], psg, gcum[:, h:h + 1], mbm[:], ALU.subtract, ALU.add, [t_psg, t_gcum, t_mbm], [t_pre])
            dtm, t_dtm = dtm_r.next()
            kb.act(dtm[:], pre[:], AF.Exp, [t_pre], [t_dtm])
            dts, t_dts = dts_r.next()
            kb.tt("pool", dts[:], dtm[:], strict[:], ALU.mult, [t_dtm, t_strict], [t_dts])
            kb.cp("act", glast[:, h:h + 1], psg[:, 127:128], [t_psg], [t_glast])
            st.append(dict(dtm=dtm, t_dtm=t_dtm, dts=dts, t_dts=t_dts))
        kb.act(egl[:], glast[:], AF.Exp, [t_glast], [t_egl])
        kb.tt("dve", kts[:], glast[:], gcum[:], ALU.subtract, [t_glast, t_gcum], [t_kts])
        kb.act(kts[:], kts[:], AF.Exp, [t_kts], [t_kts])
        for h in range(H):
            d = st[h]
            dtm, t_dtm, dts, t_dts = d["dtm"], d["t_dtm"], d["dts"], d["t_dts"]
            KT = qkvT[:, 8 + h, :]
            QT = qkvT[:, h, :]
            VT = qkvT[:, 16 + h, :]
            pkk, t_pkk = psN.next()
            pkq, t_pkq = psN.next()
            kb.mm(pkk, KT, KT, [t_qkvT], [t_pkk])
            kb.mm(pkq, KT, QT, [t_qkvT], [t_pkq])
            B, t_B = B_r.next()
            kb.stt("dve", B[:], pkk, bgc[:, h:h + 1], dts[:], ALU.mult, ALU.mult, [t_pkk, t_bgc, t_dts], [t_B])
            AIT, t_AIT = AIT_r.next()
            kb.tt("dve", AIT[:], pkq, dtm[:], ALU.mult, [t_pkq, t_dtm], [t_AIT])
            pb, t_pb = psB.next()
            kb.tr(bfv(pb), KT, ident_bf[:], [t_qkvT, t_identbf], [t_pb])
            Ktl, t_Ktl = Ktl_r.next()
            kb.act(Ktl[:], bfv(pb), AF.Copy, [t_pb, t_kts], [t_Ktl], scale=kts[:, h:h + 1])
            pb2, t_pb2 = psB.next()
            kb.tr(bfv(pb2), VT, ident_bf[:], [t_qkvT, t_identbf], [t_pb2])
            Vtm, t_Vtm = Vtm_r.next()
            kb.cp("act", Vtm[:], bfv(pb2), [t_pb2], [t_Vtm])
            d.update(B=B, t_B=t_B, AIT=AIT, t_AIT=t_AIT, Ktl=Ktl, t_Ktl=t_Ktl, Vtm=Vtm, t_Vtm=t_Vtm, KT=KT, QT=QT)
        yield "pre"
        for h in range(H):
            d = st[h]
            pt_, t_pt_ = psN.next()
            kb.tr(pt_, d["B"][:], ident[:], [d["t_B"], t_ident], [t_pt_])
            BT, t_BT = BT_r.next()
            kb.cp("act", BT[:], pt_, [t_pt_], [t_BT])
            R, t_R = R_r.next()
            kb.tt("pool", R[:], ident[:], d["B"][:], ALU.subtract, [t_ident, d["t_B"]], [t_R])
            d.update(Sk=d["B"], t_Sk=d["t_B"], SkT=BT, t_SkT=t_BT, R=R, t_R=t_R)
        yield "lv"
        NL = 6
        for lvl in range(1, NL + 1):
            last = lvl == NL
            for h in range(H):
                d = st[h]
                pT, t_pT = psN.next()
                kb.mm(pT, d["Sk"][:], d["SkT"][:], [d["t_Sk"], d["t_SkT"]], [t_pT])
                if not last:
                    pS, t_pS = psN.next()
                    kb.mm(pS, d["SkT"][:], d["Sk"][:], [d["t_Sk"], d["t_SkT"]], [t_pS])
                nT, t_nT = SB_r.next()
                kb.cp("act", nT[:], pT, [t_pT], [t_nT])
                if not last:
                    nS, t_nS = SA_r.next()
                    kb.cp("dve", nS[:], pS, [t_pS], [t_nS])
                    d.update(Sk=nS, t_Sk=t_nS)
                d.update(SkT=nT, t_SkT=t_nT)
            for h in range(H):
                d = st[h]
                pR, t_pR = psN.next()
                kb.mm(pR, d["SkT"][:], d["R"][:], [d["t_SkT"], d["t_R"]], [t_pR])
                if last:
                    PT, t_PT = PT_r.next()
                    kb.tt("dve", PT[:], pR, d["R"][:], ALU.add, [t_pR, d["t_R"]], [t_PT])
                    d.update(PT=PT, t_PT=t_PT)
                else:
                    nR, t_nR = R_r.next()
                    kb.tt("dve", nR[:], pR, d["R"][:], ALU.add, [t_pR, d["t_R"]], [t_nR])
                    d.update(R=nR, t_R=t_nR)
            yield ("inv_done" if last else "lv")
        for hg in range(0, H, 4):
            for h in range(hg, hg + 4):
                d = st[h]
                sbf, t_sbf = Sbf[h]
                px, t_px = psXO.next()
                kb.mm(px, d["KT"], sbf[:], [t_qkvT, t_sbf], [t_px])
                po1, t_po1 = psXO.next()
                kb.mm(po1, d["QT"], sbf[:], [t_qkvT, t_sbf], [t_po1])
                d.update(px=px, t_px=t_px, po1=po1, t_po1=t_po1)
            yield "sc"
            for h in range(hg, hg + 4):
                d = st[h]
                Y, t_Y = Y_r.next()
                kb.stt("dve", Y[:], d["px"], neg[:, h:h + 1], d["Vtm"][:], ALU.mult, ALU.add,
                       [d["t_px"], t_neg, d["t_Vtm"]], [t_Y])
                o1, t_o1 = o1_r.next()
                kb.act(o1[:], d["po1"], AF.Copy, [d["t_po1"], t_eg], [t_o1], scale=eg[:, h:h + 1])
                ppy, t_ppy = psX.next()
                kb.mm(ppy, d["PT"][:], Y[:], [d["t_PT"], t_Y], [t_ppy])
                vn, t_vn = vn_r.next()
                kb.act(vn[:], ppy, AF.Copy, [t_ppy, t_bgc], [t_vn], scale=bgc[:, h:h + 1])
                d.update(vn=vn, t_vn=t_vn, o1=o1, t_o1=t_o1)
            yield "sc"
            for h in range(hg, hg + 4):
                d = st[h]
                vn, t_vn = d["vn"], d["t_vn"]
                s32, t_s32 = S32[h]
                sbf, t_sbf = Sbf[h]
                pst, t_pst = psN.next()
                kb.mm(pst, d["Ktl"][:], vn[:], [d["t_Ktl"], t_vn], [t_pst])
                po2, t_po2 = psN.next()
                kb.mm(po2, d["AIT"][:], vn[:], [d["t_AIT"], t_vn], [t_po2])
                kb.stt("dve", s32[:], s32[:], egl[:, h:h + 1], pst, ALU.mult, ALU.add, [t_s32, t_egl, t_pst], [t_s32])
                kb.cp("act", sbf[:], s32[:], [t_s32], [t_sbf])
                kb.tt("dve", O_[:, h, :], po2, d["o1"][:], ALU.add, [t_po2, d["t_o1"]], [t_O])
            if hg == 0:
                yield "sc"
        osq, t_osq = osq_r.next()
        kb.act(osq[:], O_[:], AF.Square, [t_O], [t_osq], scale=float(128.0 ** -0.5))
        s8, t_s8 = s8c_r.next()
        kb.red("dve", s8[:], osq[:], [t_osq], [t_s8])
        kb.act(s8[:], s8[:], AF.Sqrt, [t_s8, t_eps], [t_s8], bias=epsc[:, 0:1])
        kb.recip(s8[:], s8[:], [t_s8], [t_s8])
        kb.tt("pool", osq[:], O_[:], s8[:, :].unsqueeze(2).to_broadcast([128, 8, 128]), ALU.mult, [t_O, t_s8], [t_osq])
        kb.tt("pool", osq[:], osq[:], dng[:, :].unsqueeze(1).to_broadcast([128, 8, 128]), ALU.mult, [t_osq, t_dng], [t_osq])
        obf, t_obf = obf_r.next()
        kb.tt("dve", obf[:].rearrange("p (h d) -> p h d", d=128), osq[:], zt[:].rearrange("p (h d) -> p h d", d=128),
              ALU.mult, [t_osq, t_zt], [t_obf])
        kb.dma("sp", ob_s[cols, :], obf[:], [t_obf], [])
        yield "done"


    gens = [chunk_gen(b) for b in range(NT)]
    for i in range(NT + 1):
        cur = gens[i] if i < NT else None
        prv = gens[i - 1] if i >= 1 else None
        cur_live, prv_live = cur is not None, prv is not None
        while cur_live or prv_live:
            if prv_live and next(prv) == "done":
                prv_live = False
            if cur_live and next(cur) == "inv_done":
                cur_live = False

    print("sbuf end C", kb.off)
    if stop == "C":
        return finish_early()
    P.barrier()
    kb.off = persist_off
    TC = 8 if NT >= 8 else NT
    wa = kb.sb([128, 4, 1024], BF16, "wa"); t_wa = T()
    wb = kb.sb([128, 8, 1024], BF16, "wb"); t_wb = T()
    wo = kb.sb([128, 8, 1024], BF16, "wo"); t_wo = T()
    wpg = kb.sb([128, 8, 1024], BF16, "wpg"); t_wpg = T()
    wpl = kb.sb([128, 2, 1024], BF16, "wpl"); t_wpl = T()
    kb.dma("gq", wa[:], w_branch_a.rearrange("(k p) c -> p k c", p=128), [], [t_wa])
    kb.dma("gq", wb[:], w_branch_b.rearrange("(k p) c -> p k c", p=128), [], [t_wb])
    kb.dma("gq", wo[:], w_out.rearrange("(k p) c -> p k c", p=128), [], [t_wo])
    kb.dma("gq", wpg[:], w_pg.rearrange("(k p) c -> p k c", p=128), [], [t_wpg])
    kb.dma("gq", wpl[:], w_ple.rearrange("(k p) c -> p k c", p=128), [], [t_wpl])
    wr = kb.sb([128, 8, 20], F32, "wr"); t_wr = T()
    kb.dma("sp", wr[:, :, 0:4], w_rg.rearrange("(k p) c -> p k c", p=128), [], [t_wr])
    kb.dma("sp", wr[:, :, 4:20], w_re.rearrange("(k p) c -> p k c", p=128), [], [t_wr])
    br = kb.sb([128, 20], F32, "br"); t_br = T()
    kb.dma("sp", br[:, 0:4], b_rg.partition_broadcast(128), [], [t_br])
    kb.dma("sp", br[:, 4:20], b_re.partition_broadcast(128), [], [t_br])
    gffn = kb.sb([128, 1024], F32, "gffn"); t_gffn = T()
    gple = kb.sb([128, 1024], F32, "gple"); t_gple = T()
    kb.dma("sp", gffn[:], norm_ffn.partition_broadcast(128), [], [t_gffn])
    kb.dma("sp", gple[:], norm_ple.partition_broadcast(128), [], [t_gple])
    yacc = [(kb.sb([128, 1024], F32, "yacc"), T()) for _ in range(TC)]
    h2T = [(kb.sb([128, 8, 128], BF16, "h2T"), T()) for _ in range(TC)]
    comb = [(kb.sb([128, 16], F32, "comb"), T()) for _ in range(TC)]
    wgu_r = kb.ring(2, [128, 8, 512], BF16, "wgu")
    wd_r = kb.ring(2, [128, 2, 1024], BF16, "wdn")
    u_r = kb.ring(3, [128, 520], F32, "ua")
    un_r = kb.ring(2, [128, 8, 65], F32, "un")
    ga_r = kb.ring(1, [128, 2048], BF16, "gates")
    obr = kb.ring(2, [128, 1024], BF16, "ob")
    oab_r = kb.ring(2, [128, 512], BF16, "oab")
    rden_r = kb.ring(2, [128, 8], F32, "rden")
    tT_r = kb.ring(2, [128, 8, 128], BF16, "tT")
    mg_r = kb.ring(1, [128, 1024], F32, "mg")
    mgb_r = kb.ring(2, [128, 1024], BF16, "mgb")
    hf_r = kb.ring(1, [128, 1024], F32, "hf")
    hfT_r = kb.ring(1, [128, 8, 128], F32, "hfT")
    hbf_r = kb.ring(2, [128, 1024], BF16, "hbf")
    jk_r = kb.ring(1, [128, 1024], BF16, "jk2")
    ss_r = kb.ring(4, [128, 1], F32, "ss2")
    lg_r = kb.ring(2, [128, 20], F32, "lg")
    sm_r = kb.ring(24, [128, 16], F32, "sm")
    sil_r = kb.ring(3, [128, 256], F32, "sil")
    act_r = kb.ring(4, [128, 256], BF16, "actb")
    actT_r = kb.ring(4, [128, 2, 128], BF16, "actT")
    pin_r = kb.ring(2, [128, 256], F32, "pin")
    pbf_r = kb.ring(2, [128, 256], BF16, "pbf")
    sg_r = kb.ring(1, [128, 1024], F32, "sg")
    psM = kb.psring([0, 1, 2, 3], 512, "psM")
    psTr = kb.psring([4, 5], 512, "psTr")
    psD = kb.psring([6, 7], 512, "psD")
    final_ops = []

    def transpose_bf(src, t_src, nk):
        ps, t_ps = psTr.next()
        psv = bfv(ps).rearrange("p (k c) -> p k c", k=8)
        for k in range(nk):
            kb.tr(psv[:, k, :], src[:, k * 128:(k + 1) * 128], ident_bf[:], [t_src, t_identbf], [t_ps])
        tT, t_tT = tT_r.next()
        kb.cp("act", tT[:, 0:nk, :], psv[:, 0:nk, :], [t_ps], [t_tT])
        return tT, t_tT

    def proj(tT, t_tT, W, t_W, nk):
        res = []
        for c in range(2):
            ps, t_ps = psM.next()
            for k in range(nk):
                kb.mm(ps, tT[:, k, :], W[:, k, c * 512:(c + 1) * 512], [t_tT, t_W], [t_ps], start=(k == 0), stop=(k == nk - 1))
            res.append((ps, t_ps))
        return res

    def rmsnorm_to(xsrc, t_x, gain, t_gain, out_ap, t_out):
        jk, t_jk = jk_r.next()
        ss, t_ss = ss_r.next()
        kb.memset("pool", ss[:], 0.0, [t_ss])
        kb.act(jk[:], xsrc, AF.Square, [t_x, t_ss], [t_jk, t_ss], scale=1.0 / 32.0, accum_out=ss[:])
        rms_rstd(ss[:], t_ss)
        kb.stt("dve", out_ap, xsrc, ss[:, 0:1], gain[:], ALU.mult, ALU.mult, [t_x, t_ss, t_gain], [t_out])

    def d_tile(c0, ti):
        t = c0 + ti
        rows = slice(t * 128, (t + 1) * 128)
        ya, t_ya = yacc[ti]
        kb.dma("sp", ya[:], x[rows, :], [], [t_ya])
        us_ = []
        for g in range(3):
            u, t_u = u_r.next()
            kb.dma("sp", u[:], ua_s[g, rows, :], [], [t_u])
            us_.append((u, t_u))
        ga, t_ga = ga_r.next()
        kb.dma("sp", ga[:], gates_s[rows, :], [], [t_ga])
        ob, t_ob = obr.next()
        kb.dma("sp", ob[:], ob_s[rows, :], [], [t_ob])
        un, t_un = un_r.next()
        unf = un[:].rearrange("p h c -> p (h c)")
        kb.tt("pool", unf, us_[0][0][:], us_[1][0][:], ALU.add, [us_[0][1], us_[1][1]], [t_un])
        kb.tt("pool", unf, unf, us_[2][0][:], ALU.add, [t_un, us_[2][1]], [t_un])
        rden, t_rden = rden_r.next()
        kb.recip(rden[:], un[:, :, 64], [t_un], [t_rden])
        oab, t_oab = oab_r.next()
        kb.tt("dve", oab[:].rearrange("p (h d) -> p h d", d=64), un[:, :, 0:64],
              rden[:, :].unsqueeze(2).to_broadcast([128, 8, 64]), ALU.mult, [t_un, t_rden], [t_oab])
        aT, t_aT = transpose_bf(oab, t_oab, 4)
        pa = proj(aT, t_aT, wa, t_wa, 4)
        mg, t_mg = mg_r.next()
        for c in range(2):
            cs = slice(c * 512, (c + 1) * 512)
            kb.tt("dve", mg[:, cs], pa[c][0], ga[:, c * 512:(c + 1) * 512], ALU.mult, [pa[c][1], t_ga], [t_mg])
        yield
        bT, t_bT = transpose_bf(ob, t_ob, 8)
        pb_ = proj(bT, t_bT, wb, t_wb, 8)
        mgb, t_mgb = mgb_r.next()
        for c in range(2):
            cs = slice(c * 512, (c + 1) * 512)
            kb.tt("dve", mgb[:, cs], pb_[c][0], ga[:, 1024 + c * 512:1024 + (c + 1) * 512], ALU.mult, [pb_[c][1], t_ga], [t_mgb])
        kb.tt("pool", mgb[:], mg[:], mgb[:], ALU.add, [t_mg, t_mgb], [t_mgb])
        yield
        mT, t_mT = transpose_bf(mgb, t_mgb, 8)
        po = proj(mT, t_mT, wo, t_wo, 8)
        for c in range(2):
            cs = slice(c * 512, (c + 1) * 512)
            kb.tt("dve", ya[:, cs], po[c][0], ya[:, cs], ALU.add, [po[c][1], t_ya], [t_ya])
        yield
        hf, t_hf = hf_r.next()
        rmsnorm_to(ya[:], t_ya, gffn, t_gffn, hf[:], t_hf)
        hbf, t_hbf = hbf_r.next()
        kb.cp("pool", hbf[:], hf[:], [t_hf], [t_hbf])
        hT2, t_hT2 = h2T[ti]
        ps, t_ps = psTr.next()
        psv = bfv(ps).rearrange("p (k c) -> p k c", k=8)
        for k in range(8):
            kb.tr(psv[:, k, :], hbf[:, k * 128:(k + 1) * 128], ident_bf[:], [t_hbf, t_identbf], [t_ps])
        kb.cp("act", hT2[:], psv, [t_ps], [t_hT2])
        yield
        hfT, t_hfT = hfT_r.next()
        for hv in range(2):
            ps, t_ps = psTr.next()
            psv4 = ps.rearrange("p (k c) -> p k c", k=4)
            for k in range(4):
                kk = hv * 4 + k
                kb.tr(psv4[:, k, :], hf[:, kk * 128:(kk + 1) * 128], ident[:], [t_hf, t_ident], [t_ps])
            kb.cp("act" if hv else "dve", hfT[:, hv * 4:hv * 4 + 4, :], psv4, [t_ps], [t_hfT])
        ps, t_ps = psD.next()
        for k in range(8):
            kb.mm(ps[:, 0:20], hfT[:, k, :], wr[:, k, :], [t_hfT, t_wr], [t_ps], start=(k == 0), stop=(k == 7))
        lg, t_lg = lg_r.next()
        kb.tt("dve", lg[:], ps[:, 0:20], br[:], ALU.add, [t_ps, t_br], [t_lg])
        cm, t_cm = comb[ti]

        def sm(n=16):
            a, t_a = sm_r.next()
            return a[:, 0:n], t_a
        gmax, t_gmax = sm(1)
        kb.red("dve", gmax, lg[:, 0:4], [t_lg], [t_gmax], op=ALU.max)
        ngmax, t_ngmax = sm(1)
        kb.ts("dve", ngmax, gmax, -1.0, ALU.mult, [t_gmax], [t_ngmax])
        gex, t_gex = sm(4)
        gsum, t_gsum = sm(1)
        kb.memset("pool", gsum, 0.0, [t_gsum])
        kb.act(gex, lg[:, 0:4], AF.Exp, [t_lg, t_ngmax, t_gsum], [t_gex, t_gsum], bias=ngmax, accum_out=gsum)
        pg, t_pg = sm(1)
        kb.recip(pg, gsum, [t_gsum], [t_pg])
        goh, t_goh = sm(4)
        kb.ts("dve", goh, lg[:, 0:4], gmax, ALU.is_ge, [t_lg, t_gmax], [t_goh])
        el, t_el = sm(16)
        kb.tt("dve", el.rearrange("p (g e) -> p g e", e=4), lg[:, 4:20].rearrange("p (g e) -> p g e", e=4),
              goh.unsqueeze(2).to_broadcast([128, 4, 4]), ALU.mult, [t_lg, t_goh], [t_el])
        sel, t_sel = sm(4)
        kb.red("dve", sel, el.rearrange("p (g e) -> p e g", e=4), [t_el], [t_sel])
        m1, t_m1 = sm(1)
        kb.red("dve", m1, sel, [t_sel], [t_m1], op=ALU.max)
        oh1, t_oh1 = sm(4)
        kb.ts("dve", oh1, sel, m1, ALU.is_ge, [t_sel, t_m1], [t_oh1])
        sel2, t_sel2 = sm(4)
        kb.stt("dve", sel2, oh1, -1e30, sel, ALU.mult, ALU.add, [t_oh1, t_sel], [t_sel2])
        m2, t_m2 = sm(1)
        kb.red("dve", m2, sel2, [t_sel2], [t_m2], op=ALU.max)
        oh2, t_oh2 = sm(4)
        kb.ts("dve", oh2, sel2, m2, ALU.is_ge, [t_sel2, t_m2], [t_oh2])
        dd, t_dd = sm(1)
        kb.tt("dve", dd, m2, m1, ALU.subtract, [t_m2, t_m1], [t_dd])
        kb.act(dd, dd, AF.Exp, [t_dd], [t_dd])
        kb.ts("dve", dd, dd, 1.0, ALU.add, [t_dd], [t_dd])
        w1, t_w1 = sm(1)
        kb.recip(w1, dd, [t_dd], [t_w1])
        w2, t_w2 = sm(1)
        kb.ts("dve", w2, w1, -1.0, ALU.mult, [t_w1], [t_w2], s2=1.0, op1=ALU.add)
        kb.tt("dve", w1, w1, pg, ALU.mult, [t_w1, t_pg], [t_w1])
        kb.tt("dve", w2, w2, pg, ALU.mult, [t_w2, t_pg], [t_w2])
        wig, t_wig = sm(4)
        kb.ts("dve", wig, oh1, w1, ALU.mult, [t_oh1, t_w1], [t_wig])
        kb.stt("dve", wig, oh2, w2, wig, ALU.mult, ALU.add, [t_oh2, t_w2, t_wig], [t_wig])
        for g in range(4):
            kb.ts("dve", cm[:, g * 4:(g + 1) * 4], wig, goh[:, g:g + 1], ALU.mult, [t_wig, t_goh], [t_cm])
        yield

    def f_tile(c0, ti):
        t = c0 + ti
        rows = slice(t * 128, (t + 1) * 128)
        ya, t_ya = yacc[ti]
        hbf, t_hbf = hbf_r.next()
        rmsnorm_to(ya[:], t_ya, gple, t_gple, hbf[:], t_hbf)
        h3T, t_h3T = transpose_bf(hbf, t_hbf, 8)
        pgate = proj(h3T, t_h3T, wpg, t_wpg, 8)
        sg, t_sg = sg_r.next()
        for c in range(2):
            cs = slice(c * 512, (c + 1) * 512)
            kb.act(sg[:, cs], pgate[c][0], AF.Sigmoid, [pgate[c][1]], [t_sg])
        pin, t_pin = pin_r.next()
        kb.dma("sp", pin[:], p_in[rows, :], [], [t_pin])
        pbf, t_pbf = pbf_r.next()
        kb.cp("pool", pbf[:], pin[:], [t_pin], [t_pbf])
        yield
        pT, t_pT = transpose_bf(pbf, t_pbf, 2)
        pple = proj(pT, t_pT, wpl, t_wpl, 2)
        for c in range(2):
            cs = slice(c * 512, (c + 1) * 512)
            kb.tt("dve", sg[:, cs], sg[:, cs], pple[c][0], ALU.mult, [t_sg, pple[c][1]], [t_sg])
        kb.tt("pool", ya[:], sg[:], ya[:], ALU.add, [t_sg, t_ya], [t_ya])
        final_ops.append(kb.dma("sp", out[rows, :], ya[:], [t_ya], []))
        yield

    def run_gens(gs):
        gs = list(gs)
        while gs:
            for g_ in list(gs):
                try:
                    next(g_)
                except StopIteration:
                    gs.remove(g_)


    for ti in range(TC):
        run_gens([d_tile(0, ti)])
    for c0 in range(0, NT, TC):
        def load_gu(e_):
            wgu, t_wgu_sb = wgu_r.next()
            kb.dma("sp", wgu[:, 0:4, :], wgu_s[e_].rearrange("(k p) c -> p k c", p=128)[:, 0:4, :], [t_wgu[e_]], [t_wgu_sb])
            kb.dma("gq", wgu[:, 4:8, :], wgu_s[e_].rearrange("(k p) c -> p k c", p=128)[:, 4:8, :], [t_wgu[e_]], [t_wgu_sb])
            return wgu, t_wgu_sb

        def load_dn(e_):
            wdn, t_wdn = wd_r.next()
            kb.dma("sp", wdn[:], wd_s[e_].rearrange("(k p) c -> p k c", p=128), [t_wd[e_]], [t_wdn])
            return wdn, t_wdn

        Wgu = {0: load_gu(0)}
        Wdn = {0: load_dn(0)}

        def exp_item(e_, ti):
            ya, t_ya = yacc[ti]
            hT2, t_hT2 = h2T[ti]
            cm, t_cm = comb[ti]
            d = {}

            def e0():
                if ti == 0 and e_ + 1 < 16:
                    Wgu[e_ + 1] = load_gu(e_ + 1)
                wgu, t_wgu_sb = Wgu[e_]
                ps, t_ps = psM.next()
                for k in range(8):
                    kb.mm(ps, hT2[:, k, :], wgu[:, k, :], [t_hT2, t_wgu_sb], [t_ps], start=(k == 0), stop=(k == 7))
                d.update(ps=ps, t_ps=t_ps)

            def e1():
                ps, t_ps = d["ps"], d["t_ps"]
                sil, t_sil = sil_r.next()
                kb.act(sil[:], ps[:, 0:256], AF.Silu, [t_ps], [t_sil])
                ab, t_ab = act_r.next()
                kb.stt("dve", ab[:], ps[:, 256:512], cm[:, e_:e_ + 1], sil[:], ALU.mult, ALU.mult, [t_ps, t_cm, t_sil], [t_ab])
                d.update(ab=ab, t_ab=t_ab)

            def e2():
                pst_, t_pst_ = psTr.next()
                pstv = bfv(pst_).rearrange("p (k c) -> p k c", k=8)
                for k in range(2):
                    kb.tr(pstv[:, k, :], d["ab"][:, k * 128:(k + 1) * 128], ident_bf[:], [d["t_ab"], t_identbf], [t_pst_])
                d.update(pstv=pstv, t_pst=t_pst_)

            def e3():
                aT, t_aT = actT_r.next()
                kb.cp("act", aT[:], d["pstv"][:, 0:2, :], [d["t_pst"]], [t_aT])
                d.update(aT=aT, t_aT=t_aT)

            def e4():
                if ti == 0 and e_ + 1 < 16:
                    Wdn[e_ + 1] = load_dn(e_ + 1)
                wdn, t_wdn = Wdn[e_]
                pds = []
                for c in range(2):
                    pd, t_pd = psD.next()
                    for k in range(2):
                        kb.mm(pd, d["aT"][:, k, :], wdn[:, k, c * 512:(c + 1) * 512], [d["t_aT"], t_wdn], [t_pd], start=(k == 0), stop=(k == 1))
                    pds.append((pd, t_pd))
                d.update(pds=pds)

            def e5():
                for c in range(2):
                    pd, t_pd = d["pds"][c]
                    cs = slice(c * 512, (c + 1) * 512)
                    kb.tt("dve", ya[:, cs], ya[:, cs], pd, ALU.add, [t_ya, t_pd], [t_ya])

            return [e0, e1, e2, e3, e4, e5]

        pipeline([exp_item(e_, ti) for e_ in range(16) for ti in range(TC)])
        for ti in range(TC + 1):
            gs = []
            if ti < TC:
                gs.append(f_tile(c0, ti))
            if ti >= 1 and c0 + TC < NT:
                gs.append(d_tile(c0 + TC, ti - 1))
            run_gens(gs)

    print("sbuf end D", kb.off, "ops", {k: len(v) for k, v in P.ops.items()})
    P.emit(nc, final_waits=final_ops)
    return nc


_CONSTS = None


def _consts():
    global _CONSTS
    if _CONSTS is None:
        _CONSTS = {
            "c_ident": np.eye(128, dtype=np.float32),
            "c_triu": np.triu(np.ones((128, 128), dtype=np.float32)),
            "c_tril": np.tril(np.ones((128, 128), dtype=np.float32)),
        }
    return _CONSTS


def make_in_map(inputs, b, S):
    f = lambda a: np.ascontiguousarray(np.asarray(a, dtype=np.float32))
    m = {
        "x": f(inputs["x"][b, :S]),
        "p": f(inputs["p"][0, b, :S]),
        "norm_mix": f(inputs["norm_mix"][0]),
        "w_in": f(inputs["w_in"][0]),
        "q_norm": f(inputs["q_norm"][0]),
        "k_norm": f(inputs["k_norm"][0]),
        "conv_w": f(np.asarray(inputs["conv_w"][0]).reshape(4, 24, 128).transpose(2, 1, 0)),
        "a_log": f(inputs["a_log"][0]),
        "dt_bias": f(inputs["dt_bias"][0]),
        "dn_out_norm": f(inputs["dn_out_norm"][0]),
        "w_branch_a": f(inputs["w_branch_a"][0]),
        "w_branch_b": f(inputs["w_branch_b"][0]),
        "w_out": f(inputs["w_out"][0]),
        "norm_ffn": f(inputs["norm_ffn"][0]),
        "w_router_group": f(inputs["w_router_group"][0]),
        "b_router_group": f(inputs["b_router_group"][0]),
        "w_router_expert": f(inputs["w_router_expert"][0]),
        "b_router_expert": f(inputs["b_router_expert"][0]),
        "w_expert_gate": f(np.asarray(inputs["w_expert_gate"][0]).reshape(16, 1024, 256)),
        "w_expert_up": f(np.asarray(inputs["w_expert_up"][0]).reshape(16, 1024, 256)),
        "w_expert_down": f(np.asarray(inputs["w_expert_down"][0]).reshape(16, 256, 1024)),
        "norm_ple": f(inputs["norm_ple"][0]),
        "w_ple": f(inputs["w_ple"][0]),
        "w_ple_gate": f(inputs["w_ple_gate"][0]),
    }
    m.update(_consts())
    return m


def kernel(**inputs):
    S = 8192
    nc = build(S)
    in_maps = [make_in_map(inputs, b, S) for b in range(8)]
    res = run_bass_kernel_spmd(nc, in_maps, core_ids=list(range(8)))
    return np.stack([np.asarray(r["out"], dtype=np.float32) for r in res.results], axis=0)
```

```python
import numpy as np
import concourse.bass as bass
import concourse.mybir as mybir
from concourse.bass_utils import run_bass_kernel_spmd

F32 = mybir.dt.float32
BF16 = mybir.dt.bfloat16
F32R = mybir.dt.float32r
ALU = mybir.AluOpType
AF = mybir.ActivationFunctionType
AX = mybir.AxisListType

ENGS = ("pe", "act", "dve", "pool")
DMAQ = ("sp", "gq")
NDSEM = 24
EPS = 1e-6
SB_BASE = 16512
SB_TOP = 229344


class T:
    __slots__ = ("name", "w", "rs", "banks")

    def __init__(self, name="", banks=()):
        self.name = name
        self.w = None
        self.rs = []
        self.banks = banks


class Bank:
    __slots__ = ("last",)

    def __init__(self):
        self.last = None


class Op:
    __slots__ = ("eng", "fn", "idx", "deps", "sig", "signo", "dslot", "dval", "clock", "gorder")


class Prog:
    def __init__(self):
        self.ops = {e: [] for e in ENGS + DMAQ}
        self.clock = {e: {} for e in ENGS + DMAQ}
        self.ndma = {q: 0 for q in DMAQ}
        self.g = 0
        self.pending = {e: [] for e in ENGS + DMAQ}

    def barrier(self):
        lasts = []
        for e in ENGS:
            if self.ops[e]:
                lasts.append(self.ops[e][-1])
        for q in DMAQ:
            lasts.extend(self.ops[q][-NDSEM:])
        self.pending["sp"] = list(lasts)
        op = self.add("sp", self.bar_fn)
        for e in ENGS + ("gq",):
            self.pending[e] = [op]

    def add(self, eng, fn, reads=(), writes=()):
        op = Op()
        op.eng = eng
        op.fn = fn
        op.idx = len(self.ops[eng])
        op.sig = False
        op.signo = None
        op.gorder = self.g
        self.g += 1
        deps = []
        for t in reads:
            if t.w is not None:
                deps.append((t.w, True))
        for t in writes:
            if t.w is not None:
                deps.append((t.w, False))
            for r in t.rs:
                deps.append((r, False))
        bks = []
        for t in tuple(reads) + tuple(writes):
            for b in t.banks:
                if b not in bks:
                    bks.append(b)
        for b in bks:
            if b.last is not None and b.last.eng != eng:
                deps.append((b.last, True))
        if self.pending[eng]:
            for d in self.pending[eng]:
                if d.eng != eng or eng in DMAQ:
                    deps.append((d, True))
            self.pending[eng] = []
        clk = self.clock[eng]
        need = {}
        for d, raw in deps:
            if d.eng == eng and eng not in DMAQ:
                if eng == "pe":
                    continue
            if d.eng in DMAQ:
                key = (d.eng, d.idx)
                if clk.get(key, False):
                    continue
                need[key] = d
            else:
                if clk.get(d.eng, -1) >= d.idx:
                    continue
                cur = need.get(d.eng)
                if cur is None or cur.idx < d.idx:
                    need[d.eng] = d
        op.deps = list(need.values())
        for d in op.deps:
            d.sig = True
            for k, v in d.clock.items():
                if isinstance(k, tuple):
                    clk[k] = True
                elif clk.get(k, -1) < v:
                    clk[k] = v
            if d.eng in DMAQ:
                clk[(d.eng, d.idx)] = True
            elif clk.get(d.eng, -1) < d.idx:
                clk[d.eng] = d.idx
        if eng in DMAQ:
            op.dslot = self.ndma[eng] % NDSEM
            op.dval = 16 * (self.ndma[eng] // NDSEM + 1)
            self.ndma[eng] += 1
            prev_i = op.idx - NDSEM
            if prev_i >= 0:
                pd = self.ops[eng][prev_i]
                if not clk.get((eng, prev_i), False):
                    op.deps.append(pd)
                    clk[(eng, prev_i)] = True
            op.sig = True
        if len(clk) > 400:
            for k in [k for k in clk if isinstance(k, tuple) and k[1] < self.ndma[k[0]] - 4 * NDSEM]:
                del clk[k]
        op.clock = dict(clk)
        if eng not in DMAQ:
            op.clock[eng] = op.idx
        self.ops[eng].append(op)
        for b in bks:
            b.last = op
        for t in reads:
            t.rs.append(op)
        for t in writes:
            t.w = op
            t.rs = []
        return op

    def emit(self, nc, final_waits=()):
        from contextlib import ExitStack
        with ExitStack() as es:
            sems = {e: es.enter_context(nc.semaphore("s_" + e)) for e in ENGS}
            dsems = {q: [es.enter_context(nc.semaphore(f"d_{q}{i}")) for i in range(NDSEM)] for q in DMAQ}
            for d in final_waits:
                d.sig = True
            for e in ENGS:
                n = 0
                for op in self.ops[e]:
                    if op.sig:
                        n += 1
                        op.signo = n
            block = es.enter_context(nc.Block())

            def waits(engobj, op):
                for d in op.deps:
                    if d.eng in DMAQ:
                        engobj.wait_ge(dsems[d.eng][d.dslot], d.dval)
                    else:
                        engobj.wait_ge(sems[d.eng], d.signo)

            def run(oplist, engobj, extra=None):
                for op in oplist:
                    waits(engobj, op)
                    ins = op.fn(engobj)
                    if op.sig:
                        if op.eng in DMAQ:
                            ins.then_inc(dsems[op.eng][op.dslot], 16)
                        else:
                            ins.then_inc(sems[op.eng], 1)
                if extra:
                    extra(engobj)

            def fin(engobj):
                for d in final_waits:
                    if d.eng in DMAQ:
                        engobj.wait_ge(dsems[d.eng][d.dslot], d.dval)
                    else:
                        engobj.wait_ge(sems[d.eng], d.signo)

            @block.tensor
            def _(e):
                run(self.ops["pe"], e)

            @block.scalar
            def _(e):
                run(self.ops["act"], e)

            @block.vector
            def _(e):
                run(self.ops["dve"], e)

            @block.gpsimd
            def _(e):
                merged = sorted(self.ops["pool"] + self.ops["gq"], key=lambda o: o.gorder)
                run(merged, e)

            @block.sync
            def _(e):
                run(self.ops["sp"], e, fin)


class KB:
    def __init__(self, nc):
        self.nc = nc
        self.P = Prog()
        self.off = SB_BASE
        self.n = 0
        self.banks = [nc.alloc_psum_tensor(f"bank{i}", [128, 512], F32) for i in range(8)]
        self.bk = [Bank() for _ in range(8)]

    def pst(self, *bank_ids):
        return T("ps", banks=tuple(self.bk[b] for b in bank_ids))

    def sb(self, shape, dt, name="t"):
        sz = int(np.prod(shape[1:])) * (2 if dt == BF16 else 4)
        sz = (sz + 31) // 32 * 32
        assert self.off + sz <= SB_TOP, f"sbuf overflow {name} {self.off} {sz}"
        self.n += 1
        t = self.nc.alloc_sbuf_tensor_at(f"{name}_{self.n}", list(shape), dt, offset=self.off)
        self.off += sz
        return t

    def ring(self, n, shape, dt, name="r"):
        return Ring([(self.sb(shape, dt, name), T(name)) for _ in range(n)])

    def psring(self, banks, nf32, name="ps"):
        per = 512 // nf32
        slots = []
        for i in range(per):
            for b in banks:
                slots.append((self.banks[b][:, i * nf32:(i + 1) * nf32], self.pst(b)))
        return Ring(slots)

    def mm(self, out, lhsT, rhs, r, w, start=True, stop=True):
        return self.P.add("pe", lambda e: e.matmul(out, lhsT=lhsT, rhs=rhs, start=start, stop=stop), reads=r, writes=w)

    def tr(self, out, in_, ident, r, w):
        return self.P.add("pe", lambda e: e.transpose(out=out, in_=in_, identity=ident), reads=r, writes=w)

    def act(self, out, in_, func, r, w, bias=None, scale=None, accum_out=None, eng="act"):
        kw = {}
        if bias is not None:
            kw["bias"] = bias
        if scale is not None:
            kw["scale"] = scale
        if accum_out is not None:
            kw["accum_out"] = accum_out
        return self.P.add("act", lambda e: e.activation(out=out, in_=in_, func=func, **kw), reads=r, writes=w)

    def cp(self, eng, out, in_, r, w):
        if eng == "act":
            return self.P.add("act", lambda e: e.copy(out=out, in_=in_), reads=r, writes=w)
        return self.P.add(eng, lambda e: e.tensor_copy(out=out, in_=in_), reads=r, writes=w)

    def tt(self, eng, out, in0, in1, op, r, w):
        return self.P.add(eng, lambda e: e.tensor_tensor(out=out, in0=in0, in1=in1, op=op), reads=r, writes=w)

    def ts(self, eng, out, in0, s1, op0, r, w, s2=None, op1=None):
        if op1 is None:
            return self.P.add(eng, lambda e: e.tensor_scalar(out=out, in0=in0, scalar1=s1, scalar2=None, op0=op0), reads=r, writes=w)
        return self.P.add(eng, lambda e: e.tensor_scalar(out=out, in0=in0, scalar1=s1, scalar2=s2, op0=op0, op1=op1), reads=r, writes=w)

    def stt(self, eng, out, in0, scalar, in1, op0, op1, r, w):
        return self.P.add(eng, lambda e: e.scalar_tensor_tensor(out=out, in0=in0, scalar=scalar, in1=in1, op0=op0, op1=op1), reads=r, writes=w)

    def red(self, eng, out, in_, r, w, op=ALU.add):
        return self.P.add(eng, lambda e: e.tensor_reduce(out=out, in_=in_, axis=AX.X, op=op), reads=r, writes=w)

    def recip(self, out, in_, r, w):
        return self.P.add("dve", lambda e: e.reciprocal(out=out, in_=in_), reads=r, writes=w)

    def memset(self, eng, ap, val, w):
        return self.P.add(eng, lambda e: e.memset(ap, val), writes=w)

    def dma(self, q, out, in_, r, w):
        return self.P.add(q, lambda e: e.dma_start(out=out, in_=in_), reads=r, writes=w)


class Ring:
    def __init__(self, slots):
        self.slots = slots
        self.i = 0

    def next(self):
        s = self.slots[self.i % len(self.slots)]
        self.i += 1
        return s


def pipeline(units):
    n = len(units)
    nst = max(len(u) for u in units)
    for i in range(n + nst - 1):
        for st in range(nst - 1, -1, -1):
            u = i - st
            if 0 <= u < n and st < len(units[u]) and units[u][st] is not None:
                units[u][st]()


def bfv(ap):
    return ap.bitcast(BF16)


def build(S, debug=False, stop=None, skip_pre=False):
    nc = bass.Bass("TRN2", target_bir_lowering=False)
    NT = S // 128
    NSP = S // 512
    kb = KB(nc)
    P = kb.P

    def din(name, shape, dt=F32):
        return nc.dram_tensor(name, list(shape), dt, kind="ExternalInput").ap()

    def dscr(name, shape, dt):
        return nc.dram_tensor(name, list(shape), dt, kind="ExternalOutput" if debug else "Internal").ap()

    x = din("x", [S, 1024])
    p_in = din("p", [S, 256])
    norm_mix = din("norm_mix", [1024])
    w_in = din("w_in", [1024, 10768])
    q_norm = din("q_norm", [64])
    k_norm = din("k_norm", [64])
    conv_w = din("conv_w", [128, 24, 4])
    a_log = din("a_log", [8])
    dt_bias = din("dt_bias", [8])
    dn_out_norm = din("dn_out_norm", [128])
    w_branch_a = din("w_branch_a", [512, 1024])
    w_branch_b = din("w_branch_b", [1024, 1024])
    w_out = din("w_out", [1024, 1024])
    norm_ffn = din("norm_ffn", [1024])
    w_rg = din("w_router_group", [1024, 4])
    b_rg = din("b_router_group", [4])
    w_re = din("w_router_expert", [1024, 16])
    b_re = din("b_router_expert", [16])
    w_eg = din("w_expert_gate", [16, 1024, 256])
    w_eu = din("w_expert_up", [16, 1024, 256])
    w_ed = din("w_expert_down", [16, 256, 1024])
    norm_ple = din("norm_ple", [1024])
    w_ple = din("w_ple", [256, 1024])
    w_pg = din("w_ple_gate", [1024, 1024])
    c_ident = din("c_ident", [128, 128])
    c_triu = din("c_triu", [128, 128])
    c_tril = din("c_tril", [128, 128])
    out = nc.dram_tensor("out", [S, 1024], F32, kind="ExternalOutput").ap()

    qkva_s = dscr("qkva_s", [9, S, 512], BF16)
    qkvb_s = dscr("qkvb_s", [24, 128, S], BF16)
    z_s = dscr("z_s", [S, 1024], BF16)
    bg_s = dscr("bg_s", [S, 16], F32)
    gates_s = dscr("gates_s", [S, 2048], BF16)
    ua_s = dscr("ua_s", [3, S, 520], F32)
    ob_s = dscr("ob_s", [S, 1024], BF16)
    wgu_s = nc.dram_tensor("wgu_s", [16, 1024, 512], BF16, kind="Internal").ap()
    wd_s = nc.dram_tensor("wd_s", [16, 256, 1024], BF16, kind="Internal").ap()

    ident = kb.sb([128, 128], F32, "ident"); t_ident = T()
    ident_bf = kb.sb([128, 128], BF16, "identbf"); t_identbf = T()
    triu = kb.sb([128, 128], F32, "triu"); t_triu = T()
    tril_bf = kb.sb([128, 128], BF16, "trilbf"); t_trilbf = T()
    triu_bf = kb.sb([128, 128], BF16, "triubf"); t_triubf = T()
    tril = kb.sb([128, 128], F32, "tril"); t_tril = T()
    mbm = kb.sb([128, 128], F32, "mbm"); t_mbm = T()
    strict = kb.sb([128, 128], F32, "strict"); t_strict = T()
    ones_bf = kb.sb([128, 128], BF16, "ones"); t_ones = T()
    epsc = kb.sb([128, 1], F32, "eps"); t_eps = T()
    eps128 = kb.sb([128, 1], F32, "eps128"); t_eps128 = T()
    kb.dma("sp", ident[:], c_ident[:, :], [], [t_ident])
    kb.dma("sp", triu[:], c_triu[:, :], [], [t_triu])
    kb.dma("sp", tril[:], c_tril[:, :], [], [t_tril])
    kb.cp("dve", ident_bf[:], ident[:], [t_ident], [t_identbf])
    kb.cp("dve", triu_bf[:], triu[:], [t_triu], [t_triubf])
    kb.cp("dve", tril_bf[:], tril[:], [t_tril], [t_trilbf])
    kb.ts("dve", mbm[:], triu[:], -1.0, ALU.add, [t_triu], [t_mbm], s2=1e9, op1=ALU.mult)
    kb.tt("dve", strict[:], triu[:], ident[:], ALU.subtract, [t_triu, t_ident], [t_strict])
    kb.memset("pool", ones_bf[:], 1.0, [t_ones])
    kb.memset("pool", epsc[:], EPS, [t_eps])
    kb.memset("pool", eps128[:], 128.0 * EPS, [t_eps128])

    t_wgu = [T() for _ in range(16)]
    t_wd = [T() for _ in range(16)]
    for e_ in range(0 if not skip_pre else 16, 16):
        kb.dma("gq", wgu_s[e_, :, 0:256], w_eg[e_], [], [t_wgu[e_]])
        kb.dma("gq", wgu_s[e_, :, 256:512], w_eu[e_], [], [t_wgu[e_]])
        kb.dma("gq", wd_s[e_], w_ed[e_], [], [t_wd[e_]])

    bar_a = kb.sb([128, 8], F32, "bar_a")
    bar_b = kb.sb([128, 8], F32, "bar_b")
    kb.memset("pool", bar_a[:], 0.0, [])
    P.bar_fn = lambda e: e.dma_start(out=bar_b[:], in_=bar_a[:])
    persist_off = kb.off

    def finish_early():
        fw = []
        for q in DMAQ:
            fw.extend(P.ops[q][-NDSEM:])
        for e in ENGS:
            if P.ops[e]:
                fw.append(P.ops[e][-1])
        P.emit(nc, final_waits=fw)
        return nc
    gain_mix = kb.sb([128, 1024], F32, "gmix"); t_gmix = T()
    kb.dma("sp", gain_mix[:], norm_mix.partition_broadcast(128), [], [t_gmix])
    hT = kb.sb([128, 8, S], BF16, "hT")
    t_hT = [T() for _ in range(NT)]
    a_off = kb.off
    xr = kb.ring(4, [128, 1024], F32, "xt")
    junk = kb.ring(2, [128, 1024], BF16, "junk")
    hb = kb.ring(3, [128, 1024], BF16, "hb")
    ssr = kb.ring(6, [128, 1], F32, "ss")
    psA0 = kb.psring([0, 1], 512, "psA0")

    def rms_rstd(ss_ap, t_ss, nfeat_scale_done=True):
        kb.act(ss_ap, ss_ap, AF.Sqrt, [t_ss, t_eps], [t_ss], bias=epsc[:, 0:1])
        kb.recip(ss_ap, ss_ap, [t_ss], [t_ss])

    def a0_unit(t):
        d = {}

        def s0():
            xt, t_xt = xr.next()
            kb.dma("sp", xt[:], x[t * 128:(t + 1) * 128, :], [], [t_xt])
            d.update(xt=xt, t_xt=t_xt)

        def s1():
            xt, t_xt = d["xt"], d["t_xt"]
            jk, t_jk = junk.next()
            ss, t_ss = ssr.next()
            kb.memset("pool", ss[:], 0.0, [t_ss])
            kb.act(jk[:], xt[:], AF.Square, [t_xt, t_ss], [t_jk, t_ss], scale=1.0 / 32.0, accum_out=ss[:])
            kb.act(ss[:], ss[:], AF.Sqrt, [t_ss, t_eps], [t_ss], bias=epsc[:, 0:1])
            d.update(ss=ss, t_ss=t_ss)

        def s2():
            xt, t_xt, ss, t_ss = d["xt"], d["t_xt"], d["ss"], d["t_ss"]
            kb.recip(ss[:], ss[:], [t_ss], [t_ss])
            h_, t_h = hb.next()
            kb.stt("dve", h_[:], xt[:], ss[:, 0:1], gain_mix[:], ALU.mult, ALU.mult, [t_xt, t_ss, t_gmix], [t_h])
            d.update(h=h_, t_h=t_h)

        def s3():
            ps, t_ps = psA0.next()
            psv = bfv(ps).rearrange("p (k c) -> p k c", k=8)
            for k in range(8):
                kb.tr(psv[:, k, :], d["h"][:, k * 128:(k + 1) * 128], ident_bf[:], [d["t_h"], t_identbf], [t_ps])
            d.update(psv=psv, t_ps=t_ps)

        def s4():
            kb.cp("act" if t % 2 else "dve", hT[:, :, t * 128:(t + 1) * 128], d["psv"], [d["t_ps"]], [t_hT[t]])

        return [s0, s1, s2, s3, s4]

    pipeline([a0_unit(t) for t in range(NT)])

    if stop == "A0":
        return finish_early()
    P.barrier()
    kb.off = a_off
    qg = kb.sb([128, 64], F32, "qg"); t_qg = T()
    kg = kb.sb([128, 64], F32, "kg"); t_kg = T()
    kb.dma("sp", qg[:], q_norm.partition_broadcast(128), [], [t_qg])
    kb.dma("sp", kg[:], k_norm.partition_broadcast(128), [], [t_kg])
    kb.ts("dve", qg[:], qg[:], 0.125, ALU.mult, [t_qg], [t_qg])
    convw = kb.sb([128, 24, 4], F32, "convw"); t_convw = T()
    kb.dma("sp", convw[:], conv_w[:, :, :], [], [t_convw])
    dtb = kb.sb([128, 8], F32, "dtb"); t_dtb = T()
    negA = kb.sb([128, 8], F32, "negA"); t_negA = T()
    kb.dma("sp", dtb[:], dt_bias.partition_broadcast(128), [], [t_dtb])
    kb.dma("sp", negA[:], a_log.partition_broadcast(128), [], [t_negA])
    kb.act(negA[:], negA[:], AF.Exp, [t_negA], [t_negA])
    kb.ts("dve", negA[:], negA[:], -1.0, ALU.mult, [t_negA], [t_negA])

    wring = kb.ring(2, [128, 8, 512], BF16, "wg")
    wsm = kb.sb([128, 8, 16], BF16, "wsm"); t_wsm = T()
    psA = kb.psring([0, 1, 2, 3, 4, 5], 512, "psA")
    psS = kb.psring([6, 7], 512, "psS")
    f32r = kb.ring(3, [128, 512], F32, "f32r")
    f32r2 = kb.ring(3, [128, 512], F32, "f32r2")
    bfr = kb.ring(4, [128, 512], BF16, "bfr")
    s8r = kb.ring(4, [128, 8], F32, "s8")
    rawr = kb.ring(3, [128, 515], F32, "raw")
    carry = [(kb.sb([128, 3], F32, "carry"), T()) for _ in range(4)]
    yr = kb.ring(5, [128, 512], F32, "y")
    sqr = kb.ring(3, [128, 512], BF16, "sqb")
    rnr = kb.ring(3, [128, 512], F32, "rn")
    bgr = kb.ring(3, [128, 16], F32, "bg")
    w_in_v = w_in.rearrange("(k p) c -> p k c", p=128)

    def col0(cg):
        if cg < 17:
            return cg * 512
        if cg == 17:
            return 8704
        return 8720 + (cg - 18) * 512

    def load_W(cg):
        c0 = col0(cg)
        if cg == 17:
            kb.dma("gq", wsm[:], w_in_v[:, :, c0:c0 + 16], [], [t_wsm])
            return wsm, t_wsm
        W, t_W = wring.next()
        kb.dma("gq", W[:, 0:4, :], w_in_v[:, 0:4, c0:c0 + 512], [], [t_W])
        kb.dma("gq", W[:, 4:8, :], w_in_v[:, 4:8, c0:c0 + 512], [], [t_W])
        return W, t_W

    def conv_unit(cg, s, cb, W, t_W):
        cbg = (cg - 9) * 4 + cb
        which = cbg // 8
        d = {}

        def sa():
            ps, t_ps = psA.next()
            for k in range(8):
                kb.mm(ps, W[:, k, cb * 128:(cb + 1) * 128], hT[:, k, s * 512:(s + 1) * 512],
                      [t_W] + t_hT[4 * s:4 * s + 4], [t_ps], start=(k == 0), stop=(k == 7))
            raw, t_raw = rawr.next()
            cy, t_cy = carry[cb]
            if s == 0:
                kb.memset("pool", raw[:, 0:3], 0.0, [t_raw])
            else:
                kb.cp("pool", raw[:, 0:3], cy[:], [t_cy], [t_raw])
            kb.cp("act", raw[:, 3:515], ps, [t_ps], [t_raw])
            kb.cp("pool", cy[:], raw[:, 512:515], [t_raw], [t_cy])
            d.update(raw=raw, t_raw=t_raw)

        def sb_():
            raw, t_raw = d["raw"], d["t_raw"]
            y, t_y = yr.next()
            kb.ts("dve", y[:], raw[:, 3:515], convw[:, cbg, 3:4], ALU.mult, [t_raw, t_convw], [t_y])
            for j in range(3):
                kb.stt("dve", y[:], raw[:, j:j + 512], convw[:, cbg, j:j + 1], y[:], ALU.mult, ALU.add,
                       [t_raw, t_convw, t_y], [t_y])
            d.update(y=y, t_y=t_y)

        def sc():
            y, t_y = d["y"], d["t_y"]
            if which == 2:
                ob, t_ob = bfr.next()
                kb.act(ob[:], y[:], AF.Silu, [t_y], [t_ob])
                kb.dma("sp", qkvb_s[cbg, :, s * 512:(s + 1) * 512], ob[:], [t_ob], [])
            else:
                kb.act(y[:], y[:], AF.Silu, [t_y], [t_y])
                sq, t_sq = sqr.next()
                kb.tt("pool", sq[:], y[:], y[:], ALU.mult, [t_y], [t_sq])
                d.update(sq=sq, t_sq=t_sq)

        def sd():
            if which == 2:
                return
            pss, t_pss = psS.next()
            kb.mm(pss, ones_bf[:], d["sq"][:], [t_ones, d["t_sq"]], [t_pss])
            rn, t_rn = rnr.next()
            if which == 0:
                kb.act(rn[:], pss, AF.Sqrt, [t_pss, t_eps128], [t_rn], bias=eps128[:, 0:1], scale=128.0)
            else:
                kb.act(rn[:], pss, AF.Sqrt, [t_pss, t_eps], [t_rn], bias=epsc[:, 0:1])
            d.update(rn=rn, t_rn=t_rn)

        def se():
            if which == 2:
                return
            rn, t_rn, y, t_y = d["rn"], d["t_rn"], d["y"], d["t_y"]
            kb.recip(rn[:], rn[:], [t_rn], [t_rn])
            ob, t_ob = bfr.next()
            kb.tt("pool", ob[:], y[:], rn[:], ALU.mult, [t_y, t_rn], [t_ob])
            kb.dma("sp", qkvb_s[cbg, :, s * 512:(s + 1) * 512], ob[:], [t_ob], [])

        return [sa, sb_, sc, sd, se]

    Wnext = load_W(0)
    for cg in range(22):
        c0 = col0(cg)
        W, t_W = Wnext
        if cg + 1 < 22:
            Wnext = load_W(cg + 1)
        if 9 <= cg <= 14:
            units = []
            for s in range(NSP):
                for cb in range(4):
                    units.append(conv_unit(cg, s, cb, W, t_W))
            pipeline(units)
        else:
            for t in range(NT):
                rows = slice(t * 128, (t + 1) * 128)
                ps, t_ps = psA.next()
                ncol = 16 if cg == 17 else 512
                pso = ps[:, 0:ncol]
                for k in range(8):
                    kb.mm(pso, hT[:, k, t * 128:(t + 1) * 128], W[:, k, :], [t_W, t_hT[t]], [t_ps],
                          start=(k == 0), stop=(k == 7))
                if cg < 6:
                    gain, t_gain = (qg, t_qg) if cg < 3 else (kg, t_kg)
                    sqf, t_sqf = f32r.next()
                    kb.act(sqf[:], ps, AF.Square, [t_ps], [t_sqf], scale=0.125)
                    s8, t_s8 = s8r.next()
                    kb.red("dve", s8[:], sqf[:].rearrange("p (h d) -> p h d", d=64), [t_sqf], [t_s8])
                    rms_rstd(s8[:], t_s8)
                    tmp, t_tmp = f32r2.next()
                    kb.tt("dve", tmp[:].rearrange("p (h d) -> p h d", d=64), ps.rearrange("p (h d) -> p h d", d=64),
                          s8[:, :].unsqueeze(2).to_broadcast([128, 8, 64]), ALU.mult, [t_ps, t_s8], [t_tmp])
                    ob, t_ob = bfr.next()
                    kb.tt("pool", ob[:].rearrange("p (h d) -> p h d", d=64), tmp[:].rearrange("p (h d) -> p h d", d=64),
                          gain[:, :].unsqueeze(1).to_broadcast([128, 8, 64]), ALU.mult, [t_tmp, t_gain], [t_ob])
                    kb.dma("sp", qkva_s[cg, rows, :], ob[:], [t_ob], [])
                elif cg < 9:
                    ob, t_ob = bfr.next()
                    kb.cp("act" if t % 2 else "dve", ob[:], ps, [t_ps], [t_ob])
                    kb.dma("sp", qkva_s[cg, rows, :], ob[:], [t_ob], [])
                elif cg in (15, 16):
                    ob, t_ob = bfr.next()
                    kb.act(ob[:], ps, AF.Silu, [t_ps], [t_ob])
                    kb.dma("sp", z_s[rows, (cg - 15) * 512:(cg - 14) * 512], ob[:], [t_ob], [])
                elif cg == 17:
                    bg, t_bg = bgr.next()
                    kb.act(bg[:, 0:8], ps[:, 0:8], AF.Sigmoid, [t_ps], [t_bg])
                    kb.tt("dve", bg[:, 8:16], ps[:, 8:16], dtb[:], ALU.add, [t_ps, t_dtb], [t_bg])
                    kb.act(bg[:, 8:16], bg[:, 8:16], AF.Exp, [t_bg], [t_bg])
                    kb.act(bg[:, 8:16], bg[:, 8:16], AF.Ln, [t_bg], [t_bg], bias=1.0)
                    kb.tt("dve", bg[:, 8:16], bg[:, 8:16], negA[:], ALU.mult, [t_bg, t_negA], [t_bg])
                    kb.dma("sp", bg_s[rows, :], bg[:], [t_bg], [])
                else:
                    ob, t_ob = bfr.next()
                    kb.act(ob[:], ps, AF.Sigmoid, [t_ps], [t_ob])
                    kb.dma("sp", gates_s[rows, (cg - 18) * 512:(cg - 17) * 512], ob[:], [t_ob], [])

    print("sbuf end A", kb.off)
    if stop == "A":
        return finish_early()
    P.barrier()
    kb.off = persist_off
    qr_ = kb.ring(3, [128, 512], BF16, "qb")
    kr_ = kb.ring(3, [128, 512], BF16, "kb")
    vst_r = kb.ring(3, [128, 512], BF16, "vst")
    vr_ = kb.ring(6, [128, 8, 65], BF16, "v1")
    for v1, t_v1 in vr_.slots:
        kb.memset("pool", v1[:, :, 64:65], 1.0, [t_v1])
    qTr = kb.ring(3, [128, 8, 128], BF16, "qT")
    for qz, t_qz in qTr.slots:
        kb.memset("pool", qz[:], 0.0, [t_qz])
    kTr = kb.ring(4, [128, 4, 128], BF16, "kT")
    er_ = kb.ring(4, [128, 4, 2, 128], BF16, "E")
    ur_ = kb.ring(3, [128, 8, 65], F32, "U")
    sc_slots = Ring([((kb.banks[0][:, :], kb.banks[1][:, :]), kb.pst(0, 1)), ((kb.banks[2][:, :], kb.banks[3][:, :]), kb.pst(2, 3))])
    pv_slots = Ring([(kb.banks[4][:, :], kb.pst(4)), (kb.banks[5][:, :], kb.pst(5))])
    psT = Ring([(kb.banks[6][:, :], kb.pst(6)), (kb.banks[7][:, :], kb.pst(7))])
    ATT = ((128, 1), (512, 4), (2048, 16))

    def attn_unit(g, dil, r_, nb, prev):
        qs = qkva_s[g].rearrange("(u r) c -> r u c", r=dil)
        ks = qkva_s[3 + g].rearrange("(u r) c -> r u c", r=dil)
        vs = qkva_s[6 + g].rearrange("(u r) c -> r u c", r=dil)
        us = ua_s[g].rearrange("(u r) c -> r u c", r=dil)
        ur = slice(nb * 128, (nb + 1) * 128)
        have_prev = nb > 0
        d = {}

        def s0():
            qb_, t_qb = qr_.next()
            kb_, t_kb = kr_.next()
            vst, t_vst = vst_r.next()
            kb.dma("sp", qb_[:], qs[r_, ur, :], [], [t_qb])
            kb.dma("sp", kb_[:], ks[r_, ur, :], [], [t_kb])
            kb.dma("sp", vst[:], vs[r_, ur, :], [], [t_vst])
            d.update(qb=qb_, t_qb=t_qb, kb=kb_, t_kb=t_kb, vst=vst, t_vst=t_vst)

        def s1():
            v1, t_v1 = vr_.next()
            kb.cp("dve", v1[:, :, 0:64], d["vst"][:].rearrange("p (h d) -> p h d", d=64), [d["t_vst"]], [t_v1])
            pt, t_pt = psT.next()
            ptv = bfv(pt).rearrange("p (a h c) -> p a h c", a=2, h=4)
            for hp in range(4):
                kb.tr(ptv[:, 0, hp, :], d["qb"][:, hp * 128:(hp + 1) * 128], ident_bf[:], [d["t_qb"], t_identbf], [t_pt])
                kb.tr(ptv[:, 1, hp, :], d["kb"][:, hp * 128:(hp + 1) * 128], ident_bf[:], [d["t_kb"], t_identbf], [t_pt])
            d.update(v1=v1, t_v1=t_v1, ptv=ptv, t_pt=t_pt)

        def s2():
            qT, t_qT = qTr.next()
            kT, t_kT = kTr.next()
            ptv, t_pt = d["ptv"], d["t_pt"]
            qTv = qT[:].rearrange("p (hp j) c -> p hp j c", j=2)
            kb.cp("dve", qTv[0:64, :, 0, :], ptv[0:64, 0], [t_pt], [t_qT])
            kb.cp("dve", qTv[64:128, :, 1, :], ptv[64:128, 0], [t_pt], [t_qT])
            kb.cp("act", kT[:], ptv[:, 1], [t_pt], [t_kT])
            d.update(qT=qT, t_qT=t_qT, kT=kT, t_kT=t_kT)

        def mk_half(half):
            hd = {}

            def h3():
                (b0, b1), t_sc = sc_slots.next()
                qT, t_qT, kT, t_kT = d["qT"], d["t_qT"], d["kT"], d["t_kT"]
                for hh in range(4):
                    h = half * 4 + hh
                    hp = h // 2
                    bank = b0 if hh < 2 else b1
                    base = (hh % 2) * 256
                    if have_prev:
                        kb.mm(bank[:, base:base + 128], prev["kT"][:, hp, :], qT[:, h, :],
                              [prev["t_kT"], t_qT], [t_sc])
                    kb.mm(bank[:, base + 128:base + 256], kT[:, hp, :], qT[:, h, :],
                          [t_kT, t_qT], [t_sc])
                hd.update(b0=b0, b1=b1, t_sc=t_sc)

            def h4():
                E, t_E = er_.next()
                t_sc = hd["t_sc"]
                for bi, bank in enumerate((hd["b0"], hd["b1"])):
                    ev = E[:, 2 * bi:2 * bi + 2, :, :]
                    bv = bank.rearrange("p (h a c) -> p h a c", h=2, a=2)
                    if have_prev:
                        kb.act(ev, bv, AF.Exp, [t_sc], [t_E])
                    else:
                        kb.act(ev[:, :, 1, :], bv[:, :, 1, :], AF.Exp, [t_sc], [t_E])
                hd.update(E=E, t_E=t_E)

            def h5():
                E, t_E = hd["E"], hd["t_E"]
                if have_prev:
                    kb.tt("pool", E[:, :, 0, :], E[:, :, 0, :], tril_bf[:, :].unsqueeze(1).to_broadcast([128, 4, 128]),
                          ALU.mult, [t_E, t_trilbf], [t_E])
                kb.tt("dve", E[:, :, 1, :], E[:, :, 1, :], triu_bf[:, :].unsqueeze(1).to_broadcast([128, 4, 128]),
                      ALU.mult, [t_E, t_triubf], [t_E])

            def h6():
                E, t_E = hd["E"], hd["t_E"]
                pv, t_pv = pv_slots.next()
                pvv = pv[:, 0:260].rearrange("p (h c) -> p h c", h=4)
                for hh in range(4):
                    h = half * 4 + hh
                    if have_prev:
                        kb.mm(pvv[:, hh, :], E[:, hh, 0, :], prev["v1"][:, h, :], [t_E, prev["t_v1"]], [t_pv], start=True, stop=False)
                    kb.mm(pvv[:, hh, :], E[:, hh, 1, :], d["v1"][:, h, :], [t_E, d["t_v1"]], [t_pv], start=not have_prev, stop=True)
                hd.update(pvv=pvv, t_pv=t_pv)

            def h7():
                if half == 0:
                    U_, t_U = ur_.next()
                    d.update(U=U_, t_U=t_U)
                U_, t_U = d["U"], d["t_U"]
                kb.cp("act" if half else "dve", U_[:, half * 4:half * 4 + 4, :], hd["pvv"], [hd["t_pv"]], [t_U])
                if half == 1:
                    kb.dma("sp", us[r_, ur, :], U_[:].rearrange("p h c -> p (h c)"), [t_U], [])

            return [h3, h4, h5, h6, h7]

        return d, [[s0, s1, s2] + mk_half(0), [None, None, None] + mk_half(1)]

    items = []
    for g, (win, dil) in enumerate(ATT):
        nblk = S // dil // 128
        for r_ in range(dil):
            prev = None
            for nb in range(nblk):
                prev, its = attn_unit(g, dil, r_, nb, prev)
                items.extend(its)
    pipeline(items)

    print("sbuf end B", kb.off)
    if stop == "B":
        return finish_early()
    P.barrier()
    kb.off = persist_off
    dng = kb.sb([128, 128], F32, "dng"); t_dng = T()
    kb.dma("sp", dng[:], dn_out_norm.partition_broadcast(128), [], [t_dng])
    S32 = [(kb.sb([128, 128], F32, "S32"), T()) for _ in range(8)]
    Sbf = [(kb.sb([128, 128], BF16, "Sbf"), T()) for _ in range(8)]
    for h in range(8):
        kb.memset("pool", S32[h][0][:], 0.0, [S32[h][1]])
        kb.memset("pool", Sbf[h][0][:], 0.0, [Sbf[h][1]])
    qkvr = kb.ring(2, [128, 24, 128], BF16, "qkvT")
    bgr2 = kb.ring(2, [128, 16], F32, "bgc")
    zr = kb.ring(2, [128, 1024], BF16, "z")
    gbc_r = kb.ring(2, [128, 8, 128], F32, "gbc")
    gcum_r = kb.ring(2, [128, 8], F32, "gcum")
    ngc_r = kb.ring(2, [128, 8], F32, "ngc")
    eg_r = kb.ring(2, [128, 8], F32, "eg")
    neg_r = kb.ring(2, [128, 8], F32, "neg")
    kts_r = kb.ring(2, [128, 8], F32, "kts")
    egl_r = kb.ring(2, [128, 8], F32, "egl")
    H = 8
    pre_r = kb.ring(4, [128, 128], F32, "pre")
    dtm_r = kb.ring(2 * H, [128, 128], F32, "dtm")
    dts_r = kb.ring(2 * H, [128, 128], F32, "dts")
    XA_r = Ring([(kb.sb([128, 256], F32R, "XA"), T("xs"), T("xr")) for _ in range(2 * H + 2)])
    XB_r = Ring([(kb.sb([128, 256], F32R, "XB"), T("xb")) for _ in range(2 * H + 2)])
    zf = kb.sb([128, 256], F32, "zf"); t_zf = T()
    kb.memset("pool", zf[:], 0.0, [t_zf])
    for xa_, t1_, t2_ in XA_r.slots:
        kb.cp("dve", xa_[:], zf[:], [t_zf], [t1_, t2_])
    for xb_, t1_ in XB_r.slots:
        kb.cp("dve", xb_[:], zf[:], [t_zf], [t1_])
    PT_r = kb.ring(2 * H, [128, 128], BF16, "PT")
    AIT_r = kb.ring(2 * H, [128, 128], BF16, "AIT")
    Ktl_r = kb.ring(2 * H, [128, 128], BF16, "Ktl")
    Vtm_r = kb.ring(2 * H, [128, 128], BF16, "Vtm")
    Y_r = kb.ring(4, [128, 128], BF16, "Y")
    vn_r = kb.ring(4, [128, 128], BF16, "vn")
    o1_r = kb.ring(4, [128, 128], F32, "o1")
    O_r = kb.ring(2, [128, 8, 128], F32, "O")
    osq_r = kb.ring(2, [128, 8, 128], F32, "osq")
    obf_r = kb.ring(2, [128, 1024], BF16, "obf")
    s8c_r = kb.ring(2, [128, 8], F32, "s8c")
    glast_r = kb.ring(2, [128, 8], F32, "glast")
    psN = kb.psring([0, 1, 2, 3], 256, "psN")
    psXO = kb.psring([4, 5], 128, "psXO")
    psG = kb.psring([6], 128, "psG")
    psX = Ring([(kb.banks[7][:, 0:128], kb.pst(7)), (kb.banks[7][:, 128:256], kb.pst(7))])
    psB = Ring([(kb.banks[7][:, 256 + 64 * i:256 + 64 * (i + 1)], kb.pst(7)) for i in range(4)])

    def chunk_gen(b):
        cols = slice(b * 128, (b + 1) * 128)
        qkvT, t_qkvT = qkvr.next()
        bgc, t_bgc = bgr2.next()
        zt, t_zt = zr.next()
        kb.dma("sp", qkvT[:], qkvb_s[:, :, cols].rearrange("c p t -> p c t"), [], [t_qkvT])
        kb.dma("sp", bgc[:], bg_s[cols, :], [], [t_bgc])
        kb.dma("sp", zt[:], z_s[cols, :], [], [t_zt])
        gcum, t_gcum = gcum_r.next()
        psg, t_psg = psG.next()
        kb.mm(psg[:, 0:8], triu[:], bgc[:, 8:16], [t_triu, t_bgc], [t_psg])
        kb.cp("dve", gcum[:], psg[:, 0:8], [t_psg], [t_gcum])
        gbc, t_gbc = gbc_r.next()
        kb.cp("pool", gbc[:], bgc[:, 8:16].unsqueeze(2).to_broadcast([128, 8, 128]), [t_bgc], [t_gbc])
        eg, t_eg = eg_r.next()
        neg, t_neg = neg_r.next()
        kb.act(eg[:], gcum[:], AF.Exp, [t_gcum], [t_eg])
        kb.ts("dve", neg[:], eg[:], -1.0, ALU.mult, [t_eg], [t_neg])
        kts, t_kts = kts_r.next()
        egl, t_egl = egl_r.next()
        O_, t_O = O_r.next()
        glast, t_glast = glast_r.next()
        st = []
        for h in range(H):
            psg, t_psg = psG.next()
            kb.mm(psg, gbc[:, h, :], triu[:], [t_gbc, t_triu], [t_psg])
            pre, t_pre = pre_r.next()
            kb.stt("dve", pre[:], psg, gcum[:, h:h + 1], mbm[:], ALU.subtract, ALU.add, [t_psg, t_gcum, t_mbm], [t_pre])
            dtm, t_dtm = dtm_r.next()
            kb.act(dtm[:], pre[:], AF.Exp, [t_pre], [t_dtm])
            dts, t_dts = dts_r.next()
            kb.tt("pool", dts[:], dtm[:], strict[:], ALU.mult, [t_dtm, t_strict], [t_dts])
            kb.cp("act", glast[:, h:h + 1], psg[:, 127:128], [t_psg], [t_glast])
            st.append(dict(dtm=dtm, t_dtm=t_dtm, dts=dts, t_dts=t_dts))
        kb.act(egl[:], glast[:], AF.Exp, [t_glast], [t_egl])
        kb.tt("dve", kts[:], glast[:], gcum[:], ALU.subtract, [t_glast, t_gcum], [t_kts])
        kb.act(kts[:], kts[:], AF.Exp, [t_kts], [t_kts])
        for h in range(H):
            d = st[h]
            dtm, t_dtm, dts, t_dts = d["dtm"], d["t_dtm"], d["dts"], d["t_dts"]
            KT = qkvT[:, 8 + h, :]
            QT = qkvT[:, h, :]
            VT = qkvT[:, 16 + h, :]
            pk, t_pk = psN.next()
            kb.mm(pk, KT, qkvT[:, :, :].rearrange("p (a h) t -> p a h t", a=3)[:, 0:2, h, :], [t_qkvT], [t_pk])
            XA0, t_B, t_XR0 = XA_r.next()
            kb.stt("dve", XA0[:, 0:128], pk[:, 128:256], bgc[:, h:h + 1], dts[:], ALU.mult, ALU.mult, [t_pk, t_bgc, t_dts], [t_B])
            AIT, t_AIT = AIT_r.next()
            kb.tt("dve", AIT[:], pk[:, 0:128], dtm[:], ALU.mult, [t_pk, t_dtm], [t_AIT])
            pb, t_pb = psB.next()
            kb.tr(bfv(pb), KT, ident_bf[:], [t_qkvT, t_identbf], [t_pb])
            Ktl, t_Ktl = Ktl_r.next()
            kb.act(Ktl[:], bfv(pb), AF.Copy, [t_pb, t_kts], [t_Ktl], scale=kts[:, h:h + 1])
            pb2, t_pb2 = psB.next()
            kb.tr(bfv(pb2), VT, ident_bf[:], [t_qkvT, t_identbf], [t_pb2])
            Vtm, t_Vtm = Vtm_r.next()
            kb.cp("act", Vtm[:], bfv(pb2), [t_pb2], [t_Vtm])
            d.update(XA0=XA0, t_B=t_B, t_XR0=t_XR0, AIT=AIT, t_AIT=t_AIT, Ktl=Ktl, t_Ktl=t_Ktl, Vtm=Vtm, t_Vtm=t_Vtm, KT=KT, QT=QT)
        yield "pre"
        for h in range(H):
            d = st[h]
            pt_, t_pt_ = psN.next()
            kb.tr(pt_[:, 0:128], d["XA0"][:, 0:128].bitcast(F32), ident[:], [d["t_B"], t_ident], [t_pt_])
            XB, t_XB = XB_r.next()
            kb.cp("act", XB[:, 0:128], pt_[:, 0:128], [t_pt_], [t_XB])
            XA1, t_S1, t_R1 = XA_r.next()
            kb.tt("dve", XA1[:, 128:256], ident[:], d["XA0"][:, 0:128].bitcast(F32), ALU.subtract, [t_ident, d["t_B"]], [t_R1])
            d.update(XA=d["XA0"], t_S=d["t_B"], t_R=d["t_XR0"], XB=XB, t_XB=t_XB, XAn=XA1, t_Sn=t_S1, t_Rn=t_R1)
        yield "lv"
        NL = 7
        for lvl in range(1, NL + 1):
            last = lvl == NL
            for h in range(H):
                d = st[h]
                XA, XB = d["XA"], d["XB"]
                p1, t_p1 = psN.next()
                kb.mm(p1, XB[:, 0:128], XA[:, :], [d["t_S"], d["t_R"], d["t_XB"]], [t_p1])
                if last:
                    PT, t_PT = PT_r.next()
                    kb.tt("dve", PT[:], p1[:, 128:256], XA[:, 128:256].bitcast(F32), ALU.add, [t_p1, d["t_R"]], [t_PT])
                    d.update(PT=PT, t_PT=t_PT)
                    continue
                p2, t_p2 = psN.next()
                kb.mm(p2, XA[:, 0:128], XB[:, :], [d["t_S"], d["t_XB"]], [t_p2])
                if lvl == 1:
                    XAn, t_Sn, t_Rn = d["XAn"], d["t_Sn"], d["t_Rn"]
                else:
                    XAn, t_Sn, t_Rn = XA_r.next()
                    kb.tt("dve", XAn[:, 128:256], p1[:, 128:256], XA[:, 128:256].bitcast(F32), ALU.add, [t_p1, d["t_R"]], [t_Rn])
                kb.cp("dve", XAn[:, 0:128], p1[:, 0:128], [t_p1], [t_Sn])
                XBn, t_XBn = XB_r.next()
                kb.cp("act", XBn[:, 0:128], p2[:, 0:128], [t_p2], [t_XBn])
                d.update(XA=XAn, t_S=t_Sn, t_R=t_Rn, XB=XBn, t_XB=t_XBn)
            yield ("inv_done" if last else "lv")
        for hg in range(0, H, 4):
            for h in range(hg, hg + 4):
                d = st[h]
                sbf, t_sbf = Sbf[h]
                px, t_px = psXO.next()
                kb.mm(px, d["KT"], sbf[:], [t_qkvT, t_sbf], [t_px])
                po1, t_po1 = psXO.next()
                kb.mm(po1, d["QT"], sbf[:], [t_qkvT, t_sbf], [t_po1])
                d.update(px=px, t_px=t_px, po1=po1, t_po1=t_po1)
            yield "sc"
            for h in range(hg, hg + 4):
                d = st[h]
                Y, t_Y = Y_r.next()
                kb.stt("dve", Y[:], d["px"], neg[:, h:h + 1], d["Vtm"][:], ALU.mult, ALU.add,
                       [d["t_px"], t_neg, d["t_Vtm"]], [t_Y])
                o1, t_o1 = o1_r.next()
                kb.act(o1[:], d["po1"], AF.Copy, [d["t_po1"], t_eg], [t_o1], scale=eg[:, h:h + 1])
                ppy, t_ppy = psX.next()
                kb.mm(ppy, d["PT"][:], Y[:], [d["t_PT"], t_Y], [t_ppy])
                vn, t_vn = vn_r.next()
                kb.act(vn[:], ppy, AF.Copy, [t_ppy, t_bgc], [t_vn], scale=bgc[:, h:h + 1])
                d.update(vn=vn, t_vn=t_vn, o1=o1, t_o1=t_o1)
            yield "sc"
            for h in range(hg, hg + 4):
                d = st[h]
                vn, t_vn = d["vn"], d["t_vn"]
                s32, t_s32 = S32[h]
                sbf, t_sbf = Sbf[h]
                pso, t_pst = psN.next()
                pst, po2, t_po2 = pso[:, 0:128], pso[:, 128:256], t_pst
                kb.mm(pst, d["Ktl"][:], vn[:], [d["t_Ktl"], t_vn], [t_pst])
                kb.mm(po2, d["AIT"][:], vn[:], [d["t_AIT"], t_vn], [t_po2])
                kb.stt("dve", s32[:], s32[:], egl[:, h:h + 1], pst, ALU.mult, ALU.add, [t_s32, t_egl, t_pst], [t_s32])
                kb.cp("act", sbf[:], s32[:], [t_s32], [t_sbf])
                kb.tt("dve", O_[:, h, :], po2, d["o1"][:], ALU.add, [t_po2, d["t_o1"]], [t_O])
            if hg == 0:
                yield "sc"
        osq, t_osq = osq_r.next()
        kb.act(osq[:], O_[:], AF.Square, [t_O], [t_osq], scale=float(128.0 ** -0.5))
        s8, t_s8 = s8c_r.next()
        kb.red("dve", s8[:], osq[:], [t_osq], [t_s8])
        kb.act(s8[:], s8[:], AF.Sqrt, [t_s8, t_eps], [t_s8], bias=epsc[:, 0:1])
        kb.recip(s8[:], s8[:], [t_s8], [t_s8])
        kb.tt("pool", osq[:], O_[:], s8[:, :].unsqueeze(2).to_broadcast([128, 8, 128]), ALU.mult, [t_O, t_s8], [t_osq])
        kb.tt("pool", osq[:], osq[:], dng[:, :].unsqueeze(1).to_broadcast([128, 8, 128]), ALU.mult, [t_osq, t_dng], [t_osq])
        obf, t_obf = obf_r.next()
        kb.tt("dve", obf[:].rearrange("p (h d) -> p h d", d=128), osq[:], zt[:].rearrange("p (h d) -> p h d", d=128),
              ALU.mult, [t_osq, t_zt], [t_obf])
        kb.dma("sp", ob_s[cols, :], obf[:], [t_obf], [])
        yield "done"


    gens = [chunk_gen(b) for b in range(NT)]
    for i in range(NT + 1):
        cur = gens[i] if i < NT else None
        prv = gens[i - 1] if i >= 1 else None
        cur_live, prv_live = cur is not None, prv is not None
        while cur_live or prv_live:
            if prv_live and next(prv) == "done":
                prv_live = False
            if cur_live and next(cur) == "inv_done":
                cur_live = False

    print("sbuf end C", kb.off)
    if stop == "C":
        return finish_early()
    P.barrier()
    kb.off = persist_off
    TC = 8 if NT >= 8 else NT
    wa = kb.sb([128, 4, 1024], BF16, "wa"); t_wa = T()
    wb = kb.sb([128, 8, 1024], BF16, "wb"); t_wb = T()
    wo = kb.sb([128, 8, 1024], BF16, "wo"); t_wo = T()
    wpg = kb.sb([128, 8, 1024], BF16, "wpg"); t_wpg = T()
    wpl = kb.sb([128, 2, 1024], BF16, "wpl"); t_wpl = T()
    kb.dma("gq", wa[:], w_branch_a.rearrange("(k p) c -> p k c", p=128), [], [t_wa])
    kb.dma("gq", wb[:], w_branch_b.rearrange("(k p) c -> p k c", p=128), [], [t_wb])
    kb.dma("gq", wo[:], w_out.rearrange("(k p) c -> p k c", p=128), [], [t_wo])
    kb.dma("gq", wpg[:], w_pg.rearrange("(k p) c -> p k c", p=128), [], [t_wpg])
    kb.dma("gq", wpl[:], w_ple.rearrange("(k p) c -> p k c", p=128), [], [t_wpl])
    wr = kb.sb([128, 8, 20], F32, "wr"); t_wr = T()
    kb.dma("sp", wr[:, :, 0:4], w_rg.rearrange("(k p) c -> p k c", p=128), [], [t_wr])
    kb.dma("sp", wr[:, :, 4:20], w_re.rearrange("(k p) c -> p k c", p=128), [], [t_wr])
    br = kb.sb([128, 20], F32, "br"); t_br = T()
    kb.dma("sp", br[:, 0:4], b_rg.partition_broadcast(128), [], [t_br])
    kb.dma("sp", br[:, 4:20], b_re.partition_broadcast(128), [], [t_br])
    gffn = kb.sb([128, 1024], F32, "gffn"); t_gffn = T()
    gple = kb.sb([128, 1024], F32, "gple"); t_gple = T()
    kb.dma("sp", gffn[:], norm_ffn.partition_broadcast(128), [], [t_gffn])
    kb.dma("sp", gple[:], norm_ple.partition_broadcast(128), [], [t_gple])
    yacc = [(kb.sb([128, 1024], F32, "yacc"), T()) for _ in range(TC)]
    h2T = [(kb.sb([128, 8, 128], BF16, "h2T"), T()) for _ in range(TC)]
    comb = [(kb.sb([128, 16], F32, "comb"), T()) for _ in range(TC)]
    wgu_r = kb.ring(2, [128, 8, 512], BF16, "wgu")
    wd_r = kb.ring(2, [128, 2, 1024], BF16, "wdn")
    u_r = kb.ring(3, [128, 520], F32, "ua")
    un_r = kb.ring(2, [128, 8, 65], F32, "un")
    ga_r = kb.ring(1, [128, 2048], BF16, "gates")
    obr = kb.ring(2, [128, 1024], BF16, "ob")
    oab_r = kb.ring(2, [128, 512], BF16, "oab")
    rden_r = kb.ring(2, [128, 8], F32, "rden")
    tT_r = kb.ring(2, [128, 8, 128], BF16, "tT")
    mg_r = kb.ring(1, [128, 1024], F32, "mg")
    mgb_r = kb.ring(2, [128, 1024], BF16, "mgb")
    hf_r = kb.ring(1, [128, 1024], F32, "hf")
    hfT_r = kb.ring(1, [128, 8, 128], F32, "hfT")
    hbf_r = kb.ring(2, [128, 1024], BF16, "hbf")
    jk_r = kb.ring(1, [128, 1024], BF16, "jk2")
    ss_r = kb.ring(4, [128, 1], F32, "ss2")
    lg_r = kb.ring(2, [128, 20], F32, "lg")
    sm_r = kb.ring(24, [128, 16], F32, "sm")
    sil_r = kb.ring(3, [128, 256], F32, "sil")
    act_r = kb.ring(4, [128, 256], BF16, "actb")
    actT_r = kb.ring(4, [128, 2, 128], BF16, "actT")
    pin_r = kb.ring(2, [128, 256], F32, "pin")
    pbf_r = kb.ring(2, [128, 256], BF16, "pbf")
    sg_r = kb.ring(1, [128, 1024], F32, "sg")
    psM = kb.psring([0, 1, 2, 3], 512, "psM")
    psTr = kb.psring([4, 5], 512, "psTr")
    psD = kb.psring([6, 7], 512, "psD")
    final_ops = []

    def transpose_bf(src, t_src, nk):
        ps, t_ps = psTr.next()
        psv = bfv(ps).rearrange("p (k c) -> p k c", k=8)
        for k in range(nk):
            kb.tr(psv[:, k, :], src[:, k * 128:(k + 1) * 128], ident_bf[:], [t_src, t_identbf], [t_ps])
        tT, t_tT = tT_r.next()
        kb.cp("act", tT[:, 0:nk, :], psv[:, 0:nk, :], [t_ps], [t_tT])
        return tT, t_tT

    def proj(tT, t_tT, W, t_W, nk):
        res = []
        for c in range(2):
            ps, t_ps = psM.next()
            for k in range(nk):
                kb.mm(ps, tT[:, k, :], W[:, k, c * 512:(c + 1) * 512], [t_tT, t_W], [t_ps], start=(k == 0), stop=(k == nk - 1))
            res.append((ps, t_ps))
        return res

    def rmsnorm_to(xsrc, t_x, gain, t_gain, out_ap, t_out):
        jk, t_jk = jk_r.next()
        ss, t_ss = ss_r.next()
        kb.memset("pool", ss[:], 0.0, [t_ss])
        kb.act(jk[:], xsrc, AF.Square, [t_x, t_ss], [t_jk, t_ss], scale=1.0 / 32.0, accum_out=ss[:])
        rms_rstd(ss[:], t_ss)
        kb.stt("dve", out_ap, xsrc, ss[:, 0:1], gain[:], ALU.mult, ALU.mult, [t_x, t_ss, t_gain], [t_out])

    def d_tile(c0, ti):
        t = c0 + ti
        rows = slice(t * 128, (t + 1) * 128)
        ya, t_ya = yacc[ti]
        kb.dma("sp", ya[:], x[rows, :], [], [t_ya])
        us_ = []
        for g in range(3):
            u, t_u = u_r.next()
            kb.dma("sp", u[:], ua_s[g, rows, :], [], [t_u])
            us_.append((u, t_u))
        ga, t_ga = ga_r.next()
        kb.dma("sp", ga[:], gates_s[rows, :], [], [t_ga])
        ob, t_ob = obr.next()
        kb.dma("sp", ob[:], ob_s[rows, :], [], [t_ob])
        un, t_un = un_r.next()
        unf = un[:].rearrange("p h c -> p (h c)")
        kb.tt("pool", unf, us_[0][0][:], us_[1][0][:], ALU.add, [us_[0][1], us_[1][1]], [t_un])
        kb.tt("pool", unf, unf, us_[2][0][:], ALU.add, [t_un, us_[2][1]], [t_un])
        rden, t_rden = rden_r.next()
        kb.recip(rden[:], un[:, :, 64], [t_un], [t_rden])
        oab, t_oab = oab_r.next()
        kb.tt("dve", oab[:].rearrange("p (h d) -> p h d", d=64), un[:, :, 0:64],
              rden[:, :].unsqueeze(2).to_broadcast([128, 8, 64]), ALU.mult, [t_un, t_rden], [t_oab])
        aT, t_aT = transpose_bf(oab, t_oab, 4)
        pa = proj(aT, t_aT, wa, t_wa, 4)
        mg, t_mg = mg_r.next()
        for c in range(2):
            cs = slice(c * 512, (c + 1) * 512)
            kb.tt("dve", mg[:, cs], pa[c][0], ga[:, c * 512:(c + 1) * 512], ALU.mult, [pa[c][1], t_ga], [t_mg])
        yield
        bT, t_bT = transpose_bf(ob, t_ob, 8)
        pb_ = proj(bT, t_bT, wb, t_wb, 8)
        mgb, t_mgb = mgb_r.next()
        for c in range(2):
            cs = slice(c * 512, (c + 1) * 512)
            kb.tt("dve", mgb[:, cs], pb_[c][0], ga[:, 1024 + c * 512:1024 + (c + 1) * 512], ALU.mult, [pb_[c][1], t_ga], [t_mgb])
        kb.tt("pool", mgb[:], mg[:], mgb[:], ALU.add, [t_mg, t_mgb], [t_mgb])
        yield
        mT, t_mT = transpose_bf(mgb, t_mgb, 8)
        po = proj(mT, t_mT, wo, t_wo, 8)
        for c in range(2):
            cs = slice(c * 512, (c + 1) * 512)
            kb.tt("dve", ya[:, cs], po[c][0], ya[:, cs], ALU.add, [po[c][1], t_ya], [t_ya])
        yield
        hf, t_hf = hf_r.next()
        rmsnorm_to(ya[:], t_ya, gffn, t_gffn, hf[:], t_hf)
        hbf, t_hbf = hbf_r.next()
        kb.cp("pool", hbf[:], hf[:], [t_hf], [t_hbf])
        hT2, t_hT2 = h2T[ti]
        ps, t_ps = psTr.next()
        psv = bfv(ps).rearrange("p (k c) -> p k c", k=8)
        for k in range(8):
            kb.tr(psv[:, k, :], hbf[:, k * 128:(k + 1) * 128], ident_bf[:], [t_hbf, t_identbf], [t_ps])
        kb.cp("act", hT2[:], psv, [t_ps], [t_hT2])
        yield
        hfT, t_hfT = hfT_r.next()
        for hv in range(2):
            ps, t_ps = psTr.next()
            psv4 = ps.rearrange("p (k c) -> p k c", k=4)
            for k in range(4):
                kk = hv * 4 + k
                kb.tr(psv4[:, k, :], hf[:, kk * 128:(kk + 1) * 128], ident[:], [t_hf, t_ident], [t_ps])
            kb.cp("act" if hv else "dve", hfT[:, hv * 4:hv * 4 + 4, :], psv4, [t_ps], [t_hfT])
        ps, t_ps = psD.next()
        for k in range(8):
            kb.mm(ps[:, 0:20], hfT[:, k, :], wr[:, k, :], [t_hfT, t_wr], [t_ps], start=(k == 0), stop=(k == 7))
        lg, t_lg = lg_r.next()
        kb.tt("dve", lg[:], ps[:, 0:20], br[:], ALU.add, [t_ps, t_br], [t_lg])
        cm, t_cm = comb[ti]

        def sm(n=16):
            a, t_a = sm_r.next()
            return a[:, 0:n], t_a
        gmax, t_gmax = sm(1)
        kb.red("dve", gmax, lg[:, 0:4], [t_lg], [t_gmax], op=ALU.max)
        ngmax, t_ngmax = sm(1)
        kb.ts("dve", ngmax, gmax, -1.0, ALU.mult, [t_gmax], [t_ngmax])
        gex, t_gex = sm(4)
        gsum, t_gsum = sm(1)
        kb.memset("pool", gsum, 0.0, [t_gsum])
        kb.act(gex, lg[:, 0:4], AF.Exp, [t_lg, t_ngmax, t_gsum], [t_gex, t_gsum], bias=ngmax, accum_out=gsum)
        pg, t_pg = sm(1)
        kb.recip(pg, gsum, [t_gsum], [t_pg])
        goh, t_goh = sm(4)
        kb.ts("dve", goh, lg[:, 0:4], gmax, ALU.is_ge, [t_lg, t_gmax], [t_goh])
        el, t_el = sm(16)
        kb.tt("dve", el.rearrange("p (g e) -> p g e", e=4), lg[:, 4:20].rearrange("p (g e) -> p g e", e=4),
              goh.unsqueeze(2).to_broadcast([128, 4, 4]), ALU.mult, [t_lg, t_goh], [t_el])
        sel, t_sel = sm(4)
        kb.red("dve", sel, el.rearrange("p (g e) -> p e g", e=4), [t_el], [t_sel])
        m1, t_m1 = sm(1)
        kb.red("dve", m1, sel, [t_sel], [t_m1], op=ALU.max)
        oh1, t_oh1 = sm(4)
        kb.ts("dve", oh1, sel, m1, ALU.is_ge, [t_sel, t_m1], [t_oh1])
        sel2, t_sel2 = sm(4)
        kb.stt("dve", sel2, oh1, -1e30, sel, ALU.mult, ALU.add, [t_oh1, t_sel], [t_sel2])
        m2, t_m2 = sm(1)
        kb.red("dve", m2, sel2, [t_sel2], [t_m2], op=ALU.max)
        oh2, t_oh2 = sm(4)
        kb.ts("dve", oh2, sel2, m2, ALU.is_ge, [t_sel2, t_m2], [t_oh2])
        dd, t_dd = sm(1)
        kb.tt("dve", dd, m2, m1, ALU.subtract, [t_m2, t_m1], [t_dd])
        kb.act(dd, dd, AF.Exp, [t_dd], [t_dd])
        kb.ts("dve", dd, dd, 1.0, ALU.add, [t_dd], [t_dd])
        w1, t_w1 = sm(1)
        kb.recip(w1, dd, [t_dd], [t_w1])
        w2, t_w2 = sm(1)
        kb.ts("dve", w2, w1, -1.0, ALU.mult, [t_w1], [t_w2], s2=1.0, op1=ALU.add)
        kb.tt("dve", w1, w1, pg, ALU.mult, [t_w1, t_pg], [t_w1])
        kb.tt("dve", w2, w2, pg, ALU.mult, [t_w2, t_pg], [t_w2])
        wig, t_wig = sm(4)
        kb.ts("dve", wig, oh1, w1, ALU.mult, [t_oh1, t_w1], [t_wig])
        kb.stt("dve", wig, oh2, w2, wig, ALU.mult, ALU.add, [t_oh2, t_w2, t_wig], [t_wig])
        for g in range(4):
            kb.ts("dve", cm[:, g * 4:(g + 1) * 4], wig, goh[:, g:g + 1], ALU.mult, [t_wig, t_goh], [t_cm])
        yield

    def f_tile(c0, ti):
        t = c0 + ti
        rows = slice(t * 128, (t + 1) * 128)
        ya, t_ya = yacc[ti]
        hbf, t_hbf = hbf_r.next()
        rmsnorm_to(ya[:], t_ya, gple, t_gple, hbf[:], t_hbf)
        h3T, t_h3T = transpose_bf(hbf, t_hbf, 8)
        pgate = proj(h3T, t_h3T, wpg, t_wpg, 8)
        sg, t_sg = sg_r.next()
        for c in range(2):
            cs = slice(c * 512, (c + 1) * 512)
            kb.act(sg[:, cs], pgate[c][0], AF.Sigmoid, [pgate[c][1]], [t_sg])
        pin, t_pin = pin_r.next()
        kb.dma("sp", pin[:], p_in[rows, :], [], [t_pin])
        pbf, t_pbf = pbf_r.next()
        kb.cp("pool", pbf[:], pin[:], [t_pin], [t_pbf])
        yield
        pT, t_pT = transpose_bf(pbf, t_pbf, 2)
        pple = proj(pT, t_pT, wpl, t_wpl, 2)
        for c in range(2):
            cs = slice(c * 512, (c + 1) * 512)
            kb.tt("dve", sg[:, cs], sg[:, cs], pple[c][0], ALU.mult, [t_sg, pple[c][1]], [t_sg])
        kb.tt("pool", ya[:], sg[:], ya[:], ALU.add, [t_sg, t_ya], [t_ya])
        final_ops.append(kb.dma("sp", out[rows, :], ya[:], [t_ya], []))
        yield

    def run_gens(gs):
        gs = list(gs)
        while gs:
            for g_ in list(gs):
                try:
                    next(g_)
                except StopIteration:
                    gs.remove(g_)


    for ti in range(TC):
        run_gens([d_tile(0, ti)])
    for c0 in range(0, NT, TC):
        def load_gu(e_):
            wgu, t_wgu_sb = wgu_r.next()
            kb.dma("sp", wgu[:, 0:4, :], wgu_s[e_].rearrange("(k p) c -> p k c", p=128)[:, 0:4, :], [t_wgu[e_]], [t_wgu_sb])
            kb.dma("gq", wgu[:, 4:8, :], wgu_s[e_].rearrange("(k p) c -> p k c", p=128)[:, 4:8, :], [t_wgu[e_]], [t_wgu_sb])
            return wgu, t_wgu_sb

        def load_dn(e_):
            wdn, t_wdn = wd_r.next()
            kb.dma("sp", wdn[:], wd_s[e_].rearrange("(k p) c -> p k c", p=128), [t_wd[e_]], [t_wdn])
            return wdn, t_wdn

        Wgu = {0: load_gu(0)}
        Wdn = {0: load_dn(0)}

        def exp_item(e_, ti):
            ya, t_ya = yacc[ti]
            hT2, t_hT2 = h2T[ti]
            cm, t_cm = comb[ti]
            d = {}

            def e0():
                if ti == 0 and e_ + 1 < 16:
                    Wgu[e_ + 1] = load_gu(e_ + 1)
                wgu, t_wgu_sb = Wgu[e_]
                ps, t_ps = psM.next()
                for k in range(8):
                    kb.mm(ps, hT2[:, k, :], wgu[:, k, :], [t_hT2, t_wgu_sb], [t_ps], start=(k == 0), stop=(k == 7))
                d.update(ps=ps, t_ps=t_ps)

            def e1():
                ps, t_ps = d["ps"], d["t_ps"]
                sil, t_sil = sil_r.next()
                kb.act(sil[:], ps[:, 0:256], AF.Silu, [t_ps], [t_sil])
                ab, t_ab = act_r.next()
                kb.stt("dve", ab[:], ps[:, 256:512], cm[:, e_:e_ + 1], sil[:], ALU.mult, ALU.mult, [t_ps, t_cm, t_sil], [t_ab])
                d.update(ab=ab, t_ab=t_ab)

            def e2():
                pst_, t_pst_ = psTr.next()
                pstv = bfv(pst_).rearrange("p (k c) -> p k c", k=8)
                for k in range(2):
                    kb.tr(pstv[:, k, :], d["ab"][:, k * 128:(k + 1) * 128], ident_bf[:], [d["t_ab"], t_identbf], [t_pst_])
                d.update(pstv=pstv, t_pst=t_pst_)

            def e3():
                aT, t_aT = actT_r.next()
                kb.cp("act", aT[:], d["pstv"][:, 0:2, :], [d["t_pst"]], [t_aT])
                d.update(aT=aT, t_aT=t_aT)

            def e4():
                if ti == 0 and e_ + 1 < 16:
                    Wdn[e_ + 1] = load_dn(e_ + 1)
                wdn, t_wdn = Wdn[e_]
                pds = []
                for c in range(2):
                    pd, t_pd = psD.next()
                    for k in range(2):
                        kb.mm(pd, d["aT"][:, k, :], wdn[:, k, c * 512:(c + 1) * 512], [d["t_aT"], t_wdn], [t_pd], start=(k == 0), stop=(k == 1))
                    pds.append((pd, t_pd))
                d.update(pds=pds)

            def e5():
                for c in range(2):
                    pd, t_pd = d["pds"][c]
                    cs = slice(c * 512, (c + 1) * 512)
                    kb.tt("dve", ya[:, cs], ya[:, cs], pd, ALU.add, [t_ya, t_pd], [t_ya])

            return [e0, e1, e2, e3, e4, e5]

        pipeline([exp_item(e_, ti) for e_ in range(16) for ti in range(TC)])
        for ti in range(TC + 1):
            gs = []
            if ti < TC:
                gs.append(f_tile(c0, ti))
            if ti >= 1 and c0 + TC < NT:
                gs.append(d_tile(c0 + TC, ti - 1))
            run_gens(gs)

    print("sbuf end D", kb.off, "ops", {k: len(v) for k, v in P.ops.items()})
    P.emit(nc, final_waits=final_ops)
    return nc


_CONSTS = None


def _consts():
    global _CONSTS
    if _CONSTS is None:
        _CONSTS = {
            "c_ident": np.eye(128, dtype=np.float32),
            "c_triu": np.triu(np.ones((128, 128), dtype=np.float32)),
            "c_tril": np.tril(np.ones((128, 128), dtype=np.float32)),
        }
    return _CONSTS


def make_in_map(inputs, b, S):
    f = lambda a: np.ascontiguousarray(np.asarray(a, dtype=np.float32))
    m = {
        "x": f(inputs["x"][b, :S]),
        "p": f(inputs["p"][0, b, :S]),
        "norm_mix": f(inputs["norm_mix"][0]),
        "w_in": f(inputs["w_in"][0]),
        "q_norm": f(inputs["q_norm"][0]),
        "k_norm": f(inputs["k_norm"][0]),
        "conv_w": f(np.asarray(inputs["conv_w"][0]).reshape(4, 24, 128).transpose(2, 1, 0)),
        "a_log": f(inputs["a_log"][0]),
        "dt_bias": f(inputs["dt_bias"][0]),
        "dn_out_norm": f(inputs["dn_out_norm"][0]),
        "w_branch_a": f(inputs["w_branch_a"][0]),
        "w_branch_b": f(inputs["w_branch_b"][0]),
        "w_out": f(inputs["w_out"][0]),
        "norm_ffn": f(inputs["norm_ffn"][0]),
        "w_router_group": f(inputs["w_router_group"][0]),
        "b_router_group": f(inputs["b_router_group"][0]),
        "w_router_expert": f(inputs["w_router_expert"][0]),
        "b_router_expert": f(inputs["b_router_expert"][0]),
        "w_expert_gate": f(np.asarray(inputs["w_expert_gate"][0]).reshape(16, 1024, 256)),
        "w_expert_up": f(np.asarray(inputs["w_expert_up"][0]).reshape(16, 1024, 256)),
        "w_expert_down": f(np.asarray(inputs["w_expert_down"][0]).reshape(16, 256, 1024)),
        "norm_ple": f(inputs["norm_ple"][0]),
        "w_ple": f(inputs["w_ple"][0]),
        "w_ple_gate": f(inputs["w_ple_gate"][0]),
    }
    m.update(_consts())
    return m


def kernel(**inputs):
    S = 8192
    nc = build(S)
    in_maps = [make_in_map(inputs, b, S) for b in range(8)]
    res = run_bass_kernel_spmd(nc, in_maps, core_ids=list(range(8)))
    return np.stack([np.asarray(r["out"], dtype=np.float32) for r in res.results], axis=0)
```

```python
import numpy as np
import concourse.bass as bass
import concourse.mybir as mybir
from concourse.bass_utils import run_bass_kernel_spmd

F32 = mybir.dt.float32
BF16 = mybir.dt.bfloat16
F32R = mybir.dt.float32r
ALU = mybir.AluOpType
AF = mybir.ActivationFunctionType
AX = mybir.AxisListType

ENGS = ("pe", "act", "dve", "pool")
DMAQ = ("sp", "gq")
NDSEM = 24
EPS = 1e-6
SB_BASE = 16512
SB_TOP = 229344


class T:
    __slots__ = ("name", "w", "rs", "banks")

    def __init__(self, name="", banks=()):
        self.name = name
        self.w = None
        self.rs = []
        self.banks = banks


class Bank:
    __slots__ = ("last",)

    def __init__(self):
        self.last = None


class Op:
    __slots__ = ("eng", "fn", "idx", "deps", "sig", "signo", "dslot", "dval", "clock", "gorder")


class Prog:
    def __init__(self):
        self.ops = {e: [] for e in ENGS + DMAQ}
        self.clock = {e: {} for e in ENGS + DMAQ}
        self.ndma = {q: 0 for q in DMAQ}
        self.g = 0
        self.pending = {e: [] for e in ENGS + DMAQ}

    def barrier(self):
        lasts = []
        for e in ENGS:
            if self.ops[e]:
                lasts.append(self.ops[e][-1])
        for q in DMAQ:
            lasts.extend(self.ops[q][-NDSEM:])
        self.pending["sp"] = list(lasts)
        op = self.add("sp", self.bar_fn)
        for e in ENGS + ("gq",):
            self.pending[e] = [op]

    def add(self, eng, fn, reads=(), writes=()):
        op = Op()
        op.eng = eng
        op.fn = fn
        op.idx = len(self.ops[eng])
        op.sig = False
        op.signo = None
        op.gorder = self.g
        self.g += 1
        deps = []
        for t in reads:
            if t.w is not None:
                deps.append((t.w, True))
        for t in writes:
            if t.w is not None:
                deps.append((t.w, False))
            for r in t.rs:
                deps.append((r, False))
        bks = []
        for t in tuple(reads) + tuple(writes):
            for b in t.banks:
                if b not in bks:
                    bks.append(b)
        for b in bks:
            if b.last is not None and b.last.eng != eng:
                deps.append((b.last, True))
        if self.pending[eng]:
            for d in self.pending[eng]:
                if d.eng != eng or eng in DMAQ:
                    deps.append((d, True))
            self.pending[eng] = []
        clk = self.clock[eng]
        need = {}
        for d, raw in deps:
            if d.eng == eng and eng not in DMAQ:
                if eng == "pe":
                    continue
            if d.eng in DMAQ:
                key = (d.eng, d.idx)
                if clk.get(key, False):
                    continue
                need[key] = d
            else:
                if clk.get(d.eng, -1) >= d.idx:
                    continue
                cur = need.get(d.eng)
                if cur is None or cur.idx < d.idx:
                    need[d.eng] = d
        op.deps = list(need.values())
        for d in op.deps:
            d.sig = True
            for k, v in d.clock.items():
                if isinstance(k, tuple):
                    clk[k] = True
                elif clk.get(k, -1) < v:
                    clk[k] = v
            if d.eng in DMAQ:
                clk[(d.eng, d.idx)] = True
            elif clk.get(d.eng, -1) < d.idx:
                clk[d.eng] = d.idx
        if eng in DMAQ:
            op.dslot = self.ndma[eng] % NDSEM
            op.dval = 16 * (self.ndma[eng] // NDSEM + 1)
            self.ndma[eng] += 1
            prev_i = op.idx - NDSEM
            if prev_i >= 0:
                pd = self.ops[eng][prev_i]
                if not clk.get((eng, prev_i), False):
                    op.deps.append(pd)
                    clk[(eng, prev_i)] = True
            op.sig = True
        if len(clk) > 400:
            for k in [k for k in clk if isinstance(k, tuple) and k[1] < self.ndma[k[0]] - 4 * NDSEM]:
                del clk[k]
        op.clock = dict(clk)
        if eng not in DMAQ:
            op.clock[eng] = op.idx
        self.ops[eng].append(op)
        for b in bks:
            b.last = op
        for t in reads:
            t.rs.append(op)
        for t in writes:
            t.w = op
            t.rs = []
        return op

    def emit(self, nc, final_waits=()):
        from contextlib import ExitStack
        with ExitStack() as es:
            sems = {e: es.enter_context(nc.semaphore("s_" + e)) for e in ENGS}
            dsems = {q: [es.enter_context(nc.semaphore(f"d_{q}{i}")) for i in range(NDSEM)] for q in DMAQ}
            for d in final_waits:
                d.sig = True
            for e in ENGS:
                n = 0
                for op in self.ops[e]:
                    if op.sig:
                        n += 1
                        op.signo = n
            block = es.enter_context(nc.Block())

            def waits(engobj, op):
                for d in op.deps:
                    if d.eng in DMAQ:
                        engobj.wait_ge(dsems[d.eng][d.dslot], d.dval)
                    else:
                        engobj.wait_ge(sems[d.eng], d.signo)

            def run(oplist, engobj, extra=None):
                for op in oplist:
                    waits(engobj, op)
                    ins = op.fn(engobj)
                    if op.sig:
                        if op.eng in DMAQ:
                            ins.then_inc(dsems[op.eng][op.dslot], 16)
                        else:
                            ins.then_inc(sems[op.eng], 1)
                if extra:
                    extra(engobj)

            def fin(engobj):
                for d in final_waits:
                    if d.eng in DMAQ:
                        engobj.wait_ge(dsems[d.eng][d.dslot], d.dval)
                    else:
                        engobj.wait_ge(sems[d.eng], d.signo)

            @block.tensor
            def _(e):
                run(self.ops["pe"], e)

            @block.scalar
            def _(e):
                run(self.ops["act"], e)

            @block.vector
            def _(e):
                run(self.ops["dve"], e)

            @block.gpsimd
            def _(e):
                merged = sorted(self.ops["pool"] + self.ops["gq"], key=lambda o: o.gorder)
                run(merged, e)

            @block.sync
            def _(e):
                run(self.ops["sp"], e, fin)


class KB:
    def __init__(self, nc):
        self.nc = nc
        self.P = Prog()
        self.off = SB_BASE
        self.n = 0
        self.banks = [nc.alloc_psum_tensor(f"bank{i}", [128, 512], F32) for i in range(8)]
        self.bk = [Bank() for _ in range(8)]

    def pst(self, *bank_ids):
        return T("ps", banks=tuple(self.bk[b] for b in bank_ids))

    def sb(self, shape, dt, name="t"):
        sz = int(np.prod(shape[1:])) * (2 if dt == BF16 else 4)
        sz = (sz + 31) // 32 * 32
        assert self.off + sz <= SB_TOP, f"sbuf overflow {name} {self.off} {sz}"
        self.n += 1
        t = self.nc.alloc_sbuf_tensor_at(f"{name}_{self.n}", list(shape), dt, offset=self.off)
        self.off += sz
        return t

    def ring(self, n, shape, dt, name="r"):
        return Ring([(self.sb(shape, dt, name), T(name)) for _ in range(n)])

    def psring(self, banks, nf32, name="ps"):
        per = 512 // nf32
        slots = []
        for i in range(per):
            for b in banks:
                slots.append((self.banks[b][:, i * nf32:(i + 1) * nf32], self.pst(b)))
        return Ring(slots)

    def mm(self, out, lhsT, rhs, r, w, start=True, stop=True):
        return self.P.add("pe", lambda e: e.matmul(out, lhsT=lhsT, rhs=rhs, start=start, stop=stop), reads=r, writes=w)

    def tr(self, out, in_, ident, r, w):
        return self.P.add("pe", lambda e: e.transpose(out=out, in_=in_, identity=ident), reads=r, writes=w)

    def act(self, out, in_, func, r, w, bias=None, scale=None, accum_out=None, eng="act"):
        kw = {}
        if bias is not None:
            kw["bias"] = bias
        if scale is not None:
            kw["scale"] = scale
        if accum_out is not None:
            kw["accum_out"] = accum_out
        return self.P.add("act", lambda e: e.activation(out=out, in_=in_, func=func, **kw), reads=r, writes=w)

    def cp(self, eng, out, in_, r, w):
        if eng == "act":
            return self.P.add("act", lambda e: e.copy(out=out, in_=in_), reads=r, writes=w)
        return self.P.add(eng, lambda e: e.tensor_copy(out=out, in_=in_), reads=r, writes=w)

    def tt(self, eng, out, in0, in1, op, r, w):
        return self.P.add(eng, lambda e: e.tensor_tensor(out=out, in0=in0, in1=in1, op=op), reads=r, writes=w)

    def ts(self, eng, out, in0, s1, op0, r, w, s2=None, op1=None):
        if op1 is None:
            return self.P.add(eng, lambda e: e.tensor_scalar(out=out, in0=in0, scalar1=s1, scalar2=None, op0=op0), reads=r, writes=w)
        return self.P.add(eng, lambda e: e.tensor_scalar(out=out, in0=in0, scalar1=s1, scalar2=s2, op0=op0, op1=op1), reads=r, writes=w)

    def stt(self, eng, out, in0, scalar, in1, op0, op1, r, w):
        return self.P.add(eng, lambda e: e.scalar_tensor_tensor(out=out, in0=in0, scalar=scalar, in1=in1, op0=op0, op1=op1), reads=r, writes=w)

    def red(self, eng, out, in_, r, w, op=ALU.add):
        return self.P.add(eng, lambda e: e.tensor_reduce(out=out, in_=in_, axis=AX.X, op=op), reads=r, writes=w)

    def recip(self, out, in_, r, w):
        return self.P.add("dve", lambda e: e.reciprocal(out=out, in_=in_), reads=r, writes=w)

    def memset(self, eng, ap, val, w):
        return self.P.add(eng, lambda e: e.memset(ap, val), writes=w)

    def dma(self, q, out, in_, r, w):
        return self.P.add(q, lambda e: e.dma_start(out=out, in_=in_), reads=r, writes=w)


class Ring:
    def __init__(self, slots):
        self.slots = slots
        self.i = 0

    def next(self):
        s = self.slots[self.i % len(self.slots)]
        self.i += 1
        return s


def pipeline(units):
    n = len(units)
    nst = max(len(u) for u in units)
    for i in range(n + nst - 1):
        for st in range(nst - 1, -1, -1):
            u = i - st
            if 0 <= u < n and st < len(units[u]) and units[u][st] is not None:
                units[u][st]()


def bfv(ap):
    return ap.bitcast(BF16)


def build(S, debug=False, stop=None, skip_pre=False):
    nc = bass.Bass("TRN2", target_bir_lowering=False)
    NT = S // 128
    NSP = S // 512
    kb = KB(nc)
    P = kb.P

    def din(name, shape, dt=F32):
        return nc.dram_tensor(name, list(shape), dt, kind="ExternalInput").ap()

    def dscr(name, shape, dt):
        return nc.dram_tensor(name, list(shape), dt, kind="ExternalOutput" if debug else "Internal").ap()

    x = din("x", [S, 1024])
    p_in = din("p", [S, 256])
    norm_mix = din("norm_mix", [1024])
    w_in = din("w_in", [1024, 10768])
    q_norm = din("q_norm", [64])
    k_norm = din("k_norm", [64])
    conv_w = din("conv_w", [128, 24, 4])
    a_log = din("a_log", [8])
    dt_bias = din("dt_bias", [8])
    dn_out_norm = din("dn_out_norm", [128])
    w_branch_a = din("w_branch_a", [512, 1024])
    w_branch_b = din("w_branch_b", [1024, 1024])
    w_out = din("w_out", [1024, 1024])
    norm_ffn = din("norm_ffn", [1024])
    w_rg = din("w_router_group", [1024, 4])
    b_rg = din("b_router_group", [4])
    w_re = din("w_router_expert", [1024, 16])
    b_re = din("b_router_expert", [16])
    w_eg = din("w_expert_gate", [16, 1024, 256])
    w_eu = din("w_expert_up", [16, 1024, 256])
    w_ed = din("w_expert_down", [16, 256, 1024])
    norm_ple = din("norm_ple", [1024])
    w_ple = din("w_ple", [256, 1024])
    w_pg = din("w_ple_gate", [1024, 1024])
    c_ident = din("c_ident", [128, 128])
    c_triu = din("c_triu", [128, 128])
    c_tril = din("c_tril", [128, 128])
    out = nc.dram_tensor("out", [S, 1024], F32, kind="ExternalOutput").ap()

    qkva_s = dscr("qkva_s", [9, S, 512], BF16)
    qkvb_s = dscr("qkvb_s", [24, 128, S], BF16)
    z_s = dscr("z_s", [S, 1024], BF16)
    bg_s = dscr("bg_s", [S, 16], F32)
    gates_s = dscr("gates_s", [S, 2048], BF16)
    ua_s = dscr("ua_s", [3, S, 520], F32)
    ob_s = dscr("ob_s", [S, 1024], BF16)
    wgu_s = nc.dram_tensor("wgu_s", [16, 1024, 512], BF16, kind="Internal").ap()
    wd_s = nc.dram_tensor("wd_s", [16, 256, 1024], BF16, kind="Internal").ap()

    ident = kb.sb([128, 128], F32, "ident"); t_ident = T()
    ident_bf = kb.sb([128, 128], BF16, "identbf"); t_identbf = T()
    triu = kb.sb([128, 128], F32, "triu"); t_triu = T()
    tril_bf = kb.sb([128, 128], BF16, "trilbf"); t_trilbf = T()
    triu_bf = kb.sb([128, 128], BF16, "triubf"); t_triubf = T()
    tril = kb.sb([128, 128], F32, "tril"); t_tril = T()
    mbm = kb.sb([128, 128], F32, "mbm"); t_mbm = T()
    strict = kb.sb([128, 128], F32, "strict"); t_strict = T()
    ones_bf = kb.sb([128, 128], BF16, "ones"); t_ones = T()
    epsc = kb.sb([128, 1], F32, "eps"); t_eps = T()
    eps128 = kb.sb([128, 1], F32, "eps128"); t_eps128 = T()
    kb.dma("sp", ident[:], c_ident[:, :], [], [t_ident])
    kb.dma("sp", triu[:], c_triu[:, :], [], [t_triu])
    kb.dma("sp", tril[:], c_tril[:, :], [], [t_tril])
    kb.cp("dve", ident_bf[:], ident[:], [t_ident], [t_identbf])
    kb.cp("dve", triu_bf[:], triu[:], [t_triu], [t_triubf])
    kb.cp("dve", tril_bf[:], tril[:], [t_tril], [t_trilbf])
    kb.ts("dve", mbm[:], triu[:], -1.0, ALU.add, [t_triu], [t_mbm], s2=1e9, op1=ALU.mult)
    kb.tt("dve", strict[:], triu[:], ident[:], ALU.subtract, [t_triu, t_ident], [t_strict])
    kb.memset("pool", ones_bf[:], 1.0, [t_ones])
    kb.memset("pool", epsc[:], EPS, [t_eps])
    kb.memset("pool", eps128[:], 128.0 * EPS, [t_eps128])

    t_wgu = [T() for _ in range(16)]
    t_wd = [T() for _ in range(16)]
    for e_ in range(0 if not skip_pre else 16, 16):
        kb.dma("gq", wgu_s[e_, :, 0:256], w_eg[e_], [], [t_wgu[e_]])
        kb.dma("gq", wgu_s[e_, :, 256:512], w_eu[e_], [], [t_wgu[e_]])
        kb.dma("gq", wd_s[e_], w_ed[e_], [], [t_wd[e_]])

    bar_a = kb.sb([128, 8], F32, "bar_a")
    bar_b = kb.sb([128, 8], F32, "bar_b")
    kb.memset("pool", bar_a[:], 0.0, [])
    P.bar_fn = lambda e: e.dma_start(out=bar_b[:], in_=bar_a[:])
    persist_off = kb.off

    def finish_early():
        fw = []
        for q in DMAQ:
            fw.extend(P.ops[q][-NDSEM:])
        for e in ENGS:
            if P.ops[e]:
                fw.append(P.ops[e][-1])
        P.emit(nc, final_waits=fw)
        return nc
    gain_mix = kb.sb([128, 1024], F32, "gmix"); t_gmix = T()
    kb.dma("sp", gain_mix[:], norm_mix.partition_broadcast(128), [], [t_gmix])
    hT = kb.sb([128, 8, S], BF16, "hT")
    t_hT = [T() for _ in range(NT)]
    a_off = kb.off
    xr = kb.ring(4, [128, 1024], F32, "xt")
    junk = kb.ring(2, [128, 1024], BF16, "junk")
    hb = kb.ring(3, [128, 1024], BF16, "hb")
    ssr = kb.ring(6, [128, 1], F32, "ss")
    psA0 = kb.psring([0, 1], 512, "psA0")

    def rms_rstd(ss_ap, t_ss, nfeat_scale_done=True):
        kb.act(ss_ap, ss_ap, AF.Sqrt, [t_ss, t_eps], [t_ss], bias=epsc[:, 0:1])
        kb.recip(ss_ap, ss_ap, [t_ss], [t_ss])

    def a0_unit(t):
        d = {}

        def s0():
            xt, t_xt = xr.next()
            kb.dma("sp", xt[:], x[t * 128:(t + 1) * 128, :], [], [t_xt])
            d.update(xt=xt, t_xt=t_xt)

        def s1():
            xt, t_xt = d["xt"], d["t_xt"]
            jk, t_jk = junk.next()
            ss, t_ss = ssr.next()
            kb.memset("pool", ss[:], 0.0, [t_ss])
            kb.act(jk[:], xt[:], AF.Square, [t_xt, t_ss], [t_jk, t_ss], scale=1.0 / 32.0, accum_out=ss[:])
            kb.act(ss[:], ss[:], AF.Sqrt, [t_ss, t_eps], [t_ss], bias=epsc[:, 0:1])
            d.update(ss=ss, t_ss=t_ss)

        def s2():
            xt, t_xt, ss, t_ss = d["xt"], d["t_xt"], d["ss"], d["t_ss"]
            kb.recip(ss[:], ss[:], [t_ss], [t_ss])
            h_, t_h = hb.next()
            kb.stt("dve", h_[:], xt[:], ss[:, 0:1], gain_mix[:], ALU.mult, ALU.mult, [t_xt, t_ss, t_gmix], [t_h])
            d.update(h=h_, t_h=t_h)

        def s3():
            ps, t_ps = psA0.next()
            psv = bfv(ps).rearrange("p (k c) -> p k c", k=8)
            for k in range(8):
                kb.tr(psv[:, k, :], d["h"][:, k * 128:(k + 1) * 128], ident_bf[:], [d["t_h"], t_identbf], [t_ps])
            d.update(psv=psv, t_ps=t_ps)

        def s4():
            kb.cp("act" if t % 2 else "dve", hT[:, :, t * 128:(t + 1) * 128], d["psv"], [d["t_ps"]], [t_hT[t]])

        return [s0, s1, s2, s3, s4]

    pipeline([a0_unit(t) for t in range(NT)])

    if stop == "A0":
        return finish_early()
    P.barrier()
    kb.off = a_off
    qg = kb.sb([128, 64], F32, "qg"); t_qg = T()
    kg = kb.sb([128, 64], F32, "kg"); t_kg = T()
    kb.dma("sp", qg[:], q_norm.partition_broadcast(128), [], [t_qg])
    kb.dma("sp", kg[:], k_norm.partition_broadcast(128), [], [t_kg])
    kb.ts("dve", qg[:], qg[:], 0.125, ALU.mult, [t_qg], [t_qg])
    convw = kb.sb([128, 24, 4], F32, "convw"); t_convw = T()
    kb.dma("sp", convw[:], conv_w[:, :, :], [], [t_convw])
    dtb = kb.sb([128, 8], F32, "dtb"); t_dtb = T()
    negA = kb.sb([128, 8], F32, "negA"); t_negA = T()
    kb.dma("sp", dtb[:], dt_bias.partition_broadcast(128), [], [t_dtb])
    kb.dma("sp", negA[:], a_log.partition_broadcast(128), [], [t_negA])
    kb.act(negA[:], negA[:], AF.Exp, [t_negA], [t_negA])
    kb.ts("dve", negA[:], negA[:], -1.0, ALU.mult, [t_negA], [t_negA])

    wring = kb.ring(2, [128, 8, 512], BF16, "wg")
    wsm = kb.sb([128, 8, 16], BF16, "wsm"); t_wsm = T()
    psA = kb.psring([0, 1, 2, 3, 4, 5], 512, "psA")
    psS = kb.psring([6, 7], 512, "psS")
    f32r = kb.ring(3, [128, 512], F32, "f32r")
    f32r2 = kb.ring(3, [128, 512], F32, "f32r2")
    bfr = kb.ring(4, [128, 512], BF16, "bfr")
    s8r = kb.ring(4, [128, 8], F32, "s8")
    rawr = kb.ring(3, [128, 515], F32, "raw")
    carry = [(kb.sb([128, 3], F32, "carry"), T()) for _ in range(4)]
    yr = kb.ring(6, [128, 512], F32, "y")
    sqr = kb.ring(3, [128, 512], BF16, "sqb")
    rnr = kb.ring(3, [128, 512], F32, "rn")
    bgr = kb.ring(3, [128, 16], F32, "bg")
    w_in_v = w_in.rearrange("(k p) c -> p k c", p=128)

    def col0(cg):
        if cg < 17:
            return cg * 512
        if cg == 17:
            return 8704
        return 8720 + (cg - 18) * 512

    def load_W(cg):
        c0 = col0(cg)
        if cg == 17:
            kb.dma("gq", wsm[:], w_in_v[:, :, c0:c0 + 16], [], [t_wsm])
            return wsm, t_wsm
        W, t_W = wring.next()
        kb.dma("gq", W[:, 0:4, :], w_in_v[:, 0:4, c0:c0 + 512], [], [t_W])
        kb.dma("gq", W[:, 4:8, :], w_in_v[:, 4:8, c0:c0 + 512], [], [t_W])
        return W, t_W

    def conv_unit(cg, s, cb, W, t_W):
        cbg = (cg - 9) * 4 + cb
        which = cbg // 8
        d = {}

        def sa():
            ps, t_ps = psA.next()
            for k in range(8):
                kb.mm(ps, W[:, k, cb * 128:(cb + 1) * 128], hT[:, k, s * 512:(s + 1) * 512],
                      [t_W] + t_hT[4 * s:4 * s + 4], [t_ps], start=(k == 0), stop=(k == 7))
            raw, t_raw = rawr.next()
            cy, t_cy = carry[cb]
            if s == 0:
                kb.memset("pool", raw[:, 0:3], 0.0, [t_raw])
            else:
                kb.cp("pool", raw[:, 0:3], cy[:], [t_cy], [t_raw])
            kb.cp("act", raw[:, 3:515], ps, [t_ps], [t_raw])
            kb.cp("pool", cy[:], raw[:, 512:515], [t_raw], [t_cy])
            y, t_y = yr.next()
            kb.act(y[:], ps, AF.Copy, [t_ps, t_convw], [t_y], scale=convw[:, cbg, 3:4])
            d.update(raw=raw, t_raw=t_raw, y=y, t_y=t_y)

        def sb_():
            raw, t_raw = d["raw"], d["t_raw"]
            y, t_y = d["y"], d["t_y"]
            for j in range(3):
                kb.stt("dve", y[:], raw[:, j:j + 512], convw[:, cbg, j:j + 1], y[:], ALU.mult, ALU.add,
                       [t_raw, t_convw, t_y], [t_y])

        def sc():
            y, t_y = d["y"], d["t_y"]
            if which == 2:
                ob, t_ob = bfr.next()
                kb.act(ob[:], y[:], AF.Silu, [t_y], [t_ob])
                kb.dma("sp", qkvb_s[cbg, :, s * 512:(s + 1) * 512], ob[:], [t_ob], [])
            else:
                kb.act(y[:], y[:], AF.Silu, [t_y], [t_y])
                sq, t_sq = sqr.next()
                kb.tt("pool", sq[:], y[:], y[:], ALU.mult, [t_y], [t_sq])
                d.update(sq=sq, t_sq=t_sq)

        def sd():
            if which == 2:
                return
            pss, t_pss = psS.next()
            kb.mm(pss, ones_bf[:], d["sq"][:], [t_ones, d["t_sq"]], [t_pss])
            rn, t_rn = rnr.next()
            if which == 0:
                kb.act(rn[:], pss, AF.Sqrt, [t_pss, t_eps128], [t_rn], bias=eps128[:, 0:1], scale=128.0)
            else:
                kb.act(rn[:], pss, AF.Sqrt, [t_pss, t_eps], [t_rn], bias=epsc[:, 0:1])
            d.update(rn=rn, t_rn=t_rn)

        def se():
            if which == 2:
                return
            rn, t_rn, y, t_y = d["rn"], d["t_rn"], d["y"], d["t_y"]
            kb.recip(rn[:], rn[:], [t_rn], [t_rn])
            ob, t_ob = bfr.next()
            kb.tt("pool", ob[:], y[:], rn[:], ALU.mult, [t_y, t_rn], [t_ob])
            kb.dma("sp", qkvb_s[cbg, :, s * 512:(s + 1) * 512], ob[:], [t_ob], [])

        return [sa, sb_, sc, sd, se]

    Wnext = load_W(0)
    for cg in range(22):
        c0 = col0(cg)
        W, t_W = Wnext
        if cg + 1 < 22:
            Wnext = load_W(cg + 1)
        if 9 <= cg <= 14:
            units = []
            for s in range(NSP):
                for cb in range(4):
                    units.append(conv_unit(cg, s, cb, W, t_W))
            pipeline(units)
        else:
            for t in range(NT):
                rows = slice(t * 128, (t + 1) * 128)
                ps, t_ps = psA.next()
                ncol = 16 if cg == 17 else 512
                pso = ps[:, 0:ncol]
                for k in range(8):
                    kb.mm(pso, hT[:, k, t * 128:(t + 1) * 128], W[:, k, :], [t_W, t_hT[t]], [t_ps],
                          start=(k == 0), stop=(k == 7))
                if cg < 6:
                    gain, t_gain = (qg, t_qg) if cg < 3 else (kg, t_kg)
                    sqf, t_sqf = f32r.next()
                    kb.act(sqf[:], ps, AF.Square, [t_ps], [t_sqf], scale=0.125)
                    s8, t_s8 = s8r.next()
                    kb.red("dve", s8[:], sqf[:].rearrange("p (h d) -> p h d", d=64), [t_sqf], [t_s8])
                    rms_rstd(s8[:], t_s8)
                    tmp, t_tmp = f32r2.next()
                    kb.tt("dve", tmp[:].rearrange("p (h d) -> p h d", d=64), ps.rearrange("p (h d) -> p h d", d=64),
                          s8[:, :].unsqueeze(2).to_broadcast([128, 8, 64]), ALU.mult, [t_ps, t_s8], [t_tmp])
                    ob, t_ob = bfr.next()
                    kb.tt("pool", ob[:].rearrange("p (h d) -> p h d", d=64), tmp[:].rearrange("p (h d) -> p h d", d=64),
                          gain[:, :].unsqueeze(1).to_broadcast([128, 8, 64]), ALU.mult, [t_tmp, t_gain], [t_ob])
                    kb.dma("sp", qkva_s[cg, rows, :], ob[:], [t_ob], [])
                elif cg < 9:
                    ob, t_ob = bfr.next()
                    kb.cp("act" if t % 2 else "dve", ob[:], ps, [t_ps], [t_ob])
                    kb.dma("sp", qkva_s[cg, rows, :], ob[:], [t_ob], [])
                elif cg in (15, 16):
                    ob, t_ob = bfr.next()
                    kb.act(ob[:], ps, AF.Silu, [t_ps], [t_ob])
                    kb.dma("sp", z_s[rows, (cg - 15) * 512:(cg - 14) * 512], ob[:], [t_ob], [])
                elif cg == 17:
                    bg, t_bg = bgr.next()
                    kb.act(bg[:, 0:8], ps[:, 0:8], AF.Sigmoid, [t_ps], [t_bg])
                    kb.tt("dve", bg[:, 8:16], ps[:, 8:16], dtb[:], ALU.add, [t_ps, t_dtb], [t_bg])
                    kb.act(bg[:, 8:16], bg[:, 8:16], AF.Exp, [t_bg], [t_bg])
                    kb.act(bg[:, 8:16], bg[:, 8:16], AF.Ln, [t_bg], [t_bg], bias=1.0)
                    kb.tt("dve", bg[:, 8:16], bg[:, 8:16], negA[:], ALU.mult, [t_bg, t_negA], [t_bg])
                    kb.dma("sp", bg_s[rows, :], bg[:], [t_bg], [])
                else:
                    ob, t_ob = bfr.next()
                    kb.act(ob[:], ps, AF.Sigmoid, [t_ps], [t_ob])
                    kb.dma("sp", gates_s[rows, (cg - 18) * 512:(cg - 17) * 512], ob[:], [t_ob], [])

    print("sbuf end A", kb.off)
    if stop == "A":
        return finish_early()
    P.barrier()
    kb.off = persist_off
    qr_ = kb.ring(3, [128, 512], BF16, "qb")
    kr_ = kb.ring(3, [128, 512], BF16, "kb")
    vst_r = kb.ring(3, [128, 512], BF16, "vst")
    vr_ = kb.ring(6, [128, 8, 65], BF16, "v1")
    for v1, t_v1 in vr_.slots:
        kb.memset("pool", v1[:, :, 64:65], 1.0, [t_v1])
    qTr = kb.ring(3, [128, 8, 128], BF16, "qT")
    for qz, t_qz in qTr.slots:
        kb.memset("pool", qz[:], 0.0, [t_qz])
    kTr = kb.ring(4, [128, 4, 128], BF16, "kT")
    er_ = kb.ring(4, [128, 4, 2, 128], BF16, "E")
    ur_ = kb.ring(3, [128, 8, 65], F32, "U")
    sc_slots = Ring([((kb.banks[0][:, :], kb.banks[1][:, :]), kb.pst(0, 1)), ((kb.banks[2][:, :], kb.banks[3][:, :]), kb.pst(2, 3))])
    pv_slots = Ring([(kb.banks[4][:, :], kb.pst(4)), (kb.banks[5][:, :], kb.pst(5))])
    psT = Ring([(kb.banks[6][:, :], kb.pst(6)), (kb.banks[7][:, :], kb.pst(7))])
    ATT = ((128, 1), (512, 4), (2048, 16))

    def attn_unit(g, dil, r_, nb, prev):
        qs = qkva_s[g].rearrange("(u r) c -> r u c", r=dil)
        ks = qkva_s[3 + g].rearrange("(u r) c -> r u c", r=dil)
        vs = qkva_s[6 + g].rearrange("(u r) c -> r u c", r=dil)
        us = ua_s[g].rearrange("(u r) c -> r u c", r=dil)
        ur = slice(nb * 128, (nb + 1) * 128)
        have_prev = nb > 0
        d = {}

        def s0():
            qb_, t_qb = qr_.next()
            kb_, t_kb = kr_.next()
            vst, t_vst = vst_r.next()
            kb.dma("sp", qb_[:], qs[r_, ur, :], [], [t_qb])
            kb.dma("sp", kb_[:], ks[r_, ur, :], [], [t_kb])
            kb.dma("sp", vst[:], vs[r_, ur, :], [], [t_vst])
            d.update(qb=qb_, t_qb=t_qb, kb=kb_, t_kb=t_kb, vst=vst, t_vst=t_vst)

        def s1():
            v1, t_v1 = vr_.next()
            kb.cp("dve", v1[:, :, 0:64], d["vst"][:].rearrange("p (h d) -> p h d", d=64), [d["t_vst"]], [t_v1])
            pt, t_pt = psT.next()
            ptv = bfv(pt).rearrange("p (a h c) -> p a h c", a=2, h=4)
            for hp in range(4):
                kb.tr(ptv[:, 0, hp, :], d["qb"][:, hp * 128:(hp + 1) * 128], ident_bf[:], [d["t_qb"], t_identbf], [t_pt])
                kb.tr(ptv[:, 1, hp, :], d["kb"][:, hp * 128:(hp + 1) * 128], ident_bf[:], [d["t_kb"], t_identbf], [t_pt])
            d.update(v1=v1, t_v1=t_v1, ptv=ptv, t_pt=t_pt)

        def s2():
            qT, t_qT = qTr.next()
            kT, t_kT = kTr.next()
            ptv, t_pt = d["ptv"], d["t_pt"]
            qTv = qT[:].rearrange("p (hp j) c -> p hp j c", j=2)
            kb.cp("dve", qTv[0:64, :, 0, :], ptv[0:64, 0], [t_pt], [t_qT])
            kb.cp("dve", qTv[64:128, :, 1, :], ptv[64:128, 0], [t_pt], [t_qT])
            kb.cp("act", kT[:], ptv[:, 1], [t_pt], [t_kT])
            d.update(qT=qT, t_qT=t_qT, kT=kT, t_kT=t_kT)

        def mk_half(half):
            hd = {}

            def h3():
                (b0, b1), t_sc = sc_slots.next()
                qT, t_qT, kT, t_kT = d["qT"], d["t_qT"], d["kT"], d["t_kT"]
                for hh in range(4):
                    h = half * 4 + hh
                    hp = h // 2
                    bank = b0 if hh < 2 else b1
                    base = (hh % 2) * 256
                    if have_prev:
                        kb.mm(bank[:, base:base + 128], prev["kT"][:, hp, :], qT[:, h, :],
                              [prev["t_kT"], t_qT], [t_sc])
                    kb.mm(bank[:, base + 128:base + 256], kT[:, hp, :], qT[:, h, :],
                          [t_kT, t_qT], [t_sc])
                hd.update(b0=b0, b1=b1, t_sc=t_sc)

            def h4():
                E, t_E = er_.next()
                t_sc = hd["t_sc"]
                for bi, bank in enumerate((hd["b0"], hd["b1"])):
                    ev = E[:, 2 * bi:2 * bi + 2, :, :]
                    bv = bank.rearrange("p (h a c) -> p h a c", h=2, a=2)
                    if have_prev:
                        kb.act(ev, bv, AF.Exp, [t_sc], [t_E])
                    else:
                        kb.act(ev[:, :, 1, :], bv[:, :, 1, :], AF.Exp, [t_sc], [t_E])
                hd.update(E=E, t_E=t_E)

            def h5():
                E, t_E = hd["E"], hd["t_E"]
                if have_prev:
                    kb.tt("pool", E[:, :, 0, :], E[:, :, 0, :], tril_bf[:, :].unsqueeze(1).to_broadcast([128, 4, 128]),
                          ALU.mult, [t_E, t_trilbf], [t_E])
                kb.tt("dve", E[:, :, 1, :], E[:, :, 1, :], triu_bf[:, :].unsqueeze(1).to_broadcast([128, 4, 128]),
                      ALU.mult, [t_E, t_triubf], [t_E])

            def h6():
                E, t_E = hd["E"], hd["t_E"]
                pv, t_pv = pv_slots.next()
                pvv = pv[:, 0:260].rearrange("p (h c) -> p h c", h=4)
                for hh in range(4):
                    h = half * 4 + hh
                    if have_prev:
                        kb.mm(pvv[:, hh, :], E[:, hh, 0, :], prev["v1"][:, h, :], [t_E, prev["t_v1"]], [t_pv], start=True, stop=False)
                    kb.mm(pvv[:, hh, :], E[:, hh, 1, :], d["v1"][:, h, :], [t_E, d["t_v1"]], [t_pv], start=not have_prev, stop=True)
                hd.update(pvv=pvv, t_pv=t_pv)

            def h7():
                if half == 0:
                    U_, t_U = ur_.next()
                    d.update(U=U_, t_U=t_U)
                U_, t_U = d["U"], d["t_U"]
                kb.cp("act" if half else "dve", U_[:, half * 4:half * 4 + 4, :], hd["pvv"], [hd["t_pv"]], [t_U])
                if half == 1:
                    kb.dma("sp", us[r_, ur, :], U_[:].rearrange("p h c -> p (h c)"), [t_U], [])

            return [h3, h4, h5, h6, h7]

        return d, [[s0, s1, s2] + mk_half(0), [None, None, None] + mk_half(1)]

    items = []
    for g, (win, dil) in enumerate(ATT):
        nblk = S // dil // 128
        for r_ in range(dil):
            prev = None
            for nb in range(nblk):
                prev, its = attn_unit(g, dil, r_, nb, prev)
                items.extend(its)
    pipeline(items)

    print("sbuf end B", kb.off)
    if stop == "B":
        return finish_early()
    P.barrier()
    kb.off = persist_off
    dng = kb.sb([128, 128], F32, "dng"); t_dng = T()
    kb.dma("sp", dng[:], dn_out_norm.partition_broadcast(128), [], [t_dng])
    S32 = [(kb.sb([128, 128], F32, "S32"), T()) for _ in range(8)]
    Sbf = [(kb.sb([128, 128], BF16, "Sbf"), T()) for _ in range(8)]
    for h in range(8):
        kb.memset("pool", S32[h][0][:], 0.0, [S32[h][1]])
        kb.memset("pool", Sbf[h][0][:], 0.0, [Sbf[h][1]])
    qkvr = kb.ring(2, [128, 24, 128], BF16, "qkvT")
    bgr2 = kb.ring(2, [128, 16], F32, "bgc")
    zr = kb.ring(2, [128, 1024], BF16, "z")
    gbc_r = kb.ring(2, [128, 8, 128], F32, "gbc")
    gcum_r = kb.ring(2, [128, 8], F32, "gcum")
    ngc_r = kb.ring(2, [128, 8], F32, "ngc")
    eg_r = kb.ring(2, [128, 8], F32, "eg")
    neg_r = kb.ring(2, [128, 8], F32, "neg")
    kts_r = kb.ring(2, [128, 8], F32, "kts")
    egl_r = kb.ring(2, [128, 8], F32, "egl")
    H = 8
    pre_r = kb.ring(4, [128, 128], F32, "pre")
    dtm_r = kb.ring(2 * H, [128, 128], F32, "dtm")
    dts_r = kb.ring(2 * H, [128, 128], F32, "dts")
    XA_r = Ring([(kb.sb([128, 256], F32R, "XA"), T("xs"), T("xr")) for _ in range(2 * H + 2)])
    XB_r = Ring([(kb.sb([128, 256], F32R, "XB"), T("xb")) for _ in range(2 * H + 2)])
    zf = kb.sb([128, 256], F32, "zf"); t_zf = T()
    kb.memset("pool", zf[:], 0.0, [t_zf])
    for xa_, t1_, t2_ in XA_r.slots:
        kb.cp("dve", xa_[:], zf[:], [t_zf], [t1_, t2_])
    for xb_, t1_ in XB_r.slots:
        kb.cp("dve", xb_[:], zf[:], [t_zf], [t1_])
    PT_r = kb.ring(2 * H, [128, 128], BF16, "PT")
    AIT_r = kb.ring(2 * H, [128, 128], BF16, "AIT")
    Ktl_r = kb.ring(2 * H, [128, 128], BF16, "Ktl")
    Vtm_r = kb.ring(2 * H, [128, 128], BF16, "Vtm")
    Y_r = kb.ring(4, [128, 128], BF16, "Y")
    vn_r = kb.ring(4, [128, 128], BF16, "vn")
    o1_r = kb.ring(4, [128, 128], F32, "o1")
    O_r = kb.ring(2, [128, 8, 128], F32, "O")
    osq_r = kb.ring(2, [128, 8, 128], F32, "osq")
    obf_r = kb.ring(2, [128, 1024], BF16, "obf")
    s8c_r = kb.ring(2, [128, 8], F32, "s8c")
    glast_r = kb.ring(2, [128, 8], F32, "glast")
    psN = kb.psring([0, 1, 2, 3], 256, "psN")
    psXO = kb.psring([4, 5], 128, "psXO")
    psG = kb.psring([6], 128, "psG")
    psX = Ring([(kb.banks[7][:, 0:128], kb.pst(7)), (kb.banks[7][:, 128:256], kb.pst(7))])
    psB = Ring([(kb.banks[7][:, 256 + 64 * i:256 + 64 * (i + 1)], kb.pst(7)) for i in range(4)])

    def chunk_gen(b):
        cols = slice(b * 128, (b + 1) * 128)
        qkvT, t_qkvT = qkvr.next()
        bgc, t_bgc = bgr2.next()
        zt, t_zt = zr.next()
        kb.dma("sp", qkvT[:], qkvb_s[:, :, cols].rearrange("c p t -> p c t"), [], [t_qkvT])
        kb.dma("sp", bgc[:], bg_s[cols, :], [], [t_bgc])
        kb.dma("sp", zt[:], z_s[cols, :], [], [t_zt])
        gcum, t_gcum = gcum_r.next()
        psg, t_psg = psG.next()
        kb.mm(psg[:, 0:8], triu[:], bgc[:, 8:16], [t_triu, t_bgc], [t_psg])
        kb.cp("dve", gcum[:], psg[:, 0:8], [t_psg], [t_gcum])
        gbc, t_gbc = gbc_r.next()
        kb.cp("pool", gbc[:], bgc[:, 8:16].unsqueeze(2).to_broadcast([128, 8, 128]), [t_bgc], [t_gbc])
        eg, t_eg = eg_r.next()
        neg, t_neg = neg_r.next()
        kb.act(eg[:], gcum[:], AF.Exp, [t_gcum], [t_eg])
        kb.ts("dve", neg[:], eg[:], -1.0, ALU.mult, [t_eg], [t_neg])
        kts, t_kts = kts_r.next()
        egl, t_egl = egl_r.next()
        O_, t_O = O_r.next()
        glast, t_glast = glast_r.next()
        st = []
        for h in range(H):
            psg, t_psg = psG.next()
            kb.mm(psg, gbc[:, h, :], triu[:], [t_gbc, t_triu], [t_psg])
            pre, t_pre = pre_r.next()
            kb.stt("dve", pre[:], psg, gcum[:, h:h + 1], mbm[:], ALU.subtract, ALU.add, [t_psg, t_gcum, t_mbm], [t_pre])
            dtm, t_dtm = dtm_r.next()
            kb.act(dtm[:], pre[:], AF.Exp, [t_pre], [t_dtm])
            dts, t_dts = dts_r.next()
            kb.tt("pool", dts[:], dtm[:], strict[:], ALU.mult, [t_dtm, t_strict], [t_dts])
            kb.cp("act", glast[:, h:h + 1], psg[:, 127:128], [t_psg], [t_glast])
            st.append(dict(dtm=dtm, t_dtm=t_dtm, dts=dts, t_dts=t_dts))
        kb.act(egl[:], glast[:], AF.Exp, [t_glast], [t_egl])
        kb.tt("dve", kts[:], glast[:], gcum[:], ALU.subtract, [t_glast, t_gcum], [t_kts])
        kb.act(kts[:], kts[:], AF.Exp, [t_kts], [t_kts])
        for h in range(H):
            d = st[h]
            dtm, t_dtm, dts, t_dts = d["dtm"], d["t_dtm"], d["dts"], d["t_dts"]
            KT = qkvT[:, 8 + h, :]
            QT = qkvT[:, h, :]
            VT = qkvT[:, 16 + h, :]
            pk, t_pk = psN.next()
            kb.mm(pk, KT, qkvT[:, :, :].rearrange("p (a h) t -> p a h t", a=3)[:, 0:2, h, :], [t_qkvT], [t_pk])
            XA0, t_B, t_XR0 = XA_r.next()
            kb.stt("dve", XA0[:, 0:128], pk[:, 128:256], bgc[:, h:h + 1], dts[:], ALU.mult, ALU.mult, [t_pk, t_bgc, t_dts], [t_B])
            AIT, t_AIT = AIT_r.next()
            kb.tt("dve", AIT[:], pk[:, 0:128], dtm[:], ALU.mult, [t_pk, t_dtm], [t_AIT])
            pb, t_pb = psB.next()
            kb.tr(bfv(pb), KT, ident_bf[:], [t_qkvT, t_identbf], [t_pb])
            Ktl, t_Ktl = Ktl_r.next()
            kb.act(Ktl[:], bfv(pb), AF.Copy, [t_pb, t_kts], [t_Ktl], scale=kts[:, h:h + 1])
            pb2, t_pb2 = psB.next()
            kb.tr(bfv(pb2), VT, ident_bf[:], [t_qkvT, t_identbf], [t_pb2])
            Vtm, t_Vtm = Vtm_r.next()
            kb.cp("act", Vtm[:], bfv(pb2), [t_pb2], [t_Vtm])
            d.update(XA0=XA0, t_B=t_B, t_XR0=t_XR0, AIT=AIT, t_AIT=t_AIT, Ktl=Ktl, t_Ktl=t_Ktl, Vtm=Vtm, t_Vtm=t_Vtm, KT=KT, QT=QT)
        yield "pre"
        for h in range(H):
            d = st[h]
            pt_, t_pt_ = psN.next()
            kb.tr(pt_[:, 0:128], d["XA0"][:, 0:128].bitcast(F32), ident[:], [d["t_B"], t_ident], [t_pt_])
            XB, t_XB = XB_r.next()
            kb.cp("act", XB[:, 0:128], pt_[:, 0:128], [t_pt_], [t_XB])
            XA1, t_S1, t_R1 = XA_r.next()
            kb.tt("dve", XA1[:, 128:256], ident[:], d["XA0"][:, 0:128].bitcast(F32), ALU.subtract, [t_ident, d["t_B"]], [t_R1])
            d.update(XA=d["XA0"], t_S=d["t_B"], t_R=d["t_XR0"], XB=XB, t_XB=t_XB, XAn=XA1, t_Sn=t_S1, t_Rn=t_R1)
        yield "lv"
        NL = 7
        for lvl in range(1, NL + 1):
            last = lvl == NL
            for h in range(H):
                d = st[h]
                XA, XB = d["XA"], d["XB"]
                p1, t_p1 = psN.next()
                kb.mm(p1, XB[:, 0:128], XA[:, :], [d["t_S"], d["t_R"], d["t_XB"]], [t_p1])
                if last:
                    PT, t_PT = PT_r.next()
                    kb.tt("dve", PT[:], p1[:, 128:256], XA[:, 128:256].bitcast(F32), ALU.add, [t_p1, d["t_R"]], [t_PT])
                    d.update(PT=PT, t_PT=t_PT)
                    continue
                p2, t_p2 = psN.next()
                kb.mm(p2, XA[:, 0:128], XB[:, :], [d["t_S"], d["t_XB"]], [t_p2])
                if lvl == 1:
                    XAn, t_Sn, t_Rn = d["XAn"], d["t_Sn"], d["t_Rn"]
                else:
                    XAn, t_Sn, t_Rn = XA_r.next()
                    kb.tt("dve", XAn[:, 128:256], p1[:, 128:256], XA[:, 128:256].bitcast(F32), ALU.add, [t_p1, d["t_R"]], [t_Rn])
                kb.cp("dve", XAn[:, 0:128], p1[:, 0:128], [t_p1], [t_Sn])
                XBn, t_XBn = XB_r.next()
                kb.cp("act", XBn[:, 0:128], p2[:, 0:128], [t_p2], [t_XBn])
                d.update(XA=XAn, t_S=t_Sn, t_R=t_Rn, XB=XBn, t_XB=t_XBn)
            yield ("inv_done" if last else "lv")
        for hg in range(0, H, 4):
            for h in range(hg, hg + 4):
                d = st[h]
                sbf, t_sbf = Sbf[h]
                px, t_px = psXO.next()
                kb.mm(px, d["KT"], sbf[:], [t_qkvT, t_sbf], [t_px])
                po1, t_po1 = psXO.next()
                kb.mm(po1, d["QT"], sbf[:], [t_qkvT, t_sbf], [t_po1])
                d.update(px=px, t_px=t_px, po1=po1, t_po1=t_po1)
            yield "sc"
            for h in range(hg, hg + 4):
                d = st[h]
                Y, t_Y = Y_r.next()
                kb.stt("dve", Y[:], d["px"], neg[:, h:h + 1], d["Vtm"][:], ALU.mult, ALU.add,
                       [d["t_px"], t_neg, d["t_Vtm"]], [t_Y])
                o1, t_o1 = o1_r.next()
                kb.act(o1[:], d["po1"], AF.Copy, [d["t_po1"], t_eg], [t_o1], scale=eg[:, h:h + 1])
                ppy, t_ppy = psX.next()
                kb.mm(ppy, d["PT"][:], Y[:], [d["t_PT"], t_Y], [t_ppy])
                vn, t_vn = vn_r.next()
                kb.act(vn[:], ppy, AF.Copy, [t_ppy, t_bgc], [t_vn], scale=bgc[:, h:h + 1])
                d.update(vn=vn, t_vn=t_vn, o1=o1, t_o1=t_o1)
            yield "sc"
            for h in range(hg, hg + 4):
                d = st[h]
                vn, t_vn = d["vn"], d["t_vn"]
                s32, t_s32 = S32[h]
                sbf, t_sbf = Sbf[h]
                pso, t_pst = psN.next()
                pst, po2, t_po2 = pso[:, 0:128], pso[:, 128:256], t_pst
                kb.mm(pst, d["Ktl"][:], vn[:], [d["t_Ktl"], t_vn], [t_pst])
                kb.mm(po2, d["AIT"][:], vn[:], [d["t_AIT"], t_vn], [t_po2])
                kb.stt("dve", s32[:], s32[:], egl[:, h:h + 1], pst, ALU.mult, ALU.add, [t_s32, t_egl, t_pst], [t_s32])
                kb.cp("act", sbf[:], s32[:], [t_s32], [t_sbf])
                kb.tt("dve", O_[:, h, :], po2, d["o1"][:], ALU.add, [t_po2, d["t_o1"]], [t_O])
            if hg == 0:
                yield "sc"
        osq, t_osq = osq_r.next()
        kb.act(osq[:], O_[:], AF.Square, [t_O], [t_osq], scale=float(128.0 ** -0.5))
        s8, t_s8 = s8c_r.next()
        kb.red("dve", s8[:], osq[:], [t_osq], [t_s8])
        kb.act(s8[:], s8[:], AF.Sqrt, [t_s8, t_eps], [t_s8], bias=epsc[:, 0:1])
        kb.recip(s8[:], s8[:], [t_s8], [t_s8])
        kb.tt("pool", osq[:], O_[:], s8[:, :].unsqueeze(2).to_broadcast([128, 8, 128]), ALU.mult, [t_O, t_s8], [t_osq])
        kb.tt("pool", osq[:], osq[:], dng[:, :].unsqueeze(1).to_broadcast([128, 8, 128]), ALU.mult, [t_osq, t_dng], [t_osq])
        obf, t_obf = obf_r.next()
        kb.tt("dve", obf[:].rearrange("p (h d) -> p h d", d=128), osq[:], zt[:].rearrange("p (h d) -> p h d", d=128),
              ALU.mult, [t_osq, t_zt], [t_obf])
        kb.dma("sp", ob_s[cols, :], obf[:], [t_obf], [])
        yield "done"


    gens = [chunk_gen(b) for b in range(NT)]
    for i in range(NT + 1):
        cur = gens[i] if i < NT else None
        prv = gens[i - 1] if i >= 1 else None
        cur_live, prv_live = cur is not None, prv is not None
        while cur_live or prv_live:
            if prv_live and next(prv) == "done":
                prv_live = False
            if cur_live and next(cur) == "inv_done":
                cur_live = False

    print("sbuf end C", kb.off)
    if stop == "C":
        return finish_early()
    P.barrier()
    kb.off = persist_off
    TC = 8 if NT >= 8 else NT
    wa = kb.sb([128, 4, 1024], BF16, "wa"); t_wa = T()
    wb = kb.sb([128, 8, 1024], BF16, "wb"); t_wb = T()
    wo = kb.sb([128, 8, 1024], BF16, "wo"); t_wo = T()
    wpg = kb.sb([128, 8, 1024], BF16, "wpg"); t_wpg = T()
    wpl = kb.sb([128, 2, 1024], BF16, "wpl"); t_wpl = T()
    kb.dma("gq", wa[:], w_branch_a.rearrange("(k p) c -> p k c", p=128), [], [t_wa])
    kb.dma("gq", wb[:], w_branch_b.rearrange("(k p) c -> p k c", p=128), [], [t_wb])
    kb.dma("gq", wo[:], w_out.rearrange("(k p) c -> p k c", p=128), [], [t_wo])
    kb.dma("gq", wpg[:], w_pg.rearrange("(k p) c -> p k c", p=128), [], [t_wpg])
    kb.dma("gq", wpl[:], w_ple.rearrange("(k p) c -> p k c", p=128), [], [t_wpl])
    wr = kb.sb([128, 8, 20], F32, "wr"); t_wr = T()
    kb.dma("sp", wr[:, :, 0:4], w_rg.rearrange("(k p) c -> p k c", p=128), [], [t_wr])
    kb.dma("sp", wr[:, :, 4:20], w_re.rearrange("(k p) c -> p k c", p=128), [], [t_wr])
    br = kb.sb([128, 20], F32, "br"); t_br = T()
    kb.dma("sp", br[:, 0:4], b_rg.partition_broadcast(128), [], [t_br])
    kb.dma("sp", br[:, 4:20], b_re.partition_broadcast(128), [], [t_br])
    gffn = kb.sb([128, 1024], F32, "gffn"); t_gffn = T()
    gple = kb.sb([128, 1024], F32, "gple"); t_gple = T()
    kb.dma("sp", gffn[:], norm_ffn.partition_broadcast(128), [], [t_gffn])
    kb.dma("sp", gple[:], norm_ple.partition_broadcast(128), [], [t_gple])
    yacc = [(kb.sb([128, 1024], F32, "yacc"), T()) for _ in range(TC)]
    h2T = [(kb.sb([128, 8, 128], BF16, "h2T"), T()) for _ in range(TC)]
    comb = [(kb.sb([128, 16], F32, "comb"), T()) for _ in range(TC)]
    wgu_r = kb.ring(2, [128, 8, 512], BF16, "wgu")
    wd_r = kb.ring(2, [128, 2, 1024], BF16, "wdn")
    u_r = kb.ring(3, [128, 520], F32, "ua")
    un_r = kb.ring(2, [128, 8, 65], F32, "un")
    ga_r = kb.ring(1, [128, 2048], BF16, "gates")
    obr = kb.ring(2, [128, 1024], BF16, "ob")
    oab_r = kb.ring(2, [128, 512], BF16, "oab")
    rden_r = kb.ring(2, [128, 8], F32, "rden")
    tT_r = kb.ring(2, [128, 8, 128], BF16, "tT")
    mg_r = kb.ring(1, [128, 1024], F32, "mg")
    mgb_r = kb.ring(2, [128, 1024], BF16, "mgb")
    hf_r = kb.ring(1, [128, 1024], F32, "hf")
    hfT_r = kb.ring(1, [128, 8, 128], F32, "hfT")
    hbf_r = kb.ring(2, [128, 1024], BF16, "hbf")
    jk_r = kb.ring(1, [128, 1024], BF16, "jk2")
    ss_r = kb.ring(4, [128, 1], F32, "ss2")
    lg_r = kb.ring(2, [128, 20], F32, "lg")
    sm_r = kb.ring(24, [128, 16], F32, "sm")
    sil_r = kb.ring(3, [128, 256], F32, "sil")
    act_r = kb.ring(4, [128, 256], BF16, "actb")
    actT_r = kb.ring(4, [128, 2, 128], BF16, "actT")
    pin_r = kb.ring(2, [128, 256], F32, "pin")
    pbf_r = kb.ring(2, [128, 256], BF16, "pbf")
    sg_r = kb.ring(1, [128, 1024], F32, "sg")
    psM = kb.psring([0, 1, 2, 3], 512, "psM")
    psTr = kb.psring([4, 5], 512, "psTr")
    psD = kb.psring([6, 7], 512, "psD")
    final_ops = []

    def transpose_bf(src, t_src, nk):
        ps, t_ps = psTr.next()
        psv = bfv(ps).rearrange("p (k c) -> p k c", k=8)
        for k in range(nk):
            kb.tr(psv[:, k, :], src[:, k * 128:(k + 1) * 128], ident_bf[:], [t_src, t_identbf], [t_ps])
        tT, t_tT = tT_r.next()
        kb.cp("act", tT[:, 0:nk, :], psv[:, 0:nk, :], [t_ps], [t_tT])
        return tT, t_tT

    def proj(tT, t_tT, W, t_W, nk):
        res = []
        for c in range(2):
            ps, t_ps = psM.next()
            for k in range(nk):
                kb.mm(ps, tT[:, k, :], W[:, k, c * 512:(c + 1) * 512], [t_tT, t_W], [t_ps], start=(k == 0), stop=(k == nk - 1))
            res.append((ps, t_ps))
        return res

    def rmsnorm_to(xsrc, t_x, gain, t_gain, out_ap, t_out):
        jk, t_jk = jk_r.next()
        ss, t_ss = ss_r.next()
        kb.memset("pool", ss[:], 0.0, [t_ss])
        kb.act(jk[:], xsrc, AF.Square, [t_x, t_ss], [t_jk, t_ss], scale=1.0 / 32.0, accum_out=ss[:])
        rms_rstd(ss[:], t_ss)
        kb.stt("dve", out_ap, xsrc, ss[:, 0:1], gain[:], ALU.mult, ALU.mult, [t_x, t_ss, t_gain], [t_out])

    def d_tile(c0, ti):
        t = c0 + ti
        rows = slice(t * 128, (t + 1) * 128)
        ya, t_ya = yacc[ti]
        kb.dma("sp", ya[:], x[rows, :], [], [t_ya])
        us_ = []
        for g in range(3):
            u, t_u = u_r.next()
            kb.dma("sp", u[:], ua_s[g, rows, :], [], [t_u])
            us_.append((u, t_u))
        ga, t_ga = ga_r.next()
        kb.dma("sp", ga[:], gates_s[rows, :], [], [t_ga])
        ob, t_ob = obr.next()
        kb.dma("sp", ob[:], ob_s[rows, :], [], [t_ob])
        un, t_un = un_r.next()
        unf = un[:].rearrange("p h c -> p (h c)")
        kb.tt("pool", unf, us_[0][0][:], us_[1][0][:], ALU.add, [us_[0][1], us_[1][1]], [t_un])
        kb.tt("pool", unf, unf, us_[2][0][:], ALU.add, [t_un, us_[2][1]], [t_un])
        rden, t_rden = rden_r.next()
        kb.recip(rden[:], un[:, :, 64], [t_un], [t_rden])
        oab, t_oab = oab_r.next()
        kb.tt("dve", oab[:].rearrange("p (h d) -> p h d", d=64), un[:, :, 0:64],
              rden[:, :].unsqueeze(2).to_broadcast([128, 8, 64]), ALU.mult, [t_un, t_rden], [t_oab])
        aT, t_aT = transpose_bf(oab, t_oab, 4)
        pa = proj(aT, t_aT, wa, t_wa, 4)
        mg, t_mg = mg_r.next()
        for c in range(2):
            cs = slice(c * 512, (c + 1) * 512)
            kb.tt("dve", mg[:, cs], pa[c][0], ga[:, c * 512:(c + 1) * 512], ALU.mult, [pa[c][1], t_ga], [t_mg])
        yield
        bT, t_bT = transpose_bf(ob, t_ob, 8)
        pb_ = proj(bT, t_bT, wb, t_wb, 8)
        mgb, t_mgb = mgb_r.next()
        for c in range(2):
            cs = slice(c * 512, (c + 1) * 512)
            kb.tt("dve", mgb[:, cs], pb_[c][0], ga[:, 1024 + c * 512:1024 + (c + 1) * 512], ALU.mult, [pb_[c][1], t_ga], [t_mgb])
        kb.tt("pool", mgb[:], mg[:], mgb[:], ALU.add, [t_mg, t_mgb], [t_mgb])
        yield
        mT, t_mT = transpose_bf(mgb, t_mgb, 8)
        po = proj(mT, t_mT, wo, t_wo, 8)
        for c in range(2):
            cs = slice(c * 512, (c + 1) * 512)
            kb.tt("dve", ya[:, cs], po[c][0], ya[:, cs], ALU.add, [po[c][1], t_ya], [t_ya])
        yield
        hf, t_hf = hf_r.next()
        rmsnorm_to(ya[:], t_ya, gffn, t_gffn, hf[:], t_hf)
        hbf, t_hbf = hbf_r.next()
        kb.cp("pool", hbf[:], hf[:], [t_hf], [t_hbf])
        hT2, t_hT2 = h2T[ti]
        ps, t_ps = psTr.next()
        psv = bfv(ps).rearrange("p (k c) -> p k c", k=8)
        for k in range(8):
            kb.tr(psv[:, k, :], hbf[:, k * 128:(k + 1) * 128], ident_bf[:], [t_hbf, t_identbf], [t_ps])
        kb.cp("act", hT2[:], psv, [t_ps], [t_hT2])
        yield
        hfT, t_hfT = hfT_r.next()
        for hv in range(2):
            ps, t_ps = psTr.next()
            psv4 = ps.rearrange("p (k c) -> p k c", k=4)
            for k in range(4):
                kk = hv * 4 + k
                kb.tr(psv4[:, k, :], hf[:, kk * 128:(kk + 1) * 128], ident[:], [t_hf, t_ident], [t_ps])
            kb.cp("act" if hv else "dve", hfT[:, hv * 4:hv * 4 + 4, :], psv4, [t_ps], [t_hfT])
        ps, t_ps = psD.next()
        for k in range(8):
            kb.mm(ps[:, 0:20], hfT[:, k, :], wr[:, k, :], [t_hfT, t_wr], [t_ps], start=(k == 0), stop=(k == 7))
        lg, t_lg = lg_r.next()
        kb.tt("dve", lg[:], ps[:, 0:20], br[:], ALU.add, [t_ps, t_br], [t_lg])
        cm, t_cm = comb[ti]

        def sm(n=16):
            a, t_a = sm_r.next()
            return a[:, 0:n], t_a
        gmax, t_gmax = sm(1)
        kb.red("dve", gmax, lg[:, 0:4], [t_lg], [t_gmax], op=ALU.max)
        ngmax, t_ngmax = sm(1)
        kb.ts("dve", ngmax, gmax, -1.0, ALU.mult, [t_gmax], [t_ngmax])
        gex, t_gex = sm(4)
        gsum, t_gsum = sm(1)
        kb.memset("pool", gsum, 0.0, [t_gsum])
        kb.act(gex, lg[:, 0:4], AF.Exp, [t_lg, t_ngmax, t_gsum], [t_gex, t_gsum], bias=ngmax, accum_out=gsum)
        pg, t_pg = sm(1)
        kb.recip(pg, gsum, [t_gsum], [t_pg])
        goh, t_goh = sm(4)
        kb.ts("dve", goh, lg[:, 0:4], gmax, ALU.is_ge, [t_lg, t_gmax], [t_goh])
        el, t_el = sm(16)
        kb.tt("dve", el.rearrange("p (g e) -> p g e", e=4), lg[:, 4:20].rearrange("p (g e) -> p g e", e=4),
              goh.unsqueeze(2).to_broadcast([128, 4, 4]), ALU.mult, [t_lg, t_goh], [t_el])
        sel, t_sel = sm(4)
        kb.red("dve", sel, el.rearrange("p (g e) -> p e g", e=4), [t_el], [t_sel])
        m1, t_m1 = sm(1)
        kb.red("dve", m1, sel, [t_sel], [t_m1], op=ALU.max)
        oh1, t_oh1 = sm(4)
        kb.ts("dve", oh1, sel, m1, ALU.is_ge, [t_sel, t_m1], [t_oh1])
        sel2, t_sel2 = sm(4)
        kb.stt("dve", sel2, oh1, -1e30, sel, ALU.mult, ALU.add, [t_oh1, t_sel], [t_sel2])
        m2, t_m2 = sm(1)
        kb.red("dve", m2, sel2, [t_sel2], [t_m2], op=ALU.max)
        oh2, t_oh2 = sm(4)
        kb.ts("dve", oh2, sel2, m2, ALU.is_ge, [t_sel2, t_m2], [t_oh2])
        dd, t_dd = sm(1)
        kb.tt("dve", dd, m2, m1, ALU.subtract, [t_m2, t_m1], [t_dd])
        kb.act(dd, dd, AF.Exp, [t_dd], [t_dd])
        kb.ts("dve", dd, dd, 1.0, ALU.add, [t_dd], [t_dd])
        w1, t_w1 = sm(1)
        kb.recip(w1, dd, [t_dd], [t_w1])
        w2, t_w2 = sm(1)
        kb.ts("dve", w2, w1, -1.0, ALU.mult, [t_w1], [t_w2], s2=1.0, op1=ALU.add)
        kb.tt("dve", w1, w1, pg, ALU.mult, [t_w1, t_pg], [t_w1])
        kb.tt("dve", w2, w2, pg, ALU.mult, [t_w2, t_pg], [t_w2])
        wig, t_wig = sm(4)
        kb.ts("dve", wig, oh1, w1, ALU.mult, [t_oh1, t_w1], [t_wig])
        kb.stt("dve", wig, oh2, w2, wig, ALU.mult, ALU.add, [t_oh2, t_w2, t_wig], [t_wig])
        for g in range(4):
            kb.ts("dve", cm[:, g * 4:(g + 1) * 4], wig, goh[:, g:g + 1], ALU.mult, [t_wig, t_goh], [t_cm])
        yield

    def f_tile(c0, ti):
        t = c0 + ti
        rows = slice(t * 128, (t + 1) * 128)
        ya, t_ya = yacc[ti]
        hbf, t_hbf = hbf_r.next()
        rmsnorm_to(ya[:], t_ya, gple, t_gple, hbf[:], t_hbf)
        h3T, t_h3T = transpose_bf(hbf, t_hbf, 8)
        pgate = proj(h3T, t_h3T, wpg, t_wpg, 8)
        sg, t_sg = sg_r.next()
        for c in range(2):
            cs = slice(c * 512, (c + 1) * 512)
            kb.act(sg[:, cs], pgate[c][0], AF.Sigmoid, [pgate[c][1]], [t_sg])
        pin, t_pin = pin_r.next()
        kb.dma("sp", pin[:], p_in[rows, :], [], [t_pin])
        pbf, t_pbf = pbf_r.next()
        kb.cp("pool", pbf[:], pin[:], [t_pin], [t_pbf])
        yield
        pT, t_pT = transpose_bf(pbf, t_pbf, 2)
        pple = proj(pT, t_pT, wpl, t_wpl, 2)
        for c in range(2):
            cs = slice(c * 512, (c + 1) * 512)
            kb.tt("dve", sg[:, cs], sg[:, cs], pple[c][0], ALU.mult, [t_sg, pple[c][1]], [t_sg])
        kb.tt("pool", ya[:], sg[:], ya[:], ALU.add, [t_sg, t_ya], [t_ya])
        final_ops.append(kb.dma("sp", out[rows, :], ya[:], [t_ya], []))
        yield

    def run_gens(gs):
        gs = list(gs)
        while gs:
            for g_ in list(gs):
                try:
                    next(g_)
                except StopIteration:
                    gs.remove(g_)


    for ti in range(TC):
        run_gens([d_tile(0, ti)])
    for c0 in range(0, NT, TC):
        def load_gu(e_):
            wgu, t_wgu_sb = wgu_r.next()
            kb.dma("sp", wgu[:, 0:4, :], wgu_s[e_].rearrange("(k p) c -> p k c", p=128)[:, 0:4, :], [t_wgu[e_]], [t_wgu_sb])
            kb.dma("gq", wgu[:, 4:8, :], wgu_s[e_].rearrange("(k p) c -> p k c", p=128)[:, 4:8, :], [t_wgu[e_]], [t_wgu_sb])
            return wgu, t_wgu_sb

        def load_dn(e_):
            wdn, t_wdn = wd_r.next()
            kb.dma("sp", wdn[:], wd_s[e_].rearrange("(k p) c -> p k c", p=128), [t_wd[e_]], [t_wdn])
            return wdn, t_wdn

        Wgu = {0: load_gu(0)}
        Wdn = {0: load_dn(0)}

        def exp_item(e_, ti):
            ya, t_ya = yacc[ti]
            hT2, t_hT2 = h2T[ti]
            cm, t_cm = comb[ti]
            d = {}

            def e0():
                if ti == 0 and e_ + 1 < 16:
                    Wgu[e_ + 1] = load_gu(e_ + 1)
                wgu, t_wgu_sb = Wgu[e_]
                ps, t_ps = psM.next()
                for k in range(8):
                    kb.mm(ps, hT2[:, k, :], wgu[:, k, :], [t_hT2, t_wgu_sb], [t_ps], start=(k == 0), stop=(k == 7))
                d.update(ps=ps, t_ps=t_ps)

            def e1():
                ps, t_ps = d["ps"], d["t_ps"]
                sil, t_sil = sil_r.next()
                kb.act(sil[:], ps[:, 0:256], AF.Silu, [t_ps], [t_sil])
                ab, t_ab = act_r.next()
                kb.stt("dve", ab[:], ps[:, 256:512], cm[:, e_:e_ + 1], sil[:], ALU.mult, ALU.mult, [t_ps, t_cm, t_sil], [t_ab])
                d.update(ab=ab, t_ab=t_ab)

            def e2():
                pst_, t_pst_ = psTr.next()
                pstv = bfv(pst_).rearrange("p (k c) -> p k c", k=8)
                for k in range(2):
                    kb.tr(pstv[:, k, :], d["ab"][:, k * 128:(k + 1) * 128], ident_bf[:], [d["t_ab"], t_identbf], [t_pst_])
                d.update(pstv=pstv, t_pst=t_pst_)

            def e3():
                aT, t_aT = actT_r.next()
                kb.cp("act", aT[:], d["pstv"][:, 0:2, :], [d["t_pst"]], [t_aT])
                d.update(aT=aT, t_aT=t_aT)

            def e4():
                if ti == 0 and e_ + 1 < 16:
                    Wdn[e_ + 1] = load_dn(e_ + 1)
                wdn, t_wdn = Wdn[e_]
                pds = []
                for c in range(2):
                    pd, t_pd = psD.next()
                    for k in range(2):
                        kb.mm(pd, d["aT"][:, k, :], wdn[:, k, c * 512:(c + 1) * 512], [d["t_aT"], t_wdn], [t_pd], start=(k == 0), stop=(k == 1))
                    pds.append((pd, t_pd))
                d.update(pds=pds)

            def e5():
                for c in range(2):
                    pd, t_pd = d["pds"][c]
                    cs = slice(c * 512, (c + 1) * 512)
                    kb.tt("dve", ya[:, cs], ya[:, cs], pd, ALU.add, [t_ya, t_pd], [t_ya])

            return [e0, e1, e2, e3, e4, e5]

        pipeline([exp_item(e_, ti) for e_ in range(16) for ti in range(TC)])
        for ti in range(TC + 1):
            gs = []
            if ti < TC:
                gs.append(f_tile(c0, ti))
            if ti >= 1 and c0 + TC < NT:
                gs.append(d_tile(c0 + TC, ti - 1))
            run_gens(gs)

    print("sbuf end D", kb.off, "ops", {k: len(v) for k, v in P.ops.items()})
    P.emit(nc, final_waits=final_ops)
    return nc


_CONSTS = None


def _consts():
    global _CONSTS
    if _CONSTS is None:
        _CONSTS = {
            "c_ident": np.eye(128, dtype=np.float32),
            "c_triu": np.triu(np.ones((128, 128), dtype=np.float32)),
            "c_tril": np.tril(np.ones((128, 128), dtype=np.float32)),
        }
    return _CONSTS


def make_in_map(inputs, b, S):
    f = lambda a: np.ascontiguousarray(np.asarray(a, dtype=np.float32))
    m = {
        "x": f(inputs["x"][b, :S]),
        "p": f(inputs["p"][0, b, :S]),
        "norm_mix": f(inputs["norm_mix"][0]),
        "w_in": f(inputs["w_in"][0]),
        "q_norm": f(inputs["q_norm"][0]),
        "k_norm": f(inputs["k_norm"][0]),
        "conv_w": f(np.asarray(inputs["conv_w"][0]).reshape(4, 24, 128).transpose(2, 1, 0)),
        "a_log": f(inputs["a_log"][0]),
        "dt_bias": f(inputs["dt_bias"][0]),
        "dn_out_norm": f(inputs["dn_out_norm"][0]),
        "w_branch_a": f(inputs["w_branch_a"][0]),
        "w_branch_b": f(inputs["w_branch_b"][0]),
        "w_out": f(inputs["w_out"][0]),
        "norm_ffn": f(inputs["norm_ffn"][0]),
        "w_router_group": f(inputs["w_router_group"][0]),
        "b_router_group": f(inputs["b_router_group"][0]),
        "w_router_expert": f(inputs["w_router_expert"][0]),
        "b_router_expert": f(inputs["b_router_expert"][0]),
        "w_expert_gate": f(np.asarray(inputs["w_expert_gate"][0]).reshape(16, 1024, 256)),
        "w_expert_up": f(np.asarray(inputs["w_expert_up"][0]).reshape(16, 1024, 256)),
        "w_expert_down": f(np.asarray(inputs["w_expert_down"][0]).reshape(16, 256, 1024)),
        "norm_ple": f(inputs["norm_ple"][0]),
        "w_ple": f(inputs["w_ple"][0]),
        "w_ple_gate": f(inputs["w_ple_gate"][0]),
    }
    m.update(_consts())
    return m


def kernel(**inputs):
    S = 8192
    nc = build(S)
    in_maps = [make_in_map(inputs, b, S) for b in range(8)]
    res = run_bass_kernel_spmd(nc, in_maps, core_ids=list(range(8)))
    return np.stack([np.asarray(r["out"], dtype=np.float32) for r in res.results], axis=0)
```

```python
import numpy as np
import concourse.bass as bass
import concourse.mybir as mybir
from concourse.bass_utils import run_bass_kernel_spmd

F32 = mybir.dt.float32
BF16 = mybir.dt.bfloat16
F32R = mybir.dt.float32r
ALU = mybir.AluOpType
AF = mybir.ActivationFunctionType
AX = mybir.AxisListType

ENGS = ("pe", "act", "dve", "pool")
DMAQ = ("sp", "gq")
NDSEM = 24
EPS = 1e-6
SB_BASE = 16512
SB_TOP = 229344


class T:
    __slots__ = ("name", "w", "rs", "banks")

    def __init__(self, name="", banks=()):
        self.name = name
        self.w = None
        self.rs = []
        self.banks = banks


class Bank:
    __slots__ = ("last",)

    def __init__(self):
        self.last = None


class Op:
    __slots__ = ("eng", "fn", "idx", "deps", "sig", "signo", "dslot", "dval", "clock", "gorder")


class Prog:
    def __init__(self):
        self.ops = {e: [] for e in ENGS + DMAQ}
        self.clock = {e: {} for e in ENGS + DMAQ}
        self.ndma = {q: 0 for q in DMAQ}
        self.g = 0
        self.pending = {e: [] for e in ENGS + DMAQ}

    def barrier(self):
        lasts = []
        for e in ENGS:
            if self.ops[e]:
                lasts.append(self.ops[e][-1])
        for q in DMAQ:
            lasts.extend(self.ops[q][-NDSEM:])
        self.pending["sp"] = list(lasts)
        op = self.add("sp", self.bar_fn)
        for e in ENGS + ("gq",):
            self.pending[e] = [op]

    def add(self, eng, fn, reads=(), writes=()):
        op = Op()
        op.eng = eng
        op.fn = fn
        op.idx = len(self.ops[eng])
        op.sig = False
        op.signo = None
        op.gorder = self.g
        self.g += 1
        deps = []
        for t in reads:
            if t.w is not None:
                deps.append((t.w, True))
        for t in writes:
            if t.w is not None:
                deps.append((t.w, False))
            for r in t.rs:
                deps.append((r, False))
        bks = []
        for t in tuple(reads) + tuple(writes):
            for b in t.banks:
                if b not in bks:
                    bks.append(b)
        for b in bks:
            if b.last is not None and b.last.eng != eng:
                deps.append((b.last, True))
        if self.pending[eng]:
            for d in self.pending[eng]:
                if d.eng != eng or eng in DMAQ:
                    deps.append((d, True))
            self.pending[eng] = []
        clk = self.clock[eng]
        need = {}
        for d, raw in deps:
            if d.eng == eng and eng not in DMAQ:
                if eng == "pe":
                    continue
            if d.eng in DMAQ:
                key = (d.eng, d.idx)
                if clk.get(key, False):
                    continue
                need[key] = d
            else:
                if clk.get(d.eng, -1) >= d.idx:
                    continue
                cur = need.get(d.eng)
                if cur is None or cur.idx < d.idx:
                    need[d.eng] = d
        op.deps = list(need.values())
        for d in op.deps:
            d.sig = True
            for k, v in d.clock.items():
                if isinstance(k, tuple):
                    clk[k] = True
                elif clk.get(k, -1) < v:
                    clk[k] = v
            if d.eng in DMAQ:
                clk[(d.eng, d.idx)] = True
            elif clk.get(d.eng, -1) < d.idx:
                clk[d.eng] = d.idx
        if eng in DMAQ:
            op.dslot = self.ndma[eng] % NDSEM
            op.dval = 16 * (self.ndma[eng] // NDSEM + 1)
            self.ndma[eng] += 1
            prev_i = op.idx - NDSEM
            if prev_i >= 0:
                pd = self.ops[eng][prev_i]
                if not clk.get((eng, prev_i), False):
                    op.deps.append(pd)
                    clk[(eng, prev_i)] = True
            op.sig = True
        if len(clk) > 400:
            for k in [k for k in clk if isinstance(k, tuple) and k[1] < self.ndma[k[0]] - 4 * NDSEM]:
                del clk[k]
        op.clock = dict(clk)
        if eng not in DMAQ:
            op.clock[eng] = op.idx
        self.ops[eng].append(op)
        for b in bks:
            b.last = op
        for t in reads:
            t.rs.append(op)
        for t in writes:
            t.w = op
            t.rs = []
        return op

    def emit(self, nc, final_waits=()):
        from contextlib import ExitStack
        with ExitStack() as es:
            sems = {e: es.enter_context(nc.semaphore("s_" + e)) for e in ENGS}
            dsems = {q: [es.enter_context(nc.semaphore(f"d_{q}{i}")) for i in range(NDSEM)] for q in DMAQ}
            for d in final_waits:
                d.sig = True
            for e in ENGS:
                n = 0
                for op in self.ops[e]:
                    if op.sig:
                        n += 1
                        op.signo = n
            block = es.enter_context(nc.Block())

            def waits(engobj, op):
                for d in op.deps:
                    if d.eng in DMAQ:
                        engobj.wait_ge(dsems[d.eng][d.dslot], d.dval)
                    else:
                        engobj.wait_ge(sems[d.eng], d.signo)

            def run(oplist, engobj, extra=None):
                for op in oplist:
                    waits(engobj, op)
                    ins = op.fn(engobj)
                    if op.sig:
                        if op.eng in DMAQ:
                            ins.then_inc(dsems[op.eng][op.dslot], 16)
                        else:
                            ins.then_inc(sems[op.eng], 1)
                if extra:
                    extra(engobj)

            def fin(engobj):
                for d in final_waits:
                    if d.eng in DMAQ:
                        engobj.wait_ge(dsems[d.eng][d.dslot], d.dval)
                    else:
                        engobj.wait_ge(sems[d.eng], d.signo)

            @block.tensor
            def _(e):
                run(self.ops["pe"], e)

            @block.scalar
            def _(e):
                run(self.ops["act"], e)

            @block.vector
            def _(e):
                run(self.ops["dve"], e)

            @block.gpsimd
            def _(e):
                merged = sorted(self.ops["pool"] + self.ops["gq"], key=lambda o: o.gorder)
                run(merged, e)

            @block.sync
            def _(e):
                run(self.ops["sp"], e, fin)


class KB:
    def __init__(self, nc):
        self.nc = nc
        self.P = Prog()
        self.off = SB_BASE
        self.n = 0
        self.banks = [nc.alloc_psum_tensor(f"bank{i}", [128, 512], F32) for i in range(8)]
        self.bk = [Bank() for _ in range(8)]

    def pst(self, *bank_ids):
        return T("ps", banks=tuple(self.bk[b] for b in bank_ids))

    def sb(self, shape, dt, name="t"):
        sz = int(np.prod(shape[1:])) * (2 if dt == BF16 else 4)
        sz = (sz + 31) // 32 * 32
        assert self.off + sz <= SB_TOP, f"sbuf overflow {name} {self.off} {sz}"
        self.n += 1
        t = self.nc.alloc_sbuf_tensor_at(f"{name}_{self.n}", list(shape), dt, offset=self.off)
        self.off += sz
        return t

    def ring(self, n, shape, dt, name="r"):
        return Ring([(self.sb(shape, dt, name), T(name)) for _ in range(n)])

    def psring(self, banks, nf32, name="ps"):
        per = 512 // nf32
        slots = []
        for i in range(per):
            for b in banks:
                slots.append((self.banks[b][:, i * nf32:(i + 1) * nf32], self.pst(b)))
        return Ring(slots)

    def mm(self, out, lhsT, rhs, r, w, start=True, stop=True):
        return self.P.add("pe", lambda e: e.matmul(out, lhsT=lhsT, rhs=rhs, start=start, stop=stop), reads=r, writes=w)

    def tr(self, out, in_, ident, r, w):
        return self.P.add("pe", lambda e: e.transpose(out=out, in_=in_, identity=ident), reads=r, writes=w)

    def act(self, out, in_, func, r, w, bias=None, scale=None, accum_out=None, eng="act"):
        kw = {}
        if bias is not None:
            kw["bias"] = bias
        if scale is not None:
            kw["scale"] = scale
        if accum_out is not None:
            kw["accum_out"] = accum_out
        return self.P.add("act", lambda e: e.activation(out=out, in_=in_, func=func, **kw), reads=r, writes=w)

    def cp(self, eng, out, in_, r, w):
        if eng == "act":
            return self.P.add("act", lambda e: e.copy(out=out, in_=in_), reads=r, writes=w)
        return self.P.add(eng, lambda e: e.tensor_copy(out=out, in_=in_), reads=r, writes=w)

    def tt(self, eng, out, in0, in1, op, r, w):
        return self.P.add(eng, lambda e: e.tensor_tensor(out=out, in0=in0, in1=in1, op=op), reads=r, writes=w)

    def ts(self, eng, out, in0, s1, op0, r, w, s2=None, op1=None):
        if op1 is None:
            return self.P.add(eng, lambda e: e.tensor_scalar(out=out, in0=in0, scalar1=s1, scalar2=None, op0=op0), reads=r, writes=w)
        return self.P.add(eng, lambda e: e.tensor_scalar(out=out, in0=in0, scalar1=s1, scalar2=s2, op0=op0, op1=op1), reads=r, writes=w)

    def stt(self, eng, out, in0, scalar, in1, op0, op1, r, w):
        return self.P.add(eng, lambda e: e.scalar_tensor_tensor(out=out, in0=in0, scalar=scalar, in1=in1, op0=op0, op1=op1), reads=r, writes=w)

    def red(self, eng, out, in_, r, w, op=ALU.add):
        return self.P.add(eng, lambda e: e.tensor_reduce(out=out, in_=in_, axis=AX.X, op=op), reads=r, writes=w)

    def recip(self, out, in_, r, w):
        return self.P.add("dve", lambda e: e.reciprocal(out=out, in_=in_), reads=r, writes=w)

    def memset(self, eng, ap, val, w):
        return self.P.add(eng, lambda e: e.memset(ap, val), writes=w)

    def dma(self, q, out, in_, r, w):
        return self.P.add(q, lambda e: e.dma_start(out=out, in_=in_), reads=r, writes=w)


class Ring:
    def __init__(self, slots):
        self.slots = slots
        self.i = 0

    def next(self):
        s = self.slots[self.i % len(self.slots)]
        self.i += 1
        return s


def pipeline(units):
    n = len(units)
    nst = max(len(u) for u in units)
    for i in range(n + nst - 1):
        for st in range(nst - 1, -1, -1):
            u = i - st
            if 0 <= u < n and st < len(units[u]) and units[u][st] is not None:
                units[u][st]()


def bfv(ap):
    return ap.bitcast(BF16)


def build(S, debug=False, stop=None, skip_pre=False):
    nc = bass.Bass("TRN2", target_bir_lowering=False)
    NT = S // 128
    NSP = S // 512
    kb = KB(nc)
    P = kb.P

    def din(name, shape, dt=F32):
        return nc.dram_tensor(name, list(shape), dt, kind="ExternalInput").ap()

    def dscr(name, shape, dt):
        return nc.dram_tensor(name, list(shape), dt, kind="ExternalOutput" if debug else "Internal").ap()

    x = din("x", [S, 1024])
    p_in = din("p", [S, 256])
    norm_mix = din("norm_mix", [1024])
    w_in = din("w_in", [1024, 10768])
    q_norm = din("q_norm", [64])
    k_norm = din("k_norm", [64])
    conv_w = din("conv_w", [128, 24, 4])
    a_log = din("a_log", [8])
    dt_bias = din("dt_bias", [8])
    dn_out_norm = din("dn_out_norm", [128])
    w_branch_a = din("w_branch_a", [512, 1024])
    w_branch_b = din("w_branch_b", [1024, 1024])
    w_out = din("w_out", [1024, 1024])
    norm_ffn = din("norm_ffn", [1024])
    w_rg = din("w_router_group", [1024, 4])
    b_rg = din("b_router_group", [4])
    w_re = din("w_router_expert", [1024, 16])
    b_re = din("b_router_expert", [16])
    w_eg = din("w_expert_gate", [16, 1024, 256])
    w_eu = din("w_expert_up", [16, 1024, 256])
    w_ed = din("w_expert_down", [16, 256, 1024])
    norm_ple = din("norm_ple", [1024])
    w_ple = din("w_ple", [256, 1024])
    w_pg = din("w_ple_gate", [1024, 1024])
    c_ident = din("c_ident", [128, 128])
    c_triu = din("c_triu", [128, 128])
    c_tril = din("c_tril", [128, 128])
    out = nc.dram_tensor("out", [S, 1024], F32, kind="ExternalOutput").ap()

    qkva_s = dscr("qkva_s", [9, S, 512], BF16)
    qkvb_s = dscr("qkvb_s", [24, 128, S], BF16)
    z_s = dscr("z_s", [S, 1024], BF16)
    bg_s = dscr("bg_s", [S, 16], F32)
    gates_s = dscr("gates_s", [S, 2048], BF16)
    ua_s = dscr("ua_s", [3, S, 520], F32)
    ob_s = dscr("ob_s", [S, 1024], BF16)
    wgu_s = nc.dram_tensor("wgu_s", [16, 1024, 512], BF16, kind="Internal").ap()
    wd_s = nc.dram_tensor("wd_s", [16, 256, 1024], BF16, kind="Internal").ap()

    ident = kb.sb([128, 128], F32, "ident"); t_ident = T()
    ident_bf = kb.sb([128, 128], BF16, "identbf"); t_identbf = T()
    triu = kb.sb([128, 128], F32, "triu"); t_triu = T()
    tril_bf = kb.sb([128, 128], BF16, "trilbf"); t_trilbf = T()
    triu_bf = kb.sb([128, 128], BF16, "triubf"); t_triubf = T()
    tril = kb.sb([128, 128], F32, "tril"); t_tril = T()
    mbm = kb.sb([128, 128], F32, "mbm"); t_mbm = T()
    strict = kb.sb([128, 128], F32, "strict"); t_strict = T()
    ones_bf = kb.sb([128, 128], BF16, "ones"); t_ones = T()
    epsc = kb.sb([128, 1], F32, "eps"); t_eps = T()
    eps128 = kb.sb([128, 1], F32, "eps128"); t_eps128 = T()
    kb.dma("sp", ident[:], c_ident[:, :], [], [t_ident])
    kb.dma("sp", triu[:], c_triu[:, :], [], [t_triu])
    kb.dma("sp", tril[:], c_tril[:, :], [], [t_tril])
    kb.cp("dve", ident_bf[:], ident[:], [t_ident], [t_identbf])
    kb.cp("dve", triu_bf[:], triu[:], [t_triu], [t_triubf])
    kb.cp("dve", tril_bf[:], tril[:], [t_tril], [t_trilbf])
    kb.ts("dve", mbm[:], triu[:], -1.0, ALU.add, [t_triu], [t_mbm], s2=1e9, op1=ALU.mult)
    kb.tt("dve", strict[:], triu[:], ident[:], ALU.subtract, [t_triu, t_ident], [t_strict])
    kb.memset("pool", ones_bf[:], 1.0, [t_ones])
    kb.memset("pool", epsc[:], EPS, [t_eps])
    kb.memset("pool", eps128[:], 128.0 * EPS, [t_eps128])

    t_wgu = [T() for _ in range(16)]
    t_wd = [T() for _ in range(16)]
    for e_ in range(0 if not skip_pre else 16, 16):
        kb.dma("gq", wgu_s[e_, :, 0:256], w_eg[e_], [], [t_wgu[e_]])
        kb.dma("gq", wgu_s[e_, :, 256:512], w_eu[e_], [], [t_wgu[e_]])
        kb.dma("gq", wd_s[e_], w_ed[e_], [], [t_wd[e_]])

    bar_a = kb.sb([128, 8], F32, "bar_a")
    bar_b = kb.sb([128, 8], F32, "bar_b")
    kb.memset("pool", bar_a[:], 0.0, [])
    P.bar_fn = lambda e: e.dma_start(out=bar_b[:], in_=bar_a[:])
    persist_off = kb.off

    def finish_early():
        fw = []
        for q in DMAQ:
            fw.extend(P.ops[q][-NDSEM:])
        for e in ENGS:
            if P.ops[e]:
                fw.append(P.ops[e][-1])
        P.emit(nc, final_waits=fw)
        return nc
    gain_mix = kb.sb([128, 1024], F32, "gmix"); t_gmix = T()
    kb.dma("sp", gain_mix[:], norm_mix.partition_broadcast(128), [], [t_gmix])
    hT = kb.sb([128, 8, S], BF16, "hT")
    t_hT = [T() for _ in range(NT)]
    a_off = kb.off
    xr = kb.ring(4, [128, 1024], F32, "xt")
    junk = kb.ring(2, [128, 1024], BF16, "junk")
    hb = kb.ring(3, [128, 1024], BF16, "hb")
    ssr = kb.ring(6, [128, 1], F32, "ss")
    psA0 = kb.psring([0, 1], 512, "psA0")

    def rms_rstd(ss_ap, t_ss, nfeat_scale_done=True):
        kb.act(ss_ap, ss_ap, AF.Sqrt, [t_ss, t_eps], [t_ss], bias=epsc[:, 0:1])
        kb.recip(ss_ap, ss_ap, [t_ss], [t_ss])

    def a0_unit(t):
        d = {}

        def s0():
            xt, t_xt = xr.next()
            kb.dma("sp", xt[:], x[t * 128:(t + 1) * 128, :], [], [t_xt])
            d.update(xt=xt, t_xt=t_xt)

        def s1():
            xt, t_xt = d["xt"], d["t_xt"]
            jk, t_jk = junk.next()
            ss, t_ss = ssr.next()
            kb.memset("pool", ss[:], 0.0, [t_ss])
            kb.act(jk[:], xt[:], AF.Square, [t_xt, t_ss], [t_jk, t_ss], scale=1.0 / 32.0, accum_out=ss[:])
            kb.act(ss[:], ss[:], AF.Sqrt, [t_ss, t_eps], [t_ss], bias=epsc[:, 0:1])
            d.update(ss=ss, t_ss=t_ss)

        def s2():
            xt, t_xt, ss, t_ss = d["xt"], d["t_xt"], d["ss"], d["t_ss"]
            kb.recip(ss[:], ss[:], [t_ss], [t_ss])
            h_, t_h = hb.next()
            kb.stt("dve", h_[:], xt[:], ss[:, 0:1], gain_mix[:], ALU.mult, ALU.mult, [t_xt, t_ss, t_gmix], [t_h])
            d.update(h=h_, t_h=t_h)

        def s3():
            ps, t_ps = psA0.next()
            psv = bfv(ps).rearrange("p (k c) -> p k c", k=8)
            for k in range(8):
                kb.tr(psv[:, k, :], d["h"][:, k * 128:(k + 1) * 128], ident_bf[:], [d["t_h"], t_identbf], [t_ps])
            d.update(psv=psv, t_ps=t_ps)

        def s4():
            kb.cp("act" if t % 2 else "dve", hT[:, :, t * 128:(t + 1) * 128], d["psv"], [d["t_ps"]], [t_hT[t]])

        return [s0, s1, s2, s3, s4]

    pipeline([a0_unit(t) for t in range(NT)])

    if stop == "A0":
        return finish_early()
    P.barrier()
    kb.off = a_off
    qg = kb.sb([128, 64], F32, "qg"); t_qg = T()
    kg = kb.sb([128, 64], F32, "kg"); t_kg = T()
    kb.dma("sp", qg[:], q_norm.partition_broadcast(128), [], [t_qg])
    kb.dma("sp", kg[:], k_norm.partition_broadcast(128), [], [t_kg])
    kb.ts("dve", qg[:], qg[:], 0.125, ALU.mult, [t_qg], [t_qg])
    convw = kb.sb([128, 24, 4], F32, "convw"); t_convw = T()
    kb.dma("sp", convw[:], conv_w[:, :, :], [], [t_convw])
    dtb = kb.sb([128, 8], F32, "dtb"); t_dtb = T()
    negA = kb.sb([128, 8], F32, "negA"); t_negA = T()
    kb.dma("sp", dtb[:], dt_bias.partition_broadcast(128), [], [t_dtb])
    kb.dma("sp", negA[:], a_log.partition_broadcast(128), [], [t_negA])
    kb.act(negA[:], negA[:], AF.Exp, [t_negA], [t_negA])
    kb.ts("dve", negA[:], negA[:], -1.0, ALU.mult, [t_negA], [t_negA])

    wring = kb.ring(2, [128, 8, 512], BF16, "wg")
    wsm = kb.sb([128, 8, 16], BF16, "wsm"); t_wsm = T()
    psA = kb.psring([0, 1, 2, 3, 4, 5], 512, "psA")
    psS = kb.psring([6, 7], 512, "psS")
    f32r = kb.ring(3, [128, 512], F32, "f32r")
    f32r2 = kb.ring(3, [128, 512], F32, "f32r2")
    bfr = kb.ring(4, [128, 512], BF16, "bfr")
    s8r = kb.ring(4, [128, 8], F32, "s8")
    rawr = kb.ring(3, [128, 515], F32, "raw")
    carry = [(kb.sb([128, 3], F32, "carry"), T()) for _ in range(4)]
    yr = kb.ring(6, [128, 512], F32, "y")
    sqr = kb.ring(3, [128, 512], BF16, "sqb")
    rnr = kb.ring(3, [128, 512], F32, "rn")
    bgr = kb.ring(3, [128, 16], F32, "bg")
    w_in_v = w_in.rearrange("(k p) c -> p k c", p=128)

    def col0(cg):
        if cg < 17:
            return cg * 512
        if cg == 17:
            return 8704
        return 8720 + (cg - 18) * 512

    def load_W(cg):
        c0 = col0(cg)
        if cg == 17:
            kb.dma("gq", wsm[:], w_in_v[:, :, c0:c0 + 16], [], [t_wsm])
            return wsm, t_wsm
        W, t_W = wring.next()
        kb.dma("gq", W[:, 0:4, :], w_in_v[:, 0:4, c0:c0 + 512], [], [t_W])
        kb.dma("gq", W[:, 4:8, :], w_in_v[:, 4:8, c0:c0 + 512], [], [t_W])
        return W, t_W

    def conv_unit(cg, s, cb, W, t_W):
        cbg = (cg - 9) * 4 + cb
        which = cbg // 8
        d = {}

        def sa():
            ps, t_ps = psA.next()
            for k in range(8):
                kb.mm(ps, W[:, k, cb * 128:(cb + 1) * 128], hT[:, k, s * 512:(s + 1) * 512],
                      [t_W] + t_hT[4 * s:4 * s + 4], [t_ps], start=(k == 0), stop=(k == 7))
            raw, t_raw = rawr.next()
            cy, t_cy = carry[cb]
            if s == 0:
                kb.memset("pool", raw[:, 0:3], 0.0, [t_raw])
            else:
                kb.cp("pool", raw[:, 0:3], cy[:], [t_cy], [t_raw])
            kb.cp("act", raw[:, 3:515], ps, [t_ps], [t_raw])
            kb.cp("pool", cy[:], raw[:, 512:515], [t_raw], [t_cy])
            y, t_y = yr.next()
            kb.act(y[:], ps, AF.Copy, [t_ps, t_convw], [t_y], scale=convw[:, cbg, 3:4])
            d.update(raw=raw, t_raw=t_raw, y=y, t_y=t_y)

        def sb_():
            raw, t_raw = d["raw"], d["t_raw"]
            y, t_y = d["y"], d["t_y"]
            for j in range(3):
                kb.stt("dve", y[:], raw[:, j:j + 512], convw[:, cbg, j:j + 1], y[:], ALU.mult, ALU.add,
                       [t_raw, t_convw, t_y], [t_y])

        def sc():
            y, t_y = d["y"], d["t_y"]
            if which == 2:
                ob, t_ob = bfr.next()
                kb.act(ob[:], y[:], AF.Silu, [t_y], [t_ob])
                kb.dma("sp", qkvb_s[cbg, :, s * 512:(s + 1) * 512], ob[:], [t_ob], [])
            else:
                kb.act(y[:], y[:], AF.Silu, [t_y], [t_y])
                sq, t_sq = sqr.next()
                kb.tt("pool", sq[:], y[:], y[:], ALU.mult, [t_y], [t_sq])
                d.update(sq=sq, t_sq=t_sq)

        def sd():
            if which == 2:
                return
            pss, t_pss = psS.next()
            kb.mm(pss, ones_bf[:], d["sq"][:], [t_ones, d["t_sq"]], [t_pss])
            rn, t_rn = rnr.next()
            if which == 0:
                kb.act(rn[:], pss, AF.Sqrt, [t_pss, t_eps128], [t_rn], bias=eps128[:, 0:1], scale=128.0)
            else:
                kb.act(rn[:], pss, AF.Sqrt, [t_pss, t_eps], [t_rn], bias=epsc[:, 0:1])
            d.update(rn=rn, t_rn=t_rn)

        def se():
            if which == 2:
                return
            rn, t_rn, y, t_y = d["rn"], d["t_rn"], d["y"], d["t_y"]
            kb.recip(rn[:], rn[:], [t_rn], [t_rn])
            ob, t_ob = bfr.next()
            kb.tt("pool", ob[:], y[:], rn[:], ALU.mult, [t_y, t_rn], [t_ob])
            kb.dma("sp", qkvb_s[cbg, :, s * 512:(s + 1) * 512], ob[:], [t_ob], [])

        return [sa, sb_, sc, sd, se]

    Wnext = load_W(0)
    for cg in range(22):
        c0 = col0(cg)
        W, t_W = Wnext
        if cg + 1 < 22:
            Wnext = load_W(cg + 1)
        if 9 <= cg <= 14:
            units = []
            for s in range(NSP):
                for cb in range(4):
                    units.append(conv_unit(cg, s, cb, W, t_W))
            pipeline(units)
        else:
            for t in range(NT):
                rows = slice(t * 128, (t + 1) * 128)
                ps, t_ps = psA.next()
                ncol = 16 if cg == 17 else 512
                pso = ps[:, 0:ncol]
                for k in range(8):
                    kb.mm(pso, hT[:, k, t * 128:(t + 1) * 128], W[:, k, :], [t_W, t_hT[t]], [t_ps],
                          start=(k == 0), stop=(k == 7))
                if cg < 6:
                    gain, t_gain = (qg, t_qg) if cg < 3 else (kg, t_kg)
                    sqf, t_sqf = f32r.next()
                    kb.act(sqf[:], ps, AF.Square, [t_ps], [t_sqf], scale=0.125)
                    s8, t_s8 = s8r.next()
                    kb.red("dve", s8[:], sqf[:].rearrange("p (h d) -> p h d", d=64), [t_sqf], [t_s8])
                    rms_rstd(s8[:], t_s8)
                    tmp, t_tmp = f32r2.next()
                    kb.tt("dve", tmp[:].rearrange("p (h d) -> p h d", d=64), ps.rearrange("p (h d) -> p h d", d=64),
                          s8[:, :].unsqueeze(2).to_broadcast([128, 8, 64]), ALU.mult, [t_ps, t_s8], [t_tmp])
                    ob, t_ob = bfr.next()
                    kb.tt("pool", ob[:].rearrange("p (h d) -> p h d", d=64), tmp[:].rearrange("p (h d) -> p h d", d=64),
                          gain[:, :].unsqueeze(1).to_broadcast([128, 8, 64]), ALU.mult, [t_tmp, t_gain], [t_ob])
                    kb.dma("sp", qkva_s[cg, rows, :], ob[:], [t_ob], [])
                elif cg < 9:
                    ob, t_ob = bfr.next()
                    kb.cp("act" if t % 2 else "dve", ob[:], ps, [t_ps], [t_ob])
                    kb.dma("sp", qkva_s[cg, rows, :], ob[:], [t_ob], [])
                elif cg in (15, 16):
                    ob, t_ob = bfr.next()
                    kb.act(ob[:], ps, AF.Silu, [t_ps], [t_ob])
                    kb.dma("sp", z_s[rows, (cg - 15) * 512:(cg - 14) * 512], ob[:], [t_ob], [])
                elif cg == 17:
                    bg, t_bg = bgr.next()
                    kb.act(bg[:, 0:8], ps[:, 0:8], AF.Sigmoid, [t_ps], [t_bg])
                    kb.tt("dve", bg[:, 8:16], ps[:, 8:16], dtb[:], ALU.add, [t_ps, t_dtb], [t_bg])
                    kb.act(bg[:, 8:16], bg[:, 8:16], AF.Exp, [t_bg], [t_bg])
                    kb.act(bg[:, 8:16], bg[:, 8:16], AF.Ln, [t_bg], [t_bg], bias=1.0)
                    kb.tt("dve", bg[:, 8:16], bg[:, 8:16], negA[:], ALU.mult, [t_bg, t_negA], [t_bg])
                    kb.dma("sp", bg_s[rows, :], bg[:], [t_bg], [])
                else:
                    ob, t_ob = bfr.next()
                    kb.act(ob[:], ps, AF.Sigmoid, [t_ps], [t_ob])
                    kb.dma("sp", gates_s[rows, (cg - 18) * 512:(cg - 17) * 512], ob[:], [t_ob], [])

    print("sbuf end A", kb.off)
    if stop == "A":
        return finish_early()
    P.barrier()
    kb.off = persist_off
    qr_ = kb.ring(3, [128, 512], BF16, "qb")
    kr_ = kb.ring(3, [128, 512], BF16, "kb")
    vst_r = kb.ring(3, [128, 512], BF16, "vst")
    vr_ = kb.ring(6, [128, 8, 65], BF16, "v1")
    for v1, t_v1 in vr_.slots:
        kb.memset("pool", v1[:, :, 64:65], 1.0, [t_v1])
    qTr = kb.ring(3, [128, 8, 128], BF16, "qT")
    for qz, t_qz in qTr.slots:
        kb.memset("pool", qz[:], 0.0, [t_qz])
    kTr = kb.ring(4, [128, 4, 128], BF16, "kT")
    er_ = kb.ring(4, [128, 4, 2, 128], BF16, "E")
    ur_ = kb.ring(3, [128, 8, 65], F32, "U")
    sc_slots = Ring([((kb.banks[0][:, :], kb.banks[1][:, :]), kb.pst(0, 1)), ((kb.banks[2][:, :], kb.banks[3][:, :]), kb.pst(2, 3))])
    pv_slots = Ring([(kb.banks[4][:, :], kb.pst(4)), (kb.banks[5][:, :], kb.pst(5))])
    psT = Ring([(kb.banks[6][:, :], kb.pst(6)), (kb.banks[7][:, :], kb.pst(7))])
    ATT = ((128, 1), (512, 4), (2048, 16))

    def attn_unit(g, dil, r_, nb, prev):
        qs = qkva_s[g].rearrange("(u r) c -> r u c", r=dil)
        ks = qkva_s[3 + g].rearrange("(u r) c -> r u c", r=dil)
        vs = qkva_s[6 + g].rearrange("(u r) c -> r u c", r=dil)
        us = ua_s[g].rearrange("(u r) c -> r u c", r=dil)
        ur = slice(nb * 128, (nb + 1) * 128)
        have_prev = nb > 0
        d = {}

        def s0():
            qb_, t_qb = qr_.next()
            kb_, t_kb = kr_.next()
            vst, t_vst = vst_r.next()
            kb.dma("sp", qb_[:], qs[r_, ur, :], [], [t_qb])
            kb.dma("sp", kb_[:], ks[r_, ur, :], [], [t_kb])
            kb.dma("sp", vst[:], vs[r_, ur, :], [], [t_vst])
            d.update(qb=qb_, t_qb=t_qb, kb=kb_, t_kb=t_kb, vst=vst, t_vst=t_vst)

        def s1():
            v1, t_v1 = vr_.next()
            kb.cp("dve", v1[:, :, 0:64], d["vst"][:].rearrange("p (h d) -> p h d", d=64), [d["t_vst"]], [t_v1])
            pt, t_pt = psT.next()
            ptv = bfv(pt).rearrange("p (a h c) -> p a h c", a=2, h=4)
            for hp in range(4):
                kb.tr(ptv[:, 0, hp, :], d["qb"][:, hp * 128:(hp + 1) * 128], ident_bf[:], [d["t_qb"], t_identbf], [t_pt])
                kb.tr(ptv[:, 1, hp, :], d["kb"][:, hp * 128:(hp + 1) * 128], ident_bf[:], [d["t_kb"], t_identbf], [t_pt])
            d.update(v1=v1, t_v1=t_v1, ptv=ptv, t_pt=t_pt)

        def s2():
            qT, t_qT = qTr.next()
            kT, t_kT = kTr.next()
            ptv, t_pt = d["ptv"], d["t_pt"]
            qTv = qT[:].rearrange("p (hp j) c -> p hp j c", j=2)
            kb.cp("dve", qTv[0:64, :, 0, :], ptv[0:64, 0], [t_pt], [t_qT])
            kb.cp("dve", qTv[64:128, :, 1, :], ptv[64:128, 0], [t_pt], [t_qT])
            kb.cp("act", kT[:], ptv[:, 1], [t_pt], [t_kT])
            d.update(qT=qT, t_qT=t_qT, kT=kT, t_kT=t_kT)

        def mk_half(half):
            hd = {}

            def h3():
                (b0, b1), t_sc = sc_slots.next()
                qT, t_qT, kT, t_kT = d["qT"], d["t_qT"], d["kT"], d["t_kT"]
                for hh in range(4):
                    h = half * 4 + hh
                    hp = h // 2
                    bank = b0 if hh < 2 else b1
                    base = (hh % 2) * 256
                    if have_prev:
                        kb.mm(bank[:, base:base + 128], prev["kT"][:, hp, :], qT[:, h, :],
                              [prev["t_kT"], t_qT], [t_sc])
                    kb.mm(bank[:, base + 128:base + 256], kT[:, hp, :], qT[:, h, :],
                          [t_kT, t_qT], [t_sc])
                hd.update(b0=b0, b1=b1, t_sc=t_sc)

            def h4():
                E, t_E = er_.next()
                t_sc = hd["t_sc"]
                for bi, bank in enumerate((hd["b0"], hd["b1"])):
                    ev = E[:, 2 * bi:2 * bi + 2, :, :]
                    bv = bank.rearrange("p (h a c) -> p h a c", h=2, a=2)
                    if have_prev:
                        kb.act(ev, bv, AF.Exp, [t_sc], [t_E])
                    else:
                        kb.act(ev[:, :, 1, :], bv[:, :, 1, :], AF.Exp, [t_sc], [t_E])
                hd.update(E=E, t_E=t_E)

            def h5():
                E, t_E = hd["E"], hd["t_E"]
                if have_prev:
                    kb.tt("pool", E[:, :, 0, :], E[:, :, 0, :], tril_bf[:, :].unsqueeze(1).to_broadcast([128, 4, 128]),
                          ALU.mult, [t_E, t_trilbf], [t_E])
                kb.tt("dve", E[:, :, 1, :], E[:, :, 1, :], triu_bf[:, :].unsqueeze(1).to_broadcast([128, 4, 128]),
                      ALU.mult, [t_E, t_triubf], [t_E])

            def h6():
                E, t_E = hd["E"], hd["t_E"]
                pv, t_pv = pv_slots.next()
                pvv = pv[:, 0:260].rearrange("p (h c) -> p h c", h=4)
                for hh in range(4):
                    h = half * 4 + hh
                    if have_prev:
                        kb.mm(pvv[:, hh, :], E[:, hh, 0, :], prev["v1"][:, h, :], [t_E, prev["t_v1"]], [t_pv], start=True, stop=False)
                    kb.mm(pvv[:, hh, :], E[:, hh, 1, :], d["v1"][:, h, :], [t_E, d["t_v1"]], [t_pv], start=not have_prev, stop=True)
                hd.update(pvv=pvv, t_pv=t_pv)

            def h7():
                if half == 0:
                    U_, t_U = ur_.next()
                    d.update(U=U_, t_U=t_U)
                U_, t_U = d["U"], d["t_U"]
                kb.cp("act" if half else "dve", U_[:, half * 4:half * 4 + 4, :], hd["pvv"], [hd["t_pv"]], [t_U])
                if half == 1:
                    kb.dma("sp", us[r_, ur, :], U_[:].rearrange("p h c -> p (h c)"), [t_U], [])

            return [h3, h4, h5, h6, h7]

        return d, [[s0, s1, s2] + mk_half(0), [None, None, None] + mk_half(1)]

    items = []
    for g, (win, dil) in enumerate(ATT):
        nblk = S // dil // 128
        for r_ in range(dil):
            prev = None
            for nb in range(nblk):
                prev, its = attn_unit(g, dil, r_, nb, prev)
                items.extend(its)
    pipeline(items)

    print("sbuf end B", kb.off)
    if stop == "B":
        return finish_early()
    P.barrier()
    kb.off = persist_off
    dng = kb.sb([128, 128], F32, "dng"); t_dng = T()
    kb.dma("sp", dng[:], dn_out_norm.partition_broadcast(128), [], [t_dng])
    S32 = [(kb.sb([128, 128], F32, "S32"), T()) for _ in range(8)]
    Sbf = [(kb.sb([128, 128], BF16, "Sbf"), T()) for _ in range(8)]
    for h in range(8):
        kb.memset("pool", S32[h][0][:], 0.0, [S32[h][1]])
        kb.memset("pool", Sbf[h][0][:], 0.0, [Sbf[h][1]])
    qkvr = kb.ring(2, [128, 24, 128], BF16, "qkvT")
    bgr2 = kb.ring(2, [128, 16], F32, "bgc")
    zr = kb.ring(2, [128, 1024], BF16, "z")
    gbc_r = kb.ring(2, [128, 8, 128], F32, "gbc")
    gcum_r = kb.ring(2, [128, 8], F32, "gcum")
    ngc_r = kb.ring(2, [128, 8], F32, "ngc")
    eg_r = kb.ring(2, [128, 8], F32, "eg")
    neg_r = kb.ring(2, [128, 8], F32, "neg")
    kts_r = kb.ring(2, [128, 8], F32, "kts")
    egl_r = kb.ring(2, [128, 8], F32, "egl")
    H = 8
    pre_r = kb.ring(4, [128, 128], F32, "pre")
    dtm_r = kb.ring(2 * H, [128, 128], F32, "dtm")
    dts_r = kb.ring(2 * H, [128, 128], F32, "dts")
    XA_r = Ring([(kb.sb([128, 256], F32R, "XA"), T("xs"), T("xr")) for _ in range(2 * H + 2)])
    XB_r = Ring([(kb.sb([128, 256], F32R, "XB"), T("xb")) for _ in range(2 * H + 2)])
    zf = kb.sb([128, 256], F32, "zf"); t_zf = T()
    kb.memset("pool", zf[:], 0.0, [t_zf])
    for xa_, t1_, t2_ in XA_r.slots:
        kb.cp("dve", xa_[:], zf[:], [t_zf], [t1_, t2_])
    for xb_, t1_ in XB_r.slots:
        kb.cp("dve", xb_[:], zf[:], [t_zf], [t1_])
    PT_r = kb.ring(2 * H, [128, 128], BF16, "PT")
    AIT_r = kb.ring(2 * H, [128, 128], BF16, "AIT")
    Ktl_r = kb.ring(2 * H, [128, 128], BF16, "Ktl")
    Vtm_r = kb.ring(2 * H, [128, 128], BF16, "Vtm")
    Y_r = kb.ring(4, [128, 128], BF16, "Y")
    vn_r = kb.ring(4, [128, 128], BF16, "vn")
    o1_r = kb.ring(4, [128, 128], F32, "o1")
    O_r = kb.ring(2, [128, 8, 128], F32, "O")
    osq_r = kb.ring(2, [128, 8, 128], F32, "osq")
    obf_r = kb.ring(2, [128, 1024], BF16, "obf")
    s8c_r = kb.ring(2, [128, 8], F32, "s8c")
    glast_r = kb.ring(2, [128, 8], F32, "glast")
    psN = kb.psring([0, 1, 2, 3], 256, "psN")
    psXO = kb.psring([4, 5], 128, "psXO")
    psG = kb.psring([6], 128, "psG")
    psX = Ring([(kb.banks[7][:, 0:128], kb.pst(7)), (kb.banks[7][:, 128:256], kb.pst(7))])
    psB = Ring([(kb.banks[7][:, 256 + 64 * i:256 + 64 * (i + 1)], kb.pst(7)) for i in range(4)])

    def chunk_gen(b):
        cols = slice(b * 128, (b + 1) * 128)
        qkvT, t_qkvT = qkvr.next()
        bgc, t_bgc = bgr2.next()
        zt, t_zt = zr.next()
        kb.dma("sp", qkvT[:], qkvb_s[:, :, cols].rearrange("c p t -> p c t"), [], [t_qkvT])
        kb.dma("sp", bgc[:], bg_s[cols, :], [], [t_bgc])
        kb.dma("sp", zt[:], z_s[cols, :], [], [t_zt])
        gcum, t_gcum = gcum_r.next()
        psg, t_psg = psG.next()
        kb.mm(psg[:, 0:8], triu[:], bgc[:, 8:16], [t_triu, t_bgc], [t_psg])
        kb.cp("dve", gcum[:], psg[:, 0:8], [t_psg], [t_gcum])
        gbc, t_gbc = gbc_r.next()
        kb.cp("pool", gbc[:], bgc[:, 8:16].unsqueeze(2).to_broadcast([128, 8, 128]), [t_bgc], [t_gbc])
        eg, t_eg = eg_r.next()
        neg, t_neg = neg_r.next()
        kb.act(eg[:], gcum[:], AF.Exp, [t_gcum], [t_eg])
        kb.ts("dve", neg[:], eg[:], -1.0, ALU.mult, [t_eg], [t_neg])
        kts, t_kts = kts_r.next()
        egl, t_egl = egl_r.next()
        O_, t_O = O_r.next()
        glast, t_glast = glast_r.next()
        st = []
        for h in range(H):
            psg, t_psg = psG.next()
            kb.mm(psg, gbc[:, h, :], triu[:], [t_gbc, t_triu], [t_psg])
            pre, t_pre = pre_r.next()
            kb.stt("dve", pre[:], psg, gcum[:, h:h + 1], mbm[:], ALU.subtract, ALU.add, [t_psg, t_gcum, t_mbm], [t_pre])
            dtm, t_dtm = dtm_r.next()
            kb.act(dtm[:], pre[:], AF.Exp, [t_pre], [t_dtm])
            dts, t_dts = dts_r.next()
            kb.tt("pool", dts[:], dtm[:], strict[:], ALU.mult, [t_dtm, t_strict], [t_dts])
            kb.cp("act", glast[:, h:h + 1], psg[:, 127:128], [t_psg], [t_glast])
            st.append(dict(dtm=dtm, t_dtm=t_dtm, dts=dts, t_dts=t_dts))
        kb.act(egl[:], glast[:], AF.Exp, [t_glast], [t_egl])
        kb.tt("dve", kts[:], glast[:], gcum[:], ALU.subtract, [t_glast, t_gcum], [t_kts])
        kb.act(kts[:], kts[:], AF.Exp, [t_kts], [t_kts])
        for h in range(H):
            d = st[h]
            dtm, t_dtm, dts, t_dts = d["dtm"], d["t_dtm"], d["dts"], d["t_dts"]
            KT = qkvT[:, 8 + h, :]
            QT = qkvT[:, h, :]
            VT = qkvT[:, 16 + h, :]
            pk, t_pk = psN.next()
            kb.mm(pk, KT, qkvT[:, :, :].rearrange("p (a h) t -> p a h t", a=3)[:, 0:2, h, :], [t_qkvT], [t_pk])
            XA0, t_B, t_XR0 = XA_r.next()
            kb.stt("dve", XA0[:, 0:128], pk[:, 128:256], bgc[:, h:h + 1], dts[:], ALU.mult, ALU.mult, [t_pk, t_bgc, t_dts], [t_B])
            AIT, t_AIT = AIT_r.next()
            kb.tt("dve", AIT[:], pk[:, 0:128], dtm[:], ALU.mult, [t_pk, t_dtm], [t_AIT])
            pb, t_pb = psB.next()
            kb.tr(bfv(pb), KT, ident_bf[:], [t_qkvT, t_identbf], [t_pb])
            Ktl, t_Ktl = Ktl_r.next()
            kb.act(Ktl[:], bfv(pb), AF.Copy, [t_pb, t_kts], [t_Ktl], scale=kts[:, h:h + 1])
            pb2, t_pb2 = psB.next()
            kb.tr(bfv(pb2), VT, ident_bf[:], [t_qkvT, t_identbf], [t_pb2])
            Vtm, t_Vtm = Vtm_r.next()
            kb.cp("act", Vtm[:], bfv(pb2), [t_pb2], [t_Vtm])
            d.update(XA0=XA0, t_B=t_B, t_XR0=t_XR0, AIT=AIT, t_AIT=t_AIT, Ktl=Ktl, t_Ktl=t_Ktl, Vtm=Vtm, t_Vtm=t_Vtm, KT=KT, QT=QT)
        yield "pre"
        for h in range(H):
            d = st[h]
            pt_, t_pt_ = psN.next()
            kb.tr(pt_[:, 0:128], d["XA0"][:, 0:128].bitcast(F32), ident[:], [d["t_B"], t_ident], [t_pt_])
            XB, t_XB = XB_r.next()
            kb.cp("act", XB[:, 0:128], pt_[:, 0:128], [t_pt_], [t_XB])
            XA1, t_S1, t_R1 = XA_r.next()
            kb.tt("dve", XA1[:, 128:256], ident[:], d["XA0"][:, 0:128].bitcast(F32), ALU.subtract, [t_ident, d["t_B"]], [t_R1])
            d.update(XA=d["XA0"], t_S=d["t_B"], t_R=d["t_XR0"], XB=XB, t_XB=t_XB, XAn=XA1, t_Sn=t_S1, t_Rn=t_R1)
        yield "lv"
        NL = 7
        for lvl in range(1, NL + 1):
            last = lvl == NL
            for h in range(H):
                d = st[h]
                XA, XB = d["XA"], d["XB"]
                p1, t_p1 = psN.next()
                kb.mm(p1, XB[:, 0:128], XA[:, :], [d["t_S"], d["t_R"], d["t_XB"]], [t_p1])
                if last:
                    PT, t_PT = PT_r.next()
                    kb.tt("dve", PT[:], p1[:, 128:256], XA[:, 128:256].bitcast(F32), ALU.add, [t_p1, d["t_R"]], [t_PT])
                    d.update(PT=PT, t_PT=t_PT)
                    continue
                p2, t_p2 = psN.next()
                kb.mm(p2, XA[:, 0:128], XB[:, :], [d["t_S"], d["t_XB"]], [t_p2])
                if lvl == 1:
                    XAn, t_Sn, t_Rn = d["XAn"], d["t_Sn"], d["t_Rn"]
                else:
                    XAn, t_Sn, t_Rn = XA_r.next()
                    kb.tt("dve", XAn[:, 128:256], p1[:, 128:256], XA[:, 128:256].bitcast(F32), ALU.add, [t_p1, d["t_R"]], [t_Rn])
                kb.cp("dve", XAn[:, 0:128], p1[:, 0:128], [t_p1], [t_Sn])
                XBn, t_XBn = XB_r.next()
                kb.cp("act", XBn[:, 0:128], p2[:, 0:128], [t_p2], [t_XBn])
                d.update(XA=XAn, t_S=t_Sn, t_R=t_Rn, XB=XBn, t_XB=t_XBn)
            yield ("inv_done" if last else "lv")
        for hg in range(0, H, 4):
            for h in range(hg, hg + 4):
                d = st[h]
                sbf, t_sbf = Sbf[h]
                px, t_px = psXO.next()
                kb.mm(px, d["KT"], sbf[:], [t_qkvT, t_sbf], [t_px])
                po1, t_po1 = psXO.next()
                kb.mm(po1, d["QT"], sbf[:], [t_qkvT, t_sbf], [t_po1])
                d.update(px=px, t_px=t_px, po1=po1, t_po1=t_po1)
            yield "sc"
            for h in range(hg, hg + 4):
                d = st[h]
                Y, t_Y = Y_r.next()
                kb.stt("dve", Y[:], d["px"], neg[:, h:h + 1], d["Vtm"][:], ALU.mult, ALU.add,
                       [d["t_px"], t_neg, d["t_Vtm"]], [t_Y])
                o1, t_o1 = o1_r.next()
                kb.act(o1[:], d["po1"], AF.Copy, [d["t_po1"], t_eg], [t_o1], scale=eg[:, h:h + 1])
                ppy, t_ppy = psX.next()
                kb.mm(ppy, d["PT"][:], Y[:], [d["t_PT"], t_Y], [t_ppy])
                vn, t_vn = vn_r.next()
                kb.act(vn[:], ppy, AF.Copy, [t_ppy, t_bgc], [t_vn], scale=bgc[:, h:h + 1])
                d.update(vn=vn, t_vn=t_vn, o1=o1, t_o1=t_o1)
            yield "sc"
            for h in range(hg, hg + 4):
                d = st[h]
                vn, t_vn = d["vn"], d["t_vn"]
                s32, t_s32 = S32[h]
                sbf, t_sbf = Sbf[h]
                pso, t_pst = psN.next()
                pst, po2, t_po2 = pso[:, 0:128], pso[:, 128:256], t_pst
                kb.mm(pst, d["Ktl"][:], vn[:], [d["t_Ktl"], t_vn], [t_pst])
                kb.mm(po2, d["AIT"][:], vn[:], [d["t_AIT"], t_vn], [t_po2])
                kb.stt("dve", s32[:], s32[:], egl[:, h:h + 1], pst, ALU.mult, ALU.add, [t_s32, t_egl, t_pst], [t_s32])
                kb.cp("act", sbf[:], s32[:], [t_s32], [t_sbf])
                kb.tt("dve", O_[:, h, :], po2, d["o1"][:], ALU.add, [t_po2, d["t_o1"]], [t_O])
            if hg == 0:
                yield "sc"
        osq, t_osq = osq_r.next()
        kb.act(osq[:], O_[:], AF.Square, [t_O], [t_osq], scale=float(128.0 ** -0.5))
        s8, t_s8 = s8c_r.next()
        kb.red("dve", s8[:], osq[:], [t_osq], [t_s8])
        kb.act(s8[:], s8[:], AF.Sqrt, [t_s8, t_eps], [t_s8], bias=epsc[:, 0:1])
        kb.recip(s8[:], s8[:], [t_s8], [t_s8])
        kb.tt("pool", osq[:], O_[:], s8[:, :].unsqueeze(2).to_broadcast([128, 8, 128]), ALU.mult, [t_O, t_s8], [t_osq])
        kb.tt("pool", osq[:], osq[:], dng[:, :].unsqueeze(1).to_broadcast([128, 8, 128]), ALU.mult, [t_osq, t_dng], [t_osq])
        obf, t_obf = obf_r.next()
        kb.tt("dve", obf[:].rearrange("p (h d) -> p h d", d=128), osq[:], zt[:].rearrange("p (h d) -> p h d", d=128),
              ALU.mult, [t_osq, t_zt], [t_obf])
        kb.dma("sp", ob_s[cols, :], obf[:], [t_obf], [])
        yield "done"


    gens = [chunk_gen(b) for b in range(NT)]
    for i in range(NT + 1):
        cur = gens[i] if i < NT else None
        prv = gens[i - 1] if i >= 1 else None
        cur_live, prv_live = cur is not None, prv is not None
        while cur_live or prv_live:
            if prv_live and next(prv) == "done":
                prv_live = False
            if cur_live and next(cur) == "inv_done":
                cur_live = False

    print("sbuf end C", kb.off)
    if stop == "C":
        return finish_early()
    P.barrier()
    kb.off = persist_off
    TC = 8 if NT >= 8 else NT
    wa = kb.sb([128, 4, 1024], BF16, "wa"); t_wa = T()
    wb = kb.sb([128, 8, 1024], BF16, "wb"); t_wb = T()
    wo = kb.sb([128, 8, 1024], BF16, "wo"); t_wo = T()
    wpg = kb.sb([128, 8, 1024], BF16, "wpg"); t_wpg = T()
    wpl = kb.sb([128, 2, 1024], BF16, "wpl"); t_wpl = T()
    kb.dma("gq", wa[:], w_branch_a.rearrange("(k p) c -> p k c", p=128), [], [t_wa])
    kb.dma("gq", wb[:], w_branch_b.rearrange("(k p) c -> p k c", p=128), [], [t_wb])
    kb.dma("gq", wo[:], w_out.rearrange("(k p) c -> p k c", p=128), [], [t_wo])
    kb.dma("gq", wpg[:], w_pg.rearrange("(k p) c -> p k c", p=128), [], [t_wpg])
    kb.dma("gq", wpl[:], w_ple.rearrange("(k p) c -> p k c", p=128), [], [t_wpl])
    wr = kb.sb([128, 8, 20], F32, "wr"); t_wr = T()
    kb.dma("sp", wr[:, :, 0:4], w_rg.rearrange("(k p) c -> p k c", p=128), [], [t_wr])
    kb.dma("sp", wr[:, :, 4:20], w_re.rearrange("(k p) c -> p k c", p=128), [], [t_wr])
    br = kb.sb([128, 20], F32, "br"); t_br = T()
    kb.dma("sp", br[:, 0:4], b_rg.partition_broadcast(128), [], [t_br])
    kb.dma("sp", br[:, 4:20], b_re.partition_broadcast(128), [], [t_br])
    gffn = kb.sb([128, 1024], F32, "gffn"); t_gffn = T()
    gple = kb.sb([128, 1024], F32, "gple"); t_gple = T()
    kb.dma("sp", gffn[:], norm_ffn.partition_broadcast(128), [], [t_gffn])
    kb.dma("sp", gple[:], norm_ple.partition_broadcast(128), [], [t_gple])
    yacc = [(kb.sb([128, 1024], F32, "yacc"), T()) for _ in range(TC)]
    h2T = [(kb.sb([128, 8, 128], BF16, "h2T"), T()) for _ in range(TC)]
    comb = [(kb.sb([128, 16], F32, "comb"), T()) for _ in range(TC)]
    wgu_r = kb.ring(2, [128, 8, 512], BF16, "wgu")
    wd_r = kb.ring(2, [128, 2, 1024], BF16, "wdn")
    u_r = kb.ring(3, [128, 520], F32, "ua")
    un_r = kb.ring(2, [128, 8, 65], F32, "un")
    ga_r = kb.ring(1, [128, 2048], BF16, "gates")
    obr = kb.ring(2, [128, 1024], BF16, "ob")
    oab_r = kb.ring(2, [128, 512], BF16, "oab")
    rden_r = kb.ring(2, [128, 8], F32, "rden")
    tT_r = kb.ring(2, [128, 8, 128], BF16, "tT")
    mg_r = kb.ring(1, [128, 1024], F32, "mg")
    mgb_r = kb.ring(2, [128, 1024], BF16, "mgb")
    hf_r = kb.ring(1, [128, 1024], F32, "hf")
    hfT_r = kb.ring(1, [128, 8, 128], F32, "hfT")
    hbf_r = kb.ring(2, [128, 1024], BF16, "hbf")
    jk_r = kb.ring(1, [128, 1024], BF16, "jk2")
    ss_r = kb.ring(4, [128, 1], F32, "ss2")
    lg_r = kb.ring(2, [128, 20], F32, "lg")
    sm_r = kb.ring(24, [128, 16], F32, "sm")
    sil_r = kb.ring(3, [128, 256], F32, "sil")
    act_r = kb.ring(4, [128, 256], BF16, "actb")
    actT_r = kb.ring(4, [128, 2, 128], BF16, "actT")
    pin_r = kb.ring(2, [128, 256], F32, "pin")
    pbf_r = kb.ring(2, [128, 256], BF16, "pbf")
    sg_r = kb.ring(1, [128, 1024], F32, "sg")
    psM = kb.psring([0, 1, 2], 512, "psM")
    psTr = kb.psring([4, 5], 512, "psTr")
    psD = kb.psring([3, 6, 7], 512, "psD")
    final_ops = []

    def transpose_bf(src, t_src, nk):
        ps, t_ps = psTr.next()
        psv = bfv(ps).rearrange("p (k c) -> p k c", k=8)
        for k in range(nk):
            kb.tr(psv[:, k, :], src[:, k * 128:(k + 1) * 128], ident_bf[:], [t_src, t_identbf], [t_ps])
        tT, t_tT = tT_r.next()
        kb.cp("act", tT[:, 0:nk, :], psv[:, 0:nk, :], [t_ps], [t_tT])
        return tT, t_tT

    def proj(tT, t_tT, W, t_W, nk):
        res = []
        for c in range(2):
            ps, t_ps = psM.next()
            for k in range(nk):
                kb.mm(ps, tT[:, k, :], W[:, k, c * 512:(c + 1) * 512], [t_tT, t_W], [t_ps], start=(k == 0), stop=(k == nk - 1))
            res.append((ps, t_ps))
        return res

    def rmsnorm_to(xsrc, t_x, gain, t_gain, out_ap, t_out):
        jk, t_jk = jk_r.next()
        ss, t_ss = ss_r.next()
        kb.memset("pool", ss[:], 0.0, [t_ss])
        kb.act(jk[:], xsrc, AF.Square, [t_x, t_ss], [t_jk, t_ss], scale=1.0 / 32.0, accum_out=ss[:])
        rms_rstd(ss[:], t_ss)
        kb.stt("dve", out_ap, xsrc, ss[:, 0:1], gain[:], ALU.mult, ALU.mult, [t_x, t_ss, t_gain], [t_out])

    def d_tile(c0, ti):
        t = c0 + ti
        rows = slice(t * 128, (t + 1) * 128)
        ya, t_ya = yacc[ti]
        kb.dma("sp", ya[:], x[rows, :], [], [t_ya])
        us_ = []
        for g in range(3):
            u, t_u = u_r.next()
            kb.dma("sp", u[:], ua_s[g, rows, :], [], [t_u])
            us_.append((u, t_u))
        ga, t_ga = ga_r.next()
        kb.dma("sp", ga[:], gates_s[rows, :], [], [t_ga])
        ob, t_ob = obr.next()
        kb.dma("sp", ob[:], ob_s[rows, :], [], [t_ob])
        un, t_un = un_r.next()
        unf = un[:].rearrange("p h c -> p (h c)")
        kb.tt("pool", unf, us_[0][0][:], us_[1][0][:], ALU.add, [us_[0][1], us_[1][1]], [t_un])
        kb.tt("pool", unf, unf, us_[2][0][:], ALU.add, [t_un, us_[2][1]], [t_un])
        rden, t_rden = rden_r.next()
        kb.recip(rden[:], un[:, :, 64], [t_un], [t_rden])
        oab, t_oab = oab_r.next()
        kb.tt("dve", oab[:].rearrange("p (h d) -> p h d", d=64), un[:, :, 0:64],
              rden[:, :].unsqueeze(2).to_broadcast([128, 8, 64]), ALU.mult, [t_un, t_rden], [t_oab])
        aT, t_aT = transpose_bf(oab, t_oab, 4)
        pa = proj(aT, t_aT, wa, t_wa, 4)
        mg, t_mg = mg_r.next()
        for c in range(2):
            cs = slice(c * 512, (c + 1) * 512)
            kb.tt("dve", mg[:, cs], pa[c][0], ga[:, c * 512:(c + 1) * 512], ALU.mult, [pa[c][1], t_ga], [t_mg])
        yield
        bT, t_bT = transpose_bf(ob, t_ob, 8)
        pb_ = proj(bT, t_bT, wb, t_wb, 8)
        mgb, t_mgb = mgb_r.next()
        for c in range(2):
            cs = slice(c * 512, (c + 1) * 512)
            kb.tt("dve", mgb[:, cs], pb_[c][0], ga[:, 1024 + c * 512:1024 + (c + 1) * 512], ALU.mult, [pb_[c][1], t_ga], [t_mgb])
        kb.tt("pool", mgb[:], mg[:], mgb[:], ALU.add, [t_mg, t_mgb], [t_mgb])
        yield
        mT, t_mT = transpose_bf(mgb, t_mgb, 8)
        po = proj(mT, t_mT, wo, t_wo, 8)
        for c in range(2):
            cs = slice(c * 512, (c + 1) * 512)
            kb.tt("dve", ya[:, cs], po[c][0], ya[:, cs], ALU.add, [po[c][1], t_ya], [t_ya])
        yield
        hf, t_hf = hf_r.next()
        rmsnorm_to(ya[:], t_ya, gffn, t_gffn, hf[:], t_hf)
        hbf, t_hbf = hbf_r.next()
        kb.cp("pool", hbf[:], hf[:], [t_hf], [t_hbf])
        hT2, t_hT2 = h2T[ti]
        ps, t_ps = psTr.next()
        psv = bfv(ps).rearrange("p (k c) -> p k c", k=8)
        for k in range(8):
            kb.tr(psv[:, k, :], hbf[:, k * 128:(k + 1) * 128], ident_bf[:], [t_hbf, t_identbf], [t_ps])
        kb.cp("act", hT2[:], psv, [t_ps], [t_hT2])
        yield
        hfT, t_hfT = hfT_r.next()
        for hv in range(2):
            ps, t_ps = psTr.next()
            psv4 = ps.rearrange("p (k c) -> p k c", k=4)
            for k in range(4):
                kk = hv * 4 + k
                kb.tr(psv4[:, k, :], hf[:, kk * 128:(kk + 1) * 128], ident[:], [t_hf, t_ident], [t_ps])
            kb.cp("act" if hv else "dve", hfT[:, hv * 4:hv * 4 + 4, :], psv4, [t_ps], [t_hfT])
        ps, t_ps = psD.next()
        for k in range(8):
            kb.mm(ps[:, 0:20], hfT[:, k, :], wr[:, k, :], [t_hfT, t_wr], [t_ps], start=(k == 0), stop=(k == 7))
        lg, t_lg = lg_r.next()
        kb.tt("dve", lg[:], ps[:, 0:20], br[:], ALU.add, [t_ps, t_br], [t_lg])
        cm, t_cm = comb[ti]

        def sm(n=16):
            a, t_a = sm_r.next()
            return a[:, 0:n], t_a
        gmax, t_gmax = sm(1)
        kb.red("dve", gmax, lg[:, 0:4], [t_lg], [t_gmax], op=ALU.max)
        ngmax, t_ngmax = sm(1)
        kb.ts("dve", ngmax, gmax, -1.0, ALU.mult, [t_gmax], [t_ngmax])
        gex, t_gex = sm(4)
        gsum, t_gsum = sm(1)
        kb.memset("pool", gsum, 0.0, [t_gsum])
        kb.act(gex, lg[:, 0:4], AF.Exp, [t_lg, t_ngmax, t_gsum], [t_gex, t_gsum], bias=ngmax, accum_out=gsum)
        pg, t_pg = sm(1)
        kb.recip(pg, gsum, [t_gsum], [t_pg])
        goh, t_goh = sm(4)
        kb.ts("dve", goh, lg[:, 0:4], gmax, ALU.is_ge, [t_lg, t_gmax], [t_goh])
        el, t_el = sm(16)
        kb.tt("dve", el.rearrange("p (g e) -> p g e", e=4), lg[:, 4:20].rearrange("p (g e) -> p g e", e=4),
              goh.unsqueeze(2).to_broadcast([128, 4, 4]), ALU.mult, [t_lg, t_goh], [t_el])
        sel, t_sel = sm(4)
        kb.red("dve", sel, el.rearrange("p (g e) -> p e g", e=4), [t_el], [t_sel])
        m1, t_m1 = sm(1)
        kb.red("dve", m1, sel, [t_sel], [t_m1], op=ALU.max)
        oh1, t_oh1 = sm(4)
        kb.ts("dve", oh1, sel, m1, ALU.is_ge, [t_sel, t_m1], [t_oh1])
        sel2, t_sel2 = sm(4)
        kb.stt("dve", sel2, oh1, -1e30, sel, ALU.mult, ALU.add, [t_oh1, t_sel], [t_sel2])
        m2, t_m2 = sm(1)
        kb.red("dve", m2, sel2, [t_sel2], [t_m2], op=ALU.max)
        oh2, t_oh2 = sm(4)
        kb.ts("dve", oh2, sel2, m2, ALU.is_ge, [t_sel2, t_m2], [t_oh2])
        dd, t_dd = sm(1)
        kb.tt("dve", dd, m2, m1, ALU.subtract, [t_m2, t_m1], [t_dd])
        kb.act(dd, dd, AF.Exp, [t_dd], [t_dd])
        kb.ts("dve", dd, dd, 1.0, ALU.add, [t_dd], [t_dd])
        w1, t_w1 = sm(1)
        kb.recip(w1, dd, [t_dd], [t_w1])
        w2, t_w2 = sm(1)
        kb.ts("dve", w2, w1, -1.0, ALU.mult, [t_w1], [t_w2], s2=1.0, op1=ALU.add)
        kb.tt("dve", w1, w1, pg, ALU.mult, [t_w1, t_pg], [t_w1])
        kb.tt("dve", w2, w2, pg, ALU.mult, [t_w2, t_pg], [t_w2])
        wig, t_wig = sm(4)
        kb.ts("dve", wig, oh1, w1, ALU.mult, [t_oh1, t_w1], [t_wig])
        kb.stt("dve", wig, oh2, w2, wig, ALU.mult, ALU.add, [t_oh2, t_w2, t_wig], [t_wig])
        for g in range(4):
            kb.ts("dve", cm[:, g * 4:(g + 1) * 4], wig, goh[:, g:g + 1], ALU.mult, [t_wig, t_goh], [t_cm])
        yield

    def f_tile(c0, ti):
        t = c0 + ti
        rows = slice(t * 128, (t + 1) * 128)
        ya, t_ya = yacc[ti]
        hbf, t_hbf = hbf_r.next()
        rmsnorm_to(ya[:], t_ya, gple, t_gple, hbf[:], t_hbf)
        h3T, t_h3T = transpose_bf(hbf, t_hbf, 8)
        pgate = proj(h3T, t_h3T, wpg, t_wpg, 8)
        sg, t_sg = sg_r.next()
        for c in range(2):
            cs = slice(c * 512, (c + 1) * 512)
            kb.act(sg[:, cs], pgate[c][0], AF.Sigmoid, [pgate[c][1]], [t_sg])
        pin, t_pin = pin_r.next()
        kb.dma("sp", pin[:], p_in[rows, :], [], [t_pin])
        pbf, t_pbf = pbf_r.next()
        kb.cp("pool", pbf[:], pin[:], [t_pin], [t_pbf])
        yield
        pT, t_pT = transpose_bf(pbf, t_pbf, 2)
        pple = proj(pT, t_pT, wpl, t_wpl, 2)
        for c in range(2):
            cs = slice(c * 512, (c + 1) * 512)
            kb.tt("dve", sg[:, cs], sg[:, cs], pple[c][0], ALU.mult, [t_sg, pple[c][1]], [t_sg])
        kb.tt("pool", ya[:], sg[:], ya[:], ALU.add, [t_sg, t_ya], [t_ya])
        final_ops.append(kb.dma("sp", out[rows, :], ya[:], [t_ya], []))
        yield

    def run_gens(gs):
        gs = list(gs)
        while gs:
            for g_ in list(gs):
                try:
                    next(g_)
                except StopIteration:
                    gs.remove(g_)


    for ti in range(TC):
        run_gens([d_tile(0, ti)])
    for c0 in range(0, NT, TC):
        def load_gu(e_):
            wgu, t_wgu_sb = wgu_r.next()
            kb.dma("sp", wgu[:, 0:4, :], wgu_s[e_].rearrange("(k p) c -> p k c", p=128)[:, 0:4, :], [t_wgu[e_]], [t_wgu_sb])
            kb.dma("gq", wgu[:, 4:8, :], wgu_s[e_].rearrange("(k p) c -> p k c", p=128)[:, 4:8, :], [t_wgu[e_]], [t_wgu_sb])
            return wgu, t_wgu_sb

        def load_dn(e_):
            wdn, t_wdn = wd_r.next()
            kb.dma("sp", wdn[:], wd_s[e_].rearrange("(k p) c -> p k c", p=128), [t_wd[e_]], [t_wdn])
            return wdn, t_wdn

        Wgu = {0: load_gu(0)}
        Wdn = {0: load_dn(0)}

        def exp_item(e_, ti):
            ya, t_ya = yacc[ti]
            hT2, t_hT2 = h2T[ti]
            cm, t_cm = comb[ti]
            d = {}

            def e0():
                if ti == 0 and e_ + 1 < 16:
                    Wgu[e_ + 1] = load_gu(e_ + 1)
                wgu, t_wgu_sb = Wgu[e_]
                ps, t_ps = psM.next()
                for k in range(8):
                    kb.mm(ps, hT2[:, k, :], wgu[:, k, :], [t_hT2, t_wgu_sb], [t_ps], start=(k == 0), stop=(k == 7))
                d.update(ps=ps, t_ps=t_ps)

            def e1():
                ps, t_ps = d["ps"], d["t_ps"]
                sil, t_sil = sil_r.next()
                kb.act(sil[:], ps[:, 0:256], AF.Silu, [t_ps], [t_sil])
                ab, t_ab = act_r.next()
                kb.stt("dve", ab[:], ps[:, 256:512], cm[:, e_:e_ + 1], sil[:], ALU.mult, ALU.mult, [t_ps, t_cm, t_sil], [t_ab])
                d.update(ab=ab, t_ab=t_ab)

            def e2():
                pst_, t_pst_ = psTr.next()
                pstv = bfv(pst_).rearrange("p (k c) -> p k c", k=8)
                for k in range(2):
                    kb.tr(pstv[:, k, :], d["ab"][:, k * 128:(k + 1) * 128], ident_bf[:], [d["t_ab"], t_identbf], [t_pst_])
                d.update(pstv=pstv, t_pst=t_pst_)

            def e3():
                aT, t_aT = actT_r.next()
                kb.cp("act", aT[:], d["pstv"][:, 0:2, :], [d["t_pst"]], [t_aT])
                d.update(aT=aT, t_aT=t_aT)

            def e4():
                if ti == 0 and e_ + 1 < 16:
                    Wdn[e_ + 1] = load_dn(e_ + 1)
                wdn, t_wdn = Wdn[e_]
                pds = []
                for c in range(2):
                    pd, t_pd = psD.next()
                    for k in range(2):
                        kb.mm(pd, d["aT"][:, k, :], wdn[:, k, c * 512:(c + 1) * 512], [d["t_aT"], t_wdn], [t_pd], start=(k == 0), stop=(k == 1))
                    pds.append((pd, t_pd))
                d.update(pds=pds)

            def e5():
                for c in range(2):
                    pd, t_pd = d["pds"][c]
                    cs = slice(c * 512, (c + 1) * 512)
                    kb.tt("dve", ya[:, cs], ya[:, cs], pd, ALU.add, [t_ya, t_pd], [t_ya])

            return [e0, e1, e2, e3, e4, e5]

        pipeline([exp_item(e_, ti) for e_ in range(16) for ti in range(TC)])
        for ti in range(TC + 1):
            gs = []
            if ti < TC:
                gs.append(f_tile(c0, ti))
            if ti >= 1 and c0 + TC < NT:
                gs.append(d_tile(c0 + TC, ti - 1))
            run_gens(gs)

    print("sbuf end D", kb.off, "ops", {k: len(v) for k, v in P.ops.items()})
    P.emit(nc, final_waits=final_ops)
    return nc


_CONSTS = None


def _consts():
    global _CONSTS
    if _CONSTS is None:
        _CONSTS = {
            "c_ident": np.eye(128, dtype=np.float32),
            "c_triu": np.triu(np.ones((128, 128), dtype=np.float32)),
            "c_tril": np.tril(np.ones((128, 128), dtype=np.float32)),
        }
    return _CONSTS


def make_in_map(inputs, b, S):
    f = lambda a: np.ascontiguousarray(np.asarray(a, dtype=np.float32))
    m = {
        "x": f(inputs["x"][b, :S]),
        "p": f(inputs["p"][0, b, :S]),
        "norm_mix": f(inputs["norm_mix"][0]),
        "w_in": f(inputs["w_in"][0]),
        "q_norm": f(inputs["q_norm"][0]),
        "k_norm": f(inputs["k_norm"][0]),
        "conv_w": f(np.asarray(inputs["conv_w"][0]).reshape(4, 24, 128).transpose(2, 1, 0)),
        "a_log": f(inputs["a_log"][0]),
        "dt_bias": f(inputs["dt_bias"][0]),
        "dn_out_norm": f(inputs["dn_out_norm"][0]),
        "w_branch_a": f(inputs["w_branch_a"][0]),
        "w_branch_b": f(inputs["w_branch_b"][0]),
        "w_out": f(inputs["w_out"][0]),
        "norm_ffn": f(inputs["norm_ffn"][0]),
        "w_router_group": f(inputs["w_router_group"][0]),
        "b_router_group": f(inputs["b_router_group"][0]),
        "w_router_expert": f(inputs["w_router_expert"][0]),
        "b_router_expert": f(inputs["b_router_expert"][0]),
        "w_expert_gate": f(np.asarray(inputs["w_expert_gate"][0]).reshape(16, 1024, 256)),
        "w_expert_up": f(np.asarray(inputs["w_expert_up"][0]).reshape(16, 1024, 256)),
        "w_expert_down": f(np.asarray(inputs["w_expert_down"][0]).reshape(16, 256, 1024)),
        "norm_ple": f(inputs["norm_ple"][0]),
        "w_ple": f(inputs["w_ple"][0]),
        "w_ple_gate": f(inputs["w_ple_gate"][0]),
    }
    m.update(_consts())
    return m


def kernel(**inputs):
    S = 8192
    nc = build(S)
    in_maps = [make_in_map(inputs, b, S) for b in range(8)]
    res = run_bass_kernel_spmd(nc, in_maps, core_ids=list(range(8)))
    return np.stack([np.asarray(r["out"], dtype=np.float32) for r in res.results], axis=0)
```
